# Optimizing a Trainium2 kernel written in Bass

```python
import math
import jax
import jax.numpy as jnp
from jax import lax
import numpy as np

D_MODEL = 1024
BATCH = 4
SEQ = 4096
DEPTH = 2

EPS = 1e-6
HEAD_DIM = 64
A_Q_HEADS = 8
A_KV_HEADS = 2
A_GROUP = A_Q_HEADS // A_KV_HEADS
WINDOW = 128
N_BUCKETS = 32
MAX_DISTANCE = 128
B_HEADS = 8
B_DK = 64
B_DV = 64
B_CONV = 4
CHUNK = 64
A_Q_W = A_Q_HEADS * HEAD_DIM
A_KV_W = A_KV_HEADS * HEAD_DIM
B_QK_W = B_HEADS * B_DK
B_V_W = B_HEADS * B_DV
B_QKV_W = 2 * B_QK_W + B_V_W
AB_SIZES = (A_Q_W, A_KV_W, A_KV_W, B_QKV_W, B_V_W, B_HEADS, B_HEADS)
AB_IN = sum(AB_SIZES)
AB_OUT = A_Q_W + B_V_W
LRU_WIDTH = D_MODEL
LRU_BLOCKS = 8
LRU_BW = LRU_WIDTH // LRU_BLOCKS
LRU_CONV = 4
LRU_C = 8.0
SC_WIDTH = D_MODEL // 2
SC_CONV = 3
CD_SIZES = (LRU_WIDTH, LRU_WIDTH, SC_WIDTH, SC_WIDTH, SC_WIDTH)
CD_IN = sum(CD_SIZES)
CD_OUT = LRU_WIDTH + SC_WIDTH
D_FF = 2816
N_EXPERTS = 8
TOP_K = 2
D_FF_EXPERT = 3584
N_EVEN = (DEPTH + 1) // 2
N_ODD = DEPTH // 2

kernel_name = 'hybrid_swa_deltanet_rglru_shortconv_moe'


def split_cols(t, sizes):
    return jnp.split(t, [int(s) for s in np.cumsum(sizes)[:-1]], axis=-1)


def rmsnorm(x, w):
    xf = x.astype(jnp.float32)
    y = xf * lax.rsqrt(jnp.mean(xf * xf, axis=-1, keepdims=True) + EPS)
    return (y * w.astype(jnp.float32)).astype(x.dtype)


def l2norm(x):
    xf = x.astype(jnp.float32)
    return xf * lax.rsqrt(jnp.sum(xf * xf, axis=-1, keepdims=True) + EPS)


def causal_dwconv(x, w, b=None):
    K = w.shape[0]
    T = x.shape[1]
    xp = jnp.pad(x, ((0, 0), (K - 1, 0), (0, 0)))
    y = xp[:, 0:T] * w[0]
    for k in range(1, K):
        y = y + xp[:, k:k + T] * w[k]
    if b is not None:
        y = y + b
    return y


def t5_causal_bucket(dist):
    max_exact = N_BUCKETS // 2
    d = np.maximum(dist, 0)
    large = max_exact + (np.log(np.maximum(d, 1) / max_exact) / math.log(MAX_DISTANCE / max_exact)
                         * (N_BUCKETS - max_exact)).astype(np.int32)
    large = np.minimum(large, N_BUCKETS - 1)
    return np.where(d < max_exact, d, large).astype(np.int32)


def swa_sink_attention(q, k, v, sinks, rel_bias):
    Bsz, T = q.shape[0], q.shape[1]
    nb = T // WINDOW
    qb = q.reshape(Bsz, nb, WINDOW, A_KV_HEADS, A_GROUP, HEAD_DIM)

    def band(t):
        tb = t.reshape(Bsz, nb, WINDOW, A_KV_HEADS, HEAD_DIM)
        prev = jnp.pad(tb[:, :-1], ((0, 0), (1, 0), (0, 0), (0, 0), (0, 0)))
        return jnp.concatenate([prev, tb], axis=2)

    kb, vb = band(k), band(v)
    logits = jnp.einsum('bnqkgd,bnskd->bnkgqs', qb, kb,
                        preferred_element_type=jnp.float32) * (HEAD_DIM ** -0.5)
    qi = np.arange(WINDOW)[:, None]
    s = np.arange(2 * WINDOW)[None, :]
    dist = qi + WINDOW - s
    in_window = (dist >= 0) & (dist < WINDOW)
    bias = rel_bias[t5_causal_bucket(dist)].astype(jnp.float32)
    bias = jnp.transpose(bias, (2, 0, 1)).reshape(A_KV_HEADS, A_GROUP, WINDOW, 2 * WINDOW)
    key_valid = (np.arange(nb)[:, None] * WINDOW - WINDOW + s) >= 0
    mask = in_window[None] & key_valid[:, None, :]
    logits = jnp.where(mask[None, :, None, None], logits + bias, -jnp.inf)
    sink = sinks.astype(jnp.float32).reshape(A_KV_HEADS, A_GROUP)[None, None, :, :, None, None]
    m = jnp.maximum(jnp.max(logits, axis=-1, keepdims=True), sink)
    p = jnp.exp(logits - m)
    p = p / (jnp.sum(p, axis=-1, keepdims=True) + jnp.exp(sink - m))
    out = jnp.einsum('bnkgqs,bnskd->bnqkgd', p.astype(v.dtype), vb)
    return out.reshape(Bsz, T, A_Q_W)


def gated_delta_rule(q, k, v, g, beta):
    Bsz, T, H, dk = q.shape
    dv = v.shape[-1]
    n = T // CHUNK

    def chunks(t):
        t = t.astype(jnp.float32).reshape((Bsz, n, CHUNK, H) + t.shape[3:])
        return jnp.moveaxis(t, 3, 1)

    q, k, v, g, beta = chunks(q), chunks(k), chunks(v), chunks(g), chunks(beta)
    q = q * (dk ** -0.5)
    g = jnp.cumsum(g, axis=-1)
    causal = np.tril(np.ones((CHUNK, CHUNK), dtype=bool))
    strict = np.tril(np.ones((CHUNK, CHUNK), dtype=bool), -1)
    decay = jnp.exp(jnp.where(causal, g[..., :, None] - g[..., None, :], -jnp.inf))
    kk = jnp.einsum('bhncd,bhnsd->bhncs', k, k)
    L = jnp.where(strict, beta[..., None] * kk * decay, 0.0)
    eye = jnp.eye(CHUNK, dtype=jnp.float32)
    Tm = lax.linalg.triangular_solve(eye + L, jnp.broadcast_to(eye, L.shape),
                                     left_side=True, lower=True, unit_diagonal=True)
    u = Tm @ (v * beta[..., None])
    w = Tm @ (k * (beta * jnp.exp(g))[..., None])
    qk = jnp.where(causal, jnp.einsum('bhncd,bhnsd->bhncs', q, k) * decay, 0.0)
    g_last = g[..., -1:]
    k_dec = k * jnp.exp(g_last - g)[..., None]
    q_dec = q * jnp.exp(g)[..., None]

    def step(S, xs):
        q_i, k_i, u_i, w_i, qk_i, gl_i = xs
        v_new = u_i - jnp.einsum('bhcd,bhde->bhce', w_i, S)
        o = jnp.einsum('bhcd,bhde->bhce', q_i, S) + jnp.einsum('bhcs,bhse->bhce', qk_i, v_new)
        S = S * jnp.exp(gl_i)[..., None] + jnp.einsum('bhcd,bhce->bhde', k_i, v_new)
        return S, o

    xs = tuple(jnp.moveaxis(t, 2, 0) for t in (q_dec, k_dec, u, w, qk, g_last))
    S0 = jnp.zeros((Bsz, H, dk, dv), jnp.float32)
    _, o = lax.scan(step, S0, xs)
    return jnp.transpose(o, (1, 0, 3, 2, 4)).reshape(Bsz, T, H, dv)


def mixer_ab(h, w_in, sinks, conv_w, a_log, dt_bias, norm_w, w_out, rel_bias):
    Bsz, T, _ = h.shape
    qa, ka, va, qkv_b, gate_b, beta_b, a_b = split_cols(h @ w_in, AB_SIZES)
    attn = swa_sink_attention(qa.reshape(Bsz, T, A_Q_HEADS, HEAD_DIM),
                              ka.reshape(Bsz, T, A_KV_HEADS, HEAD_DIM),
                              va.reshape(Bsz, T, A_KV_HEADS, HEAD_DIM), sinks, rel_bias)
    qkv_b = jax.nn.silu(causal_dwconv(qkv_b, conv_w))
    qb, kb, vb = split_cols(qkv_b, (B_QK_W, B_QK_W, B_V_W))
    qb = l2norm(qb.reshape(Bsz, T, B_HEADS, B_DK))
    kb = l2norm(kb.reshape(Bsz, T, B_HEADS, B_DK))
    vb = vb.reshape(Bsz, T, B_HEADS, B_DV)
    beta = jax.nn.sigmoid(beta_b.astype(jnp.float32))
    g = -jnp.exp(a_log.astype(jnp.float32)) * jax.nn.softplus(a_b.astype(jnp.float32) + dt_bias.astype(jnp.float32))
    o = gated_delta_rule(qb, kb, vb, g, beta)
    o = rmsnorm(o, norm_w) * jax.nn.silu(gate_b.astype(jnp.float32).reshape(Bsz, T, B_HEADS, B_DV))
    o = o.reshape(Bsz, T, B_V_W).astype(h.dtype)
    return jnp.concatenate([attn, o], axis=-1) @ w_out


def lru_combine(left, right):
    a1, b1 = left
    a2, b2 = right
    return a1 * a2, a2 * b1 + b2


def mixer_cd(h, w_in, conv_w, conv_b, gate_a_w, gate_a_b, gate_x_w, gate_x_b, lam, sconv_w, w_out):
    Bsz, T, _ = h.shape
    xc, yc, bd, cd, hd = split_cols(h @ w_in, CD_SIZES)
    xc = causal_dwconv(xc, conv_w, conv_b)
    xblk = xc.reshape(Bsz, T, LRU_BLOCKS, LRU_BW)
    r = jax.nn.sigmoid((jnp.einsum('btni,nij->btnj', xblk, gate_a_w).reshape(Bsz, T, LRU_WIDTH)
                        + gate_a_b).astype(jnp.float32))
    i = jax.nn.sigmoid((jnp.einsum('btni,nij->btnj', xblk, gate_x_w).reshape(Bsz, T, LRU_WIDTH)
                        + gate_x_b).astype(jnp.float32))
    log_a = -LRU_C * r * jax.nn.softplus(-lam.astype(jnp.float32))
    a = jnp.exp(log_a)
    b = jnp.sqrt(-jnp.expm1(2.0 * log_a)) * (i * xc.astype(jnp.float32))
    _, hs = lax.associative_scan(lru_combine, (a, b), axis=1)
    yc_out = hs.astype(h.dtype) * jax.nn.gelu(yc)
    yd_out = bd * causal_dwconv(cd * hd, sconv_w)
    return jnp.concatenate([yc_out, yd_out], axis=-1) @ w_out


def swiglu(h, wg, wu, wd):
    return (jax.nn.silu(h @ wg) * (h @ wu)) @ wd


def moe_swiglu(h, router_w, router_b, wg, wu, wd):
    logits = (h @ router_w).astype(jnp.float32) + router_b.astype(jnp.float32)
    top_val, top_idx = lax.top_k(logits, TOP_K)
    top_w = jax.nn.softmax(top_val, axis=-1)
    gates = jnp.sum(jax.nn.one_hot(top_idx, N_EXPERTS, dtype=jnp.float32) * top_w[..., None], axis=-2)
    gates = gates.astype(h.dtype)
    out = jnp.zeros_like(h)
    for e in range(N_EXPERTS):
        out = out + gates[..., e:e + 1] * swiglu(h, wg[e], wu[e], wd[e])
    return out


def setup_inputs(seed: int = 0) -> dict:
    key = jax.random.key(seed)
    ks = jax.random.split(key, 33)
    D = D_MODEL

    def nrm(k, shape, scale):
        return jax.random.normal(k, shape, jnp.float32) * scale

    def gain(k, shape):
        return 1.0 + nrm(k, shape, 0.02)

    dt = jnp.exp(jax.random.uniform(ks[12], (N_EVEN, B_HEADS), jnp.float32, math.log(1e-3), math.log(1e-1)))
    u = jax.random.uniform(ks[25], (N_ODD, LRU_WIDTH), jnp.float32, 0.9, 0.999)
    p = u ** (1.0 / LRU_C)
    return {
        'x': nrm(ks[0], (BATCH, SEQ, D), 1.0),
        'c': nrm(ks[1], (BATCH, D), 1.0),
        'rel_bias': nrm(ks[2], (N_BUCKETS, A_Q_HEADS), 0.5),
        'ada_w': nrm(ks[3], (DEPTH, D, 6 * D), 0.5 * D ** -0.5),
        'ada_b': nrm(ks[4], (DEPTH, 6 * D), 0.02),
        'norm_mix_w': gain(ks[5], (DEPTH, D)),
        'norm_ffn_w': gain(ks[6], (DEPTH, D)),
        'final_norm_w': gain(ks[7], (D,)),
        'ab_w_in': nrm(ks[8], (N_EVEN, D, AB_IN), D ** -0.5),
        'attn_sinks': nrm(ks[9], (N_EVEN, A_Q_HEADS), 0.5),
        'dn_conv_w': nrm(ks[10], (N_EVEN, B_CONV, B_QKV_W), B_CONV ** -0.5),
        'dn_a_log': jnp.log(jax.random.uniform(ks[11], (N_EVEN, B_HEADS), jnp.float32, 1.0, 16.0)),
        'dn_dt_bias': dt + jnp.log(-jnp.expm1(-dt)),
        'dn_norm_w': gain(ks[13], (N_EVEN, B_DV)),
        'ab_w_out': nrm(ks[14], (N_EVEN, AB_OUT, D), AB_OUT ** -0.5),
        'ffn_w_gate': nrm(ks[15], (N_EVEN, D, D_FF), D ** -0.5),
        'ffn_w_up': nrm(ks[16], (N_EVEN, D, D_FF), D ** -0.5),
        'ffn_w_down': nrm(ks[17], (N_EVEN, D_FF, D), D_FF ** -0.5),
        'cd_w_in': nrm(ks[18], (N_ODD, D, CD_IN), D ** -0.5),
        'lru_conv_w': nrm(ks[19], (N_ODD, LRU_CONV, LRU_WIDTH), LRU_CONV ** -0.5),
        'lru_conv_b': nrm(ks[20], (N_ODD, LRU_WIDTH), 0.01),
        'lru_gate_a_w': nrm(ks[21], (N_ODD, LRU_BLOCKS, LRU_BW, LRU_BW), LRU_BW ** -0.5),
        'lru_gate_a_b': nrm(ks[22], (N_ODD, LRU_WIDTH), 0.01),
        'lru_gate_x_w': nrm(ks[23], (N_ODD, LRU_BLOCKS, LRU_BW, LRU_BW), LRU_BW ** -0.5),
        'lru_gate_x_b': nrm(ks[24], (N_ODD, LRU_WIDTH), 0.01),
        'lru_lambda': jnp.log(p) - jnp.log1p(-p),
        'sconv_w': nrm(ks[26], (N_ODD, SC_CONV, SC_WIDTH), SC_CONV ** -0.5),
        'cd_w_out': nrm(ks[27], (N_ODD, CD_OUT, D), CD_OUT ** -0.5),
        'moe_router_w': nrm(ks[28], (N_ODD, D, N_EXPERTS), D ** -0.5),
        'moe_router_b': nrm(ks[29], (N_ODD, N_EXPERTS), 0.01),
        'moe_w_gate': nrm(ks[30], (N_ODD, N_EXPERTS, D, D_FF_EXPERT), D ** -0.5),
        'moe_w_up': nrm(ks[31], (N_ODD, N_EXPERTS, D, D_FF_EXPERT), D ** -0.5),
        'moe_w_down': nrm(ks[32], (N_ODD, N_EXPERTS, D_FF_EXPERT, D), D_FF_EXPERT ** -0.5),
    }


def reference(x, c, rel_bias, ada_w, ada_b, norm_mix_w, norm_ffn_w, final_norm_w,
              ab_w_in, attn_sinks, dn_conv_w, dn_a_log, dn_dt_bias, dn_norm_w, ab_w_out,
              ffn_w_gate, ffn_w_up, ffn_w_down,
              cd_w_in, lru_conv_w, lru_conv_b, lru_gate_a_w, lru_gate_a_b, lru_gate_x_w, lru_gate_x_b,
              lru_lambda, sconv_w, cd_w_out, moe_router_w, moe_router_b, moe_w_gate, moe_w_up, moe_w_down):
    cond = jax.nn.silu(c)
    for l in range(DEPTH):
        sh1, sc1, g1, sh2, sc2, g2 = jnp.split(cond @ ada_w[l] + ada_b[l], 6, axis=-1)
        hn = rmsnorm(x, norm_mix_w[l]) * (1.0 + sc1[:, None]) + sh1[:, None]
        if l % 2 == 0:
            e = l // 2
            mix = mixer_ab(hn, ab_w_in[e], attn_sinks[e], dn_conv_w[e], dn_a_log[e], dn_dt_bias[e],
                           dn_norm_w[e], ab_w_out[e], rel_bias)
        else:
            o = l // 2
            mix = mixer_cd(hn, cd_w_in[o], lru_conv_w[o], lru_conv_b[o], lru_gate_a_w[o], lru_gate_a_b[o],
                           lru_gate_x_w[o], lru_gate_x_b[o], lru_lambda[o], sconv_w[o], cd_w_out[o])
        x = x + g1[:, None] * mix
        hn = rmsnorm(x, norm_ffn_w[l]) * (1.0 + sc2[:, None]) + sh2[:, None]
        if l % 2 == 0:
            e = l // 2
            ffn = swiglu(hn, ffn_w_gate[e], ffn_w_up[e], ffn_w_down[e])
        else:
            o = l // 2
            ffn = moe_swiglu(hn, moe_router_w[o], moe_router_b[o], moe_w_gate[o], moe_w_up[o], moe_w_down[o])
        x = x + g2[:, None] * ffn
    return rmsnorm(x, final_norm_w)
```

```python
import numpy as np
import concourse.bass as bass
import concourse.mybir as mybir
from concourse.bass_utils import run_bass_kernel_spmd

F32 = mybir.dt.float32
BF16 = mybir.dt.bfloat16
AF = mybir.ActivationFunctionType
ALU = mybir.AluOpType
AX = mybir.AxisListType

T = 4096
TH = 2048
D = 1024
EPS = 1e-6
NEG = -30000.0
PAIRS = [[0, 1], [2, 3], [4, 5], [6, 7]]


class Tl:
    __slots__ = ("h", "name", "w", "r", "ds")

    def __init__(self, h, name):
        self.h = h
        self.name = name
        self.w = None
        self.r = {}
        self.ds = None

    def __getitem__(self, k):
        return self.h[k]


class Eng:
    def __init__(self, name, handle, sem):
        self.name = name
        self.h = handle
        self.sem = sem
        self.cnt = 0
        self.waited = {}

    def wait(self, ev):
        sem, val, _ = ev
        k = id(sem)
        if self.waited.get(k, 0) >= val:
            return
        self.waited[k] = val
        self.h.wait_ge(sem, val)


class KB:
    def __init__(self):
        self.nc = bass.Bass("TRN2", target_bir_lowering=False)
        nc = self.nc
        self.E = {}
        for n, h in (("pe", nc.tensor), ("act", nc.scalar), ("dve", nc.vector),
                     ("pool", nc.gpsimd), ("sp", nc.sync)):
            self.E[n] = Eng(n, h, nc.alloc_semaphore("sem_" + n))
        self.dall = []
        self.dfree = {}
        self.ninst = 0
        self.nps = 0
        self.pst = [Tl(nc.alloc_psum_tensor("ps%d" % i, [128, 512], F32), "ps%d" % i) for i in range(8)]
        self.nrot = 6

    def sb(self, name, shape, dt=F32):
        if not hasattr(self, "scopes"):
            self.scopes = [[]]
        cm = self.nc.sbuf_tensor(name, list(shape), dt)
        h = cm.__enter__()
        t = Tl(h, name)
        self.scopes[-1].append((cm, t))
        return t

    def push(self):
        if not hasattr(self, "scopes"):
            self.scopes = [[]]
        self.scopes.append([])

    def pop(self):
        self.barrier()
        for cm, t in reversed(self.scopes.pop()):
            if t.ds is not None:
                self.dfree.setdefault(t.ds[2], []).append(t.ds)
                t.ds = None
            cm.__exit__(None, None, None)

    def ps(self):
        t = self.pst[self.nps % self.nrot]
        self.nps += 1
        return t

    def dram(self, name, shape, dt, kind="Internal"):
        return Tl(self.nc.dram_tensor(name, list(shape), dt, kind=kind), name)

    def _deps(self, eng, reads, writes):
        E = self.E[eng]
        for t in reads:
            if t.w is not None:
                ev = t.w
                if ev[2] == eng and eng == "pe":
                    continue
                E.wait(ev)
        for t in writes:
            if t.w is not None and not (t.w[2] == eng and eng == "pe"):
                E.wait(t.w)
            for ev in t.r.values():
                if not (ev[2] == eng and eng == "pe"):
                    E.wait(ev)

    def _commit(self, ev, reads, writes):
        for t in reads:
            t.r[id(ev[0])] = ev
        for t in writes:
            t.w = ev
            t.r = {}

    def op(self, eng, fn, reads=(), writes=(), sig=True):
        E = self.E[eng]
        self._deps(eng, reads, writes)
        ins = fn(E.h)
        self.ninst += 1
        if sig:
            E.cnt += 1
            ins.then_inc(E.sem, 1)
            ev = (E.sem, E.cnt, eng)
        else:
            ev = (E.sem, E.cnt + 1, eng)
        self._commit(ev, reads, writes)
        return ins

    def _dsem(self, t, kind):
        if t.ds is None:
            fl = self.dfree.setdefault(kind, [])
            if fl:
                t.ds = fl.pop()
            else:
                t.ds = [self.nc.alloc_semaphore("dsem%d" % len(self.dall)), 0, kind]
                self.dall.append(t.ds)
        assert t.ds[2] == kind, (t.name, t.ds[2], kind)
        return t.ds

    def dma(self, q, out_ap, in_ap, reads=(), writes=(), grp="d"):
        E = self.E[q]
        self._deps(q, reads, writes)
        d = self._dsem(writes[0], "sw" if q == "pool" else "hw")
        d[1] += 16
        E.h.dma_start(out=out_ap, in_=in_ap).then_inc(d[0], 16)
        self.ninst += 1
        self._commit((d[0], d[1], "dma"), reads, writes)

    def collective(self, kind, groups, in_t, out_t, in_ap, out_ap, grp="cc"):
        E = self.E["pool"]
        self._deps("pool", [in_t], [out_t])
        d = self._dsem(out_t, "cc")
        d[1] += 1
        E.h.collective_compute(kind, ALU.bypass, replica_groups=groups,
                               ins=[in_ap], outs=[out_ap]).then_inc(d[0])
        self._commit((d[0], d[1], "dma"), [in_t], [out_t])

    def barrier(self):
        evs = []
        for n, E in self.E.items():
            if E.cnt > 0:
                evs.append((E.sem, E.cnt, n))
        for d in self.dall:
            if d[1] > 0:
                evs.append((d[0], d[1], "dma"))
        for n, E in self.E.items():
            for ev in evs:
                if ev[2] != n:
                    E.wait(ev)

    def mm(self, pst, out, lhsT, rhs, reads, start=True, stop=True, tp=None):
        kw = {}
        if tp is not None:
            kw["tile_position"] = tp
        return self.op("pe", lambda e: e.matmul(out, lhsT, rhs, start=start, stop=stop, **kw),
                       reads=reads, writes=[pst], sig=stop)

    def tr(self, pst, out, in_, ident, reads, tp=None):
        kw = {}
        if tp is not None:
            kw["tile_position"] = tp
        return self.op("pe", lambda e: e.transpose(out, in_, ident, **kw), reads=reads, writes=[pst])

    def act(self, out, in_, func, reads, writes, eng="act", **kw):
        return self.op(eng, lambda e: e.activation(out=out, in_=in_, func=func, **kw), reads=reads, writes=writes)

    def tt(self, eng, out, a, b, op, reads, writes):
        return self.op(eng, lambda e: e.tensor_tensor(out=out, in0=a, in1=b, op=op), reads=reads, writes=writes)

    def ts(self, eng, out, a, s1, s2, op0, op1, reads, writes):
        if op1 is None:
            return self.op(eng, lambda e: e.tensor_scalar(out=out, in0=a, scalar1=s1, scalar2=None, op0=op0),
                           reads=reads, writes=writes)
        return self.op(eng, lambda e: e.tensor_scalar(out=out, in0=a, scalar1=s1, scalar2=s2, op0=op0, op1=op1),
                       reads=reads, writes=writes)

    def stt(self, eng, out, a, s, b, op0, op1, reads, writes):
        return self.op(eng, lambda e: e.scalar_tensor_tensor(out=out, in0=a, scalar=s, in1=b, op0=op0, op1=op1),
                       reads=reads, writes=writes)

    def cp(self, eng, out, in_, reads, writes):
        if eng == "act":
            return self.op(eng, lambda e: e.copy(out=out, in_=in_), reads=reads, writes=writes)
        return self.op(eng, lambda e: e.tensor_copy(out=out, in_=in_), reads=reads, writes=writes)


def bc(ap, shape):
    return ap.broadcast_to(list(shape))


NC0 = 11 * 128 + 64 + 8
NC1 = 14 * 128


class Prog:
    def __init__(self, stop_after=None, debug=False, ntg=8, m0_stop=None, skip_ada=False, m1only=False, m1_stop=None):
        self.m1only = m1only
        self.m1_stop = m1_stop
        self.moeonly = False
        self.ntg = ntg
        self.nexp = 8
        self.groups = PAIRS
        self.m0_stop = m0_stop
        self.skip_ada = skip_ada
        self.kb = KB()
        self.stop_after = stop_after
        self.debug = debug
        self.inputs = {}
        self.outputs = {}

    def inp(self, name, shape, dt=F32):
        t = self.kb.dram(name, shape, dt, kind="ExternalInput")
        self.inputs[name] = t
        return t

    def outp(self, name, shape, dt=F32):
        t = self.kb.dram(name, shape, dt, kind="ExternalOutput")
        self.outputs[name] = t
        return t

    def declare(self):
        I = self.inp
        self.xfull = I("xfull", [T, D])
        self.xown = I("xown", [TH, D])
        self.flag = I("flag", [128, 2])
        self.cvec = I("cvec", [128, 8])
        self.adaw = I("adaw", [2, D, 6 * D])
        self.adab = I("adab", [2, 6 * D])
        self.nmw = I("nmw", [2, D])
        self.nfw = I("nfw", [2, D])
        self.fnw = I("fnw", [1, D])
        self.ident = I("ident", [128, 128])
        self.cmask = I("cmask", [128, 4, 64])
        self.sel = I("sel", [4, 2, 128])
        self.blk1 = I("blk1", [128, 128])
        self.win0 = I("win0", [D, NC0])
        self.cw0 = I("cw0", [128, 6, 4])
        self.dnsm = I("dnsm", [4, 2])
        self.dnw = I("dnw", [128, 1])
        self.sinkl = I("sinkl", [128, 2])
        self.biasg = I("biasg", [2, 128, 512])
        self.amask = I("amask", [2, 128, 128])
        if self.stop_after in ("adaln", "mixer0") and not self.m1only:
            self._internal()
            return
        if self.moeonly:
            self.rw = I("rw", [128, 8, 8])
            self.rb = I("rb", [1, 8])
            self.mwg = I("mwg", [self.nexp, D, 3584])
            self.mwu = I("mwu", [self.nexp, D, 3584])
            self.mwd = I("mwd", [self.nexp, 3584, D])
            self.out = self.outp("out", [TH, D])
            self.dbg_g = self.outp("dbg_g", [128, 128])
            self._internal()
            return
        if self.m1only:
            self.win1 = I("win1", [D, NC1])
            self.l1sm = I("l1sm", [128, 4, 8])
            self.gaw = I("gaw", [4, 128, 128])
            self.gxw = I("gxw", [4, 128, 128])
            self.scw = I("scw", [128, 2, 3])
            self.wout1 = I("wout1", [1536, D])
            self.out = self.outp("out", [TH, D])
            self._internal()
            return
        self.wout0 = I("wout0", [D, D])
        if self.stop_after == "op0":
            self.out = self.outp("out", [TH, D])
            self._internal()
            return
        self.wg0 = I("wg0", [D, 2816])
        self.wu0 = I("wu0", [D, 2816])
        self.wd0 = I("wd0", [2816, D])
        if self.stop_after == "ffn0":
            self.out = self.outp("out", [TH, D])
            self._internal()
            return
        self.win1 = I("win1", [D, NC1])
        self.l1sm = I("l1sm", [128, 4, 8])
        self.gaw = I("gaw", [4, 128, 128])
        self.gxw = I("gxw", [4, 128, 128])
        self.scw = I("scw", [128, 2, 3])
        self.wout1 = I("wout1", [1536, D])
        if self.stop_after != "premoe":
            self.rw = I("rw", [128, 8, 8])
            self.rb = I("rb", [1, 8])
            self.mwg = I("mwg", [8, D, 3584])
            self.mwu = I("mwu", [8, D, 3584])
            self.mwd = I("mwd", [8, 3584, D])
        self.out = self.outp("out", [TH, D])
        self._internal()

    def _internal(self):
        kb = self.kb
        self.y0s = [kb.dram("y0s%d" % i, [256, T], BF16) for i in range(2)]
        self.y0g = [kb.dram("y0g%d" % i, [512, T], BF16) for i in range(2)]
        self.h1s = [kb.dram("h1s%d" % i, [512, TH], BF16) for i in range(2)]
        self.h1g = [kb.dram("h1g%d" % i, [1024, TH], BF16) for i in range(2)]
        self.y1s = [kb.dram("y1s%d" % i, [256, T], BF16) for i in range(3)]
        self.y1g = [kb.dram("y1g%d" % i, [512, T], BF16) for i in range(3)]

    def consts(self):
        kb = self.kb
        c = {}
        self.c = c
        c["idf"] = kb.sb("idf", [128, 128], F32)
        c["idb"] = kb.sb("idb", [128, 128], BF16)
        c["cm"] = kb.sb("cm", [128, 4, 64], F32)
        c["i64b"] = kb.sb("i64b", [128, 64], BF16)
        c["sel"] = kb.sb("selc", [4, 2, 128], F32)
        c["blk1f"] = kb.sb("blk1f", [128, 128], F32)
        c["blk1"] = kb.sb("blk1b", [128, 128], BF16)
        c["ones"] = kb.sb("onesb", [128, 128], BF16)
        c["flag"] = kb.sb("flagc", [128, 2], F32)
        c["cv"] = kb.sb("cv", [128, 8], F32)
        c["cond"] = kb.sb("cond", [128, 8], F32)
        c["condB"] = kb.sb("condB", [128, 8, 128], BF16)
        c["eps"] = kb.sb("epsc", [128, 1], F32)
        c["one"] = kb.sb("onec", [128, 1], F32)
        q = "sp"
        kb.dma(q, c["idf"][:], self.ident[:, :], writes=[c["idf"]], grp="c")
        kb.dma(q, c["cm"][:], self.cmask[:, :, :], writes=[c["cm"]], grp="c")
        kb.dma(q, c["sel"][:], self.sel[:, :, :], writes=[c["sel"]], grp="c")
        kb.dma(q, c["blk1f"][:], self.blk1[:, :], writes=[c["blk1f"]], grp="c")
        kb.dma(q, c["flag"][:], self.flag[:, :], writes=[c["flag"]], grp="c")
        kb.dma(q, c["cv"][:], self.cvec[:, :], writes=[c["cv"]], grp="c")
        kb.cp("dve", c["idb"][:], c["idf"][:], [c["idf"]], [c["idb"]])
        kb.cp("dve", c["blk1"][:], c["blk1f"][:], [c["blk1f"]], [c["blk1"]])
        kb.cp("dve", c["i64b"][:], c["cm"][:, 2, :], [c["cm"]], [c["i64b"]])
        kb.op("dve", lambda e: e.memset(c["ones"][:], 1.0), writes=[c["ones"]])
        kb.op("dve", lambda e: e.memset(c["eps"][:], EPS), writes=[c["eps"]])
        kb.op("dve", lambda e: e.memset(c["one"][:], 1.0), writes=[c["one"]])
        kb.act(c["cond"][:], c["cv"][:], AF.Silu, [c["cv"]], [c["cond"]])
        kb.cp("dve", c["condB"][:], bc(c["cond"][:].unsqueeze(2), [128, 8, 128]), [c["cond"]], [c["condB"]])
        self.mods = kb.sb("mods", [128, 3, D], F32)

    def adaln(self, l, part):
        kb, c = self.kb, self.c
        kb.push()
        self.wbuf = [kb.sb("wbuf%d_%d_%d" % (i, l, part), [128, 8, 512], BF16) for i in range(2)]
        self.rowt = kb.sb("rowt_%d_%d" % (l, part), [128, 512], F32)
        self.rowt2 = kb.sb("rowt2_%d_%d" % (l, part), [128, D], F32)
        for n in range(part * 6, part * 6 + 6):
            wb = self.wbuf[n % 2]
            kb.dma("pool", wb[:], self.adaw[l, :, n * 512:(n + 1) * 512].rearrange("(k p) n -> p k n", p=128),
                   writes=[wb], grp="w")
            kb.dma("sp", self.rowt[:], bc(self.adab[l:l + 1, n * 512:(n + 1) * 512], [128, 512]),
                   writes=[self.rowt], grp="c")
            p = kb.ps()
            for k in range(8):
                kb.mm(p, p[:, :], c["condB"][:, k, :], wb[:, k, :], [c["condB"], wb], start=(k == 0), stop=(k == 7))
            kb.tt("dve", self.mods[:, (n // 2) % 3, (n % 2) * 512:(n % 2) * 512 + 512], p[:, :], self.rowt[:], ALU.add,
                  [p, self.rowt], [self.mods])
        w = self.nmw if part == 0 else self.nfw
        kb.dma("sp", self.rowt2[:], bc(w[l:l + 1, :], [128, D]), writes=[self.rowt2], grp="c")
        kb.stt("dve", self.mods[:, 1, :], self.mods[:, 1, :], 1.0, self.rowt2[:], ALU.add, ALU.mult,
               [self.mods, self.rowt2], [self.mods])
        kb.pop()

    def norm_tile(self, xt_ap, xt_tl, ia, ib, hnT_ap, hnT_tl, eng2="pool"):
        kb, c = self.kb, self.c
        s = self.scr
        kb.act(s["junk"][:], xt_ap, AF.Square, [xt_tl], [s["junk"], s["ss"]], accum_out=s["ss"][:])
        kb.act(s["rs"][:], s["ss"][:], AF.Sqrt, [s["ss"], c["eps"]], [s["rs"]], scale=1.0 / D, bias=c["eps"][:, 0:1])
        kb.op("dve", lambda e: e.reciprocal(out=s["rs"][:], in_=s["rs"][:]), reads=[s["rs"]], writes=[s["rs"]])
        kb.stt("dve", s["t1"][:], xt_ap, s["rs"][:, 0:1], self.mods[:, ia, :], ALU.mult, ALU.mult,
               [xt_tl, s["rs"], self.mods], [s["t1"]])
        kb.tt(eng2, s["hn"][:], s["t1"][:], self.mods[:, ib, :], ALU.add, [s["t1"], self.mods], [s["hn"]])
        p = kb.ps()
        pb = p[:, :].bitcast(BF16)
        for k in range(8):
            kb.tr(p, pb[:, k * 128:(k + 1) * 128], s["hn"][:, k * 128:(k + 1) * 128], c["idb"][:], [s["hn"], c["idb"]])
        kb.cp("act", hnT_ap, pb[:, 0:1024].rearrange("p (k t) -> p k t", k=8), [p], [hnT_tl])

    def alloc_scr(self):
        kb = self.kb
        s = {}
        self.scr = s
        s["junk"] = kb.sb("junk", [128, D], BF16)
        s["ss"] = kb.sb("ss", [128, 1], F32)
        s["rs"] = kb.sb("rs", [128, 1], F32)
        s["t1"] = kb.sb("t1", [128, D], F32)
        s["hn"] = kb.sb("hn", [128, D], BF16)
        self.xin = [kb.sb("xin%d" % i, [128, D], F32) for i in range(2)]

    def mixer0(self):
        kb, c = self.kb, self.c
        sb = kb.sb
        kb.push()
        w0 = sb("w0", [128, 8, NC0], BF16)
        for i in range(0, NC0, 512):
            j = min(i + 512, NC0)
            kb.dma("pool", w0[:, :, i:j], self.win0[:, i:j].rearrange("(k p) n -> p k n", p=128), writes=[w0], grp="w")
        cw = sb("cw", [128, 6, 4], F32)
        kb.dma("sp", cw[:], self.cw0[:, :, :], writes=[cw], grp="c")
        dnsm = sb("dnsmc", [4, 2], F32)
        kb.dma("sp", dnsm[:], self.dnsm[:, :], writes=[dnsm], grp="c")
        nega = sb("nega", [4, 1], F32)
        kb.act(nega[:], dnsm[:, 0:1], AF.Exp, [dnsm], [nega])
        kb.ts("dve", nega[:], nega[:], -1.0, None, ALU.mult, None, [nega], [nega])
        dnw = sb("dnwc", [128, 1], F32)
        kb.dma("sp", dnw[:], self.dnw[:, :], writes=[dnw], grp="c")
        sinkE = sb("sinkE", [128, 2], F32)
        kb.dma("sp", sinkE[:], self.sinkl[:, :], writes=[sinkE], grp="c")
        kb.act(sinkE[:], sinkE[:], AF.Exp, [sinkE], [sinkE])
        biasm = sb("biasm", [128, 2, 512], F32)
        am = sb("am", [128, 2, 128], F32)
        kb.dma("sp", biasm[:], self.biasg.h.ap().rearrange("a p n -> p a n"), writes=[biasm], grp="c")
        kb.dma("sp", am[:], self.amask.h.ap().rearrange("a p n -> p a n"), writes=[am], grp="c")
        for a in range(2):
            kb.tt("dve", biasm[:, a, :].rearrange("p (s q) -> p s q", s=4),
                  biasm[:, a, :].rearrange("p (s q) -> p s q", s=4),
                  bc(am[:, a, :].unsqueeze(1), [128, 4, 128]), ALU.add, [biasm, am], [biasm])
        rmask = sb("rmask", [4, 8, 64], F32)
        kb.op("dve", lambda e: e.memset(rmask[:], 1.0), writes=[rmask])
        kb.op("dve", lambda e: e.memset(rmask[:, :, 0:1], 0.0), writes=[rmask])

        mU8 = sb("mU8", [128, 8, 64], F32)
        mL8 = sb("mL8", [128, 8, 64], F32)
        kb.cp("dve", mU8[:], bc(c["cm"][:, 0, :].unsqueeze(1), [128, 8, 64]), [c["cm"]], [mU8])
        kb.cp("dve", mL8[:], bc(c["cm"][:, 1, :].unsqueeze(1), [128, 8, 64]), [c["cm"]], [mL8])
        hnT = sb("hnT0", [128, 8, 512], BF16)
        qaT = sb("qaT", [128, 2, 512], BF16)
        kaT = sb("kaT", [128, 2, 128 + T], BF16)
        vat = sb("vat", [128, 33, 64], BF16)
        kb.op("pool", lambda e: e.memset(kaT[:], 0.0), writes=[kaT])
        kb.op("dve", lambda e: e.memset(vat[:, 0, :], 0.0), writes=[vat])
        xpre = [sb("xpre%d" % i, [128, 3 + 512], F32) for i in range(6)]
        for i in range(6):
            kb.op("pool", lambda e, i=i: e.memset(xpre[i][:, 0:3], 0.0), writes=[xpre[i]])
        gates = sb("gates", [128, 2, 512], F32)
        tl = sb("tl", [128, 512], F32)
        cacc = sb("cacc", [128, 512], F32)
        ysil = sb("ysil", [128, 512], F32)
        sqb = sb("sqb", [128, 512], BF16)
        rstd = sb("rstd", [128, 512], F32)
        qn = [sb("qn%d" % j, [128, 512], BF16) for j in range(2)]
        qnf = [sb("qnf%d" % j, [128, 512], F32) for j in range(2)]
        i8f = sb("i8f", [128, 8, 64], F32)
        kb.cp("dve", i8f[:], bc(c["cm"][:, 2, :].unsqueeze(1), [128, 8, 64]), [c["cm"]], [i8f])
        A32 = sb("A32", [128, 8, 64], F32)
        Xs = sb("Xs", [128, 4, 64], F32)
        kn = [sb("kn%d" % j, [128, 512], BF16) for j in range(2)]
        vT = [sb("vT%d" % j, [128, 512], BF16) for j in range(2)]
        bt = sb("bt", [4, 512], F32)
        gt = sb("gt", [4, 512], F32)
        Gs = sb("Gs", [4, 512], F32)
        Es = sb("Es", [4, 512], F32)
        BEs = sb("BEs", [4, 512], F32)
        DKs = sb("DKs", [4, 512], F32)
        nbt = gt
        tk4 = sb("tk4", [128, 5, 8, 4], F32)
        TK = sb("TK", [128, 5, 8, 2], F32)
        EGLc = sb("EGLc", [128, 2, 8], F32)
        t0 = sb("t0", [128, 8, 64], F32)
        tU = sb("tU", [128, 8, 64], F32)
        tL = sb("tL", [128, 8, 64], F32)
        Du = tU
        Dl = tL
        tmpf = t0
        NTp = [sb("NTp%d" % i, [128, 8, 64], BF16) for i in range(2)]
        Np = [sb("Np%d" % i, [128, 8, 64], BF16) for i in range(2)]
        Am = sb("Am", [128, 8, 64], BF16)
        KV = sb("KV", [128, 8, 128], BF16)
        KQ = sb("KQ", [128, 8, 128], BF16)
        Wu = sb("Wu", [128, 8, 128], BF16)
        qdec = sb("qdec", [128, 8, 64], F32)
        UT = [sb("UT%d" % j, [128, 8, 64], BF16) for j in range(2)]
        RT = [sb("RT%d" % j, [128, 8, 64], BF16) for j in range(2)]
        O0 = [sb("O0%d" % j, [128, 8, 64], F32) for j in range(2)]
        Qs = [sb("Qs%d" % j, [128, 8, 64], F32) for j in range(2)]
        S32 = [sb("S32_%d" % j, [128, 64], F32) for j in range(2)]
        Sbf = [sb("Sbf_%d" % j, [128, 64], BF16) for j in range(2)]
        pre = sb("pre", [128, 64], F32)
        cS = sb("cS", [128, 64], F32)
        oT = sb("oT", [128, 8, 64], F32)
        yo = sb("yo", [128, 512], BF16)
        for j in range(2):
            kb.op("dve", lambda e, j=j: e.memset(S32[j][:], 0.0), writes=[S32[j]])
            kb.op("dve", lambda e, j=j: e.memset(Sbf[j][:], 0.0), writes=[Sbf[j]])
        PT = [sb("PT%d" % i, [128, 4, 128], BF16) for i in range(2)]
        den = sb("den", [128, 2, 128], F32)
        ao = sb("ao", [128, 2, 128], BF16)
        H = (slice(0, 64), slice(64, 128))

        for tg in range(self.ntg):
            for tt in range(4):
                xt = self.xin[tt % 2]
                r0 = tg * 512 + tt * 128
                kb.dma("sp", xt[:], self.xfull[r0:r0 + 128, :], writes=[xt], grp="x")
                self.norm_tile(xt[:], xt, 1, 0, hnT[:, :, tt * 128:(tt + 1) * 128], hnT)
            for ci in range(11):
                p = kb.ps()
                for k in range(8):
                    kb.mm(p, p[:, :], w0[:, k, ci * 128:(ci + 1) * 128], hnT[:, k, :], [w0, hnT], start=(k == 0), stop=(k == 7))
                if ci < 2:
                    kb.act(qaT[:, ci, :], p[:, :], AF.Copy, [p], [qaT], scale=0.125)
                elif ci == 2:
                    for q in range(2):
                        kb.cp("act", kaT[H[q], q, 128 + tg * 512:128 + (tg + 1) * 512], p[H[q], :], [p], [kaT])
                elif ci < 9:
                    xp = xpre[ci - 3]
                    if tg > 0:
                        kb.cp("pool", xp[:, 0:3], xp[:, 512:515], [xp], [xp])
                    kb.cp("act", xp[:, 3:515], p[:, :], [p], [xp])
                else:
                    kb.act(gates[:, ci - 9, :], p[:, :], AF.Silu, [p], [gates])
            for tt in range(4):
                p = kb.ps()
                for k in range(8):
                    kb.mm(p, p[:, 0:64], hnT[:, k, tt * 128:(tt + 1) * 128], w0[:, k, 1408:1472], [w0, hnT], start=(k == 0), stop=(k == 7))
                kb.cp("act", vat[:, 1 + tg * 4 + tt, :], p[:, 0:64], [p], [vat])
            pb_ = kb.ps()
            pd_ = kb.ps()
            for k in range(8):
                kb.mm(pb_, pb_[0:4, :], w0[:, k, 1472:1476], hnT[:, k, :], [w0, hnT], start=(k == 0), stop=(k == 7))
            for k in range(8):
                kb.mm(pd_, pd_[0:4, :], w0[:, k, 1476:1480], hnT[:, k, :], [w0, hnT], start=(k == 0), stop=(k == 7))
            if self.m0_stop == "inproj":
                continue
            kb.act(bt[:], pb_[0:4, :], AF.Sigmoid, [pb_], [bt])
            kb.act(gt[:], pd_[0:4, :], AF.Exp, [pd_, dnsm], [gt], bias=dnsm[:, 1:2])
            kb.act(gt[:], gt[:], AF.Ln, [gt, c["one"]], [gt], bias=c["one"][0:4, 0:1])
            kb.ts("dve", gt[:], gt[:], nega[:, 0:1], None, ALU.mult, None, [gt, nega], [gt])
            kb.op("dve", lambda e: e.tensor_tensor_scan(out=Gs[:], data0=rmask[:].rearrange("p a b -> p (a b)"), data1=gt[:],
                                                        initial=0.0, op0=ALU.mult, op1=ALU.add), reads=[rmask, gt], writes=[Gs])
            kb.act(Es[:], Gs[:], AF.Exp, [Gs], [Es])
            kb.tt("dve", BEs[:], bt[:], Es[:], ALU.mult, [bt, Es], [BEs])
            G3 = Gs[:].rearrange("p (a b) -> p a b", a=8)
            kb.tt("dve", DKs[:].rearrange("p (a b) -> p a b", a=8), G3, bc(G3[:, :, 63:64], [4, 8, 64]), ALU.subtract, [Gs], [DKs])
            kb.act(DKs[:], DKs[:], AF.Exp, [DKs], [DKs], scale=-1.0)
            kb.ts("dve", nbt[:], bt[:], -1.0, None, ALU.mult, None, [bt], [nbt])
            p = kb.ps()
            for qi, X in enumerate((Gs, nbt, BEs, DKs, bt)):
                for n in range(8):
                    for q in range(2):
                        kb.mm(p, p[H[q], (qi * 8 + n) * 4:(qi * 8 + n) * 4 + 4], X[:, n * 64:(n + 1) * 64], c["idf"][0:4, 0:4],
                              [X, c["idf"]], tp=(0, 64 * q))
            kb.cp("dve", tk4[:].rearrange("p a n h -> p (a n h)"), p[:, 0:160], [p], [tk4])
            for q in range(2):
                kb.cp("dve", TK[H[q], :, :, :], tk4[H[q], :, :, 2 * q:2 * q + 2], [tk4], [TK])
            p = kb.ps()
            for j in range(2):
                kb.mm(p, p[:, j * 8:j * 8 + 8], c["sel"][:, j, :], Es[:].rearrange("p (a b) -> p a b", a=8)[:, :, 63], [c["sel"], Es])
            kb.cp("dve", EGLc[:].rearrange("p j n -> p (j n)"), p[:, 0:16], [p], [EGLc])

            if self.m0_stop == "small":
                continue
            for qb in range(4):
                n = tg * 4 + qb
                kbs = [n - 1, n] if n > 0 else [n]
                for ki, kbk in enumerate(kbs):
                    sel_ = 0 if kbk == n - 1 else 1
                    pl = kb.ps()
                    for q in range(2):
                        for j in range(2):
                            sl = q * 2 + j
                            kb.mm(pl, pl[:, sl * 128:(sl + 1) * 128], kaT[:, q, 128 + kbk * 128:128 + (kbk + 1) * 128],
                                  qaT[:, j, qb * 128:(qb + 1) * 128], [kaT, qaT])
                    if self.m0_stop == "attn_mm":
                        continue
                    kb.tt("dve", tl[:], pl[:, :], biasm[:, sel_, :], ALU.add, [pl, biasm], [tl])
                    if self.m0_stop == "attn_tt":
                        continue
                    kb.act(PT[ki][:].rearrange("p s q -> p (s q)"), tl[:], AF.Exp, [tl], [PT[ki]])
                if self.m0_stop in ("attn_l", "attn_mm", "attn_tt"):
                    continue
                po = kb.ps()
                pdn = kb.ps()
                for q in range(2):
                    for j in range(2):
                        sl = q * 2 + j
                        for ki, kbk in enumerate(kbs):
                            kb.mm(po, po[H[q], j * 128:(j + 1) * 128], vat[:, 1 + kbk, :], PT[ki][:, sl, :], [vat, PT[ki]],
                                  start=(ki == 0), stop=(ki == len(kbs) - 1), tp=(0, 64 * q))
                        for ki, kbk in enumerate(kbs):
                            kb.mm(pdn, pdn[H[q], j * 128:(j + 1) * 128], c["ones"][:, 0:64], PT[ki][:, sl, :], [c["ones"], PT[ki]],
                                  start=(ki == 0), stop=(ki == len(kbs) - 1), tp=(0, 64 * q))
                if self.m0_stop == "attn_pv":
                    continue
                kb.tt("dve", den[:], pdn[:, 0:256].rearrange("p (j t) -> p j t", j=2), bc(sinkE[:].unsqueeze(2), [128, 2, 128]),
                      ALU.add, [pdn, sinkE], [den])
                kb.op("dve", lambda e: e.reciprocal(out=den[:], in_=den[:]), reads=[den], writes=[den])
                kb.tt("dve", ao[:], po[:, 0:256].rearrange("p (j t) -> p j t", j=2), den[:], ALU.mult, [po, den], [ao])
                if self.m0_stop == "attn_n":
                    continue
                for j in range(2):
                    kb.dma("sp", self.y0s[0][j * 128:(j + 1) * 128, n * 128:(n + 1) * 128], ao[:, j, :], reads=[ao], writes=[self.y0s[0]], grp="y")

            if self.m0_stop in ("attn", "attn_l", "attn_pv", "attn_n", "attn_mm", "attn_tt"):
                continue
            for ci in range(6):
                xp = xpre[ci]
                j = ci % 2
                kind = ci // 2
                kb.ts("dve", cacc[:], xp[:, 0:512], cw[:, ci, 0:1], None, ALU.mult, None, [xp, cw], [cacc])
                for k in range(1, 4):
                    kb.stt("dve", cacc[:], xp[:, k:k + 512], cw[:, ci, k:k + 1], cacc[:], ALU.mult, ALU.add, [xp, cw, cacc], [cacc])
                if kind == 2:
                    kb.act(vT[j][:], cacc[:], AF.Silu, [cacc], [vT[j]])
                    continue
                kb.act(ysil[:], cacc[:], AF.Silu, [cacc], [ysil])
                kb.act(sqb[:], ysil[:], AF.Square, [ysil], [sqb])
                p = kb.ps()
                kb.mm(p, p[:, :], c["blk1"][:], sqb[:], [c["blk1"], sqb])
                kb.act(rstd[:], p[:, :], AF.Sqrt, [p, c["eps"]], [rstd], bias=c["eps"][:, 0:1])
                kb.op("dve", lambda e: e.reciprocal(out=rstd[:], in_=rstd[:]), reads=[rstd], writes=[rstd])
                if kind == 0:
                    kb.stt("dve", qnf[j][:], ysil[:], 0.125, rstd[:], ALU.mult, ALU.mult, [ysil, rstd], [qnf[j]])
                    kb.cp("act", qn[j][:], qnf[j][:], [qnf[j]], [qn[j]])
                else:
                    kb.tt("dve", kn[j][:], ysil[:], rstd[:], ALU.mult, [ysil, rstd], [kn[j]])

            if self.m0_stop == "conv":
                continue
            for j in range(2):
                k3 = kn[j][:].rearrange("p (n c) -> p n c", n=8)
                q3 = qn[j][:].rearrange("p (n c) -> p n c", n=8)
                v3 = vT[j][:].rearrange("p (n c) -> p n c", n=8)
                pk = kb.ps()
                pv = kb.ps()
                pkb = pk[:, :].rearrange("p (n x) -> p n x", n=8)
                pvb = pv[:, :].rearrange("p (n x) -> p n x", n=8)
                for n in range(8):
                    for q in range(2):
                        kb.mm(pk, pkb[H[q], n, :], k3[H[q], n, :], c["idb"][H[q], 64 * q:64 * q + 64], [kn[j], c["idb"]], tp=(64 * q, 64 * q))
                for n in range(8):
                    for q in range(2):
                        kb.mm(pv, pvb[H[q], n, :], v3[H[q], n, :], c["idb"][H[q], 64 * q:64 * q + 64], [vT[j], c["idb"]], tp=(64 * q, 64 * q))
                if self.m0_stop == "prep0":
                    continue
                kb.tt("dve", KQ[:, :, 0:64], bc(TK[:, 3, :, j:j + 1], [128, 8, 64]), pkb, ALU.mult, [pk, TK], [KQ])
                kb.tt("dve", KV[:, :, 0:64], bc(TK[:, 2, :, j:j + 1], [128, 8, 64]), pkb, ALU.mult, [pk, TK], [KV])
                kb.tt("dve", KV[:, :, 64:128], bc(TK[:, 4, :, j:j + 1], [128, 8, 64]), pvb, ALU.mult, [pv, TK], [KV])
                if self.m0_stop == "prep1":
                    continue
                pg = kb.ps()
                pe_ = kb.ps()
                kb.mm(pg, pg[:, :], c["sel"][:, j, :], Gs[:], [c["sel"], Gs])
                kb.mm(pe_, pe_[:, :], c["sel"][:, j, :], Es[:], [c["sel"], Es])
                pg3 = pg[:, :].rearrange("p (n c) -> p n c", n=8)
                kb.tt("dve", t0[:], pg3, bc(TK[:, 0, :, j:j + 1], [128, 8, 64]), ALU.subtract, [pg, TK], [t0])
                if self.m0_stop == "prep1a":
                    continue
                kb.tt("pool", tU[:], t0[:], mU8[:], ALU.add, [t0, mU8], [tU])
                kb.stt("dve", tL[:], t0[:], -1.0, mL8[:], ALU.mult, ALU.add, [t0, mL8], [tL])
                kb.act(Du[:], tU[:], AF.Exp, [tU], [Du])
                kb.act(Dl[:], tL[:], AF.Exp, [tL], [Dl])
                if self.m0_stop == "prep1b":
                    continue
                kb.cp("act", cacc[:], pe_[:, :], [pe_], [cacc])
                kb.tt("dve", qdec[:].rearrange("p n c -> p (n c)"), cacc[:], qnf[j][:], ALU.mult, [qnf[j], cacc], [qdec])
                if self.m0_stop == "prep2":
                    continue
                pkk = kb.ps()
                pqk = kb.ps()
                kk3 = pkk[:, :].rearrange("p (n c) -> p n c", n=8)
                qk3 = pqk[:, :].rearrange("p (n c) -> p n c", n=8)
                for n in range(8):
                    for q in range(2):
                        kb.mm(pkk, kk3[H[q], n, :], k3[H[q], n, :], k3[H[q], n, :], [kn[j]], tp=(64 * q, 64 * q))
                for n in range(8):
                    for q in range(2):
                        kb.mm(pqk, qk3[H[q], n, :], k3[H[q], n, :], q3[H[q], n, :], [kn[j], qn[j]], tp=(64 * q, 64 * q))
                kb.tt("dve", tmpf[:], bc(TK[:, 1, :, j:j + 1], [128, 8, 64]), Dl[:], ALU.mult, [TK, Dl], [tmpf])
                kb.cp("act", tl[:], pkk[:, :], [pkk], [tl])
                kb.tt("dve", NTp[0][:], tl[:].rearrange("p (n c) -> p n c", n=8), tmpf[:], ALU.mult, [tl, tmpf], [NTp[0]])
                kb.cp("act", cacc[:], pqk[:, :], [pqk], [cacc])
                kb.tt("dve", KQ[:, :, 64:128], cacc[:].rearrange("p (n c) -> p n c", n=8), Du[:], ALU.mult, [cacc, Du], [KQ])
                if self.m0_stop == "prep3":
                    continue
                pn = kb.ps()
                pnb = pn[:, :].rearrange("p (n c) -> p n c", n=8)
                for n in range(8):
                    for q in range(2):
                        kb.mm(pn, pnb[H[q], n, :], NTp[0][H[q], n, :], c["idb"][H[q], 64 * q:64 * q + 64], [NTp[0], c["idb"]], tp=(64 * q, 64 * q))
                kb.cp("act", Np[0][:], pnb, [pn], [Np[0]])
                kb.cp("act", A32[:], pnb, [pn], [A32])
                kb.tt("pool", A32[:], A32[:], i8f[:], ALU.add, [A32, i8f], [A32])
                kb.cp("act", Am[:], A32[:], [A32], [Am])
                if self.m0_stop == "prep4":
                    continue
                cur = 0
                for lev in range(5):
                    nxt = 1 - cur
                    if lev < 4:
                        p1 = kb.ps()
                        p13 = p1[:, :].rearrange("p (n c) -> p n c", n=8)
                        for n in range(8):
                            for q in range(2):
                                kb.mm(p1, p13[H[q], n, :], NTp[cur][H[q], n, :], Np[cur][H[q], n, :], [NTp[cur], Np[cur]], tp=(64 * q, 64 * q))
                    p2 = kb.ps()
                    p23 = p2[:, :].rearrange("p (n c) -> p n c", n=8)
                    for n in range(8):
                        for q in range(2):
                            kb.mm(p2, p23[H[q], n, :], Np[cur][H[q], n, :], NTp[cur][H[q], n, :], [NTp[cur], Np[cur]], tp=(64 * q, 64 * q))
                    if lev < 4:
                        kb.cp("act", Np[nxt][:], p13, [p1], [Np[nxt]])
                    kb.cp("act", NTp[nxt][:], p23, [p2], [NTp[nxt]])
                    p3 = kb.ps()
                    p33 = p3[:, :].rearrange("p (n c) -> p n c", n=8)
                    for n in range(8):
                        for q in range(2):
                            kb.mm(p3, p33[H[q], n, :], NTp[nxt][H[q], n, :], Am[H[q], n, :], [NTp[nxt], Am], tp=(64 * q, 64 * q))
                    kb.cp("act", tl[:], p3[:, :], [p3], [tl])
                    kb.tt("dve", A32[:], tl[:].rearrange("p (n c) -> p n c", n=8), A32[:], ALU.add, [A32, tl], [A32])
                    kb.cp("act", Am[:], A32[:], [A32], [Am])
                    cur = nxt
                if self.m0_stop == "prep5":
                    continue
                for hf in range(2):
                    pw = kb.ps()
                    pw3 = pw[:, :].rearrange("p (n c) -> p n c", n=4)
                    for n in range(4):
                        for q in range(2):
                            kb.mm(pw, pw3[H[q], n, :], Am[H[q], hf * 4 + n, :], KV[H[q], hf * 4 + n, :], [Am, KV], tp=(64 * q, 64 * q))
                    kb.cp("act", Wu[:, hf * 4:hf * 4 + 4, :], pw3, [pw], [Wu])
                if self.m0_stop == "prep6":
                    continue
                for hf in range(2):
                    pu = kb.ps()
                    pu3 = pu[:, :].rearrange("p (n c) -> p n c", n=4)
                    for n in range(4):
                        for q in range(2):
                            kb.mm(pu, pu3[H[q], n, :], Wu[H[q], hf * 4 + n, 0:64], KQ[H[q], hf * 4 + n, :], [Wu, KQ], tp=(64 * q, 64 * q))
                    kb.cp("act", UT[j][:, hf * 4:hf * 4 + 4, :], pu3[:, :, 0:64], [pu], [UT[j]])
                    kb.cp("act", Xs[:], pu3[:, :, 64:128], [pu], [Xs])
                    kb.tt("pool", RT[j][:, hf * 4:hf * 4 + 4, :], qdec[:, hf * 4:hf * 4 + 4, :], Xs[:], ALU.subtract, [qdec, Xs], [RT[j]])
                po0 = kb.ps()
                pq_ = kb.ps()
                po03 = po0[:, :].rearrange("p (n c) -> p n c", n=8)
                pq3 = pq_[:, :].rearrange("p (n c) -> p n c", n=8)
                for n in range(8):
                    for q in range(2):
                        kb.mm(po0, po03[H[q], n, :], Wu[H[q], n, 64:128], KQ[H[q], n, 64:128], [Wu, KQ], tp=(64 * q, 64 * q))
                for n in range(8):
                    for q in range(2):
                        kb.mm(pq_, pq3[H[q], n, :], KQ[H[q], n, 0:64], Wu[H[q], n, 64:128], [Wu, KQ], tp=(64 * q, 64 * q))
                kb.cp("act", O0[j][:], po03, [po0], [O0[j]])
                kb.cp("act", Qs[j][:], pq3, [pq_], [Qs[j]])

            if self.m0_stop is not None and self.m0_stop.startswith("prep"):
                continue
            for j in range(2):
                pO = kb.pst[6 + j]
                pO3 = pO[:, :].rearrange("p (n c) -> p n c", n=8)
                for n in range(8):
                    for q in range(2):
                        kb.mm(pO, pO3[H[q], n, :], Sbf[j][H[q], :], RT[j][H[q], n, :], [Sbf[j], RT[j]], tp=(64 * q, 64 * q))
                    pS = kb.ps()
                    for q in range(2):
                        kb.mm(pS, pS[H[q], 0:64], UT[j][H[q], n, :], Sbf[j][H[q], :], [Sbf[j], UT[j]], tp=(64 * q, 64 * q))
                    kb.stt("dve", pre[:], S32[j][:], EGLc[:, j, n:n + 1], Qs[j][:, n, :], ALU.mult, ALU.add, [S32[j], EGLc, Qs[j]], [pre])
                    kb.cp("act", cS[:], pS[:, 0:64], [pS], [cS])
                    kb.tt("dve", S32[j][:], pre[:], cS[:], ALU.subtract, [pre, cS], [S32[j]])
                    kb.cp("act", Sbf[j][:], S32[j][:], [S32[j]], [Sbf[j]])
                kb.cp("act", oT[:], pO3, [pO], [oT])
                kb.tt("dve", oT[:], oT[:], O0[j][:], ALU.add, [oT, O0[j]], [oT])
                o2 = oT[:].rearrange("p n c -> p (n c)")
                kb.act(sqb[:], o2, AF.Square, [oT], [sqb])
                p = kb.ps()
                kb.mm(p, p[:, :], c["blk1"][:], sqb[:], [c["blk1"], sqb])
                kb.act(rstd[:], p[:, :], AF.Sqrt, [p, c["eps"]], [rstd], scale=1.0 / 64, bias=c["eps"][:, 0:1])
                kb.op("dve", lambda e: e.reciprocal(out=rstd[:], in_=rstd[:]), reads=[rstd], writes=[rstd])
                kb.stt("dve", ysil[:], o2, dnw[:, 0:1], rstd[:], ALU.mult, ALU.mult, [oT, dnw, rstd], [ysil])
                kb.tt("dve", yo[:], ysil[:], gates[:, j, :], ALU.mult, [ysil, gates], [yo])
                kb.dma("sp", self.y0s[1][j * 128:(j + 1) * 128, tg * 512:(tg + 1) * 512], yo[:], reads=[yo], writes=[self.y0s[1]], grp="y")
                if self.debug:
                    kb.dma("sp", self.dbg_o[j * 128:(j + 1) * 128, tg * 512:(tg + 1) * 512], o2, reads=[oT], writes=[self.dbg_o], grp="y")

    def gather(self, snd, gth):
        for a, b in zip(snd, gth):
            self.kb.collective("AllGather", self.groups, a, b, a.h.ap()[:, :], b.h.ap()[:, :])

    def outproj(self, gth, nk, wout):
        kb, c = self.kb, self.c
        sb = kb.sb
        kb.push()
        wo = sb("wo_%d" % nk, [128, nk, D], BF16)
        stg = [sb("wostg%d_%d" % (i, nk), [128, D], F32) for i in range(2)]
        for k in range(nk):
            st = stg[k % 2]
            kb.dma("sp", st[:], wout[k * 128:(k + 1) * 128, :], writes=[st])
            kb.tt("pool", wo[:, k, :], st[:], self.mods[:, 2, :], ALU.mult, [st, self.mods], [wo])
        ya = [sb("ya%d_%d" % (i, nk), [128, nk, 128], BF16) for i in range(2)]
        yb_ = [sb("yb%d_%d" % (i, nk), [128, nk, 128], BF16) for i in range(2)]
        ytmp = sb("ytmp_%d" % nk, [128, nk, 128], F32)
        yown = [sb("yown%d_%d" % (i, nk), [128, nk, 128], BF16) for i in range(2)]
        nb = len(gth)
        kpb = nk // nb
        for tt in range(16):
            A, B, Y = ya[tt % 2], yb_[tt % 2], yown[tt % 2]
            for b in range(nb):
                g3 = gth[b].h.ap().rearrange("(k p) t -> p k t", p=128)
                kb.dma("sp", A[:, b * kpb:(b + 1) * kpb, :], g3[:, :, tt * 128:(tt + 1) * 128], reads=[gth[b]], writes=[A])
                kb.dma("sp", B[:, b * kpb:(b + 1) * kpb, :], g3[:, :, TH + tt * 128:TH + (tt + 1) * 128], reads=[gth[b]], writes=[B])
            kb.ts("pool", ytmp[:], A[:], c["flag"][:, 0:1], None, ALU.mult, None, [A, c["flag"]], [ytmp])
            kb.stt("dve", Y[:], B[:], c["flag"][:, 1:2], ytmp[:], ALU.mult, ALU.add, [B, c["flag"], ytmp], [Y])
            for hf in range(2):
                p = kb.ps()
                for k in range(nk):
                    kb.mm(p, p[:, :], Y[:, k, :], wo[:, k, hf * 512:(hf + 1) * 512], [Y, wo], start=(k == 0), stop=(k == nk - 1))
                xr = self.xres[tt]
                kb.tt("dve", xr[:, hf * 512:(hf + 1) * 512], p[:, :], xr[:, hf * 512:(hf + 1) * 512], ALU.add, [p, xr], [xr])
        kb.pop()

    def load_xres(self):
        kb = self.kb
        self.xres = [kb.sb("xres%d" % i, [128, D], F32) for i in range(16)]
        for tt in range(16):
            kb.dma("sp", self.xres[tt][:], self.xown[tt * 128:(tt + 1) * 128, :], writes=[self.xres[tt]])

    def norm_own(self, hnT):
        for tt in range(16):
            self.norm_tile(self.xres[tt][:], self.xres[tt], 1, 0, hnT[:, :, tt * 128:(tt + 1) * 128], hnT)

    def ffn_expert(self, hnT, wg, wu, wd, nff, tag, gates=None, e=0):
        kb, c = self.kb, self.c
        B = self.fb
        nch = nff // 128
        ngrp = (nch + 1) // 2
        for gi in range(ngrp):
            f0 = gi * 256
            fc = min(2, nch - gi * 2)
            it = self.fcnt
            self.fcnt += 1
            wgb, wub, st, wdb, h1 = B["wg"][it % 2], B["wu"][it % 2], B["stg"][it % 2], B["wd"][it % 2], B["h1"][it % 2]
            kb.dma("pool", wgb[:, :, 0:fc * 128], wg[:, f0:f0 + fc * 128].rearrange("(k p) n -> p k n", p=128), writes=[wgb])
            kb.dma("pool", wub[:, :, 0:fc * 128], wu[:, f0:f0 + fc * 128].rearrange("(k p) n -> p k n", p=128), writes=[wub])
            kb.dma("sp", st[:, 0:fc, :], wd[f0:f0 + fc * 128, :].rearrange("(cc p) n -> p cc n", p=128), writes=[st])
            for cc in range(fc):
                kb.tt("pool", wdb[:, cc, :], st[:, cc, :], self.mods[:, 2, :], ALU.mult, [st, self.mods], [wdb])
            for cc in range(fc):
                for tg in range(4):
                    pgt = kb.ps()
                    put = kb.ps()
                    for k in range(8):
                        kb.mm(pgt, pgt[:, :], wgb[:, k, cc * 128:(cc + 1) * 128], hnT[:, k, tg * 512:(tg + 1) * 512], [wgb, hnT],
                              start=(k == 0), stop=(k == 7))
                    for k in range(8):
                        kb.mm(put, put[:, :], wub[:, k, cc * 128:(cc + 1) * 128], hnT[:, k, tg * 512:(tg + 1) * 512], [wub, hnT],
                              start=(k == 0), stop=(k == 7))
                    sl = B["sil"][(cc * 4 + tg) % 2]
                    kb.act(sl[:], pgt[:, :], AF.Silu, [pgt], [sl])
                    kb.tt("dve", h1[:, cc, tg * 512:(tg + 1) * 512], put[:, :], sl[:], ALU.mult, [put, sl], [h1])
            for tt in range(16):
                xr = self.xres[tt]
                for hf in range(2):
                    p = kb.ps()
                    for cc in range(fc):
                        kb.mm(p, p[:, :], h1[:, cc, tt * 128:(tt + 1) * 128], wdb[:, cc, hf * 512:(hf + 1) * 512], [h1, wdb],
                              start=(cc == 0), stop=(cc == fc - 1))
                    if gates is None:
                        kb.tt("dve", xr[:, hf * 512:(hf + 1) * 512], p[:, :], xr[:, hf * 512:(hf + 1) * 512], ALU.add, [p, xr], [xr])
                    else:
                        ev = B["ev"][(tt * 2 + hf) % 2]
                        kb.act(ev[:], p[:, :], AF.Copy, [p, gates], [ev], scale=gates[:, tt, e:e + 1])
                        kb.tt("pool", xr[:, hf * 512:(hf + 1) * 512], ev[:], xr[:, hf * 512:(hf + 1) * 512], ALU.add, [ev, xr], [xr])

    def alloc_ffn(self, tag):
        kb = self.kb
        B = {}
        B["wg"] = [kb.sb("fwg%d_%s" % (i, tag), [128, 8, 256], BF16) for i in range(2)]
        B["wu"] = [kb.sb("fwu%d_%s" % (i, tag), [128, 8, 256], BF16) for i in range(2)]
        B["stg"] = [kb.sb("fst%d_%s" % (i, tag), [128, 2, D], F32) for i in range(2)]
        B["wd"] = [kb.sb("fwd%d_%s" % (i, tag), [128, 2, D], BF16) for i in range(2)]
        B["h1"] = [kb.sb("fh1%d_%s" % (i, tag), [128, 2, TH], BF16) for i in range(2)]
        B["sil"] = [kb.sb("fsil%d_%s" % (i, tag), [128, 512], F32) for i in range(2)]
        B["ev"] = [kb.sb("fev%d_%s" % (i, tag), [128, 512], F32) for i in range(2)]
        self.fb = B
        self.fcnt = 0

    def ffn0(self):
        kb = self.kb
        kb.push()
        hnT = kb.sb("hnTf0", [128, 8, TH], BF16)
        self.norm_own(hnT)
        self.alloc_ffn("f0")
        self.ffn_expert(hnT, self.wg0.h.ap(), self.wu0.h.ap(), self.wd0.h.ap(), 2816, "f0")
        kb.pop()

    def moe(self):
        kb, c = self.kb, self.c
        kb.push()
        hnT = kb.sb("hnTf1", [128, 8, TH], BF16)
        self.norm_own(hnT)
        rwf = kb.sb("rwf", [128, 8, 8], F32)
        rwb = kb.sb("rwb", [128, 8, 8], BF16)
        kb.dma("sp", rwf[:], self.rw[:, :, :], writes=[rwf])
        kb.cp("dve", rwb[:], rwf[:], [rwf], [rwb])
        rbb = kb.sb("rbb", [128, 8], F32)
        kb.dma("sp", rbb[:], bc(self.rb[0:1, :], [128, 8]), writes=[rbb])
        lg = kb.sb("lg", [128, 16, 8], F32)
        p = kb.ps()
        for tt in range(16):
            for k in range(8):
                kb.mm(p, p[:, tt * 8:(tt + 1) * 8], hnT[:, k, tt * 128:(tt + 1) * 128], rwb[:, k, :], [hnT, rwb], start=(k == 0), stop=(k == 7))
        kb.cp("act", lg[:].rearrange("p a b -> p (a b)"), p[:, 0:128], [p], [lg])
        kb.tt("dve", lg[:], lg[:], bc(rbb[:].unsqueeze(1), [128, 16, 8]), ALU.add, [lg, rbb], [lg])
        m1 = kb.sb("m1", [128, 16], F32)
        m2 = kb.sb("m2", [128, 16], F32)
        eq1 = kb.sb("eq1", [128, 16, 8], F32)
        eq2 = kb.sb("eq2", [128, 16, 8], F32)
        lg2 = kb.sb("lg2", [128, 16, 8], F32)
        w1 = kb.sb("w1", [128, 16], F32)
        w2 = kb.sb("w2", [128, 16], F32)
        gates = kb.sb("gates1", [128, 16, 8], F32)
        kb.op("dve", lambda e: e.reduce_max(out=m1[:], in_=lg[:], axis=AX.X), reads=[lg], writes=[m1])
        kb.tt("dve", eq1[:], lg[:], bc(m1[:].unsqueeze(2), [128, 16, 8]), ALU.is_equal, [lg, m1], [eq1])
        kb.stt("dve", lg2[:], eq1[:], NEG, lg[:], ALU.mult, ALU.add, [eq1, lg], [lg2])
        kb.op("dve", lambda e: e.reduce_max(out=m2[:], in_=lg2[:], axis=AX.X), reads=[lg2], writes=[m2])
        kb.tt("dve", eq2[:], lg2[:], bc(m2[:].unsqueeze(2), [128, 16, 8]), ALU.is_equal, [lg2, m2], [eq2])
        kb.tt("dve", w2[:], m2[:], m1[:], ALU.subtract, [m1, m2], [w2])
        kb.act(w1[:], w2[:], AF.Sigmoid, [w2], [w1], scale=-1.0)
        kb.act(w2[:], w2[:], AF.Sigmoid, [w2], [w2])
        kb.tt("dve", eq1[:], eq1[:], bc(w1[:].unsqueeze(2), [128, 16, 8]), ALU.mult, [eq1, w1], [eq1])
        kb.tt("dve", eq2[:], eq2[:], bc(w2[:].unsqueeze(2), [128, 16, 8]), ALU.mult, [eq2, w2], [eq2])
        kb.tt("dve", gates[:], eq1[:], eq2[:], ALU.add, [eq1, eq2], [gates])
        if self.moeonly:
            kb.dma("sp", self.dbg_g[:, :], gates[:].rearrange("p a b -> p (a b)"), reads=[gates], writes=[self.dbg_g])
        self.alloc_ffn("f1")
        for e in range(self.nexp):
            self.ffn_expert(hnT, self.mwg.h.ap()[e], self.mwu.h.ap()[e], self.mwd.h.ap()[e], 3584, "f1", gates=gates, e=e)
        kb.pop()

    def mixer1(self):
        kb, c = self.kb, self.c
        sb = kb.sb
        kb.push()
        hnT = sb("hnTm1", [128, 8, TH], BF16)
        self.norm_own(hnT)
        for b in range(2):
            kb.dma("sp", self.h1s[b].h.ap().rearrange("(k p) t -> p k t", p=128), hnT[:, 4 * b:4 * b + 4, :], reads=[hnT], writes=[self.h1s[b]])
        self.gather(self.h1s, self.h1g)
        w1 = sb("w1m", [128, 8, NC1], BF16)
        for i in range(0, NC1, 512):
            j = min(i + 512, NC1)
            kb.dma("pool", w1[:, :, i:j], self.win1[:, i:j].rearrange("(k p) n -> p k n", p=128), writes=[w1])
        sm = sb("l1smc", [128, 4, 8], F32)
        kb.dma("sp", sm[:], self.l1sm[:, :, :], writes=[sm])
        scw = sb("scwc", [128, 2, 3], F32)
        kb.dma("sp", scw[:], self.scw[:, :, :], writes=[scw])
        gwf = sb("gwf", [128, 8, 128], F32)
        gwb = sb("gwb", [128, 8, 128], BF16)
        kb.dma("sp", gwf[:, 0:4, :], self.gaw.h.ap().rearrange("n i j -> i n j"), writes=[gwf])
        kb.dma("sp", gwf[:, 4:8, :], self.gxw.h.ap().rearrange("n i j -> i n j"), writes=[gwf])
        kb.cp("dve", gwb[:], gwf[:], [gwf], [gwb])
        cj = sb("cj", [128, 4], F32)
        kb.act(cj[:], sm[:, :, 7], AF.Exp, [sm], [cj], scale=-1.0)
        kb.act(cj[:], cj[:], AF.Ln, [cj, c["one"]], [cj], bias=c["one"][:, 0:1])
        kb.ts("dve", cj[:], cj[:], -8.0, None, ALU.mult, None, [cj], [cj])
        hin = sb("hin1", [128, 8, 512], BF16)
        xp1 = [sb("xp1_%d" % i, [128, 3 + 512], F32) for i in range(4)]
        xp2 = [sb("xp2_%d" % i, [128, 2 + 512], F32) for i in range(2)]
        for t_ in xp1 + xp2:
            kb.op("pool", lambda e, t_=t_: e.memset(t_[:, 0:3], 0.0), writes=[t_])
        hprev = sb("hprev", [128, 4], F32)
        kb.op("pool", lambda e: e.memset(hprev[:], 0.0), writes=[hprev])
        F = lambda n: sb(n, [128, 512], F32)
        xcv, rg, ig, av, bv, hs, yv, y2, gl, cdv = F("xcv"), F("rg"), F("ig"), F("av"), F("bv"), F("hs"), F("yv"), F("y2"), F("gl"), F("cdv")
        xcb = sb("xcb", [128, 512], BF16)
        yo = sb("yo1", [128, 512], BF16)
        g3 = [self.h1g[b].h.ap().rearrange("(r k p) t -> r p k t", r=2, p=128) for b in range(2)]

        def inproj(ci):
            p = kb.ps()
            for k in range(8):
                kb.mm(p, p[:, :], w1[:, k, ci * 128:(ci + 1) * 128], hin[:, k, :], [w1, hin], start=(k == 0), stop=(k == 7))
            return p

        for tg in range(self.ntg):
            for b in range(2):
                kb.dma("sp", hin[:, 4 * b:4 * b + 4, :], g3[b][tg // 4, :, :, (tg % 4) * 512:(tg % 4 + 1) * 512], reads=[self.h1g[b]], writes=[hin])
            for i in range(4):
                p = inproj(i)
                xp = xp1[i]
                if tg > 0:
                    kb.cp("pool", xp[:, 0:3], xp[:, 512:515], [xp], [xp])
                kb.cp("act", xp[:, 3:515], p[:, :], [p], [xp])
                kb.ts("dve", xcv[:], xp[:, 0:512], sm[:, i, 0:1], sm[:, i, 4:5], ALU.mult, ALU.add, [xp, sm], [xcv])
                for k in range(1, 4):
                    kb.stt("dve", xcv[:], xp[:, k:k + 512], sm[:, i, k:k + 1], xcv[:], ALU.mult, ALU.add, [xp, sm, xcv], [xcv])
                kb.cp("act", xcb[:], xcv[:], [xcv], [xcb])
                if self.m1_stop == "conv":
                    continue
                pr = kb.ps()
                pi = kb.ps()
                kb.mm(pr, pr[:, :], gwb[:, i, :], xcb[:], [gwb, xcb])
                kb.mm(pi, pi[:, :], gwb[:, 4 + i, :], xcb[:], [gwb, xcb])
                kb.act(rg[:], pr[:, :], AF.Sigmoid, [pr, sm], [rg], bias=sm[:, i, 5:6])
                kb.act(ig[:], pi[:, :], AF.Sigmoid, [pi, sm], [ig], bias=sm[:, i, 6:7])
                if self.m1_stop == "gate":
                    continue
                kb.act(av[:], rg[:], AF.Exp, [rg, cj], [av], scale=cj[:, i:i + 1])
                kb.tt("pool", bv[:], av[:], av[:], ALU.mult, [av], [bv])
                kb.ts("dve", bv[:], bv[:], -1.0, 1.0, ALU.mult, ALU.add, [bv], [bv])
                kb.act(bv[:], bv[:], AF.Sqrt, [bv], [bv])
                kb.tt("pool", bv[:], bv[:], ig[:], ALU.mult, [bv, ig], [bv])
                kb.tt("dve", bv[:], bv[:], xcv[:], ALU.mult, [bv, xcv], [bv])
                if self.m1_stop == "ab":
                    continue
                kb.op("dve", lambda e, i=i: e.tensor_tensor_scan(out=hs[:], data0=av[:], data1=bv[:], initial=hprev[:, i:i + 1],
                                                                 op0=ALU.mult, op1=ALU.add), reads=[av, bv, hprev], writes=[hs])
                kb.cp("dve", hprev[:, i:i + 1], hs[:, 511:512], [hs], [hprev])
                if self.m1_stop == "scan":
                    continue
                p = inproj(4 + i)
                kb.cp("act", yv[:], p[:, :], [p], [yv])
                kb.tt("pool", y2[:], yv[:], yv[:], ALU.mult, [yv], [y2])
                kb.ts("dve", y2[:], y2[:], 0.044715, 1.0, ALU.mult, ALU.add, [y2], [y2])
                kb.tt("pool", y2[:], y2[:], yv[:], ALU.mult, [y2, yv], [y2])
                kb.act(gl[:], y2[:], AF.Sigmoid, [y2], [gl], scale=1.5957691216057308)
                kb.tt("pool", gl[:], gl[:], yv[:], ALU.mult, [gl, yv], [gl])
                kb.tt("dve", yo[:], gl[:], hs[:], ALU.mult, [gl, hs], [yo])
                kb.dma("sp", self.y1s[i // 2][(i % 2) * 128:(i % 2 + 1) * 128, tg * 512:(tg + 1) * 512], yo[:], reads=[yo], writes=[self.y1s[i // 2]])
            if self.m1_stop in ("conv", "gate", "ab", "scan", "lru"):
                continue
            for i in range(2):
                pc = inproj(10 + i)
                ph = inproj(12 + i)
                xp = xp2[i]
                if tg > 0:
                    kb.cp("pool", xp[:, 0:2], xp[:, 512:514], [xp], [xp])
                kb.cp("act", cdv[:], pc[:, :], [pc], [cdv])
                kb.tt("dve", xp[:, 2:514], ph[:, :], cdv[:], ALU.mult, [ph, cdv], [xp])
                kb.ts("dve", cdv[:], xp[:, 0:512], scw[:, i, 0:1], None, ALU.mult, None, [xp, scw], [cdv])
                for k in range(1, 3):
                    kb.stt("dve", cdv[:], xp[:, k:k + 512], scw[:, i, k:k + 1], cdv[:], ALU.mult, ALU.add, [xp, scw, cdv], [cdv])
                pb_ = inproj(8 + i)
                kb.tt("dve", yo[:], pb_[:, :], cdv[:], ALU.mult, [pb_, cdv], [yo])
                kb.dma("sp", self.y1s[2][i * 128:(i + 1) * 128, tg * 512:(tg + 1) * 512], yo[:], reads=[yo], writes=[self.y1s[2]])
        kb.pop()

    def final_norm(self):
        kb, c = self.kb, self.c
        s = self.scr
        kb.push()
        fw = kb.sb("fwrow", [128, D], F32)
        kb.dma("sp", fw[:], bc(self.fnw[0:1, :], [128, D]), writes=[fw])
        ot = [kb.sb("ot%d" % i, [128, D], F32) for i in range(2)]
        for tt in range(16):
            xr = self.xres[tt]
            kb.act(s["junk"][:], xr[:], AF.Square, [xr], [s["junk"], s["ss"]], accum_out=s["ss"][:])
            kb.act(s["rs"][:], s["ss"][:], AF.Sqrt, [s["ss"], c["eps"]], [s["rs"]], scale=1.0 / D, bias=c["eps"][:, 0:1])
            kb.op("dve", lambda e: e.reciprocal(out=s["rs"][:], in_=s["rs"][:]), reads=[s["rs"]], writes=[s["rs"]])
            o = ot[tt % 2]
            kb.stt("dve", o[:], xr[:], s["rs"][:, 0:1], fw[:], ALU.mult, ALU.mult, [xr, s["rs"], fw], [o])
            kb.dma("sp", self.out[tt * 128:(tt + 1) * 128, :], o[:], reads=[o], writes=[self.out])
        kb.pop()

    def build(self):
        kb = self.kb
        self.declare()
        if self.debug and self.stop_after != "adaln":
            self.dbg_o = self.outp("dbg_o", [256, T])
            self.dbg_y0 = self.outp("dbg_y0", [512, T], BF16)
        self.consts()
        self.alloc_scr()
        if self.moeonly:
            kb.op("dve", lambda e: e.memset(self.mods[:], 1.0), writes=[self.mods])
            self.load_xres()
            self.moe()
            return self.dump_x()
        if self.m1only:
            kb.op("dve", lambda e: e.memset(self.mods[:], 1.0), writes=[self.mods])
            self.load_xres()
            self.mixer1()
            if self.m1_stop is None:
                self.gather(self.y1s, self.y1g)
                self.outproj(self.y1g, 12, self.wout1)
            return self.dump_x()
        if self.skip_ada:
            kb.op("dve", lambda e: e.memset(self.mods[:], 1.0), writes=[self.mods])
        else:
            self.adaln(0, 0)
        if self.stop_after == "adaln":
            self.dbg_mods = self.outp("dbg_mods", [128, 3 * D])
            kb.dma("sp", self.dbg_mods[:, :], self.mods[:].rearrange("p a b -> p (a b)"), reads=[self.mods], writes=[self.dbg_mods], grp="y")
            self.finish()
            return
        self.mixer0()
        kb.pop()
        if self.stop_after != "mixer0":
            self.gather(self.y0s, self.y0g)
            self.load_xres()
            self.outproj(self.y0g, 8, self.wout0)
            if self.stop_after == "op0":
                return self.dump_x()
            self.adaln(0, 1)
            self.ffn0()
            if self.stop_after == "ffn0":
                return self.dump_x()
            self.adaln(1, 0)
            self.mixer1()
            self.gather(self.y1s, self.y1g)
            self.outproj(self.y1g, 12, self.wout1)
            if self.stop_after == "premoe":
                return self.dump_x()
            self.adaln(1, 1)
            self.moe()
            self.final_norm()
            self.finish()
            return
        if self.stop_after == "mixer0":
            stg = kb.sb("stg", [128, 4, 512], BF16)
            for tg in range(self.ntg):
                for b in range(2):
                    kb.dma("sp", stg[:, 2 * b:2 * b + 2, :], self.y0s[b].h.ap()[:, tg * 512:(tg + 1) * 512].rearrange("(a p) t -> p a t", p=128),
                           reads=[self.y0s[b]], writes=[stg], grp="x")
                kb.dma("sp", self.dbg_y0.h.ap()[:, tg * 512:(tg + 1) * 512].rearrange("(a p) t -> p a t", p=128), stg[:],
                       reads=[stg], writes=[self.dbg_y0], grp="y")
            self.finish()
            return

    def dump_x(self):
        kb = self.kb
        for tt in range(16):
            kb.dma("sp", self.out[tt * 128:(tt + 1) * 128, :], self.xres[tt][:], reads=[self.xres[tt]], writes=[self.out])
        self.finish()

    def finish(self):
        kb = self.kb
        kb.barrier()


def bucket_tab():
    dist = np.arange(128)
    max_exact = 16
    d = np.maximum(dist, 0)
    large = max_exact + (np.log(np.maximum(d, 1) / max_exact) / np.log(128 / max_exact) * (32 - max_exact)).astype(np.int32)
    large = np.minimum(large, 31)
    return np.where(d < max_exact, d, large).astype(np.int32)


def host_inputs(inputs, c):
    b, half = c // 2, c % 2
    f32 = np.float32
    m = {}
    x = inputs["x"]
    m["xfull"] = np.ascontiguousarray(x[b])
    m["xown"] = np.ascontiguousarray(x[b, half * TH:(half + 1) * TH])
    fl = np.zeros((128, 2), f32)
    fl[:, 0] = 1.0 - half
    fl[:, 1] = half
    m["flag"] = fl
    m["cvec"] = np.ascontiguousarray(inputs["c"][b].reshape(8, 128).T)
    m["adaw"] = inputs["ada_w"]
    m["adab"] = inputs["ada_b"]
    m["nmw"] = inputs["norm_mix_w"]
    m["nfw"] = inputs["norm_ffn_w"]
    m["fnw"] = inputs["final_norm_w"].reshape(1, D)
    m["ident"] = np.eye(128, dtype=f32)
    r = np.arange(64)[:, None]
    cc = np.arange(64)[None, :]
    cm = np.zeros((128, 4, 64), f32)
    for q in range(2):
        cm[q * 64:(q + 1) * 64, 0] = np.where(cc >= r, 0.0, NEG)
        cm[q * 64:(q + 1) * 64, 1] = np.where(r > cc, 0.0, NEG)
        cm[q * 64:(q + 1) * 64, 2] = np.eye(64)
    m["cmask"] = cm
    sel = np.zeros((4, 2, 128), f32)
    for q in range(2):
        for j in range(2):
            sel[2 * q + j, j, q * 64:(q + 1) * 64] = 1.0
    m["sel"] = sel
    blk = np.zeros((128, 128), f32)
    blk[:64, :64] = 1.0
    blk[64:, 64:] = 1.0
    m["blk1"] = blk
    w = inputs["ab_w_in"][0]
    HA = lambda q, j: 4 * half + 2 * q + j
    cols = []
    for j in range(2):
        for q in range(2):
            cols += list(range(HA(q, j) * 64, HA(q, j) * 64 + 64))
    cols += list(range(512 + half * 64, 512 + half * 64 + 64)) * 2
    dncols = []
    for base in (768, 768 + 512, 768 + 1024, 2304):
        for j in range(2):
            for q in range(2):
                cols += list(range(base + HA(q, j) * 64, base + HA(q, j) * 64 + 64))
                if base < 2304:
                    dncols += list(range(base - 768 + HA(q, j) * 64, base - 768 + HA(q, j) * 64 + 64))
    cols += list(range(640 + half * 64, 640 + half * 64 + 64))
    cols += [2816 + 4 * half + hl for hl in range(4)]
    cols += [2824 + 4 * half + hl for hl in range(4)]
    assert len(cols) == NC0
    m["win0"] = np.ascontiguousarray(w[:, cols])
    cw = inputs["dn_conv_w"][0][:, dncols]
    m["cw0"] = np.ascontiguousarray(cw.reshape(4, 6, 128).transpose(2, 1, 0))
    hl = [4 * half + i for i in range(4)]
    m["dnsm"] = np.ascontiguousarray(np.stack([inputs["dn_a_log"][0][hl], inputs["dn_dt_bias"][0][hl]], axis=1))
    m["dnw"] = np.ascontiguousarray(np.tile(inputs["dn_norm_w"][0], 2).reshape(128, 1))
    sk = np.zeros((128, 2), f32)
    for q in range(2):
        for j in range(2):
            sk[q * 64:(q + 1) * 64, j] = inputs["attn_sinks"][0][HA(q, j)]
    m["sinkl"] = sk
    bt = bucket_tab()
    s_ = np.arange(128)[:, None]
    qi = np.arange(128)[None, :]
    bg = np.zeros((2, 128, 4, 128), f32)
    am = np.zeros((2, 128, 128), f32)
    for a in range(2):
        dist = qi + 128 - (s_ + 128 * a)
        valid = (dist >= 0) & (dist < 128)
        bk = bt[np.clip(dist, 0, 127)]
        am[a] = np.where(valid, 0.0, NEG)
        for q in range(2):
            for j in range(2):
                bg[a, :, q * 2 + j, :] = inputs["rel_bias"][bk, HA(q, j)]
    m["biasg"] = bg.reshape(2, 128, 512)
    m["amask"] = am
    rows = []
    for base in (0, 512):
        for r_ in range(2):
            for j in range(2):
                for q in range(2):
                    Hh = 4 * r_ + 2 * q + j
                    rows += list(range(base + Hh * 64, base + Hh * 64 + 64))
    m["wout0"] = np.ascontiguousarray(inputs["ab_w_out"][0][rows])
    m["wg0"] = inputs["ffn_w_gate"][0]
    m["wu0"] = inputs["ffn_w_up"][0]
    m["wd0"] = inputs["ffn_w_down"][0]
    w1 = inputs["cd_w_in"][0]
    cols = []
    for base in (0, 1024):
        for i in range(4):
            cols += list(range(base + (4 * half + i) * 128, base + (4 * half + i + 1) * 128))
    for base in (2048, 2560, 3072):
        for i in range(2):
            cols += list(range(base + (2 * half + i) * 128, base + (2 * half + i + 1) * 128))
    assert len(cols) == NC1
    m["win1"] = np.ascontiguousarray(w1[:, cols])
    ch = np.arange(512) + 512 * half
    sm = np.zeros((128, 4, 8), f32)
    sm[:, :, 0:4] = inputs["lru_conv_w"][0][:, ch].reshape(4, 4, 128).transpose(2, 1, 0)
    sm[:, :, 4] = inputs["lru_conv_b"][0][ch].reshape(4, 128).T
    sm[:, :, 5] = inputs["lru_gate_a_b"][0][ch].reshape(4, 128).T
    sm[:, :, 6] = inputs["lru_gate_x_b"][0][ch].reshape(4, 128).T
    sm[:, :, 7] = inputs["lru_lambda"][0][ch].reshape(4, 128).T
    m["l1sm"] = sm
    m["gaw"] = np.ascontiguousarray(inputs["lru_gate_a_w"][0][4 * half:4 * half + 4])
    m["gxw"] = np.ascontiguousarray(inputs["lru_gate_x_w"][0][4 * half:4 * half + 4])
    sc = inputs["sconv_w"][0][:, 256 * half:256 * half + 256]
    m["scw"] = np.ascontiguousarray(sc.reshape(3, 2, 128).transpose(2, 1, 0))
    rows = []
    for b_ in range(2):
        for r_ in range(2):
            for i in (2 * b_, 2 * b_ + 1):
                rows += list(range((4 * r_ + i) * 128, (4 * r_ + i + 1) * 128))
    for r_ in range(2):
        for i in range(2):
            rows += list(range(1024 + (2 * r_ + i) * 128, 1024 + (2 * r_ + i + 1) * 128))
    m["wout1"] = np.ascontiguousarray(inputs["cd_w_out"][0][rows])
    m["rw"] = np.ascontiguousarray(inputs["moe_router_w"][0].reshape(8, 128, 8).transpose(1, 0, 2))
    m["rb"] = inputs["moe_router_b"][0].reshape(1, 8)
    m["mwg"] = inputs["moe_w_gate"][0]
    m["mwu"] = inputs["moe_w_up"][0]
    m["mwd"] = inputs["moe_w_down"][0]
    return {k: np.ascontiguousarray(v, dtype=np.float32) for k, v in m.items()}


def run(inputs, stop_after=None, debug=False, **kw):
    pg = Prog(stop_after=stop_after, debug=debug, **kw)
    pg.build()
    in_maps = []
    for c in range(8):
        hm = host_inputs(inputs, c)
        in_maps.append({k: hm[k] for k in pg.inputs})
    res = run_bass_kernel_spmd(pg.kb.nc, in_maps, core_ids=list(range(8)))
    return res, pg


def kernel(**inputs):
    inputs = {k: np.asarray(v) for k, v in inputs.items()}
    res, pg = run(inputs)
    out = np.zeros((4, T, D), np.float32)
    for c in range(8):
        out[c // 2, (c % 2) * TH:(c % 2 + 1) * TH] = res.results[c]["out"]
    return out
```

```python
import numpy as np
import concourse.bass as bass
import concourse.mybir as mybir
from concourse.bass_utils import run_bass_kernel_spmd

F32 = mybir.dt.float32
BF16 = mybir.dt.bfloat16
AF = mybir.ActivationFunctionType
ALU = mybir.AluOpType
AX = mybir.AxisListType

T = 4096
TH = 2048
D = 1024
EPS = 1e-6
NEG = -30000.0
PAIRS = [[0, 1], [2, 3], [4, 5], [6, 7]]


class Tl:
    __slots__ = ("h", "name", "w", "r", "ds")

    def __init__(self, h, name):
        self.h = h
        self.name = name
        self.w = None
        self.r = {}
        self.ds = None

    def __getitem__(self, k):
        return self.h[k]


class Eng:
    def __init__(self, name, handle, sem):
        self.name = name
        self.h = handle
        self.sem = sem
        self.cnt = 0
        self.waited = {}

    def wait(self, ev):
        sem, val, _ = ev
        k = id(sem)
        if self.waited.get(k, 0) >= val:
            return
        self.waited[k] = val
        self.h.wait_ge(sem, val)


class KB:
    def __init__(self):
        self.nc = bass.Bass("TRN2", target_bir_lowering=False)
        nc = self.nc
        self.E = {}
        for n, h in (("pe", nc.tensor), ("act", nc.scalar), ("dve", nc.vector),
                     ("pool", nc.gpsimd), ("sp", nc.sync)):
            self.E[n] = Eng(n, h, nc.alloc_semaphore("sem_" + n))
        self.dall = []
        self.dfree = {}
        self.ninst = 0
        self.nps = 0
        self.pst = [Tl(nc.alloc_psum_tensor("ps%d" % i, [128, 512], F32), "ps%d" % i) for i in range(8)]
        self.nrot = 6

    def sb(self, name, shape, dt=F32):
        if not hasattr(self, "scopes"):
            self.scopes = [[]]
        cm = self.nc.sbuf_tensor(name, list(shape), dt)
        h = cm.__enter__()
        t = Tl(h, name)
        self.scopes[-1].append((cm, t))
        return t

    def push(self):
        if not hasattr(self, "scopes"):
            self.scopes = [[]]
        self.scopes.append([])

    def pop(self):
        self.barrier()
        for cm, t in reversed(self.scopes.pop()):
            if t.ds is not None:
                self.dfree.setdefault(t.ds[2], []).append(t.ds)
                t.ds = None
            cm.__exit__(None, None, None)

    def ps(self):
        t = self.pst[self.nps % self.nrot]
        self.nps += 1
        return t

    def dram(self, name, shape, dt, kind="Internal"):
        return Tl(self.nc.dram_tensor(name, list(shape), dt, kind=kind), name)

    def _deps(self, eng, reads, writes):
        E = self.E[eng]
        for t in reads:
            if t.w is not None:
                ev = t.w
                if ev[2] == eng and eng == "pe":
                    continue
                E.wait(ev)
        for t in writes:
            if t.w is not None and not (t.w[2] == eng and eng == "pe"):
                E.wait(t.w)
            for ev in t.r.values():
                if not (ev[2] == eng and eng == "pe"):
                    E.wait(ev)

    def _commit(self, ev, reads, writes):
        for t in reads:
            t.r[id(ev[0])] = ev
        for t in writes:
            t.w = ev
            t.r = {}

    def op(self, eng, fn, reads=(), writes=(), sig=True):
        E = self.E[eng]
        self._deps(eng, reads, writes)
        ins = fn(E.h)
        self.ninst += 1
        if sig:
            E.cnt += 1
            ins.then_inc(E.sem, 1)
            ev = (E.sem, E.cnt, eng)
        else:
            ev = (E.sem, E.cnt + 1, eng)
        self._commit(ev, reads, writes)
        return ins

    def _dsem(self, t, kind):
        if t.ds is None:
            fl = self.dfree.setdefault(kind, [])
            if fl:
                t.ds = fl.pop()
            else:
                t.ds = [self.nc.alloc_semaphore("dsem%d" % len(self.dall)), 0, kind]
                self.dall.append(t.ds)
        assert t.ds[2] == kind, (t.name, t.ds[2], kind)
        return t.ds

    def dma(self, q, out_ap, in_ap, reads=(), writes=(), grp="d"):
        E = self.E[q]
        self._deps(q, reads, writes)
        d = self._dsem(writes[0], "sw" if q == "pool" else "hw")
        d[1] += 16
        E.h.dma_start(out=out_ap, in_=in_ap).then_inc(d[0], 16)
        self.ninst += 1
        self._commit((d[0], d[1], "dma"), reads, writes)

    def collective(self, kind, groups, in_t, out_t, in_ap, out_ap, grp="cc"):
        E = self.E["pool"]
        self._deps("pool", [in_t], [out_t])
        d = self._dsem(out_t, "cc")
        d[1] += 1
        E.h.collective_compute(kind, ALU.bypass, replica_groups=groups,
                               ins=[in_ap], outs=[out_ap]).then_inc(d[0])
        self._commit((d[0], d[1], "dma"), [in_t], [out_t])

    def barrier(self):
        evs = []
        for n, E in self.E.items():
            if E.cnt > 0:
                evs.append((E.sem, E.cnt, n))
        for d in self.dall:
            if d[1] > 0:
                evs.append((d[0], d[1], "dma"))
        for n, E in self.E.items():
            for ev in evs:
                if ev[2] != n:
                    E.wait(ev)

    def mm(self, pst, out, lhsT, rhs, reads, start=True, stop=True, tp=None):
        kw = {}
        if tp is not None:
            kw["tile_position"] = tp
        return self.op("pe", lambda e: e.matmul(out, lhsT, rhs, start=start, stop=stop, **kw),
                       reads=reads, writes=[pst], sig=stop)

    def tr(self, pst, out, in_, ident, reads, tp=None):
        kw = {}
        if tp is not None:
            kw["tile_position"] = tp
        return self.op("pe", lambda e: e.transpose(out, in_, ident, **kw), reads=reads, writes=[pst])

    def act(self, out, in_, func, reads, writes, eng="act", **kw):
        return self.op(eng, lambda e: e.activation(out=out, in_=in_, func=func, **kw), reads=reads, writes=writes)

    def tt(self, eng, out, a, b, op, reads, writes):
        return self.op(eng, lambda e: e.tensor_tensor(out=out, in0=a, in1=b, op=op), reads=reads, writes=writes)

    def ts(self, eng, out, a, s1, s2, op0, op1, reads, writes):
        if op1 is None:
            return self.op(eng, lambda e: e.tensor_scalar(out=out, in0=a, scalar1=s1, scalar2=None, op0=op0),
                           reads=reads, writes=writes)
        return self.op(eng, lambda e: e.tensor_scalar(out=out, in0=a, scalar1=s1, scalar2=s2, op0=op0, op1=op1),
                       reads=reads, writes=writes)

    def stt(self, eng, out, a, s, b, op0, op1, reads, writes):
        return self.op(eng, lambda e: e.scalar_tensor_tensor(out=out, in0=a, scalar=s, in1=b, op0=op0, op1=op1),
                       reads=reads, writes=writes)

    def cp(self, eng, out, in_, reads, writes):
        if eng == "act":
            return self.op(eng, lambda e: e.copy(out=out, in_=in_), reads=reads, writes=writes)
        return self.op(eng, lambda e: e.tensor_copy(out=out, in_=in_), reads=reads, writes=writes)


def bc(ap, shape):
    return ap.broadcast_to(list(shape))


NC0 = 11 * 128 + 64 + 8
NC1 = 14 * 128


class Prog:
    def __init__(self, stop_after=None, debug=False, ntg=8, m0_stop=None, skip_ada=False, m1only=False, m1_stop=None):
        self.m1only = m1only
        self.m1_stop = m1_stop
        self.moeonly = False
        self.ntg = ntg
        self.nexp = 8
        self.groups = PAIRS
        self.m0_stop = m0_stop
        self.skip_ada = skip_ada
        self.kb = KB()
        self.stop_after = stop_after
        self.debug = debug
        self.inputs = {}
        self.outputs = {}

    def inp(self, name, shape, dt=F32):
        t = self.kb.dram(name, shape, dt, kind="ExternalInput")
        self.inputs[name] = t
        return t

    def outp(self, name, shape, dt=F32):
        t = self.kb.dram(name, shape, dt, kind="ExternalOutput")
        self.outputs[name] = t
        return t

    def declare(self):
        I = self.inp
        self.xfull = I("xfull", [T, D])
        self.xown = I("xown", [TH, D])
        self.flag = I("flag", [128, 2])
        self.cvec = I("cvec", [128, 8])
        self.adaw = I("adaw", [2, D, 6 * D])
        self.adab = I("adab", [2, 6 * D])
        self.nmw = I("nmw", [2, D])
        self.nfw = I("nfw", [2, D])
        self.fnw = I("fnw", [1, D])
        self.ident = I("ident", [128, 128])
        self.cmask = I("cmask", [128, 4, 64])
        self.sel = I("sel", [4, 2, 128])
        self.blk1 = I("blk1", [128, 128])
        self.win0 = I("win0", [D, NC0])
        self.cw0 = I("cw0", [128, 6, 4])
        self.dnsm = I("dnsm", [4, 2])
        self.dnw = I("dnw", [128, 1])
        self.sinkl = I("sinkl", [128, 2])
        self.biasg = I("biasg", [2, 128, 512])
        self.amask = I("amask", [2, 128, 128])
        if self.stop_after in ("adaln", "mixer0") and not self.m1only:
            self._internal()
            return
        if self.moeonly:
            self.rw = I("rw", [128, 8, 8])
            self.rb = I("rb", [1, 8])
            self.mwg = I("mwg", [self.nexp, D, 3584])
            self.mwu = I("mwu", [self.nexp, D, 3584])
            self.mwd = I("mwd", [self.nexp, 3584, D])
            self.out = self.outp("out", [TH, D])
            self.dbg_g = self.outp("dbg_g", [128, 128])
            self._internal()
            return
        if self.m1only:
            self.win1 = I("win1", [D, NC1])
            self.l1sm = I("l1sm", [128, 4, 8])
            self.gaw = I("gaw", [4, 128, 128])
            self.gxw = I("gxw", [4, 128, 128])
            self.scw = I("scw", [128, 2, 3])
            self.wout1 = I("wout1", [1536, D])
            self.out = self.outp("out", [TH, D])
            self._internal()
            return
        self.wout0 = I("wout0", [D, D])
        if self.stop_after == "op0":
            self.out = self.outp("out", [TH, D])
            self._internal()
            return
        self.wg0 = I("wg0", [D, 2816])
        self.wu0 = I("wu0", [D, 2816])
        self.wd0 = I("wd0", [2816, D])
        if self.stop_after == "ffn0":
            self.out = self.outp("out", [TH, D])
            self._internal()
            return
        self.win1 = I("win1", [D, NC1])
        self.l1sm = I("l1sm", [128, 4, 8])
        self.gaw = I("gaw", [4, 128, 128])
        self.gxw = I("gxw", [4, 128, 128])
        self.scw = I("scw", [128, 2, 3])
        self.wout1 = I("wout1", [1536, D])
        if self.stop_after != "premoe":
            self.rw = I("rw", [128, 8, 8])
            self.rb = I("rb", [1, 8])
            self.mwg = I("mwg", [8, D, 3584])
            self.mwu = I("mwu", [8, D, 3584])
            self.mwd = I("mwd", [8, 3584, D])
        self.out = self.outp("out", [TH, D])
        self._internal()

    def _internal(self):
        kb = self.kb
        self.y0s = [kb.dram("y0s%d" % i, [256, T], BF16) for i in range(2)]
        self.y0g = [kb.dram("y0g%d" % i, [512, T], BF16) for i in range(2)]
        self.h1s = [kb.dram("h1s%d" % i, [512, TH], BF16) for i in range(2)]
        self.h1g = [kb.dram("h1g%d" % i, [1024, TH], BF16) for i in range(2)]
        self.y1s = [kb.dram("y1s%d" % i, [256, T], BF16) for i in range(3)]
        self.y1g = [kb.dram("y1g%d" % i, [512, T], BF16) for i in range(3)]

    def consts(self):
        kb = self.kb
        c = {}
        self.c = c
        c["idf"] = kb.sb("idf", [128, 128], F32)
        c["idb"] = kb.sb("idb", [128, 128], BF16)
        c["cm"] = kb.sb("cm", [128, 4, 64], F32)
        c["i64b"] = kb.sb("i64b", [128, 64], BF16)
        c["sel"] = kb.sb("selc", [4, 2, 128], F32)
        c["blk1f"] = kb.sb("blk1f", [128, 128], F32)
        c["blk1"] = kb.sb("blk1b", [128, 128], BF16)
        c["ones"] = kb.sb("onesb", [128, 128], BF16)
        c["flag"] = kb.sb("flagc", [128, 2], F32)
        c["cv"] = kb.sb("cv", [128, 8], F32)
        c["cond"] = kb.sb("cond", [128, 8], F32)
        c["condB"] = kb.sb("condB", [128, 8, 128], BF16)
        c["eps"] = kb.sb("epsc", [128, 1], F32)
        c["one"] = kb.sb("onec", [128, 1], F32)
        q = "sp"
        kb.dma(q, c["idf"][:], self.ident[:, :], writes=[c["idf"]], grp="c")
        kb.dma(q, c["cm"][:], self.cmask[:, :, :], writes=[c["cm"]], grp="c")
        kb.dma(q, c["sel"][:], self.sel[:, :, :], writes=[c["sel"]], grp="c")
        kb.dma(q, c["blk1f"][:], self.blk1[:, :], writes=[c["blk1f"]], grp="c")
        kb.dma(q, c["flag"][:], self.flag[:, :], writes=[c["flag"]], grp="c")
        kb.dma(q, c["cv"][:], self.cvec[:, :], writes=[c["cv"]], grp="c")
        kb.cp("dve", c["idb"][:], c["idf"][:], [c["idf"]], [c["idb"]])
        kb.cp("dve", c["blk1"][:], c["blk1f"][:], [c["blk1f"]], [c["blk1"]])
        kb.cp("dve", c["i64b"][:], c["cm"][:, 2, :], [c["cm"]], [c["i64b"]])
        kb.op("dve", lambda e: e.memset(c["ones"][:], 1.0), writes=[c["ones"]])
        kb.op("dve", lambda e: e.memset(c["eps"][:], EPS), writes=[c["eps"]])
        kb.op("dve", lambda e: e.memset(c["one"][:], 1.0), writes=[c["one"]])
        kb.act(c["cond"][:], c["cv"][:], AF.Silu, [c["cv"]], [c["cond"]])
        kb.cp("dve", c["condB"][:], bc(c["cond"][:].unsqueeze(2), [128, 8, 128]), [c["cond"]], [c["condB"]])
        self.mods = kb.sb("mods", [128, 3, D], F32)

    def adaln(self, l, part):
        kb, c = self.kb, self.c
        kb.push()
        self.wbuf = [kb.sb("wbuf%d_%d_%d" % (i, l, part), [128, 8, 512], BF16) for i in range(2)]
        self.rowt = kb.sb("rowt_%d_%d" % (l, part), [128, 512], F32)
        self.rowt2 = kb.sb("rowt2_%d_%d" % (l, part), [128, D], F32)
        for n in range(part * 6, part * 6 + 6):
            wb = self.wbuf[n % 2]
            kb.dma("pool", wb[:], self.adaw[l, :, n * 512:(n + 1) * 512].rearrange("(k p) n -> p k n", p=128),
                   writes=[wb], grp="w")
            kb.dma("sp", self.rowt[:], bc(self.adab[l:l + 1, n * 512:(n + 1) * 512], [128, 512]),
                   writes=[self.rowt], grp="c")
            p = kb.ps()
            for k in range(8):
                kb.mm(p, p[:, :], c["condB"][:, k, :], wb[:, k, :], [c["condB"], wb], start=(k == 0), stop=(k == 7))
            kb.tt("dve", self.mods[:, (n // 2) % 3, (n % 2) * 512:(n % 2) * 512 + 512], p[:, :], self.rowt[:], ALU.add,
                  [p, self.rowt], [self.mods])
        w = self.nmw if part == 0 else self.nfw
        kb.dma("sp", self.rowt2[:], bc(w[l:l + 1, :], [128, D]), writes=[self.rowt2], grp="c")
        kb.stt("dve", self.mods[:, 1, :], self.mods[:, 1, :], 1.0, self.rowt2[:], ALU.add, ALU.mult,
               [self.mods, self.rowt2], [self.mods])
        kb.pop()

    def norm_tile(self, xt_ap, xt_tl, ia, ib, hnT_ap, hnT_tl, eng2="pool"):
        kb, c = self.kb, self.c
        s = self.scr
        kb.act(s["junk"][:], xt_ap, AF.Square, [xt_tl], [s["junk"], s["ss"]], accum_out=s["ss"][:])
        kb.act(s["rs"][:], s["ss"][:], AF.Sqrt, [s["ss"], c["eps"]], [s["rs"]], scale=1.0 / D, bias=c["eps"][:, 0:1])
        kb.op("dve", lambda e: e.reciprocal(out=s["rs"][:], in_=s["rs"][:]), reads=[s["rs"]], writes=[s["rs"]])
        kb.stt("dve", s["t1"][:], xt_ap, s["rs"][:, 0:1], self.mods[:, ia, :], ALU.mult, ALU.mult,
               [xt_tl, s["rs"], self.mods], [s["t1"]])
        kb.tt(eng2, s["hn"][:], s["t1"][:], self.mods[:, ib, :], ALU.add, [s["t1"], self.mods], [s["hn"]])
        p = kb.ps()
        pb = p[:, :].bitcast(BF16)
        for k in range(8):
            kb.tr(p, pb[:, k * 128:(k + 1) * 128], s["hn"][:, k * 128:(k + 1) * 128], c["idb"][:], [s["hn"], c["idb"]])
        kb.cp("act", hnT_ap, pb[:, 0:1024].rearrange("p (k t) -> p k t", k=8), [p], [hnT_tl])

    def alloc_scr(self):
        kb = self.kb
        s = {}
        self.scr = s
        s["junk"] = kb.sb("junk", [128, D], BF16)
        s["ss"] = kb.sb("ss", [128, 1], F32)
        s["rs"] = kb.sb("rs", [128, 1], F32)
        s["t1"] = kb.sb("t1", [128, D], F32)
        s["hn"] = kb.sb("hn", [128, D], BF16)
        self.xin = [kb.sb("xin%d" % i, [128, D], F32) for i in range(2)]

    def mixer0(self):
        kb, c = self.kb, self.c
        sb = kb.sb
        kb.push()
        w0 = sb("w0", [128, 8, NC0], BF16)
        for i in range(0, NC0, 512):
            j = min(i + 512, NC0)
            kb.dma("pool", w0[:, :, i:j], self.win0[:, i:j].rearrange("(k p) n -> p k n", p=128), writes=[w0], grp="w")
        cw = sb("cw", [128, 6, 4], F32)
        kb.dma("sp", cw[:], self.cw0[:, :, :], writes=[cw], grp="c")
        dnsm = sb("dnsmc", [4, 2], F32)
        kb.dma("sp", dnsm[:], self.dnsm[:, :], writes=[dnsm], grp="c")
        nega = sb("nega", [4, 1], F32)
        kb.act(nega[:], dnsm[:, 0:1], AF.Exp, [dnsm], [nega])
        kb.ts("dve", nega[:], nega[:], -1.0, None, ALU.mult, None, [nega], [nega])
        dnw = sb("dnwc", [128, 1], F32)
        kb.dma("sp", dnw[:], self.dnw[:, :], writes=[dnw], grp="c")
        sinkE = sb("sinkE", [128, 2], F32)
        kb.dma("sp", sinkE[:], self.sinkl[:, :], writes=[sinkE], grp="c")
        kb.act(sinkE[:], sinkE[:], AF.Exp, [sinkE], [sinkE])
        biasm = sb("biasm", [128, 2, 512], F32)
        am = sb("am", [128, 2, 128], F32)
        kb.dma("sp", biasm[:], self.biasg.h.ap().rearrange("a p n -> p a n"), writes=[biasm], grp="c")
        kb.dma("sp", am[:], self.amask.h.ap().rearrange("a p n -> p a n"), writes=[am], grp="c")
        for a in range(2):
            kb.tt("dve", biasm[:, a, :].rearrange("p (s q) -> p s q", s=4),
                  biasm[:, a, :].rearrange("p (s q) -> p s q", s=4),
                  bc(am[:, a, :].unsqueeze(1), [128, 4, 128]), ALU.add, [biasm, am], [biasm])
        rmask = sb("rmask", [4, 8, 64], F32)
        kb.op("dve", lambda e: e.memset(rmask[:], 1.0), writes=[rmask])
        kb.op("dve", lambda e: e.memset(rmask[:, :, 0:1], 0.0), writes=[rmask])

        mU8 = sb("mU8", [128, 8, 64], F32)
        mL8 = sb("mL8", [128, 8, 64], F32)
        kb.cp("dve", mU8[:], bc(c["cm"][:, 0, :].unsqueeze(1), [128, 8, 64]), [c["cm"]], [mU8])
        kb.cp("dve", mL8[:], bc(c["cm"][:, 1, :].unsqueeze(1), [128, 8, 64]), [c["cm"]], [mL8])
        hnT = sb("hnT0", [128, 8, 512], BF16)
        qaT = sb("qaT", [128, 2, 512], BF16)
        kaT = sb("kaT", [128, 2, 128 + T], BF16)
        vat = sb("vat", [128, 33, 64], BF16)
        kb.op("pool", lambda e: e.memset(kaT[:], 0.0), writes=[kaT])
        kb.op("dve", lambda e: e.memset(vat[:, 0, :], 0.0), writes=[vat])
        xpre = [sb("xpre%d" % i, [128, 3 + 512], F32) for i in range(6)]
        for i in range(6):
            kb.op("pool", lambda e, i=i: e.memset(xpre[i][:, 0:3], 0.0), writes=[xpre[i]])
        gates = sb("gates", [128, 2, 512], F32)
        tl = sb("tl", [128, 512], F32)
        cacc = sb("cacc", [128, 512], F32)
        ysil = sb("ysil", [128, 512], F32)
        sqb = sb("sqb", [128, 512], BF16)
        rstd = sb("rstd", [128, 512], F32)
        qn = [sb("qn%d" % j, [128, 512], BF16) for j in range(2)]
        qnf = [sb("qnf%d" % j, [128, 512], F32) for j in range(2)]
        i8f = sb("i8f", [128, 8, 64], F32)
        kb.cp("dve", i8f[:], bc(c["cm"][:, 2, :].unsqueeze(1), [128, 8, 64]), [c["cm"]], [i8f])
        A32 = sb("A32", [128, 8, 64], F32)
        Xs = sb("Xs", [128, 4, 64], F32)
        kn = [sb("kn%d" % j, [128, 512], BF16) for j in range(2)]
        vT = [sb("vT%d" % j, [128, 512], BF16) for j in range(2)]
        bt = sb("bt", [4, 512], F32)
        gt = sb("gt", [4, 512], F32)
        Gs = sb("Gs", [4, 512], F32)
        Es = sb("Es", [4, 512], F32)
        BEs = sb("BEs", [4, 512], F32)
        DKs = sb("DKs", [4, 512], F32)
        nbt = gt
        tk4 = sb("tk4", [128, 5, 8, 4], F32)
        TK = sb("TK", [128, 5, 8, 2], F32)
        EGLc = sb("EGLc", [128, 2, 8], F32)
        t0 = sb("t0", [128, 8, 64], F32)
        tU = sb("tU", [128, 8, 64], F32)
        tL = sb("tL", [128, 8, 64], F32)
        Du = tU
        Dl = tL
        tmpf = t0
        NTp = [sb("NTp%d" % i, [128, 8, 64], BF16) for i in range(2)]
        Np = [sb("Np%d" % i, [128, 8, 64], BF16) for i in range(2)]
        Am = sb("Am", [128, 8, 64], BF16)
        KV = sb("KV", [128, 8, 128], BF16)
        KQ = sb("KQ", [128, 8, 128], BF16)
        Wu = sb("Wu", [128, 8, 128], BF16)
        qdec = sb("qdec", [128, 8, 64], F32)
        UT = [sb("UT%d" % j, [128, 8, 64], BF16) for j in range(2)]
        RT = [sb("RT%d" % j, [128, 8, 64], BF16) for j in range(2)]
        O0 = [sb("O0%d" % j, [128, 8, 64], F32) for j in range(2)]
        Qs = [sb("Qs%d" % j, [128, 8, 64], F32) for j in range(2)]
        S32 = [sb("S32_%d" % j, [128, 64], F32) for j in range(2)]
        Sbf = [sb("Sbf_%d" % j, [128, 64], BF16) for j in range(2)]
        pre = sb("pre", [128, 64], F32)
        cS = sb("cS", [128, 64], F32)
        oT = sb("oT", [128, 8, 64], F32)
        yo = sb("yo", [128, 512], BF16)
        for j in range(2):
            kb.op("dve", lambda e, j=j: e.memset(S32[j][:], 0.0), writes=[S32[j]])
            kb.op("dve", lambda e, j=j: e.memset(Sbf[j][:], 0.0), writes=[Sbf[j]])
        PT = [sb("PT%d" % i, [128, 4, 128], BF16) for i in range(2)]
        den = sb("den", [128, 2, 128], F32)
        ao = sb("ao", [128, 2, 128], BF16)
        H = (slice(0, 64), slice(64, 128))

        for tg in range(self.ntg):
            for tt in range(4):
                xt = self.xin[tt % 2]
                r0 = tg * 512 + tt * 128
                kb.dma("sp", xt[:], self.xfull[r0:r0 + 128, :], writes=[xt], grp="x")
                self.norm_tile(xt[:], xt, 1, 0, hnT[:, :, tt * 128:(tt + 1) * 128], hnT)
            for ci in range(11):
                p = kb.ps()
                for k in range(8):
                    kb.mm(p, p[:, :], w0[:, k, ci * 128:(ci + 1) * 128], hnT[:, k, :], [w0, hnT], start=(k == 0), stop=(k == 7))
                if ci < 2:
                    kb.act(qaT[:, ci, :], p[:, :], AF.Copy, [p], [qaT], scale=0.125)
                elif ci == 2:
                    for q in range(2):
                        kb.cp("act", kaT[H[q], q, 128 + tg * 512:128 + (tg + 1) * 512], p[H[q], :], [p], [kaT])
                elif ci < 9:
                    xp = xpre[ci - 3]
                    if tg > 0:
                        kb.cp("pool", xp[:, 0:3], xp[:, 512:515], [xp], [xp])
                    kb.cp("act", xp[:, 3:515], p[:, :], [p], [xp])
                else:
                    kb.act(gates[:, ci - 9, :], p[:, :], AF.Silu, [p], [gates])
            for tt in range(4):
                p = kb.ps()
                for k in range(8):
                    kb.mm(p, p[:, 0:64], hnT[:, k, tt * 128:(tt + 1) * 128], w0[:, k, 1408:1472], [w0, hnT], start=(k == 0), stop=(k == 7))
                kb.cp("act", vat[:, 1 + tg * 4 + tt, :], p[:, 0:64], [p], [vat])
            pb_ = kb.ps()
            pd_ = kb.ps()
            for k in range(8):
                kb.mm(pb_, pb_[0:4, :], w0[:, k, 1472:1476], hnT[:, k, :], [w0, hnT], start=(k == 0), stop=(k == 7))
            for k in range(8):
                kb.mm(pd_, pd_[0:4, :], w0[:, k, 1476:1480], hnT[:, k, :], [w0, hnT], start=(k == 0), stop=(k == 7))
            if self.m0_stop == "inproj":
                continue
            kb.act(bt[:], pb_[0:4, :], AF.Sigmoid, [pb_], [bt])
            kb.act(gt[:], pd_[0:4, :], AF.Exp, [pd_, dnsm], [gt], bias=dnsm[:, 1:2])
            kb.act(gt[:], gt[:], AF.Ln, [gt, c["one"]], [gt], bias=c["one"][0:4, 0:1])
            kb.ts("dve", gt[:], gt[:], nega[:, 0:1], None, ALU.mult, None, [gt, nega], [gt])
            kb.op("dve", lambda e: e.tensor_tensor_scan(out=Gs[:], data0=rmask[:].rearrange("p a b -> p (a b)"), data1=gt[:],
                                                        initial=0.0, op0=ALU.mult, op1=ALU.add), reads=[rmask, gt], writes=[Gs])
            kb.act(Es[:], Gs[:], AF.Exp, [Gs], [Es])
            kb.tt("dve", BEs[:], bt[:], Es[:], ALU.mult, [bt, Es], [BEs])
            G3 = Gs[:].rearrange("p (a b) -> p a b", a=8)
            kb.tt("dve", DKs[:].rearrange("p (a b) -> p a b", a=8), G3, bc(G3[:, :, 63:64], [4, 8, 64]), ALU.subtract, [Gs], [DKs])
            kb.act(DKs[:], DKs[:], AF.Exp, [DKs], [DKs], scale=-1.0)
            kb.ts("dve", nbt[:], bt[:], -1.0, None, ALU.mult, None, [bt], [nbt])
            p = kb.ps()
            for qi, X in enumerate((Gs, nbt, BEs, DKs, bt)):
                for n in range(8):
                    for q in range(2):
                        kb.mm(p, p[H[q], (qi * 8 + n) * 4:(qi * 8 + n) * 4 + 4], X[:, n * 64:(n + 1) * 64], c["idf"][0:4, 0:4],
                              [X, c["idf"]], tp=(0, 64 * q))
            kb.cp("dve", tk4[:].rearrange("p a n h -> p (a n h)"), p[:, 0:160], [p], [tk4])
            for q in range(2):
                kb.cp("dve", TK[H[q], :, :, :], tk4[H[q], :, :, 2 * q:2 * q + 2], [tk4], [TK])
            p = kb.ps()
            for j in range(2):
                kb.mm(p, p[:, j * 8:j * 8 + 8], c["sel"][:, j, :], Es[:].rearrange("p (a b) -> p a b", a=8)[:, :, 63], [c["sel"], Es])
            kb.cp("dve", EGLc[:].rearrange("p j n -> p (j n)"), p[:, 0:16], [p], [EGLc])

            if self.m0_stop == "small":
                continue
            for qb in range(4):
                n = tg * 4 + qb
                kbs = [n - 1, n] if n > 0 else [n]
                for ki, kbk in enumerate(kbs):
                    sel_ = 0 if kbk == n - 1 else 1
                    pl = kb.ps()
                    for q in range(2):
                        for j in range(2):
                            sl = q * 2 + j
                            kb.mm(pl, pl[:, sl * 128:(sl + 1) * 128], kaT[:, q, 128 + kbk * 128:128 + (kbk + 1) * 128],
                                  qaT[:, j, qb * 128:(qb + 1) * 128], [kaT, qaT])
                    if self.m0_stop == "attn_mm":
                        continue
                    kb.tt("dve", tl[:], pl[:, :], biasm[:, sel_, :], ALU.add, [pl, biasm], [tl])
                    if self.m0_stop == "attn_tt":
                        continue
                    kb.act(PT[ki][:].rearrange("p s q -> p (s q)"), tl[:], AF.Exp, [tl], [PT[ki]])
                if self.m0_stop in ("attn_l", "attn_mm", "attn_tt"):
                    continue
                po = kb.ps()
                pdn = kb.ps()
                for q in range(2):
                    for j in range(2):
                        sl = q * 2 + j
                        for ki, kbk in enumerate(kbs):
                            kb.mm(po, po[H[q], j * 128:(j + 1) * 128], vat[:, 1 + kbk, :], PT[ki][:, sl, :], [vat, PT[ki]],
                                  start=(ki == 0), stop=(ki == len(kbs) - 1), tp=(0, 64 * q))
                        for ki, kbk in enumerate(kbs):
                            kb.mm(pdn, pdn[H[q], j * 128:(j + 1) * 128], c["ones"][:, 0:64], PT[ki][:, sl, :], [c["ones"], PT[ki]],
                                  start=(ki == 0), stop=(ki == len(kbs) - 1), tp=(0, 64 * q))
                if self.m0_stop == "attn_pv":
                    continue
                kb.tt("dve", den[:], pdn[:, 0:256].rearrange("p (j t) -> p j t", j=2), bc(sinkE[:].unsqueeze(2), [128, 2, 128]),
                      ALU.add, [pdn, sinkE], [den])
                kb.op("dve", lambda e: e.reciprocal(out=den[:], in_=den[:]), reads=[den], writes=[den])
                kb.tt("dve", ao[:], po[:, 0:256].rearrange("p (j t) -> p j t", j=2), den[:], ALU.mult, [po, den], [ao])
                if self.m0_stop == "attn_n":
                    continue
                for j in range(2):
                    kb.dma("sp", self.y0s[0][j * 128:(j + 1) * 128, n * 128:(n + 1) * 128], ao[:, j, :], reads=[ao], writes=[self.y0s[0]], grp="y")

            if self.m0_stop in ("attn", "attn_l", "attn_pv", "attn_n", "attn_mm", "attn_tt"):
                continue
            for ci in range(6):
                xp = xpre[ci]
                j = ci % 2
                kind = ci // 2
                kb.ts("dve", cacc[:], xp[:, 0:512], cw[:, ci, 0:1], None, ALU.mult, None, [xp, cw], [cacc])
                for k in range(1, 4):
                    kb.stt("dve", cacc[:], xp[:, k:k + 512], cw[:, ci, k:k + 1], cacc[:], ALU.mult, ALU.add, [xp, cw, cacc], [cacc])
                if kind == 2:
                    kb.act(vT[j][:], cacc[:], AF.Silu, [cacc], [vT[j]])
                    continue
                kb.act(ysil[:], cacc[:], AF.Silu, [cacc], [ysil])
                kb.act(sqb[:], ysil[:], AF.Square, [ysil], [sqb])
                p = kb.ps()
                kb.mm(p, p[:, :], c["blk1"][:], sqb[:], [c["blk1"], sqb])
                kb.act(rstd[:], p[:, :], AF.Sqrt, [p, c["eps"]], [rstd], bias=c["eps"][:, 0:1])
                kb.op("dve", lambda e: e.reciprocal(out=rstd[:], in_=rstd[:]), reads=[rstd], writes=[rstd])
                if kind == 0:
                    kb.stt("dve", qnf[j][:], ysil[:], 0.125, rstd[:], ALU.mult, ALU.mult, [ysil, rstd], [qnf[j]])
                    kb.cp("act", qn[j][:], qnf[j][:], [qnf[j]], [qn[j]])
                else:
                    kb.tt("dve", kn[j][:], ysil[:], rstd[:], ALU.mult, [ysil, rstd], [kn[j]])

            if self.m0_stop == "conv":
                continue
            for j in range(2):
                k3 = kn[j][:].rearrange("p (n c) -> p n c", n=8)
                q3 = qn[j][:].rearrange("p (n c) -> p n c", n=8)
                v3 = vT[j][:].rearrange("p (n c) -> p n c", n=8)
                pk = kb.ps()
                pv = kb.ps()
                pkb = pk[:, :].rearrange("p (n x) -> p n x", n=8)
                pvb = pv[:, :].rearrange("p (n x) -> p n x", n=8)
                for n in range(8):
                    for q in range(2):
                        kb.mm(pk, pkb[H[q], n, :], k3[H[q], n, :], c["idb"][H[q], 64 * q:64 * q + 64], [kn[j], c["idb"]], tp=(64 * q, 64 * q))
                for n in range(8):
                    for q in range(2):
                        kb.mm(pv, pvb[H[q], n, :], v3[H[q], n, :], c["idb"][H[q], 64 * q:64 * q + 64], [vT[j], c["idb"]], tp=(64 * q, 64 * q))
                if self.m0_stop == "prep0":
                    continue
                kb.tt("dve", KQ[:, :, 0:64], bc(TK[:, 3, :, j:j + 1], [128, 8, 64]), pkb, ALU.mult, [pk, TK], [KQ])
                kb.tt("dve", KV[:, :, 0:64], bc(TK[:, 2, :, j:j + 1], [128, 8, 64]), pkb, ALU.mult, [pk, TK], [KV])
                kb.tt("dve", KV[:, :, 64:128], bc(TK[:, 4, :, j:j + 1], [128, 8, 64]), pvb, ALU.mult, [pv, TK], [KV])
                if self.m0_stop == "prep1":
                    continue
                pg = kb.ps()
                pe_ = kb.ps()
                kb.mm(pg, pg[:, :], c["sel"][:, j, :], Gs[:], [c["sel"], Gs])
                kb.mm(pe_, pe_[:, :], c["sel"][:, j, :], Es[:], [c["sel"], Es])
                pg3 = pg[:, :].rearrange("p (n c) -> p n c", n=8)
                kb.tt("dve", t0[:], pg3, bc(TK[:, 0, :, j:j + 1], [128, 8, 64]), ALU.subtract, [pg, TK], [t0])
                if self.m0_stop == "prep1a":
                    continue
                kb.tt("pool", tU[:], t0[:], mU8[:], ALU.add, [t0, mU8], [tU])
                kb.stt("dve", tL[:], t0[:], -1.0, mL8[:], ALU.mult, ALU.add, [t0, mL8], [tL])
                kb.act(Du[:], tU[:], AF.Exp, [tU], [Du])
                kb.act(Dl[:], tL[:], AF.Exp, [tL], [Dl])
                if self.m0_stop == "prep1b":
                    continue
                kb.cp("act", cacc[:], pe_[:, :], [pe_], [cacc])
                kb.tt("dve", qdec[:].rearrange("p n c -> p (n c)"), cacc[:], qnf[j][:], ALU.mult, [qnf[j], cacc], [qdec])
                if self.m0_stop == "prep2":
                    continue
                pkk = kb.ps()
                pqk = kb.ps()
                kk3 = pkk[:, :].rearrange("p (n c) -> p n c", n=8)
                qk3 = pqk[:, :].rearrange("p (n c) -> p n c", n=8)
                for n in range(8):
                    for q in range(2):
                        kb.mm(pkk, kk3[H[q], n, :], k3[H[q], n, :], k3[H[q], n, :], [kn[j]], tp=(64 * q, 64 * q))
                for n in range(8):
                    for q in range(2):
                        kb.mm(pqk, qk3[H[q], n, :], k3[H[q], n, :], q3[H[q], n, :], [kn[j], qn[j]], tp=(64 * q, 64 * q))
                kb.tt("dve", tmpf[:], bc(TK[:, 1, :, j:j + 1], [128, 8, 64]), Dl[:], ALU.mult, [TK, Dl], [tmpf])
                kb.cp("act", tl[:], pkk[:, :], [pkk], [tl])
                kb.tt("dve", NTp[0][:], tl[:].rearrange("p (n c) -> p n c", n=8), tmpf[:], ALU.mult, [tl, tmpf], [NTp[0]])
                kb.cp("act", cacc[:], pqk[:, :], [pqk], [cacc])
                kb.tt("dve", KQ[:, :, 64:128], cacc[:].rearrange("p (n c) -> p n c", n=8), Du[:], ALU.mult, [cacc, Du], [KQ])
                if self.m0_stop == "prep3":
                    continue
                pn = kb.ps()
                pnb = pn[:, :].rearrange("p (n c) -> p n c", n=8)
                for n in range(8):
                    for q in range(2):
                        kb.mm(pn, pnb[H[q], n, :], NTp[0][H[q], n, :], c["idb"][H[q], 64 * q:64 * q + 64], [NTp[0], c["idb"]], tp=(64 * q, 64 * q))
                kb.cp("act", Np[0][:], pnb, [pn], [Np[0]])
                kb.cp("act", A32[:], pnb, [pn], [A32])
                kb.tt("pool", A32[:], A32[:], i8f[:], ALU.add, [A32, i8f], [A32])
                kb.cp("act", Am[:], A32[:], [A32], [Am])
                if self.m0_stop == "prep4":
                    continue
                cur = 0
                for lev in range(5):
                    nxt = 1 - cur
                    if lev < 4:
                        p1 = kb.ps()
                        p13 = p1[:, :].rearrange("p (n c) -> p n c", n=8)
                        for n in range(8):
                            for q in range(2):
                                kb.mm(p1, p13[H[q], n, :], NTp[cur][H[q], n, :], Np[cur][H[q], n, :], [NTp[cur], Np[cur]], tp=(64 * q, 64 * q))
                    p2 = kb.ps()
                    p23 = p2[:, :].rearrange("p (n c) -> p n c", n=8)
                    for n in range(8):
                        for q in range(2):
                            kb.mm(p2, p23[H[q], n, :], Np[cur][H[q], n, :], NTp[cur][H[q], n, :], [NTp[cur], Np[cur]], tp=(64 * q, 64 * q))
                    if lev < 4:
                        kb.cp("act", Np[nxt][:], p13, [p1], [Np[nxt]])
                    kb.cp("act", NTp[nxt][:], p23, [p2], [NTp[nxt]])
                    p3 = kb.ps()
                    p33 = p3[:, :].rearrange("p (n c) -> p n c", n=8)
                    for n in range(8):
                        for q in range(2):
                            kb.mm(p3, p33[H[q], n, :], NTp[nxt][H[q], n, :], Am[H[q], n, :], [NTp[nxt], Am], tp=(64 * q, 64 * q))
                    kb.cp("act", tl[:], p3[:, :], [p3], [tl])
                    kb.tt("dve", A32[:], tl[:].rearrange("p (n c) -> p n c", n=8), A32[:], ALU.add, [A32, tl], [A32])
                    kb.cp("act", Am[:], A32[:], [A32], [Am])
                    cur = nxt
                if self.m0_stop == "prep5":
                    continue
                for hf in range(2):
                    pw = kb.ps()
                    pw3 = pw[:, :].rearrange("p (n c) -> p n c", n=4)
                    for n in range(4):
                        for q in range(2):
                            kb.mm(pw, pw3[H[q], n, :], Am[H[q], hf * 4 + n, :], KV[H[q], hf * 4 + n, :], [Am, KV], tp=(64 * q, 64 * q))
                    kb.cp("act", Wu[:, hf * 4:hf * 4 + 4, :], pw3, [pw], [Wu])
                if self.m0_stop == "prep6":
                    continue
                for hf in range(2):
                    pu = kb.ps()
                    pu3 = pu[:, :].rearrange("p (n c) -> p n c", n=4)
                    for n in range(4):
                        for q in range(2):
                            kb.mm(pu, pu3[H[q], n, :], Wu[H[q], hf * 4 + n, 0:64], KQ[H[q], hf * 4 + n, :], [Wu, KQ], tp=(64 * q, 64 * q))
                    kb.cp("act", UT[j][:, hf * 4:hf * 4 + 4, :], pu3[:, :, 0:64], [pu], [UT[j]])
                    kb.cp("act", Xs[:], pu3[:, :, 64:128], [pu], [Xs])
                    kb.tt("pool", RT[j][:, hf * 4:hf * 4 + 4, :], qdec[:, hf * 4:hf * 4 + 4, :], Xs[:], ALU.subtract, [qdec, Xs], [RT[j]])
                po0 = kb.ps()
                pq_ = kb.ps()
                po03 = po0[:, :].rearrange("p (n c) -> p n c", n=8)
                pq3 = pq_[:, :].rearrange("p (n c) -> p n c", n=8)
                for n in range(8):
                    for q in range(2):
                        kb.mm(po0, po03[H[q], n, :], Wu[H[q], n, 64:128], KQ[H[q], n, 64:128], [Wu, KQ], tp=(64 * q, 64 * q))
                for n in range(8):
                    for q in range(2):
                        kb.mm(pq_, pq3[H[q], n, :], KQ[H[q], n, 0:64], Wu[H[q], n, 64:128], [Wu, KQ], tp=(64 * q, 64 * q))
                kb.cp("act", O0[j][:], po03, [po0], [O0[j]])
                kb.cp("act", Qs[j][:], pq3, [pq_], [Qs[j]])

            if self.m0_stop is not None and self.m0_stop.startswith("prep"):
                continue
            for j in range(2):
                pO = kb.pst[6 + j]
                pO3 = pO[:, :].rearrange("p (n c) -> p n c", n=8)
                for n in range(8):
                    for q in range(2):
                        kb.mm(pO, pO3[H[q], n, :], Sbf[j][H[q], :], RT[j][H[q], n, :], [Sbf[j], RT[j]], tp=(64 * q, 64 * q))
                    pS = kb.ps()
                    for q in range(2):
                        kb.mm(pS, pS[H[q], 0:64], UT[j][H[q], n, :], Sbf[j][H[q], :], [Sbf[j], UT[j]], tp=(64 * q, 64 * q))
                    kb.stt("dve", pre[:], S32[j][:], EGLc[:, j, n:n + 1], Qs[j][:, n, :], ALU.mult, ALU.add, [S32[j], EGLc, Qs[j]], [pre])
                    kb.cp("act", cS[:], pS[:, 0:64], [pS], [cS])
                    kb.tt("dve", S32[j][:], pre[:], cS[:], ALU.subtract, [pre, cS], [S32[j]])
                    kb.cp("act", Sbf[j][:], S32[j][:], [S32[j]], [Sbf[j]])
                kb.cp("act", oT[:], pO3, [pO], [oT])
                kb.tt("dve", oT[:], oT[:], O0[j][:], ALU.add, [oT, O0[j]], [oT])
                o2 = oT[:].rearrange("p n c -> p (n c)")
                kb.act(sqb[:], o2, AF.Square, [oT], [sqb])
                p = kb.ps()
                kb.mm(p, p[:, :], c["blk1"][:], sqb[:], [c["blk1"], sqb])
                kb.act(rstd[:], p[:, :], AF.Sqrt, [p, c["eps"]], [rstd], scale=1.0 / 64, bias=c["eps"][:, 0:1])
                kb.op("dve", lambda e: e.reciprocal(out=rstd[:], in_=rstd[:]), reads=[rstd], writes=[rstd])
                kb.stt("dve", ysil[:], o2, dnw[:, 0:1], rstd[:], ALU.mult, ALU.mult, [oT, dnw, rstd], [ysil])
                kb.tt("dve", yo[:], ysil[:], gates[:, j, :], ALU.mult, [ysil, gates], [yo])
                kb.dma("sp", self.y0s[1][j * 128:(j + 1) * 128, tg * 512:(tg + 1) * 512], yo[:], reads=[yo], writes=[self.y0s[1]], grp="y")
                if self.debug:
                    kb.dma("sp", self.dbg_o[j * 128:(j + 1) * 128, tg * 512:(tg + 1) * 512], o2, reads=[oT], writes=[self.dbg_o], grp="y")

    def gather(self, snd, gth):
        for a, b in zip(snd, gth):
            self.kb.collective("AllGather", self.groups, a, b, a.h.ap()[:, :], b.h.ap()[:, :])

    def outproj(self, gth, nk, wout):
        kb, c = self.kb, self.c
        sb = kb.sb
        kb.push()
        wo = sb("wo_%d" % nk, [128, nk, D], BF16)
        stg = [sb("wostg%d_%d" % (i, nk), [128, D], F32) for i in range(2)]
        for k in range(nk):
            st = stg[k % 2]
            kb.dma("sp", st[:], wout[k * 128:(k + 1) * 128, :], writes=[st])
            kb.tt("pool", wo[:, k, :], st[:], self.mods[:, 2, :], ALU.mult, [st, self.mods], [wo])
        ya = [sb("ya%d_%d" % (i, nk), [128, nk, 128], BF16) for i in range(2)]
        yb_ = [sb("yb%d_%d" % (i, nk), [128, nk, 128], BF16) for i in range(2)]
        ytmp = sb("ytmp_%d" % nk, [128, nk, 128], F32)
        yown = [sb("yown%d_%d" % (i, nk), [128, nk, 128], BF16) for i in range(2)]
        nb = len(gth)
        kpb = nk // nb
        for tt in range(16):
            A, B, Y = ya[tt % 2], yb_[tt % 2], yown[tt % 2]
            for b in range(nb):
                g3 = gth[b].h.ap().rearrange("(k p) t -> p k t", p=128)
                kb.dma("sp", A[:, b * kpb:(b + 1) * kpb, :], g3[:, :, tt * 128:(tt + 1) * 128], reads=[gth[b]], writes=[A])
                kb.dma("sp", B[:, b * kpb:(b + 1) * kpb, :], g3[:, :, TH + tt * 128:TH + (tt + 1) * 128], reads=[gth[b]], writes=[B])
            kb.ts("pool", ytmp[:], A[:], c["flag"][:, 0:1], None, ALU.mult, None, [A, c["flag"]], [ytmp])
            kb.stt("dve", Y[:], B[:], c["flag"][:, 1:2], ytmp[:], ALU.mult, ALU.add, [B, c["flag"], ytmp], [Y])
            for hf in range(2):
                p = kb.ps()
                for k in range(nk):
                    kb.mm(p, p[:, :], Y[:, k, :], wo[:, k, hf * 512:(hf + 1) * 512], [Y, wo], start=(k == 0), stop=(k == nk - 1))
                xr = self.xres[tt]
                kb.tt("dve", xr[:, hf * 512:(hf + 1) * 512], p[:, :], xr[:, hf * 512:(hf + 1) * 512], ALU.add, [p, xr], [xr])
        kb.pop()

    def load_xres(self):
        kb = self.kb
        self.xres = [kb.sb("xres%d" % i, [128, D], F32) for i in range(16)]
        for tt in range(16):
            kb.dma("sp", self.xres[tt][:], self.xown[tt * 128:(tt + 1) * 128, :], writes=[self.xres[tt]])

    def norm_own(self, hnT):
        for tt in range(16):
            self.norm_tile(self.xres[tt][:], self.xres[tt], 1, 0, hnT[:, :, tt * 128:(tt + 1) * 128], hnT)

    def ffn_run(self, hnT, specs, gates=None):
        kb, c = self.kb, self.c
        B = self.fb
        groups = []
        for (wg, wu, wd, nff, e) in specs:
            nch = nff // 128
            for gi in range((nch + 1) // 2):
                groups.append((wg, wu, wd, gi * 256, min(2, nch - gi * 2), e))

        def load(it):
            wg, wu, wd, f0, fc, e = groups[it]
            wgb, wub, st, wdb = B["wg"][it % 2], B["wu"][it % 2], B["stg"][it % 2], B["wd"][it % 2]
            kb.dma("pool", wgb[:, :, 0:fc * 128], wg[:, f0:f0 + fc * 128].rearrange("(k p) n -> p k n", p=128), writes=[wgb])
            kb.dma("pool", wub[:, :, 0:fc * 128], wu[:, f0:f0 + fc * 128].rearrange("(k p) n -> p k n", p=128), writes=[wub])
            kb.dma("sp", st[:, 0:fc, :], wd[f0:f0 + fc * 128, :].rearrange("(cc p) n -> p cc n", p=128), writes=[st])
            for cc in range(fc):
                kb.tt("pool", wdb[:, cc, :], st[:, cc, :], self.mods[:, 2, :], ALU.mult, [st, self.mods], [wdb])

        def compute(it):
            wg, wu, wd, f0, fc, e = groups[it]
            wgb, wub, wdb, h1 = B["wg"][it % 2], B["wu"][it % 2], B["wd"][it % 2], B["h1"][it % 2]
            for cc in range(fc):
                for tg in range(4):
                    pgt = kb.ps()
                    put = kb.ps()
                    for k in range(8):
                        kb.mm(pgt, pgt[:, :], wgb[:, k, cc * 128:(cc + 1) * 128], hnT[:, k, tg * 512:(tg + 1) * 512], [wgb, hnT],
                              start=(k == 0), stop=(k == 7))
                    for k in range(8):
                        kb.mm(put, put[:, :], wub[:, k, cc * 128:(cc + 1) * 128], hnT[:, k, tg * 512:(tg + 1) * 512], [wub, hnT],
                              start=(k == 0), stop=(k == 7))
                    sl = B["sil"][(cc * 4 + tg) % 2]
                    kb.act(sl[:], pgt[:, :], AF.Silu, [pgt], [sl])
                    kb.tt("dve", h1[:, cc, tg * 512:(tg + 1) * 512], put[:, :], sl[:], ALU.mult, [put, sl], [h1])
            for tt in range(16):
                xr = self.xres[tt]
                for hf in range(2):
                    p = kb.ps()
                    for cc in range(fc):
                        kb.mm(p, p[:, :], h1[:, cc, tt * 128:(tt + 1) * 128], wdb[:, cc, hf * 512:(hf + 1) * 512], [h1, wdb],
                              start=(cc == 0), stop=(cc == fc - 1))
                    if gates is None:
                        kb.tt("dve", xr[:, hf * 512:(hf + 1) * 512], p[:, :], xr[:, hf * 512:(hf + 1) * 512], ALU.add, [p, xr], [xr])
                    else:
                        ev = B["ev"][(tt * 2 + hf) % 2]
                        kb.act(ev[:], p[:, :], AF.Copy, [p, gates], [ev], scale=gates[:, tt, e:e + 1])
                        kb.tt("dve", xr[:, hf * 512:(hf + 1) * 512], ev[:], xr[:, hf * 512:(hf + 1) * 512], ALU.add, [ev, xr], [xr])

        load(0)
        for it in range(len(groups)):
            if it + 1 < len(groups):
                load(it + 1)
            compute(it)

    def alloc_ffn(self, tag):
        kb = self.kb
        B = {}
        B["wg"] = [kb.sb("fwg%d_%s" % (i, tag), [128, 8, 256], BF16) for i in range(2)]
        B["wu"] = [kb.sb("fwu%d_%s" % (i, tag), [128, 8, 256], BF16) for i in range(2)]
        B["stg"] = [kb.sb("fst%d_%s" % (i, tag), [128, 2, D], F32) for i in range(2)]
        B["wd"] = [kb.sb("fwd%d_%s" % (i, tag), [128, 2, D], BF16) for i in range(2)]
        B["h1"] = [kb.sb("fh1%d_%s" % (i, tag), [128, 2, TH], BF16) for i in range(2)]
        B["sil"] = [kb.sb("fsil%d_%s" % (i, tag), [128, 512], F32) for i in range(2)]
        B["ev"] = [kb.sb("fev%d_%s" % (i, tag), [128, 512], F32) for i in range(2)]
        self.fb = B
        self.fcnt = 0

    def ffn0(self):
        kb = self.kb
        kb.push()
        hnT = kb.sb("hnTf0", [128, 8, TH], BF16)
        self.norm_own(hnT)
        self.alloc_ffn("f0")
        self.ffn_run(hnT, [(self.wg0.h.ap(), self.wu0.h.ap(), self.wd0.h.ap(), 2816, 0)])
        kb.pop()

    def moe(self):
        kb, c = self.kb, self.c
        kb.push()
        hnT = kb.sb("hnTf1", [128, 8, TH], BF16)
        self.norm_own(hnT)
        rwf = kb.sb("rwf", [128, 8, 8], F32)
        rwb = kb.sb("rwb", [128, 8, 8], BF16)
        kb.dma("sp", rwf[:], self.rw[:, :, :], writes=[rwf])
        kb.cp("dve", rwb[:], rwf[:], [rwf], [rwb])
        rbb = kb.sb("rbb", [128, 8], F32)
        kb.dma("sp", rbb[:], bc(self.rb[0:1, :], [128, 8]), writes=[rbb])
        lg = kb.sb("lg", [128, 16, 8], F32)
        p = kb.ps()
        for tt in range(16):
            for k in range(8):
                kb.mm(p, p[:, tt * 8:(tt + 1) * 8], hnT[:, k, tt * 128:(tt + 1) * 128], rwb[:, k, :], [hnT, rwb], start=(k == 0), stop=(k == 7))
        kb.cp("act", lg[:].rearrange("p a b -> p (a b)"), p[:, 0:128], [p], [lg])
        kb.tt("dve", lg[:], lg[:], bc(rbb[:].unsqueeze(1), [128, 16, 8]), ALU.add, [lg, rbb], [lg])
        m1 = kb.sb("m1", [128, 16], F32)
        m2 = kb.sb("m2", [128, 16], F32)
        eq1 = kb.sb("eq1", [128, 16, 8], F32)
        eq2 = kb.sb("eq2", [128, 16, 8], F32)
        lg2 = kb.sb("lg2", [128, 16, 8], F32)
        w1 = kb.sb("w1", [128, 16], F32)
        w2 = kb.sb("w2", [128, 16], F32)
        gates = kb.sb("gates1", [128, 16, 8], F32)
        kb.op("dve", lambda e: e.reduce_max(out=m1[:], in_=lg[:], axis=AX.X), reads=[lg], writes=[m1])
        kb.tt("dve", eq1[:], lg[:], bc(m1[:].unsqueeze(2), [128, 16, 8]), ALU.is_equal, [lg, m1], [eq1])
        kb.stt("dve", lg2[:], eq1[:], NEG, lg[:], ALU.mult, ALU.add, [eq1, lg], [lg2])
        kb.op("dve", lambda e: e.reduce_max(out=m2[:], in_=lg2[:], axis=AX.X), reads=[lg2], writes=[m2])
        kb.tt("dve", eq2[:], lg2[:], bc(m2[:].unsqueeze(2), [128, 16, 8]), ALU.is_equal, [lg2, m2], [eq2])
        kb.tt("dve", w2[:], m2[:], m1[:], ALU.subtract, [m1, m2], [w2])
        kb.act(w1[:], w2[:], AF.Sigmoid, [w2], [w1], scale=-1.0)
        kb.act(w2[:], w2[:], AF.Sigmoid, [w2], [w2])
        kb.tt("dve", eq1[:], eq1[:], bc(w1[:].unsqueeze(2), [128, 16, 8]), ALU.mult, [eq1, w1], [eq1])
        kb.tt("dve", eq2[:], eq2[:], bc(w2[:].unsqueeze(2), [128, 16, 8]), ALU.mult, [eq2, w2], [eq2])
        kb.tt("dve", gates[:], eq1[:], eq2[:], ALU.add, [eq1, eq2], [gates])
        if self.moeonly:
            kb.dma("sp", self.dbg_g[:, :], gates[:].rearrange("p a b -> p (a b)"), reads=[gates], writes=[self.dbg_g])
        self.alloc_ffn("f1")
        self.ffn_run(hnT, [(self.mwg.h.ap()[e], self.mwu.h.ap()[e], self.mwd.h.ap()[e], 3584, e) for e in range(self.nexp)], gates=gates)
        kb.pop()

    def mixer1(self):
        kb, c = self.kb, self.c
        sb = kb.sb
        kb.push()
        hnT = sb("hnTm1", [128, 8, TH], BF16)
        self.norm_own(hnT)
        for b in range(2):
            kb.dma("sp", self.h1s[b].h.ap().rearrange("(k p) t -> p k t", p=128), hnT[:, 4 * b:4 * b + 4, :], reads=[hnT], writes=[self.h1s[b]])
        self.gather(self.h1s, self.h1g)
        w1 = sb("w1m", [128, 8, NC1], BF16)
        for i in range(0, NC1, 512):
            j = min(i + 512, NC1)
            kb.dma("pool", w1[:, :, i:j], self.win1[:, i:j].rearrange("(k p) n -> p k n", p=128), writes=[w1])
        sm = sb("l1smc", [128, 4, 8], F32)
        kb.dma("sp", sm[:], self.l1sm[:, :, :], writes=[sm])
        scw = sb("scwc", [128, 2, 3], F32)
        kb.dma("sp", scw[:], self.scw[:, :, :], writes=[scw])
        gwf = sb("gwf", [128, 8, 128], F32)
        gwb = sb("gwb", [128, 8, 128], BF16)
        kb.dma("sp", gwf[:, 0:4, :], self.gaw.h.ap().rearrange("n i j -> i n j"), writes=[gwf])
        kb.dma("sp", gwf[:, 4:8, :], self.gxw.h.ap().rearrange("n i j -> i n j"), writes=[gwf])
        kb.cp("dve", gwb[:], gwf[:], [gwf], [gwb])
        cj = sb("cj", [128, 4], F32)
        kb.act(cj[:], sm[:, :, 7], AF.Exp, [sm], [cj], scale=-1.0)
        kb.act(cj[:], cj[:], AF.Ln, [cj, c["one"]], [cj], bias=c["one"][:, 0:1])
        kb.ts("dve", cj[:], cj[:], -8.0, None, ALU.mult, None, [cj], [cj])
        hin = sb("hin1", [128, 8, 512], BF16)
        xp1 = [sb("xp1_%d" % i, [128, 3 + 512], F32) for i in range(4)]
        xp2 = [sb("xp2_%d" % i, [128, 2 + 512], F32) for i in range(2)]
        for t_ in xp1 + xp2:
            kb.op("pool", lambda e, t_=t_: e.memset(t_[:, 0:3], 0.0), writes=[t_])
        hprev = sb("hprev", [128, 4], F32)
        kb.op("pool", lambda e: e.memset(hprev[:], 0.0), writes=[hprev])
        F = lambda n: sb(n, [128, 512], F32)
        xcv, rg, ig, av, bv, hs, yv, y2, gl, cdv = F("xcv"), F("rg"), F("ig"), F("av"), F("bv"), F("hs"), F("yv"), F("y2"), F("gl"), F("cdv")
        xcb = sb("xcb", [128, 512], BF16)
        yo = sb("yo1", [128, 512], BF16)
        g3 = [self.h1g[b].h.ap().rearrange("(r k p) t -> r p k t", r=2, p=128) for b in range(2)]

        def inproj(ci):
            p = kb.ps()
            for k in range(8):
                kb.mm(p, p[:, :], w1[:, k, ci * 128:(ci + 1) * 128], hin[:, k, :], [w1, hin], start=(k == 0), stop=(k == 7))
            return p

        for tg in range(self.ntg):
            for b in range(2):
                kb.dma("sp", hin[:, 4 * b:4 * b + 4, :], g3[b][tg // 4, :, :, (tg % 4) * 512:(tg % 4 + 1) * 512], reads=[self.h1g[b]], writes=[hin])
            for i in range(4):
                p = inproj(i)
                xp = xp1[i]
                if tg > 0:
                    kb.cp("pool", xp[:, 0:3], xp[:, 512:515], [xp], [xp])
                kb.cp("act", xp[:, 3:515], p[:, :], [p], [xp])
                kb.ts("dve", xcv[:], xp[:, 0:512], sm[:, i, 0:1], sm[:, i, 4:5], ALU.mult, ALU.add, [xp, sm], [xcv])
                for k in range(1, 4):
                    kb.stt("dve", xcv[:], xp[:, k:k + 512], sm[:, i, k:k + 1], xcv[:], ALU.mult, ALU.add, [xp, sm, xcv], [xcv])
                kb.cp("act", xcb[:], xcv[:], [xcv], [xcb])
                if self.m1_stop == "conv":
                    continue
                pr = kb.ps()
                pi = kb.ps()
                kb.mm(pr, pr[:, :], gwb[:, i, :], xcb[:], [gwb, xcb])
                kb.mm(pi, pi[:, :], gwb[:, 4 + i, :], xcb[:], [gwb, xcb])
                kb.act(rg[:], pr[:, :], AF.Sigmoid, [pr, sm], [rg], bias=sm[:, i, 5:6])
                kb.act(ig[:], pi[:, :], AF.Sigmoid, [pi, sm], [ig], bias=sm[:, i, 6:7])
                if self.m1_stop == "gate":
                    continue
                kb.act(av[:], rg[:], AF.Exp, [rg, cj], [av], scale=cj[:, i:i + 1])
                kb.tt("pool", bv[:], av[:], av[:], ALU.mult, [av], [bv])
                kb.ts("dve", bv[:], bv[:], -1.0, 1.0, ALU.mult, ALU.add, [bv], [bv])
                kb.act(bv[:], bv[:], AF.Sqrt, [bv], [bv])
                kb.tt("pool", bv[:], bv[:], ig[:], ALU.mult, [bv, ig], [bv])
                kb.tt("dve", bv[:], bv[:], xcv[:], ALU.mult, [bv, xcv], [bv])
                if self.m1_stop == "ab":
                    continue
                kb.op("dve", lambda e, i=i: e.tensor_tensor_scan(out=hs[:], data0=av[:], data1=bv[:], initial=hprev[:, i:i + 1],
                                                                 op0=ALU.mult, op1=ALU.add), reads=[av, bv, hprev], writes=[hs])
                kb.cp("dve", hprev[:, i:i + 1], hs[:, 511:512], [hs], [hprev])
                if self.m1_stop == "scan":
                    continue
                p = inproj(4 + i)
                kb.cp("act", yv[:], p[:, :], [p], [yv])
                kb.tt("pool", y2[:], yv[:], yv[:], ALU.mult, [yv], [y2])
                kb.ts("dve", y2[:], y2[:], 0.044715, 1.0, ALU.mult, ALU.add, [y2], [y2])
                kb.tt("pool", y2[:], y2[:], yv[:], ALU.mult, [y2, yv], [y2])
                kb.act(gl[:], y2[:], AF.Sigmoid, [y2], [gl], scale=1.5957691216057308)
                kb.tt("pool", gl[:], gl[:], yv[:], ALU.mult, [gl, yv], [gl])
                kb.tt("dve", yo[:], gl[:], hs[:], ALU.mult, [gl, hs], [yo])
                kb.dma("sp", self.y1s[i // 2][(i % 2) * 128:(i % 2 + 1) * 128, tg * 512:(tg + 1) * 512], yo[:], reads=[yo], writes=[self.y1s[i // 2]])
            if self.m1_stop in ("conv", "gate", "ab", "scan", "lru"):
                continue
            for i in range(2):
                pc = inproj(10 + i)
                ph = inproj(12 + i)
                xp = xp2[i]
                if tg > 0:
                    kb.cp("pool", xp[:, 0:2], xp[:, 512:514], [xp], [xp])
                kb.cp("act", cdv[:], pc[:, :], [pc], [cdv])
                kb.tt("dve", xp[:, 2:514], ph[:, :], cdv[:], ALU.mult, [ph, cdv], [xp])
                kb.ts("dve", cdv[:], xp[:, 0:512], scw[:, i, 0:1], None, ALU.mult, None, [xp, scw], [cdv])
                for k in range(1, 3):
                    kb.stt("dve", cdv[:], xp[:, k:k + 512], scw[:, i, k:k + 1], cdv[:], ALU.mult, ALU.add, [xp, scw, cdv], [cdv])
                pb_ = inproj(8 + i)
                kb.tt("dve", yo[:], pb_[:, :], cdv[:], ALU.mult, [pb_, cdv], [yo])
                kb.dma("sp", self.y1s[2][i * 128:(i + 1) * 128, tg * 512:(tg + 1) * 512], yo[:], reads=[yo], writes=[self.y1s[2]])
        kb.pop()

    def final_norm(self):
        kb, c = self.kb, self.c
        s = self.scr
        kb.push()
        fw = kb.sb("fwrow", [128, D], F32)
        kb.dma("sp", fw[:], bc(self.fnw[0:1, :], [128, D]), writes=[fw])
        ot = [kb.sb("ot%d" % i, [128, D], F32) for i in range(2)]
        for tt in range(16):
            xr = self.xres[tt]
            kb.act(s["junk"][:], xr[:], AF.Square, [xr], [s["junk"], s["ss"]], accum_out=s["ss"][:])
            kb.act(s["rs"][:], s["ss"][:], AF.Sqrt, [s["ss"], c["eps"]], [s["rs"]], scale=1.0 / D, bias=c["eps"][:, 0:1])
            kb.op("dve", lambda e: e.reciprocal(out=s["rs"][:], in_=s["rs"][:]), reads=[s["rs"]], writes=[s["rs"]])
            o = ot[tt % 2]
            kb.stt("dve", o[:], xr[:], s["rs"][:, 0:1], fw[:], ALU.mult, ALU.mult, [xr, s["rs"], fw], [o])
            kb.dma("sp", self.out[tt * 128:(tt + 1) * 128, :], o[:], reads=[o], writes=[self.out])
        kb.pop()

    def build(self):
        kb = self.kb
        self.declare()
        if self.debug and self.stop_after != "adaln":
            self.dbg_o = self.outp("dbg_o", [256, T])
            self.dbg_y0 = self.outp("dbg_y0", [512, T], BF16)
        self.consts()
        self.alloc_scr()
        if self.moeonly:
            kb.op("dve", lambda e: e.memset(self.mods[:], 1.0), writes=[self.mods])
            self.load_xres()
            self.moe()
            return self.dump_x()
        if self.m1only:
            kb.op("dve", lambda e: e.memset(self.mods[:], 1.0), writes=[self.mods])
            self.load_xres()
            self.mixer1()
            if self.m1_stop is None:
                self.gather(self.y1s, self.y1g)
                self.outproj(self.y1g, 12, self.wout1)
            return self.dump_x()
        if self.skip_ada:
            kb.op("dve", lambda e: e.memset(self.mods[:], 1.0), writes=[self.mods])
        else:
            self.adaln(0, 0)
        if self.stop_after == "adaln":
            self.dbg_mods = self.outp("dbg_mods", [128, 3 * D])
            kb.dma("sp", self.dbg_mods[:, :], self.mods[:].rearrange("p a b -> p (a b)"), reads=[self.mods], writes=[self.dbg_mods], grp="y")
            self.finish()
            return
        self.mixer0()
        kb.pop()
        if self.stop_after != "mixer0":
            self.gather(self.y0s, self.y0g)
            self.load_xres()
            self.outproj(self.y0g, 8, self.wout0)
            if self.stop_after == "op0":
                return self.dump_x()
            self.adaln(0, 1)
            self.ffn0()
            if self.stop_after == "ffn0":
                return self.dump_x()
            self.adaln(1, 0)
            self.mixer1()
            self.gather(self.y1s, self.y1g)
            self.outproj(self.y1g, 12, self.wout1)
            if self.stop_after == "premoe":
                return self.dump_x()
            self.adaln(1, 1)
            self.moe()
            self.final_norm()
            self.finish()
            return
        if self.stop_after == "mixer0":
            stg = kb.sb("stg", [128, 4, 512], BF16)
            for tg in range(self.ntg):
                for b in range(2):
                    kb.dma("sp", stg[:, 2 * b:2 * b + 2, :], self.y0s[b].h.ap()[:, tg * 512:(tg + 1) * 512].rearrange("(a p) t -> p a t", p=128),
                           reads=[self.y0s[b]], writes=[stg], grp="x")
                kb.dma("sp", self.dbg_y0.h.ap()[:, tg * 512:(tg + 1) * 512].rearrange("(a p) t -> p a t", p=128), stg[:],
                       reads=[stg], writes=[self.dbg_y0], grp="y")
            self.finish()
            return

    def dump_x(self):
        kb = self.kb
        for tt in range(16):
            kb.dma("sp", self.out[tt * 128:(tt + 1) * 128, :], self.xres[tt][:], reads=[self.xres[tt]], writes=[self.out])
        self.finish()

    def finish(self):
        kb = self.kb
        kb.barrier()


def bucket_tab():
    dist = np.arange(128)
    max_exact = 16
    d = np.maximum(dist, 0)
    large = max_exact + (np.log(np.maximum(d, 1) / max_exact) / np.log(128 / max_exact) * (32 - max_exact)).astype(np.int32)
    large = np.minimum(large, 31)
    return np.where(d < max_exact, d, large).astype(np.int32)


def host_inputs(inputs, c):
    b, half = c // 2, c % 2
    f32 = np.float32
    m = {}
    x = inputs["x"]
    m["xfull"] = np.ascontiguousarray(x[b])
    m["xown"] = np.ascontiguousarray(x[b, half * TH:(half + 1) * TH])
    fl = np.zeros((128, 2), f32)
    fl[:, 0] = 1.0 - half
    fl[:, 1] = half
    m["flag"] = fl
    m["cvec"] = np.ascontiguousarray(inputs["c"][b].reshape(8, 128).T)
    m["adaw"] = inputs["ada_w"]
    m["adab"] = inputs["ada_b"]
    m["nmw"] = inputs["norm_mix_w"]
    m["nfw"] = inputs["norm_ffn_w"]
    m["fnw"] = inputs["final_norm_w"].reshape(1, D)
    m["ident"] = np.eye(128, dtype=f32)
    r = np.arange(64)[:, None]
    cc = np.arange(64)[None, :]
    cm = np.zeros((128, 4, 64), f32)
    for q in range(2):
        cm[q * 64:(q + 1) * 64, 0] = np.where(cc >= r, 0.0, NEG)
        cm[q * 64:(q + 1) * 64, 1] = np.where(r > cc, 0.0, NEG)
        cm[q * 64:(q + 1) * 64, 2] = np.eye(64)
    m["cmask"] = cm
    sel = np.zeros((4, 2, 128), f32)
    for q in range(2):
        for j in range(2):
            sel[2 * q + j, j, q * 64:(q + 1) * 64] = 1.0
    m["sel"] = sel
    blk = np.zeros((128, 128), f32)
    blk[:64, :64] = 1.0
    blk[64:, 64:] = 1.0
    m["blk1"] = blk
    w = inputs["ab_w_in"][0]
    HA = lambda q, j: 4 * half + 2 * q + j
    cols = []
    for j in range(2):
        for q in range(2):
            cols += list(range(HA(q, j) * 64, HA(q, j) * 64 + 64))
    cols += list(range(512 + half * 64, 512 + half * 64 + 64)) * 2
    dncols = []
    for base in (768, 768 + 512, 768 + 1024, 2304):
        for j in range(2):
            for q in range(2):
                cols += list(range(base + HA(q, j) * 64, base + HA(q, j) * 64 + 64))
                if base < 2304:
                    dncols += list(range(base - 768 + HA(q, j) * 64, base - 768 + HA(q, j) * 64 + 64))
    cols += list(range(640 + half * 64, 640 + half * 64 + 64))
    cols += [2816 + 4 * half + hl for hl in range(4)]
    cols += [2824 + 4 * half + hl for hl in range(4)]
    assert len(cols) == NC0
    m["win0"] = np.ascontiguousarray(w[:, cols])
    cw = inputs["dn_conv_w"][0][:, dncols]
    m["cw0"] = np.ascontiguousarray(cw.reshape(4, 6, 128).transpose(2, 1, 0))
    hl = [4 * half + i for i in range(4)]
    m["dnsm"] = np.ascontiguousarray(np.stack([inputs["dn_a_log"][0][hl], inputs["dn_dt_bias"][0][hl]], axis=1))
    m["dnw"] = np.ascontiguousarray(np.tile(inputs["dn_norm_w"][0], 2).reshape(128, 1))
    sk = np.zeros((128, 2), f32)
    for q in range(2):
        for j in range(2):
            sk[q * 64:(q + 1) * 64, j] = inputs["attn_sinks"][0][HA(q, j)]
    m["sinkl"] = sk
    bt = bucket_tab()
    s_ = np.arange(128)[:, None]
    qi = np.arange(128)[None, :]
    bg = np.zeros((2, 128, 4, 128), f32)
    am = np.zeros((2, 128, 128), f32)
    for a in range(2):
        dist = qi + 128 - (s_ + 128 * a)
        valid = (dist >= 0) & (dist < 128)
        bk = bt[np.clip(dist, 0, 127)]
        am[a] = np.where(valid, 0.0, NEG)
        for q in range(2):
            for j in range(2):
                bg[a, :, q * 2 + j, :] = inputs["rel_bias"][bk, HA(q, j)]
    m["biasg"] = bg.reshape(2, 128, 512)
    m["amask"] = am
    rows = []
    for base in (0, 512):
        for r_ in range(2):
            for j in range(2):
                for q in range(2):
                    Hh = 4 * r_ + 2 * q + j
                    rows += list(range(base + Hh * 64, base + Hh * 64 + 64))
    m["wout0"] = np.ascontiguousarray(inputs["ab_w_out"][0][rows])
    m["wg0"] = inputs["ffn_w_gate"][0]
    m["wu0"] = inputs["ffn_w_up"][0]
    m["wd0"] = inputs["ffn_w_down"][0]
    w1 = inputs["cd_w_in"][0]
    cols = []
    for base in (0, 1024):
        for i in range(4):
            cols += list(range(base + (4 * half + i) * 128, base + (4 * half + i + 1) * 128))
    for base in (2048, 2560, 3072):
        for i in range(2):
            cols += list(range(base + (2 * half + i) * 128, base + (2 * half + i + 1) * 128))
    assert len(cols) == NC1
    m["win1"] = np.ascontiguousarray(w1[:, cols])
    ch = np.arange(512) + 512 * half
    sm = np.zeros((128, 4, 8), f32)
    sm[:, :, 0:4] = inputs["lru_conv_w"][0][:, ch].reshape(4, 4, 128).transpose(2, 1, 0)
    sm[:, :, 4] = inputs["lru_conv_b"][0][ch].reshape(4, 128).T
    sm[:, :, 5] = inputs["lru_gate_a_b"][0][ch].reshape(4, 128).T
    sm[:, :, 6] = inputs["lru_gate_x_b"][0][ch].reshape(4, 128).T
    sm[:, :, 7] = inputs["lru_lambda"][0][ch].reshape(4, 128).T
    m["l1sm"] = sm
    m["gaw"] = np.ascontiguousarray(inputs["lru_gate_a_w"][0][4 * half:4 * half + 4])
    m["gxw"] = np.ascontiguousarray(inputs["lru_gate_x_w"][0][4 * half:4 * half + 4])
    sc = inputs["sconv_w"][0][:, 256 * half:256 * half + 256]
    m["scw"] = np.ascontiguousarray(sc.reshape(3, 2, 128).transpose(2, 1, 0))
    rows = []
    for b_ in range(2):
        for r_ in range(2):
            for i in (2 * b_, 2 * b_ + 1):
                rows += list(range((4 * r_ + i) * 128, (4 * r_ + i + 1) * 128))
    for r_ in range(2):
        for i in range(2):
            rows += list(range(1024 + (2 * r_ + i) * 128, 1024 + (2 * r_ + i + 1) * 128))
    m["wout1"] = np.ascontiguousarray(inputs["cd_w_out"][0][rows])
    m["rw"] = np.ascontiguousarray(inputs["moe_router_w"][0].reshape(8, 128, 8).transpose(1, 0, 2))
    m["rb"] = inputs["moe_router_b"][0].reshape(1, 8)
    m["mwg"] = inputs["moe_w_gate"][0]
    m["mwu"] = inputs["moe_w_up"][0]
    m["mwd"] = inputs["moe_w_down"][0]
    return {k: np.ascontiguousarray(v, dtype=np.float32) for k, v in m.items()}


def run(inputs, stop_after=None, debug=False, **kw):
    pg = Prog(stop_after=stop_after, debug=debug, **kw)
    pg.build()
    in_maps = []
    for c in range(8):
        hm = host_inputs(inputs, c)
        in_maps.append({k: hm[k] for k in pg.inputs})
    res = run_bass_kernel_spmd(pg.kb.nc, in_maps, core_ids=list(range(8)))
    return res, pg


def kernel(**inputs):
    inputs = {k: np.asarray(v) for k, v in inputs.items()}
    res, pg = run(inputs)
    out = np.zeros((4, T, D), np.float32)
    for c in range(8):
        out[c // 2, (c % 2) * TH:(c % 2 + 1) * TH] = res.results[c]["out"]
    return out
```

```python
import numpy as np
import concourse.bass as bass
import concourse.mybir as mybir
from concourse.bass_utils import run_bass_kernel_spmd

F32 = mybir.dt.float32
BF16 = mybir.dt.bfloat16
AF = mybir.ActivationFunctionType
ALU = mybir.AluOpType
AX = mybir.AxisListType

T = 4096
TH = 2048
D = 1024
EPS = 1e-6
NEG = -30000.0
PAIRS = [[0, 1], [2, 3], [4, 5], [6, 7]]


class Tl:
    __slots__ = ("h", "name", "w", "r", "ds")

    def __init__(self, h, name):
        self.h = h
        self.name = name
        self.w = None
        self.r = {}
        self.ds = None

    def __getitem__(self, k):
        return self.h[k]


class Eng:
    def __init__(self, name, handle, sem):
        self.name = name
        self.h = handle
        self.sem = sem
        self.cnt = 0
        self.waited = {}

    def wait(self, ev):
        sem, val, _ = ev
        k = id(sem)
        if self.waited.get(k, 0) >= val:
            return
        self.waited[k] = val
        self.h.wait_ge(sem, val)


class KB:
    def __init__(self):
        self.nc = bass.Bass("TRN2", target_bir_lowering=False)
        nc = self.nc
        self.E = {}
        for n, h in (("pe", nc.tensor), ("act", nc.scalar), ("dve", nc.vector),
                     ("pool", nc.gpsimd), ("sp", nc.sync)):
            self.E[n] = Eng(n, h, nc.alloc_semaphore("sem_" + n))
        self.dall = []
        self.dfree = {}
        self.ninst = 0
        self.nps = 0
        self.pst = [Tl(nc.alloc_psum_tensor("ps%d" % i, [128, 512], F32), "ps%d" % i) for i in range(8)]
        self.nrot = 6

    def sb(self, name, shape, dt=F32):
        if not hasattr(self, "scopes"):
            self.scopes = [[]]
        cm = self.nc.sbuf_tensor(name, list(shape), dt)
        h = cm.__enter__()
        t = Tl(h, name)
        self.scopes[-1].append((cm, t))
        return t

    def push(self):
        if not hasattr(self, "scopes"):
            self.scopes = [[]]
        self.scopes.append([])

    def pop(self):
        self.barrier()
        for cm, t in reversed(self.scopes.pop()):
            if t.ds is not None:
                self.dfree.setdefault(t.ds[2], []).append(t.ds)
                t.ds = None
            cm.__exit__(None, None, None)

    def ps(self):
        t = self.pst[self.nps % self.nrot]
        self.nps += 1
        return t

    def dram(self, name, shape, dt, kind="Internal"):
        return Tl(self.nc.dram_tensor(name, list(shape), dt, kind=kind), name)

    def _deps(self, eng, reads, writes):
        E = self.E[eng]
        for t in reads:
            if t.w is not None:
                ev = t.w
                if ev[2] == eng and eng == "pe":
                    continue
                E.wait(ev)
        for t in writes:
            if t.w is not None and not (t.w[2] == eng and eng == "pe"):
                E.wait(t.w)
            for ev in t.r.values():
                if not (ev[2] == eng and eng == "pe"):
                    E.wait(ev)

    def _commit(self, ev, reads, writes):
        for t in reads:
            t.r[id(ev[0])] = ev
        for t in writes:
            t.w = ev
            t.r = {}

    def op(self, eng, fn, reads=(), writes=(), sig=True):
        E = self.E[eng]
        self._deps(eng, reads, writes)
        ins = fn(E.h)
        self.ninst += 1
        if sig:
            E.cnt += 1
            ins.then_inc(E.sem, 1)
            ev = (E.sem, E.cnt, eng)
        else:
            ev = (E.sem, E.cnt + 1, eng)
        self._commit(ev, reads, writes)
        return ins

    def _dsem(self, t, kind):
        if t.ds is None:
            fl = self.dfree.setdefault(kind, [])
            if fl:
                t.ds = fl.pop()
            else:
                t.ds = [self.nc.alloc_semaphore("dsem%d" % len(self.dall)), 0, kind]
                self.dall.append(t.ds)
        assert t.ds[2] == kind, (t.name, t.ds[2], kind)
        return t.ds

    def dma(self, q, out_ap, in_ap, reads=(), writes=(), grp="d"):
        E = self.E[q]
        self._deps(q, reads, writes)
        d = self._dsem(writes[0], "sw" if q == "pool" else "hw")
        d[1] += 16
        E.h.dma_start(out=out_ap, in_=in_ap).then_inc(d[0], 16)
        self.ninst += 1
        self._commit((d[0], d[1], "dma"), reads, writes)

    def collective(self, kind, groups, in_t, out_t, in_ap, out_ap, grp="cc"):
        E = self.E["pool"]
        self._deps("pool", [in_t], [out_t])
        d = self._dsem(out_t, "cc")
        d[1] += 1
        E.h.collective_compute(kind, ALU.bypass, replica_groups=groups,
                               ins=[in_ap], outs=[out_ap]).then_inc(d[0])
        self._commit((d[0], d[1], "dma"), [in_t], [out_t])

    def barrier(self):
        evs = []
        for n, E in self.E.items():
            if E.cnt > 0:
                evs.append((E.sem, E.cnt, n))
        for d in self.dall:
            if d[1] > 0:
                evs.append((d[0], d[1], "dma"))
        for n, E in self.E.items():
            for ev in evs:
                if ev[2] != n:
                    E.wait(ev)

    def mm(self, pst, out, lhsT, rhs, reads, start=True, stop=True, tp=None):
        kw = {}
        if tp is not None:
            kw["tile_position"] = tp
        return self.op("pe", lambda e: e.matmul(out, lhsT, rhs, start=start, stop=stop, **kw),
                       reads=reads, writes=[pst], sig=stop)

    def tr(self, pst, out, in_, ident, reads, tp=None):
        kw = {}
        if tp is not None:
            kw["tile_position"] = tp
        return self.op("pe", lambda e: e.transpose(out, in_, ident, **kw), reads=reads, writes=[pst])

    def act(self, out, in_, func, reads, writes, eng="act", **kw):
        return self.op(eng, lambda e: e.activation(out=out, in_=in_, func=func, **kw), reads=reads, writes=writes)

    def tt(self, eng, out, a, b, op, reads, writes):
        return self.op(eng, lambda e: e.tensor_tensor(out=out, in0=a, in1=b, op=op), reads=reads, writes=writes)

    def ts(self, eng, out, a, s1, s2, op0, op1, reads, writes):
        if op1 is None:
            return self.op(eng, lambda e: e.tensor_scalar(out=out, in0=a, scalar1=s1, scalar2=None, op0=op0),
                           reads=reads, writes=writes)
        return self.op(eng, lambda e: e.tensor_scalar(out=out, in0=a, scalar1=s1, scalar2=s2, op0=op0, op1=op1),
                       reads=reads, writes=writes)

    def stt(self, eng, out, a, s, b, op0, op1, reads, writes):
        return self.op(eng, lambda e: e.scalar_tensor_tensor(out=out, in0=a, scalar=s, in1=b, op0=op0, op1=op1),
                       reads=reads, writes=writes)

    def cp(self, eng, out, in_, reads, writes):
        if eng == "act":
            return self.op(eng, lambda e: e.copy(out=out, in_=in_), reads=reads, writes=writes)
        return self.op(eng, lambda e: e.tensor_copy(out=out, in_=in_), reads=reads, writes=writes)


def bc(ap, shape):
    return ap.broadcast_to(list(shape))


NC0 = 11 * 128 + 64 + 8
NC1 = 14 * 128


class Prog:
    def __init__(self, stop_after=None, debug=False, ntg=8, m0_stop=None, skip_ada=False, m1only=False, m1_stop=None):
        self.m1only = m1only
        self.m1_stop = m1_stop
        self.moeonly = False
        self.ntg = ntg
        self.nexp = 8
        self.groups = PAIRS
        self.m0_stop = m0_stop
        self.skip_ada = skip_ada
        self.kb = KB()
        self.stop_after = stop_after
        self.debug = debug
        self.inputs = {}
        self.outputs = {}

    def inp(self, name, shape, dt=F32):
        t = self.kb.dram(name, shape, dt, kind="ExternalInput")
        self.inputs[name] = t
        return t

    def outp(self, name, shape, dt=F32):
        t = self.kb.dram(name, shape, dt, kind="ExternalOutput")
        self.outputs[name] = t
        return t

    def declare(self):
        I = self.inp
        self.xfull = I("xfull", [T, D])
        self.xown = I("xown", [TH, D])
        self.flag = I("flag", [128, 2])
        self.cvec = I("cvec", [128, 8])
        self.adaw = I("adaw", [2, D, 6 * D])
        self.adab = I("adab", [2, 6 * D])
        self.nmw = I("nmw", [2, D])
        self.nfw = I("nfw", [2, D])
        self.fnw = I("fnw", [1, D])
        self.ident = I("ident", [128, 128])
        self.cmask = I("cmask", [128, 4, 64])
        self.sel = I("sel", [4, 2, 128])
        self.blk1 = I("blk1", [128, 128])
        self.win0 = I("win0", [D, NC0])
        self.cw0 = I("cw0", [128, 6, 4])
        self.dnsm = I("dnsm", [4, 2])
        self.dnw = I("dnw", [128, 1])
        self.sinkl = I("sinkl", [128, 2])
        self.biasg = I("biasg", [2, 128, 512])
        self.amask = I("amask", [2, 128, 128])
        if self.stop_after in ("adaln", "mixer0") and not self.m1only:
            self._internal()
            return
        if self.moeonly:
            self.rw = I("rw", [128, 8, 8])
            self.rb = I("rb", [1, 8])
            self.mwg = I("mwg", [self.nexp, D, 3584])
            self.mwu = I("mwu", [self.nexp, D, 3584])
            self.mwd = I("mwd", [self.nexp, 3584, D])
            self.out = self.outp("out", [TH, D])
            self.dbg_g = self.outp("dbg_g", [128, 128])
            self._internal()
            return
        if self.m1only:
            self.win1 = I("win1", [D, NC1])
            self.l1sm = I("l1sm", [128, 4, 8])
            self.gaw = I("gaw", [4, 128, 128])
            self.gxw = I("gxw", [4, 128, 128])
            self.scw = I("scw", [128, 2, 3])
            self.wout1 = I("wout1", [1536, D])
            self.out = self.outp("out", [TH, D])
            self._internal()
            return
        self.wout0 = I("wout0", [D, D])
        if self.stop_after == "op0":
            self.out = self.outp("out", [TH, D])
            self._internal()
            return
        self.wg0 = I("wg0", [D, 2816])
        self.wu0 = I("wu0", [D, 2816])
        self.wd0 = I("wd0", [2816, D])
        if self.stop_after == "ffn0":
            self.out = self.outp("out", [TH, D])
            self._internal()
            return
        self.win1 = I("win1", [D, NC1])
        self.l1sm = I("l1sm", [128, 4, 8])
        self.gaw = I("gaw", [4, 128, 128])
        self.gxw = I("gxw", [4, 128, 128])
        self.scw = I("scw", [128, 2, 3])
        self.wout1 = I("wout1", [1536, D])
        if self.stop_after != "premoe":
            self.rw = I("rw", [128, 8, 8])
            self.rb = I("rb", [1, 8])
            self.mwg = I("mwg", [8, D, 3584])
            self.mwu = I("mwu", [8, D, 3584])
            self.mwd = I("mwd", [8, 3584, D])
        self.out = self.outp("out", [TH, D])
        self._internal()

    def _internal(self):
        kb = self.kb
        self.y0s = [kb.dram("y0s%d" % i, [256, T], BF16) for i in range(2)]
        self.y0g = [kb.dram("y0g%d" % i, [512, T], BF16) for i in range(2)]
        self.h1s = [kb.dram("h1s%d" % i, [512, TH], BF16) for i in range(2)]
        self.h1g = [kb.dram("h1g%d" % i, [1024, TH], BF16) for i in range(2)]
        self.y1s = [kb.dram("y1s%d" % i, [256, T], BF16) for i in range(3)]
        self.y1g = [kb.dram("y1g%d" % i, [512, T], BF16) for i in range(3)]

    def consts(self):
        kb = self.kb
        c = {}
        self.c = c
        c["idf"] = kb.sb("idf", [128, 128], F32)
        c["idb"] = kb.sb("idb", [128, 128], BF16)
        c["cm"] = kb.sb("cm", [128, 4, 64], F32)
        c["i64b"] = kb.sb("i64b", [128, 64], BF16)
        c["sel"] = kb.sb("selc", [4, 2, 128], F32)
        c["blk1f"] = kb.sb("blk1f", [128, 128], F32)
        c["blk1"] = kb.sb("blk1b", [128, 128], BF16)
        c["ones"] = kb.sb("onesb", [128, 128], BF16)
        c["flag"] = kb.sb("flagc", [128, 2], F32)
        c["cv"] = kb.sb("cv", [128, 8], F32)
        c["cond"] = kb.sb("cond", [128, 8], F32)
        c["condB"] = kb.sb("condB", [128, 8, 128], BF16)
        c["eps"] = kb.sb("epsc", [128, 1], F32)
        c["one"] = kb.sb("onec", [128, 1], F32)
        q = "sp"
        kb.dma(q, c["idf"][:], self.ident[:, :], writes=[c["idf"]], grp="c")
        kb.dma(q, c["cm"][:], self.cmask[:, :, :], writes=[c["cm"]], grp="c")
        kb.dma(q, c["sel"][:], self.sel[:, :, :], writes=[c["sel"]], grp="c")
        kb.dma(q, c["blk1f"][:], self.blk1[:, :], writes=[c["blk1f"]], grp="c")
        kb.dma(q, c["flag"][:], self.flag[:, :], writes=[c["flag"]], grp="c")
        kb.dma(q, c["cv"][:], self.cvec[:, :], writes=[c["cv"]], grp="c")
        kb.cp("dve", c["idb"][:], c["idf"][:], [c["idf"]], [c["idb"]])
        kb.cp("dve", c["blk1"][:], c["blk1f"][:], [c["blk1f"]], [c["blk1"]])
        kb.cp("dve", c["i64b"][:], c["cm"][:, 2, :], [c["cm"]], [c["i64b"]])
        kb.op("dve", lambda e: e.memset(c["ones"][:], 1.0), writes=[c["ones"]])
        kb.op("dve", lambda e: e.memset(c["eps"][:], EPS), writes=[c["eps"]])
        kb.op("dve", lambda e: e.memset(c["one"][:], 1.0), writes=[c["one"]])
        kb.act(c["cond"][:], c["cv"][:], AF.Silu, [c["cv"]], [c["cond"]])
        kb.cp("dve", c["condB"][:], bc(c["cond"][:].unsqueeze(2), [128, 8, 128]), [c["cond"]], [c["condB"]])
        self.mods = kb.sb("mods", [128, 3, D], F32)

    def adaln(self, l, part):
        kb, c = self.kb, self.c
        kb.push()
        self.wbuf = [kb.sb("wbuf%d_%d_%d" % (i, l, part), [128, 8, 512], BF16) for i in range(2)]
        self.rowt = kb.sb("rowt_%d_%d" % (l, part), [128, 512], F32)
        self.rowt2 = kb.sb("rowt2_%d_%d" % (l, part), [128, D], F32)
        for n in range(part * 6, part * 6 + 6):
            wb = self.wbuf[n % 2]
            kb.dma("pool", wb[:], self.adaw[l, :, n * 512:(n + 1) * 512].rearrange("(k p) n -> p k n", p=128),
                   writes=[wb], grp="w")
            kb.dma("sp", self.rowt[:], bc(self.adab[l:l + 1, n * 512:(n + 1) * 512], [128, 512]),
                   writes=[self.rowt], grp="c")
            p = kb.ps()
            for k in range(8):
                kb.mm(p, p[:, :], c["condB"][:, k, :], wb[:, k, :], [c["condB"], wb], start=(k == 0), stop=(k == 7))
            kb.tt("dve", self.mods[:, (n // 2) % 3, (n % 2) * 512:(n % 2) * 512 + 512], p[:, :], self.rowt[:], ALU.add,
                  [p, self.rowt], [self.mods])
        w = self.nmw if part == 0 else self.nfw
        kb.dma("sp", self.rowt2[:], bc(w[l:l + 1, :], [128, D]), writes=[self.rowt2], grp="c")
        kb.stt("dve", self.mods[:, 1, :], self.mods[:, 1, :], 1.0, self.rowt2[:], ALU.add, ALU.mult,
               [self.mods, self.rowt2], [self.mods])
        kb.pop()

    def norm_tile(self, xt_ap, xt_tl, ia, ib, hnT_ap, hnT_tl, eng2="dve"):
        kb, c = self.kb, self.c
        s = self.scr
        kb.act(s["junk"][:], xt_ap, AF.Square, [xt_tl], [s["junk"], s["ss"]], accum_out=s["ss"][:])
        kb.act(s["rs"][:], s["ss"][:], AF.Sqrt, [s["ss"], c["eps"]], [s["rs"]], scale=1.0 / D, bias=c["eps"][:, 0:1])
        kb.op("dve", lambda e: e.reciprocal(out=s["rs"][:], in_=s["rs"][:]), reads=[s["rs"]], writes=[s["rs"]])
        kb.stt("dve", s["t1"][:], xt_ap, s["rs"][:, 0:1], self.mods[:, ia, :], ALU.mult, ALU.mult,
               [xt_tl, s["rs"], self.mods], [s["t1"]])
        kb.tt(eng2, s["hn"][:], s["t1"][:], self.mods[:, ib, :], ALU.add, [s["t1"], self.mods], [s["hn"]])
        p = kb.ps()
        pb = p[:, :].bitcast(BF16)
        for k in range(8):
            kb.tr(p, pb[:, k * 128:(k + 1) * 128], s["hn"][:, k * 128:(k + 1) * 128], c["idb"][:], [s["hn"], c["idb"]])
        kb.cp("act", hnT_ap, pb[:, 0:1024].rearrange("p (k t) -> p k t", k=8), [p], [hnT_tl])

    def alloc_scr(self):
        kb = self.kb
        s = {}
        self.scr = s
        s["junk"] = kb.sb("junk", [128, D], BF16)
        s["ss"] = kb.sb("ss", [128, 1], F32)
        s["rs"] = kb.sb("rs", [128, 1], F32)
        s["t1"] = kb.sb("t1", [128, D], F32)
        s["hn"] = kb.sb("hn", [128, D], BF16)
        self.xin = [kb.sb("xin%d" % i, [128, D], F32) for i in range(2)]

    def mixer0(self):
        kb, c = self.kb, self.c
        sb = kb.sb
        kb.push()
        w0 = sb("w0", [128, 8, NC0], BF16)
        for i in range(0, NC0, 512):
            j = min(i + 512, NC0)
            kb.dma("pool", w0[:, :, i:j], self.win0[:, i:j].rearrange("(k p) n -> p k n", p=128), writes=[w0], grp="w")
        cw = sb("cw", [128, 6, 4], F32)
        kb.dma("sp", cw[:], self.cw0[:, :, :], writes=[cw], grp="c")
        dnsm = sb("dnsmc", [4, 2], F32)
        kb.dma("sp", dnsm[:], self.dnsm[:, :], writes=[dnsm], grp="c")
        nega = sb("nega", [4, 1], F32)
        kb.act(nega[:], dnsm[:, 0:1], AF.Exp, [dnsm], [nega])
        kb.ts("dve", nega[:], nega[:], -1.0, None, ALU.mult, None, [nega], [nega])
        dnw = sb("dnwc", [128, 1], F32)
        kb.dma("sp", dnw[:], self.dnw[:, :], writes=[dnw], grp="c")
        sinkE = sb("sinkE", [128, 2], F32)
        kb.dma("sp", sinkE[:], self.sinkl[:, :], writes=[sinkE], grp="c")
        kb.act(sinkE[:], sinkE[:], AF.Exp, [sinkE], [sinkE])
        biasm = sb("biasm", [128, 2, 512], F32)
        am = sb("am", [128, 2, 128], F32)
        kb.dma("sp", biasm[:], self.biasg.h.ap().rearrange("a p n -> p a n"), writes=[biasm], grp="c")
        kb.dma("sp", am[:], self.amask.h.ap().rearrange("a p n -> p a n"), writes=[am], grp="c")
        for a in range(2):
            kb.tt("dve", biasm[:, a, :].rearrange("p (s q) -> p s q", s=4),
                  biasm[:, a, :].rearrange("p (s q) -> p s q", s=4),
                  bc(am[:, a, :].unsqueeze(1), [128, 4, 128]), ALU.add, [biasm, am], [biasm])
        rmask = sb("rmask", [4, 8, 64], F32)
        kb.op("dve", lambda e: e.memset(rmask[:], 1.0), writes=[rmask])
        kb.op("dve", lambda e: e.memset(rmask[:, :, 0:1], 0.0), writes=[rmask])

        mU8 = sb("mU8", [128, 8, 64], F32)
        mL8 = sb("mL8", [128, 8, 64], F32)
        kb.cp("dve", mU8[:], bc(c["cm"][:, 0, :].unsqueeze(1), [128, 8, 64]), [c["cm"]], [mU8])
        kb.cp("dve", mL8[:], bc(c["cm"][:, 1, :].unsqueeze(1), [128, 8, 64]), [c["cm"]], [mL8])
        hnT = sb("hnT0", [128, 8, 512], BF16)
        qaT = sb("qaT", [128, 2, 512], BF16)
        kaT = sb("kaT", [128, 2, 128 + T], BF16)
        vat = sb("vat", [128, 33, 64], BF16)
        kb.op("pool", lambda e: e.memset(kaT[:], 0.0), writes=[kaT])
        kb.op("dve", lambda e: e.memset(vat[:, 0, :], 0.0), writes=[vat])
        xpre = [sb("xpre%d" % i, [128, 3 + 512], F32) for i in range(6)]
        for i in range(6):
            kb.op("pool", lambda e, i=i: e.memset(xpre[i][:, 0:3], 0.0), writes=[xpre[i]])
        gates = sb("gates", [128, 2, 512], F32)
        tl = sb("tl", [128, 512], F32)
        cacc = sb("cacc", [128, 512], F32)
        ysil = sb("ysil", [128, 512], F32)
        sqb = sb("sqb", [128, 512], BF16)
        rstd = sb("rstd", [128, 512], F32)
        qn = [sb("qn%d" % j, [128, 512], BF16) for j in range(2)]
        qnf = [sb("qnf%d" % j, [128, 512], F32) for j in range(2)]
        i8f = sb("i8f", [128, 8, 64], F32)
        kb.cp("dve", i8f[:], bc(c["cm"][:, 2, :].unsqueeze(1), [128, 8, 64]), [c["cm"]], [i8f])
        A32 = sb("A32", [128, 8, 64], F32)
        Xs = sb("Xs", [128, 4, 64], F32)
        kn = [sb("kn%d" % j, [128, 512], BF16) for j in range(2)]
        vT = [sb("vT%d" % j, [128, 512], BF16) for j in range(2)]
        bt = sb("bt", [4, 512], F32)
        gt = sb("gt", [4, 512], F32)
        Gs = sb("Gs", [4, 512], F32)
        Es = sb("Es", [4, 512], F32)
        BEs = sb("BEs", [4, 512], F32)
        DKs = sb("DKs", [4, 512], F32)
        nbt = gt
        tk4 = sb("tk4", [128, 5, 8, 4], F32)
        TK = sb("TK", [128, 5, 8, 2], F32)
        EGLc = sb("EGLc", [128, 2, 8], F32)
        t0 = sb("t0", [128, 8, 64], F32)
        tU = sb("tU", [128, 8, 64], F32)
        tL = sb("tL", [128, 8, 64], F32)
        Du = tU
        Dl = tL
        tmpf = t0
        NTp = [sb("NTp%d" % i, [128, 8, 64], BF16) for i in range(2)]
        Np = [sb("Np%d" % i, [128, 8, 64], BF16) for i in range(2)]
        Am = sb("Am", [128, 8, 64], BF16)
        KV = sb("KV", [128, 8, 128], BF16)
        KQ = sb("KQ", [128, 8, 128], BF16)
        Wu = sb("Wu", [128, 8, 128], BF16)
        qdec = sb("qdec", [128, 8, 64], F32)
        UT = [sb("UT%d" % j, [128, 8, 64], BF16) for j in range(2)]
        RT = [sb("RT%d" % j, [128, 8, 64], BF16) for j in range(2)]
        O0 = [sb("O0%d" % j, [128, 8, 64], F32) for j in range(2)]
        Qs = [sb("Qs%d" % j, [128, 8, 64], F32) for j in range(2)]
        S32 = [sb("S32_%d" % j, [128, 64], F32) for j in range(2)]
        Sbf = [sb("Sbf_%d" % j, [128, 64], BF16) for j in range(2)]
        pre = [sb("pre%d" % j, [128, 64], F32) for j in range(2)]
        cS = [sb("cS%d" % j, [128, 64], F32) for j in range(2)]
        oT = sb("oT", [128, 8, 64], F32)
        yo = sb("yo", [128, 512], BF16)
        for j in range(2):
            kb.op("dve", lambda e, j=j: e.memset(S32[j][:], 0.0), writes=[S32[j]])
            kb.op("dve", lambda e, j=j: e.memset(Sbf[j][:], 0.0), writes=[Sbf[j]])
        PT = [sb("PT%d" % i, [128, 4, 128], BF16) for i in range(2)]
        den = sb("den", [128, 2, 128], F32)
        ao = sb("ao", [128, 2, 128], BF16)
        H = (slice(0, 64), slice(64, 128))

        for tg in range(self.ntg):
            for tt in range(4):
                xt = self.xin[tt % 2]
                r0 = tg * 512 + tt * 128
                kb.dma("sp", xt[:], self.xfull[r0:r0 + 128, :], writes=[xt], grp="x")
                self.norm_tile(xt[:], xt, 1, 0, hnT[:, :, tt * 128:(tt + 1) * 128], hnT)
            for ci in range(11):
                p = kb.ps()
                for k in range(8):
                    kb.mm(p, p[:, :], w0[:, k, ci * 128:(ci + 1) * 128], hnT[:, k, :], [w0, hnT], start=(k == 0), stop=(k == 7))
                if ci < 2:
                    kb.act(qaT[:, ci, :], p[:, :], AF.Copy, [p], [qaT], scale=0.125)
                elif ci == 2:
                    for q in range(2):
                        kb.cp("act", kaT[H[q], q, 128 + tg * 512:128 + (tg + 1) * 512], p[H[q], :], [p], [kaT])
                elif ci < 9:
                    xp = xpre[ci - 3]
                    if tg > 0:
                        kb.cp("pool", xp[:, 0:3], xp[:, 512:515], [xp], [xp])
                    kb.cp("act", xp[:, 3:515], p[:, :], [p], [xp])
                else:
                    kb.act(gates[:, ci - 9, :], p[:, :], AF.Silu, [p], [gates])
            for tt in range(4):
                p = kb.ps()
                for k in range(8):
                    kb.mm(p, p[:, 0:64], hnT[:, k, tt * 128:(tt + 1) * 128], w0[:, k, 1408:1472], [w0, hnT], start=(k == 0), stop=(k == 7))
                kb.cp("act", vat[:, 1 + tg * 4 + tt, :], p[:, 0:64], [p], [vat])
            pb_ = kb.ps()
            pd_ = kb.ps()
            for k in range(8):
                kb.mm(pb_, pb_[0:4, :], w0[:, k, 1472:1476], hnT[:, k, :], [w0, hnT], start=(k == 0), stop=(k == 7))
            for k in range(8):
                kb.mm(pd_, pd_[0:4, :], w0[:, k, 1476:1480], hnT[:, k, :], [w0, hnT], start=(k == 0), stop=(k == 7))
            kb.act(bt[:], pb_[0:4, :], AF.Sigmoid, [pb_], [bt])
            kb.act(gt[:], pd_[0:4, :], AF.Exp, [pd_, dnsm], [gt], bias=dnsm[:, 1:2])
            kb.act(gt[:], gt[:], AF.Ln, [gt, c["one"]], [gt], bias=c["one"][0:4, 0:1])
            kb.ts("dve", gt[:], gt[:], nega[:, 0:1], None, ALU.mult, None, [gt, nega], [gt])
            kb.op("dve", lambda e: e.tensor_tensor_scan(out=Gs[:], data0=rmask[:].rearrange("p a b -> p (a b)"), data1=gt[:],
                                                        initial=0.0, op0=ALU.mult, op1=ALU.add), reads=[rmask, gt], writes=[Gs])
            kb.act(Es[:], Gs[:], AF.Exp, [Gs], [Es])
            kb.tt("dve", BEs[:], bt[:], Es[:], ALU.mult, [bt, Es], [BEs])
            G3 = Gs[:].rearrange("p (a b) -> p a b", a=8)
            kb.tt("dve", DKs[:].rearrange("p (a b) -> p a b", a=8), G3, bc(G3[:, :, 63:64], [4, 8, 64]), ALU.subtract, [Gs], [DKs])
            kb.act(DKs[:], DKs[:], AF.Exp, [DKs], [DKs], scale=-1.0)
            kb.ts("dve", nbt[:], bt[:], -1.0, None, ALU.mult, None, [bt], [nbt])
            p = kb.ps()
            for qi, X in enumerate((Gs, nbt, BEs, DKs, bt)):
                for n in range(8):
                    for q in range(2):
                        kb.mm(p, p[H[q], (qi * 8 + n) * 4:(qi * 8 + n) * 4 + 4], X[:, n * 64:(n + 1) * 64], c["idf"][0:4, 0:4],
                              [X, c["idf"]], tp=(0, 64 * q))
            kb.cp("dve", tk4[:].rearrange("p a n h -> p (a n h)"), p[:, 0:160], [p], [tk4])
            for q in range(2):
                kb.cp("dve", TK[H[q], :, :, :], tk4[H[q], :, :, 2 * q:2 * q + 2], [tk4], [TK])
            p = kb.ps()
            for j in range(2):
                kb.mm(p, p[:, j * 8:j * 8 + 8], c["sel"][:, j, :], Es[:].rearrange("p (a b) -> p a b", a=8)[:, :, 63], [c["sel"], Es])
            kb.cp("dve", EGLc[:].rearrange("p j n -> p (j n)"), p[:, 0:16], [p], [EGLc])

            def gen_attn():
                for qb in range(4):
                    n = tg * 4 + qb
                    kbs = [n - 1, n] if n > 0 else [n]
                    for ki, kbk in enumerate(kbs):
                        sel_ = 0 if kbk == n - 1 else 1
                        pl = kb.ps()
                        for q in range(2):
                            for j in range(2):
                                sl = q * 2 + j
                                kb.mm(pl, pl[:, sl * 128:(sl + 1) * 128], kaT[:, q, 128 + kbk * 128:128 + (kbk + 1) * 128],
                                      qaT[:, j, qb * 128:(qb + 1) * 128], [kaT, qaT])
                        kb.tt("dve", tl[:], pl[:, :], biasm[:, sel_, :], ALU.add, [pl, biasm], [tl])
                        kb.act(PT[ki][:].rearrange("p s q -> p (s q)"), tl[:], AF.Exp, [tl], [PT[ki]])
                        yield
                    po = kb.ps()
                    pdn = kb.ps()
                    for q in range(2):
                        for j in range(2):
                            sl = q * 2 + j
                            for ki, kbk in enumerate(kbs):
                                kb.mm(po, po[H[q], j * 128:(j + 1) * 128], vat[:, 1 + kbk, :], PT[ki][:, sl, :], [vat, PT[ki]],
                                      start=(ki == 0), stop=(ki == len(kbs) - 1), tp=(0, 64 * q))
                            for ki, kbk in enumerate(kbs):
                                kb.mm(pdn, pdn[H[q], j * 128:(j + 1) * 128], c["ones"][:, 0:64], PT[ki][:, sl, :], [c["ones"], PT[ki]],
                                      start=(ki == 0), stop=(ki == len(kbs) - 1), tp=(0, 64 * q))
                    kb.tt("dve", den[:], pdn[:, 0:256].rearrange("p (j t) -> p j t", j=2), bc(sinkE[:].unsqueeze(2), [128, 2, 128]),
                          ALU.add, [pdn, sinkE], [den])
                    kb.op("dve", lambda e: e.reciprocal(out=den[:], in_=den[:]), reads=[den], writes=[den])
                    kb.tt("dve", ao[:], po[:, 0:256].rearrange("p (j t) -> p j t", j=2), den[:], ALU.mult, [po, den], [ao])
                    for j in range(2):
                        kb.dma("sp", self.y0s[0][j * 128:(j + 1) * 128, n * 128:(n + 1) * 128], ao[:, j, :], reads=[ao], writes=[self.y0s[0]], grp="y")
                    yield

            def gen_dn():
                for ci in range(6):
                    xp = xpre[ci]
                    j = ci % 2
                    kind = ci // 2
                    kb.ts("dve", cacc[:], xp[:, 0:512], cw[:, ci, 0:1], None, ALU.mult, None, [xp, cw], [cacc])
                    for k in range(1, 4):
                        kb.stt("dve", cacc[:], xp[:, k:k + 512], cw[:, ci, k:k + 1], cacc[:], ALU.mult, ALU.add, [xp, cw, cacc], [cacc])
                    if kind == 2:
                        kb.act(vT[j][:], cacc[:], AF.Silu, [cacc], [vT[j]])
                        yield
                        continue
                    kb.act(ysil[:], cacc[:], AF.Silu, [cacc], [ysil])
                    kb.act(sqb[:], ysil[:], AF.Square, [ysil], [sqb])
                    p = kb.ps()
                    kb.mm(p, p[:, :], c["blk1"][:], sqb[:], [c["blk1"], sqb])
                    kb.act(rstd[:], p[:, :], AF.Sqrt, [p, c["eps"]], [rstd], bias=c["eps"][:, 0:1])
                    kb.op("dve", lambda e: e.reciprocal(out=rstd[:], in_=rstd[:]), reads=[rstd], writes=[rstd])
                    if kind == 0:
                        kb.stt("dve", qnf[j][:], ysil[:], 0.125, rstd[:], ALU.mult, ALU.mult, [ysil, rstd], [qnf[j]])
                        kb.cp("act", qn[j][:], qnf[j][:], [qnf[j]], [qn[j]])
                    else:
                        kb.tt("dve", kn[j][:], ysil[:], rstd[:], ALU.mult, [ysil, rstd], [kn[j]])
                    yield

                for j in range(2):
                    k3 = kn[j][:].rearrange("p (n c) -> p n c", n=8)
                    q3 = qn[j][:].rearrange("p (n c) -> p n c", n=8)
                    v3 = vT[j][:].rearrange("p (n c) -> p n c", n=8)
                    pk = kb.ps()
                    pv = kb.ps()
                    pkb = pk[:, :].rearrange("p (n x) -> p n x", n=8)
                    pvb = pv[:, :].rearrange("p (n x) -> p n x", n=8)
                    for n in range(8):
                        for q in range(2):
                            kb.mm(pk, pkb[H[q], n, :], k3[H[q], n, :], c["idb"][H[q], 64 * q:64 * q + 64], [kn[j], c["idb"]], tp=(64 * q, 64 * q))
                    for n in range(8):
                        for q in range(2):
                            kb.mm(pv, pvb[H[q], n, :], v3[H[q], n, :], c["idb"][H[q], 64 * q:64 * q + 64], [vT[j], c["idb"]], tp=(64 * q, 64 * q))
                    kb.tt("dve", KQ[:, :, 0:64], bc(TK[:, 3, :, j:j + 1], [128, 8, 64]), pkb, ALU.mult, [pk, TK], [KQ])
                    kb.tt("dve", KV[:, :, 0:64], bc(TK[:, 2, :, j:j + 1], [128, 8, 64]), pkb, ALU.mult, [pk, TK], [KV])
                    kb.tt("dve", KV[:, :, 64:128], bc(TK[:, 4, :, j:j + 1], [128, 8, 64]), pvb, ALU.mult, [pv, TK], [KV])
                    yield
                    pg = kb.ps()
                    pe_ = kb.ps()
                    kb.mm(pg, pg[:, :], c["sel"][:, j, :], Gs[:], [c["sel"], Gs])
                    kb.mm(pe_, pe_[:, :], c["sel"][:, j, :], Es[:], [c["sel"], Es])
                    pg3 = pg[:, :].rearrange("p (n c) -> p n c", n=8)
                    kb.tt("dve", t0[:], pg3, bc(TK[:, 0, :, j:j + 1], [128, 8, 64]), ALU.subtract, [pg, TK], [t0])
                    kb.tt("pool", tU[:], t0[:], mU8[:], ALU.add, [t0, mU8], [tU])
                    kb.stt("dve", tL[:], t0[:], -1.0, mL8[:], ALU.mult, ALU.add, [t0, mL8], [tL])
                    kb.act(Du[:], tU[:], AF.Exp, [tU], [Du])
                    kb.act(Dl[:], tL[:], AF.Exp, [tL], [Dl])
                    kb.cp("act", cacc[:], pe_[:, :], [pe_], [cacc])
                    kb.tt("dve", qdec[:].rearrange("p n c -> p (n c)"), cacc[:], qnf[j][:], ALU.mult, [qnf[j], cacc], [qdec])
                    yield
                    pkk = kb.ps()
                    pqk = kb.ps()
                    kk3 = pkk[:, :].rearrange("p (n c) -> p n c", n=8)
                    qk3 = pqk[:, :].rearrange("p (n c) -> p n c", n=8)
                    for n in range(8):
                        for q in range(2):
                            kb.mm(pkk, kk3[H[q], n, :], k3[H[q], n, :], k3[H[q], n, :], [kn[j]], tp=(64 * q, 64 * q))
                    for n in range(8):
                        for q in range(2):
                            kb.mm(pqk, qk3[H[q], n, :], k3[H[q], n, :], q3[H[q], n, :], [kn[j], qn[j]], tp=(64 * q, 64 * q))
                    kb.tt("dve", tmpf[:], bc(TK[:, 1, :, j:j + 1], [128, 8, 64]), Dl[:], ALU.mult, [TK, Dl], [tmpf])
                    kb.cp("act", tl[:], pkk[:, :], [pkk], [tl])
                    kb.tt("dve", NTp[0][:], tl[:].rearrange("p (n c) -> p n c", n=8), tmpf[:], ALU.mult, [tl, tmpf], [NTp[0]])
                    kb.cp("act", cacc[:], pqk[:, :], [pqk], [cacc])
                    kb.tt("dve", KQ[:, :, 64:128], cacc[:].rearrange("p (n c) -> p n c", n=8), Du[:], ALU.mult, [cacc, Du], [KQ])
                    yield
                    pn = kb.ps()
                    pnb = pn[:, :].rearrange("p (n c) -> p n c", n=8)
                    for n in range(8):
                        for q in range(2):
                            kb.mm(pn, pnb[H[q], n, :], NTp[0][H[q], n, :], c["idb"][H[q], 64 * q:64 * q + 64], [NTp[0], c["idb"]], tp=(64 * q, 64 * q))
                    kb.cp("act", Np[0][:], pnb, [pn], [Np[0]])
                    kb.cp("act", A32[:], pnb, [pn], [A32])
                    kb.tt("pool", A32[:], A32[:], i8f[:], ALU.add, [A32, i8f], [A32])
                    kb.cp("act", Am[:], A32[:], [A32], [Am])
                    cur = 0
                    for lev in range(5):
                        nxt = 1 - cur
                        if lev < 4:
                            p1 = kb.ps()
                            p13 = p1[:, :].rearrange("p (n c) -> p n c", n=8)
                            for n in range(8):
                                for q in range(2):
                                    kb.mm(p1, p13[H[q], n, :], NTp[cur][H[q], n, :], Np[cur][H[q], n, :], [NTp[cur], Np[cur]], tp=(64 * q, 64 * q))
                        p2 = kb.ps()
                        p23 = p2[:, :].rearrange("p (n c) -> p n c", n=8)
                        for n in range(8):
                            for q in range(2):
                                kb.mm(p2, p23[H[q], n, :], Np[cur][H[q], n, :], NTp[cur][H[q], n, :], [NTp[cur], Np[cur]], tp=(64 * q, 64 * q))
                        if lev < 4:
                            kb.cp("act", Np[nxt][:], p13, [p1], [Np[nxt]])
                        kb.cp("act", NTp[nxt][:], p23, [p2], [NTp[nxt]])
                        p3 = kb.ps()
                        p33 = p3[:, :].rearrange("p (n c) -> p n c", n=8)
                        for n in range(8):
                            for q in range(2):
                                kb.mm(p3, p33[H[q], n, :], NTp[nxt][H[q], n, :], Am[H[q], n, :], [NTp[nxt], Am], tp=(64 * q, 64 * q))
                        kb.cp("act", tl[:], p3[:, :], [p3], [tl])
                        kb.tt("dve", A32[:], tl[:].rearrange("p (n c) -> p n c", n=8), A32[:], ALU.add, [A32, tl], [A32])
                        kb.cp("act", Am[:], A32[:], [A32], [Am])
                        cur = nxt
                        yield
                    yield
                    for hf in range(2):
                        pw = kb.ps()
                        pw3 = pw[:, :].rearrange("p (n c) -> p n c", n=4)
                        for n in range(4):
                            for q in range(2):
                                kb.mm(pw, pw3[H[q], n, :], Am[H[q], hf * 4 + n, :], KV[H[q], hf * 4 + n, :], [Am, KV], tp=(64 * q, 64 * q))
                        kb.cp("act", Wu[:, hf * 4:hf * 4 + 4, :], pw3, [pw], [Wu])
                    yield
                    for hf in range(2):
                        pu = kb.ps()
                        pu3 = pu[:, :].rearrange("p (n c) -> p n c", n=4)
                        for n in range(4):
                            for q in range(2):
                                kb.mm(pu, pu3[H[q], n, :], Wu[H[q], hf * 4 + n, 0:64], KQ[H[q], hf * 4 + n, :], [Wu, KQ], tp=(64 * q, 64 * q))
                        kb.cp("act", UT[j][:, hf * 4:hf * 4 + 4, :], pu3[:, :, 0:64], [pu], [UT[j]])
                        kb.cp("act", Xs[:], pu3[:, :, 64:128], [pu], [Xs])
                        kb.tt("pool", RT[j][:, hf * 4:hf * 4 + 4, :], qdec[:, hf * 4:hf * 4 + 4, :], Xs[:], ALU.subtract, [qdec, Xs], [RT[j]])
                    po0 = kb.ps()
                    pq_ = kb.ps()
                    po03 = po0[:, :].rearrange("p (n c) -> p n c", n=8)
                    pq3 = pq_[:, :].rearrange("p (n c) -> p n c", n=8)
                    for n in range(8):
                        for q in range(2):
                            kb.mm(po0, po03[H[q], n, :], Wu[H[q], n, 64:128], KQ[H[q], n, 64:128], [Wu, KQ], tp=(64 * q, 64 * q))
                    for n in range(8):
                        for q in range(2):
                            kb.mm(pq_, pq3[H[q], n, :], KQ[H[q], n, 0:64], Wu[H[q], n, 64:128], [Wu, KQ], tp=(64 * q, 64 * q))
                    kb.cp("act", O0[j][:], po03, [po0], [O0[j]])
                    kb.cp("act", Qs[j][:], pq3, [pq_], [Qs[j]])
                    yield

            alive = [gen_attn(), gen_dn()]
            while alive:
                for g_ in list(alive):
                    try:
                        next(g_)
                    except StopIteration:
                        alive.remove(g_)

            pOs = [kb.pst[6], kb.pst[7]]
            pO3s = [pp[:, :].rearrange("p (n c) -> p n c", n=8) for pp in pOs]
            for n in range(8):
                for j in range(2):
                    for q in range(2):
                        kb.mm(pOs[j], pO3s[j][H[q], n, :], Sbf[j][H[q], :], RT[j][H[q], n, :], [Sbf[j], RT[j]], tp=(64 * q, 64 * q))
                    pS = kb.ps()
                    for q in range(2):
                        kb.mm(pS, pS[H[q], 0:64], UT[j][H[q], n, :], Sbf[j][H[q], :], [Sbf[j], UT[j]], tp=(64 * q, 64 * q))
                    kb.stt("dve", pre[j][:], S32[j][:], EGLc[:, j, n:n + 1], Qs[j][:, n, :], ALU.mult, ALU.add, [S32[j], EGLc, Qs[j]], [pre[j]])
                    kb.cp("act", cS[j][:], pS[:, 0:64], [pS], [cS[j]])
                    kb.tt("dve", S32[j][:], pre[j][:], cS[j][:], ALU.subtract, [pre[j], cS[j]], [S32[j]])
                    kb.cp("act", Sbf[j][:], S32[j][:], [S32[j]], [Sbf[j]])
            for j in range(2):
                pO = pOs[j]
                pO3 = pO3s[j]
                kb.cp("act", oT[:], pO3, [pO], [oT])
                kb.tt("dve", oT[:], oT[:], O0[j][:], ALU.add, [oT, O0[j]], [oT])
                o2 = oT[:].rearrange("p n c -> p (n c)")
                kb.act(sqb[:], o2, AF.Square, [oT], [sqb])
                p = kb.ps()
                kb.mm(p, p[:, :], c["blk1"][:], sqb[:], [c["blk1"], sqb])
                kb.act(rstd[:], p[:, :], AF.Sqrt, [p, c["eps"]], [rstd], scale=1.0 / 64, bias=c["eps"][:, 0:1])
                kb.op("dve", lambda e: e.reciprocal(out=rstd[:], in_=rstd[:]), reads=[rstd], writes=[rstd])
                kb.stt("dve", ysil[:], o2, dnw[:, 0:1], rstd[:], ALU.mult, ALU.mult, [oT, dnw, rstd], [ysil])
                kb.tt("dve", yo[:], ysil[:], gates[:, j, :], ALU.mult, [ysil, gates], [yo])
                kb.dma("sp", self.y0s[1][j * 128:(j + 1) * 128, tg * 512:(tg + 1) * 512], yo[:], reads=[yo], writes=[self.y0s[1]], grp="y")
                if self.debug:
                    kb.dma("sp", self.dbg_o[j * 128:(j + 1) * 128, tg * 512:(tg + 1) * 512], o2, reads=[oT], writes=[self.dbg_o], grp="y")

    def gather(self, snd, gth):
        for a, b in zip(snd, gth):
            self.kb.collective("AllGather", self.groups, a, b, a.h.ap()[:, :], b.h.ap()[:, :])

    def outproj(self, gth, nk, wout):
        kb, c = self.kb, self.c
        sb = kb.sb
        kb.push()
        wo = sb("wo_%d" % nk, [128, nk, D], BF16)
        stg = [sb("wostg%d_%d" % (i, nk), [128, D], F32) for i in range(2)]
        for k in range(nk):
            st = stg[k % 2]
            kb.dma("sp", st[:], wout[k * 128:(k + 1) * 128, :], writes=[st])
            kb.tt("dve", wo[:, k, :], st[:], self.mods[:, 2, :], ALU.mult, [st, self.mods], [wo])
        ya = [sb("ya%d_%d" % (i, nk), [128, nk, 128], BF16) for i in range(2)]
        yb_ = [sb("yb%d_%d" % (i, nk), [128, nk, 128], BF16) for i in range(2)]
        ytmp = sb("ytmp_%d" % nk, [128, nk, 128], F32)
        yown = [sb("yown%d_%d" % (i, nk), [128, nk, 128], BF16) for i in range(2)]
        nb = len(gth)
        kpb = nk // nb
        for tt in range(16):
            A, B, Y = ya[tt % 2], yb_[tt % 2], yown[tt % 2]
            for b in range(nb):
                g3 = gth[b].h.ap().rearrange("(k p) t -> p k t", p=128)
                kb.dma("sp", A[:, b * kpb:(b + 1) * kpb, :], g3[:, :, tt * 128:(tt + 1) * 128], reads=[gth[b]], writes=[A])
                kb.dma("sp", B[:, b * kpb:(b + 1) * kpb, :], g3[:, :, TH + tt * 128:TH + (tt + 1) * 128], reads=[gth[b]], writes=[B])
            kb.ts("dve", ytmp[:], A[:], c["flag"][:, 0:1], None, ALU.mult, None, [A, c["flag"]], [ytmp])
            kb.stt("dve", Y[:], B[:], c["flag"][:, 1:2], ytmp[:], ALU.mult, ALU.add, [B, c["flag"], ytmp], [Y])
            for hf in range(2):
                p = kb.ps()
                for k in range(nk):
                    kb.mm(p, p[:, :], Y[:, k, :], wo[:, k, hf * 512:(hf + 1) * 512], [Y, wo], start=(k == 0), stop=(k == nk - 1))
                xr = self.xres[tt]
                kb.tt("dve", xr[:, hf * 512:(hf + 1) * 512], p[:, :], xr[:, hf * 512:(hf + 1) * 512], ALU.add, [p, xr], [xr])
        kb.pop()

    def load_xres(self):
        kb = self.kb
        self.xres = [kb.sb("xres%d" % i, [128, D], F32) for i in range(16)]
        for tt in range(16):
            kb.dma("sp", self.xres[tt][:], self.xown[tt * 128:(tt + 1) * 128, :], writes=[self.xres[tt]])

    def norm_own(self, hnT):
        for tt in range(16):
            self.norm_tile(self.xres[tt][:], self.xres[tt], 1, 0, hnT[:, :, tt * 128:(tt + 1) * 128], hnT)

    def ffn_run(self, hnT, specs, gates=None):
        kb, c = self.kb, self.c
        B = self.fb
        groups = []
        for (wg, wu, wd, nff, e) in specs:
            nch = nff // 128
            for gi in range((nch + 1) // 2):
                groups.append((wg, wu, wd, gi * 256, min(2, nch - gi * 2), e))

        def load(it):
            wg, wu, wd, f0, fc, e = groups[it]
            wgb, wub, st, wdb = B["wg"][it % 2], B["wu"][it % 2], B["stg"][it % 2], B["wd"][it % 2]
            kb.dma("pool", wgb[:, :, 0:fc * 128], wg[:, f0:f0 + fc * 128].rearrange("(k p) n -> p k n", p=128), writes=[wgb])
            kb.dma("pool", wub[:, :, 0:fc * 128], wu[:, f0:f0 + fc * 128].rearrange("(k p) n -> p k n", p=128), writes=[wub])
            kb.dma("sp", st[:, 0:fc, :], wd[f0:f0 + fc * 128, :].rearrange("(cc p) n -> p cc n", p=128), writes=[st])
            for cc in range(fc):
                kb.tt("pool", wdb[:, cc, :], st[:, cc, :], self.mods[:, 2, :], ALU.mult, [st, self.mods], [wdb])

        def compute(it):
            wg, wu, wd, f0, fc, e = groups[it]
            wgb, wub, wdb, h1 = B["wg"][it % 2], B["wu"][it % 2], B["wd"][it % 2], B["h1"][it % 2]
            for cc in range(fc):
                for tg in range(4):
                    pgt = kb.ps()
                    put = kb.ps()
                    for k in range(8):
                        kb.mm(pgt, pgt[:, :], wgb[:, k, cc * 128:(cc + 1) * 128], hnT[:, k, tg * 512:(tg + 1) * 512], [wgb, hnT],
                              start=(k == 0), stop=(k == 7))
                    for k in range(8):
                        kb.mm(put, put[:, :], wub[:, k, cc * 128:(cc + 1) * 128], hnT[:, k, tg * 512:(tg + 1) * 512], [wub, hnT],
                              start=(k == 0), stop=(k == 7))
                    sl = B["sil"][(cc * 4 + tg) % 2]
                    kb.act(sl[:], pgt[:, :], AF.Silu, [pgt], [sl])
                    kb.tt("dve", h1[:, cc, tg * 512:(tg + 1) * 512], put[:, :], sl[:], ALU.mult, [put, sl], [h1])
            for tt in range(16):
                xr = self.xres[tt]
                for hf in range(2):
                    p = kb.ps()
                    for cc in range(fc):
                        kb.mm(p, p[:, :], h1[:, cc, tt * 128:(tt + 1) * 128], wdb[:, cc, hf * 512:(hf + 1) * 512], [h1, wdb],
                              start=(cc == 0), stop=(cc == fc - 1))
                    if gates is None:
                        kb.tt("dve", xr[:, hf * 512:(hf + 1) * 512], p[:, :], xr[:, hf * 512:(hf + 1) * 512], ALU.add, [p, xr], [xr])
                    else:
                        ev = B["ev"][(tt * 2 + hf) % 2]
                        kb.act(ev[:], p[:, :], AF.Copy, [p, gates], [ev], scale=gates[:, tt, e:e + 1])
                        kb.tt("dve", xr[:, hf * 512:(hf + 1) * 512], ev[:], xr[:, hf * 512:(hf + 1) * 512], ALU.add, [ev, xr], [xr])

        load(0)
        for it in range(len(groups)):
            if it + 1 < len(groups):
                load(it + 1)
            compute(it)

    def alloc_ffn(self, tag):
        kb = self.kb
        B = {}
        B["wg"] = [kb.sb("fwg%d_%s" % (i, tag), [128, 8, 256], BF16) for i in range(2)]
        B["wu"] = [kb.sb("fwu%d_%s" % (i, tag), [128, 8, 256], BF16) for i in range(2)]
        B["stg"] = [kb.sb("fst%d_%s" % (i, tag), [128, 2, D], F32) for i in range(2)]
        B["wd"] = [kb.sb("fwd%d_%s" % (i, tag), [128, 2, D], BF16) for i in range(2)]
        B["h1"] = [kb.sb("fh1%d_%s" % (i, tag), [128, 2, TH], BF16) for i in range(2)]
        B["sil"] = [kb.sb("fsil%d_%s" % (i, tag), [128, 512], F32) for i in range(2)]
        B["ev"] = [kb.sb("fev%d_%s" % (i, tag), [128, 512], F32) for i in range(2)]
        self.fb = B
        self.fcnt = 0

    def ffn0(self):
        kb = self.kb
        kb.push()
        hnT = kb.sb("hnTf0", [128, 8, TH], BF16)
        self.norm_own(hnT)
        self.alloc_ffn("f0")
        self.ffn_run(hnT, [(self.wg0.h.ap(), self.wu0.h.ap(), self.wd0.h.ap(), 2816, 0)])
        kb.pop()

    def moe(self):
        kb, c = self.kb, self.c
        kb.push()
        hnT = kb.sb("hnTf1", [128, 8, TH], BF16)
        self.norm_own(hnT)
        rwf = kb.sb("rwf", [128, 8, 8], F32)
        rwb = kb.sb("rwb", [128, 8, 8], BF16)
        kb.dma("sp", rwf[:], self.rw[:, :, :], writes=[rwf])
        kb.cp("dve", rwb[:], rwf[:], [rwf], [rwb])
        rbb = kb.sb("rbb", [128, 8], F32)
        kb.dma("sp", rbb[:], bc(self.rb[0:1, :], [128, 8]), writes=[rbb])
        lg = kb.sb("lg", [128, 16, 8], F32)
        p = kb.ps()
        for tt in range(16):
            for k in range(8):
                kb.mm(p, p[:, tt * 8:(tt + 1) * 8], hnT[:, k, tt * 128:(tt + 1) * 128], rwb[:, k, :], [hnT, rwb], start=(k == 0), stop=(k == 7))
        kb.cp("act", lg[:].rearrange("p a b -> p (a b)"), p[:, 0:128], [p], [lg])
        kb.tt("dve", lg[:], lg[:], bc(rbb[:].unsqueeze(1), [128, 16, 8]), ALU.add, [lg, rbb], [lg])
        m1 = kb.sb("m1", [128, 16], F32)
        m2 = kb.sb("m2", [128, 16], F32)
        eq1 = kb.sb("eq1", [128, 16, 8], F32)
        eq2 = kb.sb("eq2", [128, 16, 8], F32)
        lg2 = kb.sb("lg2", [128, 16, 8], F32)
        w1 = kb.sb("w1", [128, 16], F32)
        w2 = kb.sb("w2", [128, 16], F32)
        gates = kb.sb("gates1", [128, 16, 8], F32)
        kb.op("dve", lambda e: e.reduce_max(out=m1[:], in_=lg[:], axis=AX.X), reads=[lg], writes=[m1])
        kb.tt("dve", eq1[:], lg[:], bc(m1[:].unsqueeze(2), [128, 16, 8]), ALU.is_equal, [lg, m1], [eq1])
        kb.stt("dve", lg2[:], eq1[:], NEG, lg[:], ALU.mult, ALU.add, [eq1, lg], [lg2])
        kb.op("dve", lambda e: e.reduce_max(out=m2[:], in_=lg2[:], axis=AX.X), reads=[lg2], writes=[m2])
        kb.tt("dve", eq2[:], lg2[:], bc(m2[:].unsqueeze(2), [128, 16, 8]), ALU.is_equal, [lg2, m2], [eq2])
        kb.tt("dve", w2[:], m2[:], m1[:], ALU.subtract, [m1, m2], [w2])
        kb.act(w1[:], w2[:], AF.Sigmoid, [w2], [w1], scale=-1.0)
        kb.act(w2[:], w2[:], AF.Sigmoid, [w2], [w2])
        kb.tt("dve", eq1[:], eq1[:], bc(w1[:].unsqueeze(2), [128, 16, 8]), ALU.mult, [eq1, w1], [eq1])
        kb.tt("dve", eq2[:], eq2[:], bc(w2[:].unsqueeze(2), [128, 16, 8]), ALU.mult, [eq2, w2], [eq2])
        kb.tt("dve", gates[:], eq1[:], eq2[:], ALU.add, [eq1, eq2], [gates])
        if self.moeonly:
            kb.dma("sp", self.dbg_g[:, :], gates[:].rearrange("p a b -> p (a b)"), reads=[gates], writes=[self.dbg_g])
        self.alloc_ffn("f1")
        self.ffn_run(hnT, [(self.mwg.h.ap()[e], self.mwu.h.ap()[e], self.mwd.h.ap()[e], 3584, e) for e in range(self.nexp)], gates=gates)
        kb.pop()

    def mixer1(self):
        kb, c = self.kb, self.c
        sb = kb.sb
        kb.push()
        hnT = sb("hnTm1", [128, 8, TH], BF16)
        self.norm_own(hnT)
        for b in range(2):
            kb.dma("sp", self.h1s[b].h.ap().rearrange("(k p) t -> p k t", p=128), hnT[:, 4 * b:4 * b + 4, :], reads=[hnT], writes=[self.h1s[b]])
        self.gather(self.h1s, self.h1g)
        w1 = sb("w1m", [128, 8, NC1], BF16)
        for i in range(0, NC1, 512):
            j = min(i + 512, NC1)
            kb.dma("pool", w1[:, :, i:j], self.win1[:, i:j].rearrange("(k p) n -> p k n", p=128), writes=[w1])
        sm = sb("l1smc", [128, 4, 8], F32)
        kb.dma("sp", sm[:], self.l1sm[:, :, :], writes=[sm])
        scw = sb("scwc", [128, 2, 3], F32)
        kb.dma("sp", scw[:], self.scw[:, :, :], writes=[scw])
        gwf = sb("gwf", [128, 8, 128], F32)
        gwb = sb("gwb", [128, 8, 128], BF16)
        kb.dma("sp", gwf[:, 0:4, :], self.gaw.h.ap().rearrange("n i j -> i n j"), writes=[gwf])
        kb.dma("sp", gwf[:, 4:8, :], self.gxw.h.ap().rearrange("n i j -> i n j"), writes=[gwf])
        kb.cp("dve", gwb[:], gwf[:], [gwf], [gwb])
        cj = sb("cj", [128, 4], F32)
        kb.act(cj[:], sm[:, :, 7], AF.Exp, [sm], [cj], scale=-1.0)
        kb.act(cj[:], cj[:], AF.Ln, [cj, c["one"]], [cj], bias=c["one"][:, 0:1])
        kb.ts("dve", cj[:], cj[:], -8.0, None, ALU.mult, None, [cj], [cj])
        hin = sb("hin1", [128, 8, 512], BF16)
        xp1 = [sb("xp1_%d" % i, [128, 3 + 512], F32) for i in range(4)]
        xp2 = [sb("xp2_%d" % i, [128, 2 + 512], F32) for i in range(2)]
        for t_ in xp1 + xp2:
            kb.op("pool", lambda e, t_=t_: e.memset(t_[:, 0:3], 0.0), writes=[t_])
        hprev = sb("hprev", [128, 4], F32)
        kb.op("pool", lambda e: e.memset(hprev[:], 0.0), writes=[hprev])
        F = lambda n: sb(n, [128, 512], F32)
        xcv, rg, ig, av, bv, hs, yv, y2, gl, cdv = F("xcv"), F("rg"), F("ig"), F("av"), F("bv"), F("hs"), F("yv"), F("y2"), F("gl"), F("cdv")
        xcb = sb("xcb", [128, 512], BF16)
        yo = sb("yo1", [128, 512], BF16)
        g3 = [self.h1g[b].h.ap().rearrange("(r k p) t -> r p k t", r=2, p=128) for b in range(2)]

        def inproj(ci):
            p = kb.ps()
            for k in range(8):
                kb.mm(p, p[:, :], w1[:, k, ci * 128:(ci + 1) * 128], hin[:, k, :], [w1, hin], start=(k == 0), stop=(k == 7))
            return p

        for tg in range(self.ntg):
            for b in range(2):
                kb.dma("sp", hin[:, 4 * b:4 * b + 4, :], g3[b][tg // 4, :, :, (tg % 4) * 512:(tg % 4 + 1) * 512], reads=[self.h1g[b]], writes=[hin])
            for i in range(4):
                p = inproj(i)
                xp = xp1[i]
                if tg > 0:
                    kb.cp("pool", xp[:, 0:3], xp[:, 512:515], [xp], [xp])
                kb.cp("act", xp[:, 3:515], p[:, :], [p], [xp])
                kb.ts("dve", xcv[:], xp[:, 0:512], sm[:, i, 0:1], sm[:, i, 4:5], ALU.mult, ALU.add, [xp, sm], [xcv])
                for k in range(1, 4):
                    kb.stt("dve", xcv[:], xp[:, k:k + 512], sm[:, i, k:k + 1], xcv[:], ALU.mult, ALU.add, [xp, sm, xcv], [xcv])
                kb.cp("act", xcb[:], xcv[:], [xcv], [xcb])
                if self.m1_stop == "conv":
                    continue
                pr = kb.ps()
                pi = kb.ps()
                kb.mm(pr, pr[:, :], gwb[:, i, :], xcb[:], [gwb, xcb])
                kb.mm(pi, pi[:, :], gwb[:, 4 + i, :], xcb[:], [gwb, xcb])
                kb.act(rg[:], pr[:, :], AF.Sigmoid, [pr, sm], [rg], bias=sm[:, i, 5:6])
                kb.act(ig[:], pi[:, :], AF.Sigmoid, [pi, sm], [ig], bias=sm[:, i, 6:7])
                if self.m1_stop == "gate":
                    continue
                kb.act(av[:], rg[:], AF.Exp, [rg, cj], [av], scale=cj[:, i:i + 1])
                kb.tt("pool", bv[:], av[:], av[:], ALU.mult, [av], [bv])
                kb.ts("dve", bv[:], bv[:], -1.0, 1.0, ALU.mult, ALU.add, [bv], [bv])
                kb.act(bv[:], bv[:], AF.Sqrt, [bv], [bv])
                kb.tt("pool", bv[:], bv[:], ig[:], ALU.mult, [bv, ig], [bv])
                kb.tt("dve", bv[:], bv[:], xcv[:], ALU.mult, [bv, xcv], [bv])
                if self.m1_stop == "ab":
                    continue
                kb.op("dve", lambda e, i=i: e.tensor_tensor_scan(out=hs[:], data0=av[:], data1=bv[:], initial=hprev[:, i:i + 1],
                                                                 op0=ALU.mult, op1=ALU.add), reads=[av, bv, hprev], writes=[hs])
                kb.cp("dve", hprev[:, i:i + 1], hs[:, 511:512], [hs], [hprev])
                if self.m1_stop == "scan":
                    continue
                p = inproj(4 + i)
                kb.cp("act", yv[:], p[:, :], [p], [yv])
                kb.tt("pool", y2[:], yv[:], yv[:], ALU.mult, [yv], [y2])
                kb.ts("dve", y2[:], y2[:], 0.044715, 1.0, ALU.mult, ALU.add, [y2], [y2])
                kb.tt("pool", y2[:], y2[:], yv[:], ALU.mult, [y2, yv], [y2])
                kb.act(gl[:], y2[:], AF.Sigmoid, [y2], [gl], scale=1.5957691216057308)
                kb.tt("pool", gl[:], gl[:], yv[:], ALU.mult, [gl, yv], [gl])
                kb.tt("dve", yo[:], gl[:], hs[:], ALU.mult, [gl, hs], [yo])
                kb.dma("sp", self.y1s[i // 2][(i % 2) * 128:(i % 2 + 1) * 128, tg * 512:(tg + 1) * 512], yo[:], reads=[yo], writes=[self.y1s[i // 2]])
            if self.m1_stop in ("conv", "gate", "ab", "scan", "lru"):
                continue
            for i in range(2):
                pc = inproj(10 + i)
                ph = inproj(12 + i)
                xp = xp2[i]
                if tg > 0:
                    kb.cp("pool", xp[:, 0:2], xp[:, 512:514], [xp], [xp])
                kb.cp("act", cdv[:], pc[:, :], [pc], [cdv])
                kb.tt("dve", xp[:, 2:514], ph[:, :], cdv[:], ALU.mult, [ph, cdv], [xp])
                kb.ts("dve", cdv[:], xp[:, 0:512], scw[:, i, 0:1], None, ALU.mult, None, [xp, scw], [cdv])
                for k in range(1, 3):
                    kb.stt("dve", cdv[:], xp[:, k:k + 512], scw[:, i, k:k + 1], cdv[:], ALU.mult, ALU.add, [xp, scw, cdv], [cdv])
                pb_ = inproj(8 + i)
                kb.tt("dve", yo[:], pb_[:, :], cdv[:], ALU.mult, [pb_, cdv], [yo])
                kb.dma("sp", self.y1s[2][i * 128:(i + 1) * 128, tg * 512:(tg + 1) * 512], yo[:], reads=[yo], writes=[self.y1s[2]])
        kb.pop()

    def final_norm(self):
        kb, c = self.kb, self.c
        s = self.scr
        kb.push()
        fw = kb.sb("fwrow", [128, D], F32)
        kb.dma("sp", fw[:], bc(self.fnw[0:1, :], [128, D]), writes=[fw])
        ot = [kb.sb("ot%d" % i, [128, D], F32) for i in range(2)]
        for tt in range(16):
            xr = self.xres[tt]
            kb.act(s["junk"][:], xr[:], AF.Square, [xr], [s["junk"], s["ss"]], accum_out=s["ss"][:])
            kb.act(s["rs"][:], s["ss"][:], AF.Sqrt, [s["ss"], c["eps"]], [s["rs"]], scale=1.0 / D, bias=c["eps"][:, 0:1])
            kb.op("dve", lambda e: e.reciprocal(out=s["rs"][:], in_=s["rs"][:]), reads=[s["rs"]], writes=[s["rs"]])
            o = ot[tt % 2]
            kb.stt("dve", o[:], xr[:], s["rs"][:, 0:1], fw[:], ALU.mult, ALU.mult, [xr, s["rs"], fw], [o])
            kb.dma("sp", self.out[tt * 128:(tt + 1) * 128, :], o[:], reads=[o], writes=[self.out])
        kb.pop()

    def build(self):
        kb = self.kb
        self.declare()
        if self.debug and self.stop_after != "adaln":
            self.dbg_o = self.outp("dbg_o", [256, T])
            self.dbg_y0 = self.outp("dbg_y0", [512, T], BF16)
        self.consts()
        self.alloc_scr()
        if self.moeonly:
            kb.op("dve", lambda e: e.memset(self.mods[:], 1.0), writes=[self.mods])
            self.load_xres()
            self.moe()
            return self.dump_x()
        if self.m1only:
            kb.op("dve", lambda e: e.memset(self.mods[:], 1.0), writes=[self.mods])
            self.load_xres()
            self.mixer1()
            if self.m1_stop is None:
                self.gather(self.y1s, self.y1g)
                self.outproj(self.y1g, 12, self.wout1)
            return self.dump_x()
        if self.skip_ada:
            kb.op("dve", lambda e: e.memset(self.mods[:], 1.0), writes=[self.mods])
        else:
            self.adaln(0, 0)
        if self.stop_after == "adaln":
            self.dbg_mods = self.outp("dbg_mods", [128, 3 * D])
            kb.dma("sp", self.dbg_mods[:, :], self.mods[:].rearrange("p a b -> p (a b)"), reads=[self.mods], writes=[self.dbg_mods], grp="y")
            self.finish()
            return
        self.mixer0()
        kb.pop()
        if self.stop_after != "mixer0":
            self.gather(self.y0s, self.y0g)
            self.load_xres()
            self.outproj(self.y0g, 8, self.wout0)
            if self.stop_after == "op0":
                return self.dump_x()
            self.adaln(0, 1)
            self.ffn0()
            if self.stop_after == "ffn0":
                return self.dump_x()
            self.adaln(1, 0)
            self.mixer1()
            self.gather(self.y1s, self.y1g)
            self.outproj(self.y1g, 12, self.wout1)
            if self.stop_after == "premoe":
                return self.dump_x()
            self.adaln(1, 1)
            self.moe()
            self.final_norm()
            self.finish()
            return
        if self.stop_after == "mixer0":
            stg = kb.sb("stg", [128, 4, 512], BF16)
            for tg in range(self.ntg):
                for b in range(2):
                    kb.dma("sp", stg[:, 2 * b:2 * b + 2, :], self.y0s[b].h.ap()[:, tg * 512:(tg + 1) * 512].rearrange("(a p) t -> p a t", p=128),
                           reads=[self.y0s[b]], writes=[stg], grp="x")
                kb.dma("sp", self.dbg_y0.h.ap()[:, tg * 512:(tg + 1) * 512].rearrange("(a p) t -> p a t", p=128), stg[:],
                       reads=[stg], writes=[self.dbg_y0], grp="y")
            self.finish()
            return

    def dump_x(self):
        kb = self.kb
        for tt in range(16):
            kb.dma("sp", self.out[tt * 128:(tt + 1) * 128, :], self.xres[tt][:], reads=[self.xres[tt]], writes=[self.out])
        self.finish()

    def finish(self):
        kb = self.kb
        kb.barrier()


def bucket_tab():
    dist = np.arange(128)
    max_exact = 16
    d = np.maximum(dist, 0)
    large = max_exact + (np.log(np.maximum(d, 1) / max_exact) / np.log(128 / max_exact) * (32 - max_exact)).astype(np.int32)
    large = np.minimum(large, 31)
    return np.where(d < max_exact, d, large).astype(np.int32)


def host_inputs(inputs, c):
    b, half = c // 2, c % 2
    f32 = np.float32
    m = {}
    x = inputs["x"]
    m["xfull"] = np.ascontiguousarray(x[b])
    m["xown"] = np.ascontiguousarray(x[b, half * TH:(half + 1) * TH])
    fl = np.zeros((128, 2), f32)
    fl[:, 0] = 1.0 - half
    fl[:, 1] = half
    m["flag"] = fl
    m["cvec"] = np.ascontiguousarray(inputs["c"][b].reshape(8, 128).T)
    m["adaw"] = inputs["ada_w"]
    m["adab"] = inputs["ada_b"]
    m["nmw"] = inputs["norm_mix_w"]
    m["nfw"] = inputs["norm_ffn_w"]
    m["fnw"] = inputs["final_norm_w"].reshape(1, D)
    m["ident"] = np.eye(128, dtype=f32)
    r = np.arange(64)[:, None]
    cc = np.arange(64)[None, :]
    cm = np.zeros((128, 4, 64), f32)
    for q in range(2):
        cm[q * 64:(q + 1) * 64, 0] = np.where(cc >= r, 0.0, NEG)
        cm[q * 64:(q + 1) * 64, 1] = np.where(r > cc, 0.0, NEG)
        cm[q * 64:(q + 1) * 64, 2] = np.eye(64)
    m["cmask"] = cm
    sel = np.zeros((4, 2, 128), f32)
    for q in range(2):
        for j in range(2):
            sel[2 * q + j, j, q * 64:(q + 1) * 64] = 1.0
    m["sel"] = sel
    blk = np.zeros((128, 128), f32)
    blk[:64, :64] = 1.0
    blk[64:, 64:] = 1.0
    m["blk1"] = blk
    w = inputs["ab_w_in"][0]
    HA = lambda q, j: 4 * half + 2 * q + j
    cols = []
    for j in range(2):
        for q in range(2):
            cols += list(range(HA(q, j) * 64, HA(q, j) * 64 + 64))
    cols += list(range(512 + half * 64, 512 + half * 64 + 64)) * 2
    dncols = []
    for base in (768, 768 + 512, 768 + 1024, 2304):
        for j in range(2):
            for q in range(2):
                cols += list(range(base + HA(q, j) * 64, base + HA(q, j) * 64 + 64))
                if base < 2304:
                    dncols += list(range(base - 768 + HA(q, j) * 64, base - 768 + HA(q, j) * 64 + 64))
    cols += list(range(640 + half * 64, 640 + half * 64 + 64))
    cols += [2816 + 4 * half + hl for hl in range(4)]
    cols += [2824 + 4 * half + hl for hl in range(4)]
    assert len(cols) == NC0
    m["win0"] = np.ascontiguousarray(w[:, cols])
    cw = inputs["dn_conv_w"][0][:, dncols]
    m["cw0"] = np.ascontiguousarray(cw.reshape(4, 6, 128).transpose(2, 1, 0))
    hl = [4 * half + i for i in range(4)]
    m["dnsm"] = np.ascontiguousarray(np.stack([inputs["dn_a_log"][0][hl], inputs["dn_dt_bias"][0][hl]], axis=1))
    m["dnw"] = np.ascontiguousarray(np.tile(inputs["dn_norm_w"][0], 2).reshape(128, 1))
    sk = np.zeros((128, 2), f32)
    for q in range(2):
        for j in range(2):
            sk[q * 64:(q + 1) * 64, j] = inputs["attn_sinks"][0][HA(q, j)]
    m["sinkl"] = sk
    bt = bucket_tab()
    s_ = np.arange(128)[:, None]
    qi = np.arange(128)[None, :]
    bg = np.zeros((2, 128, 4, 128), f32)
    am = np.zeros((2, 128, 128), f32)
    for a in range(2):
        dist = qi + 128 - (s_ + 128 * a)
        valid = (dist >= 0) & (dist < 128)
        bk = bt[np.clip(dist, 0, 127)]
        am[a] = np.where(valid, 0.0, NEG)
        for q in range(2):
            for j in range(2):
                bg[a, :, q * 2 + j, :] = inputs["rel_bias"][bk, HA(q, j)]
    m["biasg"] = bg.reshape(2, 128, 512)
    m["amask"] = am
    rows = []
    for base in (0, 512):
        for r_ in range(2):
            for j in range(2):
                for q in range(2):
                    Hh = 4 * r_ + 2 * q + j
                    rows += list(range(base + Hh * 64, base + Hh * 64 + 64))
    m["wout0"] = np.ascontiguousarray(inputs["ab_w_out"][0][rows])
    m["wg0"] = inputs["ffn_w_gate"][0]
    m["wu0"] = inputs["ffn_w_up"][0]
    m["wd0"] = inputs["ffn_w_down"][0]
    w1 = inputs["cd_w_in"][0]
    cols = []
    for base in (0, 1024):
        for i in range(4):
            cols += list(range(base + (4 * half + i) * 128, base + (4 * half + i + 1) * 128))
    for base in (2048, 2560, 3072):
        for i in range(2):
            cols += list(range(base + (2 * half + i) * 128, base + (2 * half + i + 1) * 128))
    assert len(cols) == NC1
    m["win1"] = np.ascontiguousarray(w1[:, cols])
    ch = np.arange(512) + 512 * half
    sm = np.zeros((128, 4, 8), f32)
    sm[:, :, 0:4] = inputs["lru_conv_w"][0][:, ch].reshape(4, 4, 128).transpose(2, 1, 0)
    sm[:, :, 4] = inputs["lru_conv_b"][0][ch].reshape(4, 128).T
    sm[:, :, 5] = inputs["lru_gate_a_b"][0][ch].reshape(4, 128).T
    sm[:, :, 6] = inputs["lru_gate_x_b"][0][ch].reshape(4, 128).T
    sm[:, :, 7] = inputs["lru_lambda"][0][ch].reshape(4, 128).T
    m["l1sm"] = sm
    m["gaw"] = np.ascontiguousarray(inputs["lru_gate_a_w"][0][4 * half:4 * half + 4])
    m["gxw"] = np.ascontiguousarray(inputs["lru_gate_x_w"][0][4 * half:4 * half + 4])
    sc = inputs["sconv_w"][0][:, 256 * half:256 * half + 256]
    m["scw"] = np.ascontiguousarray(sc.reshape(3, 2, 128).transpose(2, 1, 0))
    rows = []
    for b_ in range(2):
        for r_ in range(2):
            for i in (2 * b_, 2 * b_ + 1):
                rows += list(range((4 * r_ + i) * 128, (4 * r_ + i + 1) * 128))
    for r_ in range(2):
        for i in range(2):
            rows += list(range(1024 + (2 * r_ + i) * 128, 1024 + (2 * r_ + i + 1) * 128))
    m["wout1"] = np.ascontiguousarray(inputs["cd_w_out"][0][rows])
    m["rw"] = np.ascontiguousarray(inputs["moe_router_w"][0].reshape(8, 128, 8).transpose(1, 0, 2))
    m["rb"] = inputs["moe_router_b"][0].reshape(1, 8)
    m["mwg"] = inputs["moe_w_gate"][0]
    m["mwu"] = inputs["moe_w_up"][0]
    m["mwd"] = inputs["moe_w_down"][0]
    return {k: np.ascontiguousarray(v, dtype=np.float32) for k, v in m.items()}


def run(inputs, stop_after=None, debug=False, **kw):
    pg = Prog(stop_after=stop_after, debug=debug, **kw)
    pg.build()
    in_maps = []
    for c in range(8):
        hm = host_inputs(inputs, c)
        in_maps.append({k: hm[k] for k in pg.inputs})
    res = run_bass_kernel_spmd(pg.kb.nc, in_maps, core_ids=list(range(8)))
    return res, pg


def kernel(**inputs):
    inputs = {k: np.asarray(v) for k, v in inputs.items()}
    res, pg = run(inputs)
    out = np.zeros((4, T, D), np.float32)
    for c in range(8):
        out[c // 2, (c % 2) * TH:(c % 2 + 1) * TH] = res.results[c]["out"]
    return out
```

```python
import numpy as np
import concourse.bass as bass
import concourse.mybir as mybir
from concourse.bass_utils import run_bass_kernel_spmd

F32 = mybir.dt.float32
BF16 = mybir.dt.bfloat16
AF = mybir.ActivationFunctionType
ALU = mybir.AluOpType
AX = mybir.AxisListType

T = 4096
TH = 2048
D = 1024
EPS = 1e-6
NEG = -30000.0
PAIRS = [[0, 1], [2, 3], [4, 5], [6, 7]]


class Tl:
    __slots__ = ("h", "name", "w", "r", "ds")

    def __init__(self, h, name):
        self.h = h
        self.name = name
        self.w = None
        self.r = {}
        self.ds = None

    def __getitem__(self, k):
        return self.h[k]


class Eng:
    def __init__(self, name, handle, sem):
        self.name = name
        self.h = handle
        self.sem = sem
        self.cnt = 0
        self.waited = {}

    def wait(self, ev):
        sem, val, _ = ev
        k = id(sem)
        if self.waited.get(k, 0) >= val:
            return
        self.waited[k] = val
        self.h.wait_ge(sem, val)


class KB:
    def __init__(self):
        self.nc = bass.Bass("TRN2", target_bir_lowering=False)
        nc = self.nc
        self.E = {}
        for n, h in (("pe", nc.tensor), ("act", nc.scalar), ("dve", nc.vector),
                     ("pool", nc.gpsimd), ("sp", nc.sync)):
            self.E[n] = Eng(n, h, nc.alloc_semaphore("sem_" + n))
        self.dall = []
        self.dfree = {}
        self.ninst = 0
        self.nps = 0
        self.pst = [Tl(nc.alloc_psum_tensor("ps%d" % i, [128, 512], F32), "ps%d" % i) for i in range(8)]
        self.nrot = 6

    def sb(self, name, shape, dt=F32):
        if not hasattr(self, "scopes"):
            self.scopes = [[]]
        cm = self.nc.sbuf_tensor(name, list(shape), dt)
        h = cm.__enter__()
        t = Tl(h, name)
        self.scopes[-1].append((cm, t))
        return t

    def push(self):
        if not hasattr(self, "scopes"):
            self.scopes = [[]]
        self.scopes.append([])

    def pop(self):
        self.barrier()
        for cm, t in reversed(self.scopes.pop()):
            if t.ds is not None:
                self.dfree.setdefault(t.ds[2], []).append(t.ds)
                t.ds = None
            cm.__exit__(None, None, None)

    def ps(self):
        t = self.pst[self.nps % self.nrot]
        self.nps += 1
        return t

    def dram(self, name, shape, dt, kind="Internal"):
        return Tl(self.nc.dram_tensor(name, list(shape), dt, kind=kind), name)

    def _deps(self, eng, reads, writes):
        E = self.E[eng]
        for t in reads:
            if t.w is not None:
                ev = t.w
                if ev[2] == eng and eng == "pe":
                    continue
                E.wait(ev)
        for t in writes:
            if t.w is not None and not (t.w[2] == eng and eng == "pe"):
                E.wait(t.w)
            for ev in t.r.values():
                if not (ev[2] == eng and eng == "pe"):
                    E.wait(ev)

    def _commit(self, ev, reads, writes):
        for t in reads:
            t.r[id(ev[0])] = ev
        for t in writes:
            t.w = ev
            t.r = {}

    def op(self, eng, fn, reads=(), writes=(), sig=True):
        E = self.E[eng]
        self._deps(eng, reads, writes)
        ins = fn(E.h)
        self.ninst += 1
        if sig:
            E.cnt += 1
            ins.then_inc(E.sem, 1)
            ev = (E.sem, E.cnt, eng)
        else:
            ev = (E.sem, E.cnt + 1, eng)
        self._commit(ev, reads, writes)
        return ins

    def _dsem(self, t, kind):
        if t.ds is None:
            fl = self.dfree.setdefault(kind, [])
            if fl:
                t.ds = fl.pop()
            else:
                t.ds = [self.nc.alloc_semaphore("dsem%d" % len(self.dall)), 0, kind]
                self.dall.append(t.ds)
        assert t.ds[2] == kind, (t.name, t.ds[2], kind)
        return t.ds

    def dma(self, q, out_ap, in_ap, reads=(), writes=(), grp="d"):
        E = self.E[q]
        self._deps(q, reads, writes)
        d = self._dsem(writes[0], "sw" if q == "pool" else "hw")
        d[1] += 16
        E.h.dma_start(out=out_ap, in_=in_ap).then_inc(d[0], 16)
        self.ninst += 1
        self._commit((d[0], d[1], "dma"), reads, writes)

    def collective(self, kind, groups, in_t, out_t, in_ap, out_ap, grp="cc"):
        E = self.E["pool"]
        self._deps("pool", [in_t], [out_t])
        d = self._dsem(out_t, "cc")
        d[1] += 1
        E.h.collective_compute(kind, ALU.bypass, replica_groups=groups,
                               ins=[in_ap], outs=[out_ap]).then_inc(d[0])
        self._commit((d[0], d[1], "dma"), [in_t], [out_t])

    def barrier(self):
        evs = []
        for n, E in self.E.items():
            if E.cnt > 0:
                evs.append((E.sem, E.cnt, n))
        for d in self.dall:
            if d[1] > 0:
                evs.append((d[0], d[1], "dma"))
        for n, E in self.E.items():
            for ev in evs:
                if ev[2] != n:
                    E.wait(ev)

    def mm(self, pst, out, lhsT, rhs, reads, start=True, stop=True, tp=None):
        kw = {}
        if tp is not None:
            kw["tile_position"] = tp
        return self.op("pe", lambda e: e.matmul(out, lhsT, rhs, start=start, stop=stop, **kw),
                       reads=reads, writes=[pst], sig=stop)

    def tr(self, pst, out, in_, ident, reads, tp=None):
        kw = {}
        if tp is not None:
            kw["tile_position"] = tp
        return self.op("pe", lambda e: e.transpose(out, in_, ident, **kw), reads=reads, writes=[pst])

    def act(self, out, in_, func, reads, writes, eng="act", **kw):
        return self.op(eng, lambda e: e.activation(out=out, in_=in_, func=func, **kw), reads=reads, writes=writes)

    def tt(self, eng, out, a, b, op, reads, writes):
        return self.op(eng, lambda e: e.tensor_tensor(out=out, in0=a, in1=b, op=op), reads=reads, writes=writes)

    def ts(self, eng, out, a, s1, s2, op0, op1, reads, writes):
        if op1 is None:
            return self.op(eng, lambda e: e.tensor_scalar(out=out, in0=a, scalar1=s1, scalar2=None, op0=op0),
                           reads=reads, writes=writes)
        return self.op(eng, lambda e: e.tensor_scalar(out=out, in0=a, scalar1=s1, scalar2=s2, op0=op0, op1=op1),
                       reads=reads, writes=writes)

    def stt(self, eng, out, a, s, b, op0, op1, reads, writes):
        return self.op(eng, lambda e: e.scalar_tensor_tensor(out=out, in0=a, scalar=s, in1=b, op0=op0, op1=op1),
                       reads=reads, writes=writes)

    def cp(self, eng, out, in_, reads, writes):
        if eng == "act":
            return self.op(eng, lambda e: e.copy(out=out, in_=in_), reads=reads, writes=writes)
        return self.op(eng, lambda e: e.tensor_copy(out=out, in_=in_), reads=reads, writes=writes)


def bc(ap, shape):
    return ap.broadcast_to(list(shape))


NC0 = 11 * 128 + 64 + 8
NC1 = 14 * 128


class Prog:
    def __init__(self, stop_after=None, debug=False, ntg=8, m0_stop=None, skip_ada=False, m1only=False, m1_stop=None):
        self.m1only = m1only
        self.m1_stop = m1_stop
        self.moeonly = False
        self.ntg = ntg
        self.nexp = 8
        self.groups = PAIRS
        self.m0_stop = m0_stop
        self.skip_ada = skip_ada
        self.kb = KB()
        self.stop_after = stop_after
        self.debug = debug
        self.inputs = {}
        self.outputs = {}

    def inp(self, name, shape, dt=F32):
        t = self.kb.dram(name, shape, dt, kind="ExternalInput")
        self.inputs[name] = t
        return t

    def outp(self, name, shape, dt=F32):
        t = self.kb.dram(name, shape, dt, kind="ExternalOutput")
        self.outputs[name] = t
        return t

    def declare(self):
        I = self.inp
        self.xfull = I("xfull", [T, D])
        self.xown = I("xown", [TH, D])
        self.flag = I("flag", [128, 2])
        self.cvec = I("cvec", [128, 8])
        self.adaw = I("adaw", [2, D, 6 * D])
        self.adab = I("adab", [2, 6 * D])
        self.nmw = I("nmw", [2, D])
        self.nfw = I("nfw", [2, D])
        self.fnw = I("fnw", [1, D])
        self.ident = I("ident", [128, 128])
        self.cmask = I("cmask", [128, 4, 64])
        self.sel = I("sel", [4, 2, 128])
        self.blk1 = I("blk1", [128, 128])
        self.win0 = I("win0", [D, NC0])
        self.cw0 = I("cw0", [128, 6, 4])
        self.dnsm = I("dnsm", [4, 2])
        self.dnw = I("dnw", [128, 1])
        self.sinkl = I("sinkl", [128, 2])
        self.biasg = I("biasg", [2, 128, 512])
        self.amask = I("amask", [2, 128, 128])
        if self.stop_after in ("adaln", "mixer0") and not self.m1only:
            self._internal()
            return
        if self.moeonly:
            self.rw = I("rw", [128, 8, 8])
            self.rb = I("rb", [1, 8])
            self.mwg = I("mwg", [self.nexp, D, 3584])
            self.mwu = I("mwu", [self.nexp, D, 3584])
            self.mwd = I("mwd", [self.nexp, 3584, D])
            self.out = self.outp("out", [TH, D])
            self.dbg_g = self.outp("dbg_g", [128, 128])
            self._internal()
            return
        if self.m1only:
            self.win1 = I("win1", [D, NC1])
            self.l1sm = I("l1sm", [128, 4, 8])
            self.gaw = I("gaw", [4, 128, 128])
            self.gxw = I("gxw", [4, 128, 128])
            self.scw = I("scw", [128, 2, 3])
            self.wout1 = I("wout1", [1536, D])
            self.out = self.outp("out", [TH, D])
            self._internal()
            return
        self.wout0 = I("wout0", [D, D])
        if self.stop_after == "op0":
            self.out = self.outp("out", [TH, D])
            self._internal()
            return
        self.wg0 = I("wg0", [D, 2816])
        self.wu0 = I("wu0", [D, 2816])
        self.wd0 = I("wd0", [2816, D])
        if self.stop_after == "ffn0":
            self.out = self.outp("out", [TH, D])
            self._internal()
            return
        self.win1 = I("win1", [D, NC1])
        self.l1sm = I("l1sm", [128, 4, 8])
        self.gaw = I("gaw", [4, 128, 128])
        self.gxw = I("gxw", [4, 128, 128])
        self.scw = I("scw", [128, 2, 3])
        self.wout1 = I("wout1", [1536, D])
        if self.stop_after != "premoe":
            self.rw = I("rw", [128, 8, 8])
            self.rb = I("rb", [1, 8])
            self.mwg = I("mwg", [8, D, 3584])
            self.mwu = I("mwu", [8, D, 3584])
            self.mwd = I("mwd", [8, 3584, D])
        self.out = self.outp("out", [TH, D])
        self._internal()

    def _internal(self):
        kb = self.kb
        self.y0s = [kb.dram("y0s%d" % i, [256, T], BF16) for i in range(2)]
        self.y0g = [kb.dram("y0g%d" % i, [512, T], BF16) for i in range(2)]
        self.h1s = [kb.dram("h1s%d" % i, [512, TH], BF16) for i in range(2)]
        self.h1g = [kb.dram("h1g%d" % i, [1024, TH], BF16) for i in range(2)]
        self.y1s = [kb.dram("y1s%d" % i, [256, T], BF16) for i in range(3)]
        self.y1g = [kb.dram("y1g%d" % i, [512, T], BF16) for i in range(3)]

    def consts(self):
        kb = self.kb
        c = {}
        self.c = c
        c["idf"] = kb.sb("idf", [128, 128], F32)
        c["idb"] = kb.sb("idb", [128, 128], BF16)
        c["cm"] = kb.sb("cm", [128, 4, 64], F32)
        c["i64b"] = kb.sb("i64b", [128, 64], BF16)
        c["sel"] = kb.sb("selc", [4, 2, 128], F32)
        c["blk1f"] = kb.sb("blk1f", [128, 128], F32)
        c["blk1"] = kb.sb("blk1b", [128, 128], BF16)
        c["ones"] = kb.sb("onesb", [128, 128], BF16)
        c["flag"] = kb.sb("flagc", [128, 2], F32)
        c["cv"] = kb.sb("cv", [128, 8], F32)
        c["cond"] = kb.sb("cond", [128, 8], F32)
        c["condB"] = kb.sb("condB", [128, 8, 128], BF16)
        c["eps"] = kb.sb("epsc", [128, 1], F32)
        c["one"] = kb.sb("onec", [128, 1], F32)
        q = "sp"
        kb.dma(q, c["idf"][:], self.ident[:, :], writes=[c["idf"]], grp="c")
        kb.dma(q, c["cm"][:], self.cmask[:, :, :], writes=[c["cm"]], grp="c")
        kb.dma(q, c["sel"][:], self.sel[:, :, :], writes=[c["sel"]], grp="c")
        kb.dma(q, c["blk1f"][:], self.blk1[:, :], writes=[c["blk1f"]], grp="c")
        kb.dma(q, c["flag"][:], self.flag[:, :], writes=[c["flag"]], grp="c")
        kb.dma(q, c["cv"][:], self.cvec[:, :], writes=[c["cv"]], grp="c")
        kb.cp("dve", c["idb"][:], c["idf"][:], [c["idf"]], [c["idb"]])
        kb.cp("dve", c["blk1"][:], c["blk1f"][:], [c["blk1f"]], [c["blk1"]])
        kb.cp("dve", c["i64b"][:], c["cm"][:, 2, :], [c["cm"]], [c["i64b"]])
        kb.op("dve", lambda e: e.memset(c["ones"][:], 1.0), writes=[c["ones"]])
        kb.op("dve", lambda e: e.memset(c["eps"][:], EPS), writes=[c["eps"]])
        kb.op("dve", lambda e: e.memset(c["one"][:], 1.0), writes=[c["one"]])
        kb.act(c["cond"][:], c["cv"][:], AF.Silu, [c["cv"]], [c["cond"]])
        kb.cp("dve", c["condB"][:], bc(c["cond"][:].unsqueeze(2), [128, 8, 128]), [c["cond"]], [c["condB"]])
        self.mods = kb.sb("mods", [128, 3, D], F32)

    def adaln(self, l, part):
        kb, c = self.kb, self.c
        kb.push()
        self.wbuf = [kb.sb("wbuf%d_%d_%d" % (i, l, part), [128, 8, 512], BF16) for i in range(2)]
        self.rowt = kb.sb("rowt_%d_%d" % (l, part), [128, 512], F32)
        self.rowt2 = kb.sb("rowt2_%d_%d" % (l, part), [128, D], F32)
        for n in range(part * 6, part * 6 + 6):
            wb = self.wbuf[n % 2]
            kb.dma("pool", wb[:], self.adaw[l, :, n * 512:(n + 1) * 512].rearrange("(k p) n -> p k n", p=128),
                   writes=[wb], grp="w")
            kb.dma("sp", self.rowt[:], bc(self.adab[l:l + 1, n * 512:(n + 1) * 512], [128, 512]),
                   writes=[self.rowt], grp="c")
            p = kb.ps()
            for k in range(8):
                kb.mm(p, p[:, :], c["condB"][:, k, :], wb[:, k, :], [c["condB"], wb], start=(k == 0), stop=(k == 7))
            kb.tt("dve", self.mods[:, (n // 2) % 3, (n % 2) * 512:(n % 2) * 512 + 512], p[:, :], self.rowt[:], ALU.add,
                  [p, self.rowt], [self.mods])
        w = self.nmw if part == 0 else self.nfw
        kb.dma("sp", self.rowt2[:], bc(w[l:l + 1, :], [128, D]), writes=[self.rowt2], grp="c")
        kb.stt("dve", self.mods[:, 1, :], self.mods[:, 1, :], 1.0, self.rowt2[:], ALU.add, ALU.mult,
               [self.mods, self.rowt2], [self.mods])
        kb.pop()

    def norm_tile(self, xt_ap, xt_tl, ia, ib, hnT_ap, hnT_tl, eng2="dve"):
        kb, c = self.kb, self.c
        s = self.scr
        kb.act(s["junk"][:], xt_ap, AF.Square, [xt_tl], [s["junk"], s["ss"]], accum_out=s["ss"][:])
        kb.act(s["rs"][:], s["ss"][:], AF.Sqrt, [s["ss"], c["eps"]], [s["rs"]], scale=1.0 / D, bias=c["eps"][:, 0:1])
        kb.op("dve", lambda e: e.reciprocal(out=s["rs"][:], in_=s["rs"][:]), reads=[s["rs"]], writes=[s["rs"]])
        kb.stt("dve", s["t1"][:], xt_ap, s["rs"][:, 0:1], self.mods[:, ia, :], ALU.mult, ALU.mult,
               [xt_tl, s["rs"], self.mods], [s["t1"]])
        kb.tt(eng2, s["hn"][:], s["t1"][:], self.mods[:, ib, :], ALU.add, [s["t1"], self.mods], [s["hn"]])
        p = kb.ps()
        pb = p[:, :].bitcast(BF16)
        for k in range(8):
            kb.tr(p, pb[:, k * 128:(k + 1) * 128], s["hn"][:, k * 128:(k + 1) * 128], c["idb"][:], [s["hn"], c["idb"]])
        kb.cp("act", hnT_ap, pb[:, 0:1024].rearrange("p (k t) -> p k t", k=8), [p], [hnT_tl])

    def alloc_scr(self):
        kb = self.kb
        s = {}
        self.scr = s
        s["junk"] = kb.sb("junk", [128, D], BF16)
        s["ss"] = kb.sb("ss", [128, 1], F32)
        s["rs"] = kb.sb("rs", [128, 1], F32)
        s["t1"] = kb.sb("t1", [128, D], F32)
        s["hn"] = kb.sb("hn", [128, D], BF16)
        self.xin = [kb.sb("xin%d" % i, [128, D], F32) for i in range(2)]

    def mixer0(self):
        kb, c = self.kb, self.c
        sb = kb.sb
        kb.push()
        w0 = sb("w0", [128, 8, NC0], BF16)
        for i in range(0, NC0, 512):
            j = min(i + 512, NC0)
            kb.dma("pool", w0[:, :, i:j], self.win0[:, i:j].rearrange("(k p) n -> p k n", p=128), writes=[w0], grp="w")
        cw = sb("cw", [128, 6, 4], F32)
        kb.dma("sp", cw[:], self.cw0[:, :, :], writes=[cw], grp="c")
        dnsm = sb("dnsmc", [4, 2], F32)
        kb.dma("sp", dnsm[:], self.dnsm[:, :], writes=[dnsm], grp="c")
        nega = sb("nega", [4, 1], F32)
        kb.act(nega[:], dnsm[:, 0:1], AF.Exp, [dnsm], [nega])
        kb.ts("dve", nega[:], nega[:], -1.0, None, ALU.mult, None, [nega], [nega])
        dnw = sb("dnwc", [128, 1], F32)
        kb.dma("sp", dnw[:], self.dnw[:, :], writes=[dnw], grp="c")
        sinkE = sb("sinkE", [128, 2], F32)
        kb.dma("sp", sinkE[:], self.sinkl[:, :], writes=[sinkE], grp="c")
        kb.act(sinkE[:], sinkE[:], AF.Exp, [sinkE], [sinkE])
        biasm = sb("biasm", [128, 2, 512], F32)
        am = sb("am", [128, 2, 128], F32)
        kb.dma("sp", biasm[:], self.biasg.h.ap().rearrange("a p n -> p a n"), writes=[biasm], grp="c")
        kb.dma("sp", am[:], self.amask.h.ap().rearrange("a p n -> p a n"), writes=[am], grp="c")
        for a in range(2):
            kb.tt("dve", biasm[:, a, :].rearrange("p (s q) -> p s q", s=4),
                  biasm[:, a, :].rearrange("p (s q) -> p s q", s=4),
                  bc(am[:, a, :].unsqueeze(1), [128, 4, 128]), ALU.add, [biasm, am], [biasm])
        rmask = sb("rmask", [4, 8, 64], F32)
        kb.op("dve", lambda e: e.memset(rmask[:], 1.0), writes=[rmask])
        kb.op("dve", lambda e: e.memset(rmask[:, :, 0:1], 0.0), writes=[rmask])

        mU8 = sb("mU8", [128, 8, 64], F32)
        mL8 = sb("mL8", [128, 8, 64], F32)
        kb.cp("dve", mU8[:], bc(c["cm"][:, 0, :].unsqueeze(1), [128, 8, 64]), [c["cm"]], [mU8])
        kb.cp("dve", mL8[:], bc(c["cm"][:, 1, :].unsqueeze(1), [128, 8, 64]), [c["cm"]], [mL8])
        hnT = sb("hnT0", [128, 8, 512], BF16)
        qaT = sb("qaT", [128, 2, 512], BF16)
        kaT = sb("kaT", [128, 2, 128 + T], BF16)
        vat = sb("vat", [128, 33, 64], BF16)
        kb.op("pool", lambda e: e.memset(kaT[:], 0.0), writes=[kaT])
        kb.op("dve", lambda e: e.memset(vat[:, 0, :], 0.0), writes=[vat])
        xpre = [sb("xpre%d" % i, [128, 3 + 512], F32) for i in range(6)]
        for i in range(6):
            kb.op("pool", lambda e, i=i: e.memset(xpre[i][:, 0:3], 0.0), writes=[xpre[i]])
        gates = sb("gates", [128, 2, 512], F32)
        tl = sb("tl", [128, 512], F32)
        cacc = sb("cacc", [128, 512], F32)
        ysil = sb("ysil", [128, 512], F32)
        sqb = sb("sqb", [128, 512], BF16)
        rstd = sb("rstd", [128, 512], F32)
        qn = [sb("qn%d" % j, [128, 512], BF16) for j in range(2)]
        qnf = [sb("qnf%d" % j, [128, 512], F32) for j in range(2)]
        i8f = sb("i8f", [128, 8, 64], F32)
        kb.cp("dve", i8f[:], bc(c["cm"][:, 2, :].unsqueeze(1), [128, 8, 64]), [c["cm"]], [i8f])
        A32 = sb("A32", [128, 8, 64], F32)
        Xs = sb("Xs", [128, 4, 64], F32)
        kn = [sb("kn%d" % j, [128, 512], BF16) for j in range(2)]
        vT = [sb("vT%d" % j, [128, 512], BF16) for j in range(2)]
        bt = sb("bt", [4, 512], F32)
        gt = sb("gt", [4, 512], F32)
        Gs = sb("Gs", [4, 512], F32)
        Es = sb("Es", [4, 512], F32)
        BEs = sb("BEs", [4, 512], F32)
        DKs = sb("DKs", [4, 512], F32)
        nbt = gt
        tk4 = sb("tk4", [128, 5, 8, 4], F32)
        TK = sb("TK", [128, 5, 8, 2], F32)
        EGLc = sb("EGLc", [128, 2, 8], F32)
        t0 = sb("t0", [128, 8, 64], F32)
        tU = sb("tU", [128, 8, 64], F32)
        tL = sb("tL", [128, 8, 64], F32)
        Du = tU
        Dl = tL
        tmpf = t0
        NTp = [sb("NTp%d" % i, [128, 8, 64], BF16) for i in range(2)]
        Np = [sb("Np%d" % i, [128, 8, 64], BF16) for i in range(2)]
        Am = sb("Am", [128, 8, 64], BF16)
        KV = sb("KV", [128, 8, 128], BF16)
        KQ = sb("KQ", [128, 8, 128], BF16)
        Wu = sb("Wu", [128, 8, 128], BF16)
        qdec = sb("qdec", [128, 8, 64], F32)
        UT = [sb("UT%d" % j, [128, 8, 64], BF16) for j in range(2)]
        RT = [sb("RT%d" % j, [128, 8, 64], BF16) for j in range(2)]
        O0 = [sb("O0%d" % j, [128, 8, 64], F32) for j in range(2)]
        Qs = [sb("Qs%d" % j, [128, 8, 64], F32) for j in range(2)]
        S32 = [sb("S32_%d" % j, [128, 64], F32) for j in range(2)]
        Sbf = [sb("Sbf_%d" % j, [128, 64], BF16) for j in range(2)]
        pre = [sb("pre%d" % j, [128, 64], F32) for j in range(2)]
        cS = [sb("cS%d" % j, [128, 64], F32) for j in range(2)]
        oT = sb("oT", [128, 8, 64], F32)
        yo = sb("yo", [128, 512], BF16)
        for j in range(2):
            kb.op("dve", lambda e, j=j: e.memset(S32[j][:], 0.0), writes=[S32[j]])
            kb.op("dve", lambda e, j=j: e.memset(Sbf[j][:], 0.0), writes=[Sbf[j]])
        PT = [sb("PT%d" % i, [128, 4, 128], BF16) for i in range(2)]
        den = sb("den", [128, 2, 128], F32)
        ao = sb("ao", [128, 2, 128], BF16)
        H = (slice(0, 64), slice(64, 128))

        for tg in range(self.ntg):
            for tt in range(4):
                xt = self.xin[tt % 2]
                r0 = tg * 512 + tt * 128
                kb.dma("sp", xt[:], self.xfull[r0:r0 + 128, :], writes=[xt], grp="x")
                self.norm_tile(xt[:], xt, 1, 0, hnT[:, :, tt * 128:(tt + 1) * 128], hnT)
            for ci in range(11):
                p = kb.ps()
                for k in range(8):
                    kb.mm(p, p[:, :], w0[:, k, ci * 128:(ci + 1) * 128], hnT[:, k, :], [w0, hnT], start=(k == 0), stop=(k == 7))
                if ci < 2:
                    kb.act(qaT[:, ci, :], p[:, :], AF.Copy, [p], [qaT], scale=0.125)
                elif ci == 2:
                    for q in range(2):
                        kb.cp("act", kaT[H[q], q, 128 + tg * 512:128 + (tg + 1) * 512], p[H[q], :], [p], [kaT])
                elif ci < 9:
                    xp = xpre[ci - 3]
                    if tg > 0:
                        kb.cp("pool", xp[:, 0:3], xp[:, 512:515], [xp], [xp])
                    kb.cp("act", xp[:, 3:515], p[:, :], [p], [xp])
                else:
                    kb.act(gates[:, ci - 9, :], p[:, :], AF.Silu, [p], [gates])
            for tt in range(4):
                p = kb.ps()
                for k in range(8):
                    kb.mm(p, p[:, 0:64], hnT[:, k, tt * 128:(tt + 1) * 128], w0[:, k, 1408:1472], [w0, hnT], start=(k == 0), stop=(k == 7))
                kb.cp("act", vat[:, 1 + tg * 4 + tt, :], p[:, 0:64], [p], [vat])
            pb_ = kb.ps()
            pd_ = kb.ps()
            for k in range(8):
                kb.mm(pb_, pb_[0:4, :], w0[:, k, 1472:1476], hnT[:, k, :], [w0, hnT], start=(k == 0), stop=(k == 7))
            for k in range(8):
                kb.mm(pd_, pd_[0:4, :], w0[:, k, 1476:1480], hnT[:, k, :], [w0, hnT], start=(k == 0), stop=(k == 7))
            kb.act(bt[:], pb_[0:4, :], AF.Sigmoid, [pb_], [bt])
            kb.act(gt[:], pd_[0:4, :], AF.Exp, [pd_, dnsm], [gt], bias=dnsm[:, 1:2])
            kb.act(gt[:], gt[:], AF.Ln, [gt, c["one"]], [gt], bias=c["one"][0:4, 0:1])
            kb.ts("dve", gt[:], gt[:], nega[:, 0:1], None, ALU.mult, None, [gt, nega], [gt])
            kb.op("dve", lambda e: e.tensor_tensor_scan(out=Gs[:], data0=rmask[:].rearrange("p a b -> p (a b)"), data1=gt[:],
                                                        initial=0.0, op0=ALU.mult, op1=ALU.add), reads=[rmask, gt], writes=[Gs])
            kb.act(Es[:], Gs[:], AF.Exp, [Gs], [Es])
            kb.tt("dve", BEs[:], bt[:], Es[:], ALU.mult, [bt, Es], [BEs])
            G3 = Gs[:].rearrange("p (a b) -> p a b", a=8)
            kb.tt("dve", DKs[:].rearrange("p (a b) -> p a b", a=8), G3, bc(G3[:, :, 63:64], [4, 8, 64]), ALU.subtract, [Gs], [DKs])
            kb.act(DKs[:], DKs[:], AF.Exp, [DKs], [DKs], scale=-1.0)
            kb.ts("dve", nbt[:], bt[:], -1.0, None, ALU.mult, None, [bt], [nbt])
            p = kb.ps()
            for qi, X in enumerate((Gs, nbt, BEs, DKs, bt)):
                for n in range(8):
                    for q in range(2):
                        kb.mm(p, p[H[q], (qi * 8 + n) * 4:(qi * 8 + n) * 4 + 4], X[:, n * 64:(n + 1) * 64], c["idf"][0:4, 0:4],
                              [X, c["idf"]], tp=(0, 64 * q))
            kb.cp("dve", tk4[:].rearrange("p a n h -> p (a n h)"), p[:, 0:160], [p], [tk4])
            for q in range(2):
                kb.cp("dve", TK[H[q], :, :, :], tk4[H[q], :, :, 2 * q:2 * q + 2], [tk4], [TK])
            p = kb.ps()
            for j in range(2):
                kb.mm(p, p[:, j * 8:j * 8 + 8], c["sel"][:, j, :], Es[:].rearrange("p (a b) -> p a b", a=8)[:, :, 63], [c["sel"], Es])
            kb.cp("dve", EGLc[:].rearrange("p j n -> p (j n)"), p[:, 0:16], [p], [EGLc])

            def gen_attn():
                for qb in range(4):
                    n = tg * 4 + qb
                    kbs = [n - 1, n] if n > 0 else [n]
                    for ki, kbk in enumerate(kbs):
                        sel_ = 0 if kbk == n - 1 else 1
                        pl = kb.ps()
                        for q in range(2):
                            for j in range(2):
                                sl = q * 2 + j
                                kb.mm(pl, pl[:, sl * 128:(sl + 1) * 128], kaT[:, q, 128 + kbk * 128:128 + (kbk + 1) * 128],
                                      qaT[:, j, qb * 128:(qb + 1) * 128], [kaT, qaT])
                        kb.tt("dve", tl[:], pl[:, :], biasm[:, sel_, :], ALU.add, [pl, biasm], [tl])
                        kb.act(PT[ki][:].rearrange("p s q -> p (s q)"), tl[:], AF.Exp, [tl], [PT[ki]])
                        yield
                    po = kb.ps()
                    pdn = kb.ps()
                    for q in range(2):
                        for j in range(2):
                            sl = q * 2 + j
                            for ki, kbk in enumerate(kbs):
                                kb.mm(po, po[H[q], j * 128:(j + 1) * 128], vat[:, 1 + kbk, :], PT[ki][:, sl, :], [vat, PT[ki]],
                                      start=(ki == 0), stop=(ki == len(kbs) - 1), tp=(0, 64 * q))
                            for ki, kbk in enumerate(kbs):
                                kb.mm(pdn, pdn[H[q], j * 128:(j + 1) * 128], c["ones"][:, 0:64], PT[ki][:, sl, :], [c["ones"], PT[ki]],
                                      start=(ki == 0), stop=(ki == len(kbs) - 1), tp=(0, 64 * q))
                    kb.tt("dve", den[:], pdn[:, 0:256].rearrange("p (j t) -> p j t", j=2), bc(sinkE[:].unsqueeze(2), [128, 2, 128]),
                          ALU.add, [pdn, sinkE], [den])
                    kb.op("dve", lambda e: e.reciprocal(out=den[:], in_=den[:]), reads=[den], writes=[den])
                    kb.tt("dve", ao[:], po[:, 0:256].rearrange("p (j t) -> p j t", j=2), den[:], ALU.mult, [po, den], [ao])
                    for j in range(2):
                        kb.dma("sp", self.y0s[0][j * 128:(j + 1) * 128, n * 128:(n + 1) * 128], ao[:, j, :], reads=[ao], writes=[self.y0s[0]], grp="y")
                    yield

            def gen_dn():
                for ci in range(6):
                    xp = xpre[ci]
                    j = ci % 2
                    kind = ci // 2
                    kb.ts("dve", cacc[:], xp[:, 0:512], cw[:, ci, 0:1], None, ALU.mult, None, [xp, cw], [cacc])
                    for k in range(1, 4):
                        kb.stt("dve", cacc[:], xp[:, k:k + 512], cw[:, ci, k:k + 1], cacc[:], ALU.mult, ALU.add, [xp, cw, cacc], [cacc])
                    if kind == 2:
                        kb.act(vT[j][:], cacc[:], AF.Silu, [cacc], [vT[j]])
                        yield
                        continue
                    kb.act(ysil[:], cacc[:], AF.Silu, [cacc], [ysil])
                    kb.act(sqb[:], ysil[:], AF.Square, [ysil], [sqb])
                    p = kb.ps()
                    kb.mm(p, p[:, :], c["blk1"][:], sqb[:], [c["blk1"], sqb])
                    kb.act(rstd[:], p[:, :], AF.Sqrt, [p, c["eps"]], [rstd], bias=c["eps"][:, 0:1])
                    kb.op("dve", lambda e: e.reciprocal(out=rstd[:], in_=rstd[:]), reads=[rstd], writes=[rstd])
                    if kind == 0:
                        kb.stt("dve", qnf[j][:], ysil[:], 0.125, rstd[:], ALU.mult, ALU.mult, [ysil, rstd], [qnf[j]])
                        kb.cp("act", qn[j][:], qnf[j][:], [qnf[j]], [qn[j]])
                    else:
                        kb.tt("dve", kn[j][:], ysil[:], rstd[:], ALU.mult, [ysil, rstd], [kn[j]])
                    yield

                for j in range(2):
                    k3 = kn[j][:].rearrange("p (n c) -> p n c", n=8)
                    q3 = qn[j][:].rearrange("p (n c) -> p n c", n=8)
                    v3 = vT[j][:].rearrange("p (n c) -> p n c", n=8)
                    pk = kb.ps()
                    pv = kb.ps()
                    pkb = pk[:, :].rearrange("p (n x) -> p n x", n=8)
                    pvb = pv[:, :].rearrange("p (n x) -> p n x", n=8)
                    for n in range(8):
                        for q in range(2):
                            kb.mm(pk, pkb[H[q], n, :], k3[H[q], n, :], c["idb"][H[q], 64 * q:64 * q + 64], [kn[j], c["idb"]], tp=(64 * q, 64 * q))
                    for n in range(8):
                        for q in range(2):
                            kb.mm(pv, pvb[H[q], n, :], v3[H[q], n, :], c["idb"][H[q], 64 * q:64 * q + 64], [vT[j], c["idb"]], tp=(64 * q, 64 * q))
                    kb.tt("dve", KQ[:, :, 0:64], bc(TK[:, 3, :, j:j + 1], [128, 8, 64]), pkb, ALU.mult, [pk, TK], [KQ])
                    kb.tt("dve", KV[:, :, 0:64], bc(TK[:, 2, :, j:j + 1], [128, 8, 64]), pkb, ALU.mult, [pk, TK], [KV])
                    kb.tt("dve", KV[:, :, 64:128], bc(TK[:, 4, :, j:j + 1], [128, 8, 64]), pvb, ALU.mult, [pv, TK], [KV])
                    yield
                    pg = kb.ps()
                    pe_ = kb.ps()
                    kb.mm(pg, pg[:, :], c["sel"][:, j, :], Gs[:], [c["sel"], Gs])
                    kb.mm(pe_, pe_[:, :], c["sel"][:, j, :], Es[:], [c["sel"], Es])
                    pg3 = pg[:, :].rearrange("p (n c) -> p n c", n=8)
                    kb.tt("dve", t0[:], pg3, bc(TK[:, 0, :, j:j + 1], [128, 8, 64]), ALU.subtract, [pg, TK], [t0])
                    kb.tt("pool", tU[:], t0[:], mU8[:], ALU.add, [t0, mU8], [tU])
                    kb.stt("dve", tL[:], t0[:], -1.0, mL8[:], ALU.mult, ALU.add, [t0, mL8], [tL])
                    kb.act(Du[:], tU[:], AF.Exp, [tU], [Du])
                    kb.act(Dl[:], tL[:], AF.Exp, [tL], [Dl])
                    kb.cp("act", cacc[:], pe_[:, :], [pe_], [cacc])
                    kb.tt("dve", qdec[:].rearrange("p n c -> p (n c)"), cacc[:], qnf[j][:], ALU.mult, [qnf[j], cacc], [qdec])
                    yield
                    pkk = kb.ps()
                    pqk = kb.ps()
                    kk3 = pkk[:, :].rearrange("p (n c) -> p n c", n=8)
                    qk3 = pqk[:, :].rearrange("p (n c) -> p n c", n=8)
                    for n in range(8):
                        for q in range(2):
                            kb.mm(pkk, kk3[H[q], n, :], k3[H[q], n, :], k3[H[q], n, :], [kn[j]], tp=(64 * q, 64 * q))
                    for n in range(8):
                        for q in range(2):
                            kb.mm(pqk, qk3[H[q], n, :], k3[H[q], n, :], q3[H[q], n, :], [kn[j], qn[j]], tp=(64 * q, 64 * q))
                    kb.tt("dve", tmpf[:], bc(TK[:, 1, :, j:j + 1], [128, 8, 64]), Dl[:], ALU.mult, [TK, Dl], [tmpf])
                    kb.cp("act", tl[:], pkk[:, :], [pkk], [tl])
                    kb.tt("dve", NTp[0][:], tl[:].rearrange("p (n c) -> p n c", n=8), tmpf[:], ALU.mult, [tl, tmpf], [NTp[0]])
                    kb.cp("act", cacc[:], pqk[:, :], [pqk], [cacc])
                    kb.tt("dve", KQ[:, :, 64:128], cacc[:].rearrange("p (n c) -> p n c", n=8), Du[:], ALU.mult, [cacc, Du], [KQ])
                    yield
                    pn = kb.ps()
                    pnb = pn[:, :].rearrange("p (n c) -> p n c", n=8)
                    for n in range(8):
                        for q in range(2):
                            kb.mm(pn, pnb[H[q], n, :], NTp[0][H[q], n, :], c["idb"][H[q], 64 * q:64 * q + 64], [NTp[0], c["idb"]], tp=(64 * q, 64 * q))
                    kb.cp("act", Np[0][:], pnb, [pn], [Np[0]])
                    kb.cp("act", A32[:], pnb, [pn], [A32])
                    kb.tt("pool", A32[:], A32[:], i8f[:], ALU.add, [A32, i8f], [A32])
                    kb.cp("act", Am[:], A32[:], [A32], [Am])
                    cur = 0
                    for lev in range(5):
                        nxt = 1 - cur
                        if lev < 4:
                            p1 = kb.ps()
                            p13 = p1[:, :].rearrange("p (n c) -> p n c", n=8)
                            for n in range(8):
                                for q in range(2):
                                    kb.mm(p1, p13[H[q], n, :], NTp[cur][H[q], n, :], Np[cur][H[q], n, :], [NTp[cur], Np[cur]], tp=(64 * q, 64 * q))
                        p2 = kb.ps()
                        p23 = p2[:, :].rearrange("p (n c) -> p n c", n=8)
                        for n in range(8):
                            for q in range(2):
                                kb.mm(p2, p23[H[q], n, :], Np[cur][H[q], n, :], NTp[cur][H[q], n, :], [NTp[cur], Np[cur]], tp=(64 * q, 64 * q))
                        if lev < 4:
                            kb.cp("act", Np[nxt][:], p13, [p1], [Np[nxt]])
                        kb.cp("act", NTp[nxt][:], p23, [p2], [NTp[nxt]])
                        p3 = kb.ps()
                        p33 = p3[:, :].rearrange("p (n c) -> p n c", n=8)
                        for n in range(8):
                            for q in range(2):
                                kb.mm(p3, p33[H[q], n, :], NTp[nxt][H[q], n, :], Am[H[q], n, :], [NTp[nxt], Am], tp=(64 * q, 64 * q))
                        kb.cp("act", tl[:], p3[:, :], [p3], [tl])
                        kb.tt("dve", A32[:], tl[:].rearrange("p (n c) -> p n c", n=8), A32[:], ALU.add, [A32, tl], [A32])
                        kb.cp("act", Am[:], A32[:], [A32], [Am])
                        cur = nxt
                        yield
                    yield
                    for hf in range(2):
                        pw = kb.ps()
                        pw3 = pw[:, :].rearrange("p (n c) -> p n c", n=4)
                        for n in range(4):
                            for q in range(2):
                                kb.mm(pw, pw3[H[q], n, :], Am[H[q], hf * 4 + n, :], KV[H[q], hf * 4 + n, :], [Am, KV], tp=(64 * q, 64 * q))
                        kb.cp("act", Wu[:, hf * 4:hf * 4 + 4, :], pw3, [pw], [Wu])
                    yield
                    for hf in range(2):
                        pu = kb.ps()
                        pu3 = pu[:, :].rearrange("p (n c) -> p n c", n=4)
                        for n in range(4):
                            for q in range(2):
                                kb.mm(pu, pu3[H[q], n, :], Wu[H[q], hf * 4 + n, 0:64], KQ[H[q], hf * 4 + n, :], [Wu, KQ], tp=(64 * q, 64 * q))
                        kb.cp("act", UT[j][:, hf * 4:hf * 4 + 4, :], pu3[:, :, 0:64], [pu], [UT[j]])
                        kb.cp("act", Xs[:], pu3[:, :, 64:128], [pu], [Xs])
                        kb.tt("pool", RT[j][:, hf * 4:hf * 4 + 4, :], qdec[:, hf * 4:hf * 4 + 4, :], Xs[:], ALU.subtract, [qdec, Xs], [RT[j]])
                    po0 = kb.ps()
                    pq_ = kb.ps()
                    po03 = po0[:, :].rearrange("p (n c) -> p n c", n=8)
                    pq3 = pq_[:, :].rearrange("p (n c) -> p n c", n=8)
                    for n in range(8):
                        for q in range(2):
                            kb.mm(po0, po03[H[q], n, :], Wu[H[q], n, 64:128], KQ[H[q], n, 64:128], [Wu, KQ], tp=(64 * q, 64 * q))
                    for n in range(8):
                        for q in range(2):
                            kb.mm(pq_, pq3[H[q], n, :], KQ[H[q], n, 0:64], Wu[H[q], n, 64:128], [Wu, KQ], tp=(64 * q, 64 * q))
                    kb.cp("act", O0[j][:], po03, [po0], [O0[j]])
                    kb.cp("act", Qs[j][:], pq3, [pq_], [Qs[j]])
                    yield

            alive = [gen_attn(), gen_dn()]
            while alive:
                for g_ in list(alive):
                    try:
                        next(g_)
                    except StopIteration:
                        alive.remove(g_)

            pOs = [kb.pst[6], kb.pst[7]]
            pO3s = [pp[:, :].rearrange("p (n c) -> p n c", n=8) for pp in pOs]
            for n in range(8):
                for j in range(2):
                    for q in range(2):
                        kb.mm(pOs[j], pO3s[j][H[q], n, :], Sbf[j][H[q], :], RT[j][H[q], n, :], [Sbf[j], RT[j]], tp=(64 * q, 64 * q))
                    pS = kb.ps()
                    for q in range(2):
                        kb.mm(pS, pS[H[q], 0:64], UT[j][H[q], n, :], Sbf[j][H[q], :], [Sbf[j], UT[j]], tp=(64 * q, 64 * q))
                    kb.stt("dve", pre[j][:], S32[j][:], EGLc[:, j, n:n + 1], Qs[j][:, n, :], ALU.mult, ALU.add, [S32[j], EGLc, Qs[j]], [pre[j]])
                    kb.cp("act", cS[j][:], pS[:, 0:64], [pS], [cS[j]])
                    kb.tt("dve", S32[j][:], pre[j][:], cS[j][:], ALU.subtract, [pre[j], cS[j]], [S32[j]])
                    kb.cp("act", Sbf[j][:], S32[j][:], [S32[j]], [Sbf[j]])
            for j in range(2):
                pO = pOs[j]
                pO3 = pO3s[j]
                kb.cp("act", oT[:], pO3, [pO], [oT])
                kb.tt("dve", oT[:], oT[:], O0[j][:], ALU.add, [oT, O0[j]], [oT])
                o2 = oT[:].rearrange("p n c -> p (n c)")
                kb.act(sqb[:], o2, AF.Square, [oT], [sqb])
                p = kb.ps()
                kb.mm(p, p[:, :], c["blk1"][:], sqb[:], [c["blk1"], sqb])
                kb.act(rstd[:], p[:, :], AF.Sqrt, [p, c["eps"]], [rstd], scale=1.0 / 64, bias=c["eps"][:, 0:1])
                kb.op("dve", lambda e: e.reciprocal(out=rstd[:], in_=rstd[:]), reads=[rstd], writes=[rstd])
                kb.stt("dve", ysil[:], o2, dnw[:, 0:1], rstd[:], ALU.mult, ALU.mult, [oT, dnw, rstd], [ysil])
                kb.tt("dve", yo[:], ysil[:], gates[:, j, :], ALU.mult, [ysil, gates], [yo])
                kb.dma("sp", self.y0s[1][j * 128:(j + 1) * 128, tg * 512:(tg + 1) * 512], yo[:], reads=[yo], writes=[self.y0s[1]], grp="y")
                if self.debug:
                    kb.dma("sp", self.dbg_o[j * 128:(j + 1) * 128, tg * 512:(tg + 1) * 512], o2, reads=[oT], writes=[self.dbg_o], grp="y")

    def gather(self, snd, gth):
        for a, b in zip(snd, gth):
            self.kb.collective("AllGather", self.groups, a, b, a.h.ap()[:, :], b.h.ap()[:, :])

    def outproj(self, gth, nk, wout):
        kb, c = self.kb, self.c
        sb = kb.sb
        kb.push()
        wo = sb("wo_%d" % nk, [128, nk, D], BF16)
        stg = [sb("wostg%d_%d" % (i, nk), [128, D], F32) for i in range(2)]
        for k in range(nk):
            st = stg[k % 2]
            kb.dma("sp", st[:], wout[k * 128:(k + 1) * 128, :], writes=[st])
            kb.tt("dve", wo[:, k, :], st[:], self.mods[:, 2, :], ALU.mult, [st, self.mods], [wo])
        ya = [sb("ya%d_%d" % (i, nk), [128, nk, 128], BF16) for i in range(2)]
        yb_ = [sb("yb%d_%d" % (i, nk), [128, nk, 128], BF16) for i in range(2)]
        ytmp = sb("ytmp_%d" % nk, [128, nk, 128], F32)
        yown = [sb("yown%d_%d" % (i, nk), [128, nk, 128], BF16) for i in range(2)]
        nb = len(gth)
        kpb = nk // nb
        for tt in range(16):
            A, B, Y = ya[tt % 2], yb_[tt % 2], yown[tt % 2]
            for b in range(nb):
                g3 = gth[b].h.ap().rearrange("(k p) t -> p k t", p=128)
                kb.dma("sp", A[:, b * kpb:(b + 1) * kpb, :], g3[:, :, tt * 128:(tt + 1) * 128], reads=[gth[b]], writes=[A])
                kb.dma("sp", B[:, b * kpb:(b + 1) * kpb, :], g3[:, :, TH + tt * 128:TH + (tt + 1) * 128], reads=[gth[b]], writes=[B])
            kb.ts("dve", ytmp[:], A[:], c["flag"][:, 0:1], None, ALU.mult, None, [A, c["flag"]], [ytmp])
            kb.stt("dve", Y[:], B[:], c["flag"][:, 1:2], ytmp[:], ALU.mult, ALU.add, [B, c["flag"], ytmp], [Y])
            for hf in range(2):
                p = kb.ps()
                for k in range(nk):
                    kb.mm(p, p[:, :], Y[:, k, :], wo[:, k, hf * 512:(hf + 1) * 512], [Y, wo], start=(k == 0), stop=(k == nk - 1))
                xr = self.xres[tt]
                kb.tt("dve", xr[:, hf * 512:(hf + 1) * 512], p[:, :], xr[:, hf * 512:(hf + 1) * 512], ALU.add, [p, xr], [xr])
        kb.pop()

    def load_xres(self):
        kb = self.kb
        self.xres = [kb.sb("xres%d" % i, [128, D], F32) for i in range(16)]
        for tt in range(16):
            kb.dma("sp", self.xres[tt][:], self.xown[tt * 128:(tt + 1) * 128, :], writes=[self.xres[tt]])

    def norm_own(self, hnT):
        for tt in range(16):
            self.norm_tile(self.xres[tt][:], self.xres[tt], 1, 0, hnT[:, :, tt * 128:(tt + 1) * 128], hnT)

    def ffn_run(self, hnT, specs, gates=None):
        kb, c = self.kb, self.c
        B = self.fb
        groups = []
        for (wg, wu, wd, nff, e) in specs:
            nch = nff // 128
            for gi in range((nch + 1) // 2):
                groups.append((wg, wu, wd, gi * 256, min(2, nch - gi * 2), e))

        def load_gu(it):
            wg, wu, wd, f0, fc, e = groups[it]
            wgb, wub = B["wg"][it % 2], B["wu"][it % 2]
            kb.dma("pool", wgb[:, :, 0:fc * 128], wg[:, f0:f0 + fc * 128].rearrange("(k p) n -> p k n", p=128), writes=[wgb])
            kb.dma("pool", wub[:, :, 0:fc * 128], wu[:, f0:f0 + fc * 128].rearrange("(k p) n -> p k n", p=128), writes=[wub])

        def load_d(it):
            wg, wu, wd, f0, fc, e = groups[it]
            st, wdb = B["stg"][it % 2], B["wd"][it % 2]
            kb.dma("sp", st[:, 0:fc, :], wd[f0:f0 + fc * 128, :].rearrange("(cc p) n -> p cc n", p=128), writes=[st])
            for cc in range(fc):
                kb.tt("pool", wdb[:, cc, :], st[:, cc, :], self.mods[:, 2, :], ALU.mult, [st, self.mods], [wdb])

        def gen_up(it):
            wg, wu, wd, f0, fc, e = groups[it]
            wgb, wub, h1 = B["wg"][it % 2], B["wu"][it % 2], B["h1"][it % 2]
            for cc in range(fc):
                for tg in range(4):
                    pgt = kb.ps()
                    put = kb.ps()
                    for k in range(8):
                        kb.mm(pgt, pgt[:, :], wgb[:, k, cc * 128:(cc + 1) * 128], hnT[:, k, tg * 512:(tg + 1) * 512], [wgb, hnT],
                              start=(k == 0), stop=(k == 7))
                    for k in range(8):
                        kb.mm(put, put[:, :], wub[:, k, cc * 128:(cc + 1) * 128], hnT[:, k, tg * 512:(tg + 1) * 512], [wub, hnT],
                              start=(k == 0), stop=(k == 7))
                    sl = B["sil"][(cc * 4 + tg) % 2]
                    kb.act(sl[:], pgt[:, :], AF.Silu, [pgt], [sl])
                    kb.tt("dve", h1[:, cc, tg * 512:(tg + 1) * 512], put[:, :], sl[:], ALU.mult, [put, sl], [h1])
                    yield

        def gen_down(it):
            wg, wu, wd, f0, fc, e = groups[it]
            wdb, h1 = B["wd"][it % 2], B["h1"][it % 2]
            for tt in range(16):
                xr = self.xres[tt]
                for hf in range(2):
                    p = kb.ps()
                    for cc in range(fc):
                        kb.mm(p, p[:, :], h1[:, cc, tt * 128:(tt + 1) * 128], wdb[:, cc, hf * 512:(hf + 1) * 512], [h1, wdb],
                              start=(cc == 0), stop=(cc == fc - 1))
                    if gates is None:
                        kb.tt("dve", xr[:, hf * 512:(hf + 1) * 512], p[:, :], xr[:, hf * 512:(hf + 1) * 512], ALU.add, [p, xr], [xr])
                    else:
                        ev = B["ev"][(tt * 2 + hf) % 2]
                        kb.act(ev[:], p[:, :], AF.Copy, [p, gates], [ev], scale=gates[:, tt, e:e + 1])
                        kb.tt("dve", xr[:, hf * 512:(hf + 1) * 512], ev[:], xr[:, hf * 512:(hf + 1) * 512], ALU.add, [ev, xr], [xr])
                if tt % 2 == 1:
                    yield

        n = len(groups)
        for it in range(min(2, n)):
            load_gu(it)
            load_d(it)
        for _ in gen_up(0):
            pass
        for it in range(n):
            if it + 2 < n:
                load_gu(it + 2)
            alive = [gen_down(it)]
            if it + 1 < n:
                alive.append(gen_up(it + 1))
            while alive:
                for g_ in list(alive):
                    try:
                        next(g_)
                    except StopIteration:
                        alive.remove(g_)
            if it + 2 < n:
                load_d(it + 2)

    def alloc_ffn(self, tag):
        kb = self.kb
        B = {}
        B["wg"] = [kb.sb("fwg%d_%s" % (i, tag), [128, 8, 256], BF16) for i in range(2)]
        B["wu"] = [kb.sb("fwu%d_%s" % (i, tag), [128, 8, 256], BF16) for i in range(2)]
        B["stg"] = [kb.sb("fst%d_%s" % (i, tag), [128, 2, D], F32) for i in range(2)]
        B["wd"] = [kb.sb("fwd%d_%s" % (i, tag), [128, 2, D], BF16) for i in range(2)]
        B["h1"] = [kb.sb("fh1%d_%s" % (i, tag), [128, 2, TH], BF16) for i in range(2)]
        B["sil"] = [kb.sb("fsil%d_%s" % (i, tag), [128, 512], F32) for i in range(2)]
        B["ev"] = [kb.sb("fev%d_%s" % (i, tag), [128, 512], F32) for i in range(2)]
        self.fb = B
        self.fcnt = 0

    def ffn0(self):
        kb = self.kb
        kb.push()
        hnT = kb.sb("hnTf0", [128, 8, TH], BF16)
        self.norm_own(hnT)
        self.alloc_ffn("f0")
        self.ffn_run(hnT, [(self.wg0.h.ap(), self.wu0.h.ap(), self.wd0.h.ap(), 2816, 0)])
        kb.pop()

    def moe(self):
        kb, c = self.kb, self.c
        kb.push()
        hnT = kb.sb("hnTf1", [128, 8, TH], BF16)
        self.norm_own(hnT)
        rwf = kb.sb("rwf", [128, 8, 8], F32)
        rwb = kb.sb("rwb", [128, 8, 8], BF16)
        kb.dma("sp", rwf[:], self.rw[:, :, :], writes=[rwf])
        kb.cp("dve", rwb[:], rwf[:], [rwf], [rwb])
        rbb = kb.sb("rbb", [128, 8], F32)
        kb.dma("sp", rbb[:], bc(self.rb[0:1, :], [128, 8]), writes=[rbb])
        lg = kb.sb("lg", [128, 16, 8], F32)
        p = kb.ps()
        for tt in range(16):
            for k in range(8):
                kb.mm(p, p[:, tt * 8:(tt + 1) * 8], hnT[:, k, tt * 128:(tt + 1) * 128], rwb[:, k, :], [hnT, rwb], start=(k == 0), stop=(k == 7))
        kb.cp("act", lg[:].rearrange("p a b -> p (a b)"), p[:, 0:128], [p], [lg])
        kb.tt("dve", lg[:], lg[:], bc(rbb[:].unsqueeze(1), [128, 16, 8]), ALU.add, [lg, rbb], [lg])
        m1 = kb.sb("m1", [128, 16], F32)
        m2 = kb.sb("m2", [128, 16], F32)
        eq1 = kb.sb("eq1", [128, 16, 8], F32)
        eq2 = kb.sb("eq2", [128, 16, 8], F32)
        lg2 = kb.sb("lg2", [128, 16, 8], F32)
        w1 = kb.sb("w1", [128, 16], F32)
        w2 = kb.sb("w2", [128, 16], F32)
        gates = kb.sb("gates1", [128, 16, 8], F32)
        kb.op("dve", lambda e: e.reduce_max(out=m1[:], in_=lg[:], axis=AX.X), reads=[lg], writes=[m1])
        kb.tt("dve", eq1[:], lg[:], bc(m1[:].unsqueeze(2), [128, 16, 8]), ALU.is_equal, [lg, m1], [eq1])
        kb.stt("dve", lg2[:], eq1[:], NEG, lg[:], ALU.mult, ALU.add, [eq1, lg], [lg2])
        kb.op("dve", lambda e: e.reduce_max(out=m2[:], in_=lg2[:], axis=AX.X), reads=[lg2], writes=[m2])
        kb.tt("dve", eq2[:], lg2[:], bc(m2[:].unsqueeze(2), [128, 16, 8]), ALU.is_equal, [lg2, m2], [eq2])
        kb.tt("dve", w2[:], m2[:], m1[:], ALU.subtract, [m1, m2], [w2])
        kb.act(w1[:], w2[:], AF.Sigmoid, [w2], [w1], scale=-1.0)
        kb.act(w2[:], w2[:], AF.Sigmoid, [w2], [w2])
        kb.tt("dve", eq1[:], eq1[:], bc(w1[:].unsqueeze(2), [128, 16, 8]), ALU.mult, [eq1, w1], [eq1])
        kb.tt("dve", eq2[:], eq2[:], bc(w2[:].unsqueeze(2), [128, 16, 8]), ALU.mult, [eq2, w2], [eq2])
        kb.tt("dve", gates[:], eq1[:], eq2[:], ALU.add, [eq1, eq2], [gates])
        if self.moeonly:
            kb.dma("sp", self.dbg_g[:, :], gates[:].rearrange("p a b -> p (a b)"), reads=[gates], writes=[self.dbg_g])
        self.alloc_ffn("f1")
        self.ffn_run(hnT, [(self.mwg.h.ap()[e], self.mwu.h.ap()[e], self.mwd.h.ap()[e], 3584, e) for e in range(self.nexp)], gates=gates)
        kb.pop()

    def mixer1(self):
        kb, c = self.kb, self.c
        sb = kb.sb
        kb.push()
        hnT = sb("hnTm1", [128, 8, TH], BF16)
        self.norm_own(hnT)
        for b in range(2):
            kb.dma("sp", self.h1s[b].h.ap().rearrange("(k p) t -> p k t", p=128), hnT[:, 4 * b:4 * b + 4, :], reads=[hnT], writes=[self.h1s[b]])
        self.gather(self.h1s, self.h1g)
        w1 = sb("w1m", [128, 8, NC1], BF16)
        for i in range(0, NC1, 512):
            j = min(i + 512, NC1)
            kb.dma("pool", w1[:, :, i:j], self.win1[:, i:j].rearrange("(k p) n -> p k n", p=128), writes=[w1])
        sm = sb("l1smc", [128, 4, 8], F32)
        kb.dma("sp", sm[:], self.l1sm[:, :, :], writes=[sm])
        scw = sb("scwc", [128, 2, 3], F32)
        kb.dma("sp", scw[:], self.scw[:, :, :], writes=[scw])
        gwf = sb("gwf", [128, 8, 128], F32)
        gwb = sb("gwb", [128, 8, 128], BF16)
        kb.dma("sp", gwf[:, 0:4, :], self.gaw.h.ap().rearrange("n i j -> i n j"), writes=[gwf])
        kb.dma("sp", gwf[:, 4:8, :], self.gxw.h.ap().rearrange("n i j -> i n j"), writes=[gwf])
        kb.cp("dve", gwb[:], gwf[:], [gwf], [gwb])
        cj = sb("cj", [128, 4], F32)
        kb.act(cj[:], sm[:, :, 7], AF.Exp, [sm], [cj], scale=-1.0)
        kb.act(cj[:], cj[:], AF.Ln, [cj, c["one"]], [cj], bias=c["one"][:, 0:1])
        kb.ts("dve", cj[:], cj[:], -8.0, None, ALU.mult, None, [cj], [cj])
        hin = sb("hin1", [128, 8, 512], BF16)
        xp1 = [sb("xp1_%d" % i, [128, 3 + 512], F32) for i in range(4)]
        xp2 = [sb("xp2_%d" % i, [128, 2 + 512], F32) for i in range(2)]
        for t_ in xp1 + xp2:
            kb.op("pool", lambda e, t_=t_: e.memset(t_[:, 0:3], 0.0), writes=[t_])
        hprev = sb("hprev", [128, 4], F32)
        kb.op("pool", lambda e: e.memset(hprev[:], 0.0), writes=[hprev])
        F = lambda n: sb(n, [128, 512], F32)
        xcv, rg, ig, av, bv, hs, yv, y2, gl, cdv = F("xcv"), F("rg"), F("ig"), F("av"), F("bv"), F("hs"), F("yv"), F("y2"), F("gl"), F("cdv")
        xcb = sb("xcb", [128, 512], BF16)
        yo = sb("yo1", [128, 512], BF16)
        g3 = [self.h1g[b].h.ap().rearrange("(r k p) t -> r p k t", r=2, p=128) for b in range(2)]

        def inproj(ci):
            p = kb.ps()
            for k in range(8):
                kb.mm(p, p[:, :], w1[:, k, ci * 128:(ci + 1) * 128], hin[:, k, :], [w1, hin], start=(k == 0), stop=(k == 7))
            return p

        for tg in range(self.ntg):
            for b in range(2):
                kb.dma("sp", hin[:, 4 * b:4 * b + 4, :], g3[b][tg // 4, :, :, (tg % 4) * 512:(tg % 4 + 1) * 512], reads=[self.h1g[b]], writes=[hin])
            for i in range(4):
                p = inproj(i)
                xp = xp1[i]
                if tg > 0:
                    kb.cp("pool", xp[:, 0:3], xp[:, 512:515], [xp], [xp])
                kb.cp("act", xp[:, 3:515], p[:, :], [p], [xp])
                kb.ts("dve", xcv[:], xp[:, 0:512], sm[:, i, 0:1], sm[:, i, 4:5], ALU.mult, ALU.add, [xp, sm], [xcv])
                for k in range(1, 4):
                    kb.stt("dve", xcv[:], xp[:, k:k + 512], sm[:, i, k:k + 1], xcv[:], ALU.mult, ALU.add, [xp, sm, xcv], [xcv])
                kb.cp("act", xcb[:], xcv[:], [xcv], [xcb])
                if self.m1_stop == "conv":
                    continue
                pr = kb.ps()
                pi = kb.ps()
                kb.mm(pr, pr[:, :], gwb[:, i, :], xcb[:], [gwb, xcb])
                kb.mm(pi, pi[:, :], gwb[:, 4 + i, :], xcb[:], [gwb, xcb])
                kb.act(rg[:], pr[:, :], AF.Sigmoid, [pr, sm], [rg], bias=sm[:, i, 5:6])
                kb.act(ig[:], pi[:, :], AF.Sigmoid, [pi, sm], [ig], bias=sm[:, i, 6:7])
                if self.m1_stop == "gate":
                    continue
                kb.act(av[:], rg[:], AF.Exp, [rg, cj], [av], scale=cj[:, i:i + 1])
                kb.tt("pool", bv[:], av[:], av[:], ALU.mult, [av], [bv])
                kb.ts("dve", bv[:], bv[:], -1.0, 1.0, ALU.mult, ALU.add, [bv], [bv])
                kb.act(bv[:], bv[:], AF.Sqrt, [bv], [bv])
                kb.tt("pool", bv[:], bv[:], ig[:], ALU.mult, [bv, ig], [bv])
                kb.tt("dve", bv[:], bv[:], xcv[:], ALU.mult, [bv, xcv], [bv])
                if self.m1_stop == "ab":
                    continue
                kb.op("dve", lambda e, i=i: e.tensor_tensor_scan(out=hs[:], data0=av[:], data1=bv[:], initial=hprev[:, i:i + 1],
                                                                 op0=ALU.mult, op1=ALU.add), reads=[av, bv, hprev], writes=[hs])
                kb.cp("dve", hprev[:, i:i + 1], hs[:, 511:512], [hs], [hprev])
                if self.m1_stop == "scan":
                    continue
                p = inproj(4 + i)
                kb.cp("act", yv[:], p[:, :], [p], [yv])
                kb.tt("pool", y2[:], yv[:], yv[:], ALU.mult, [yv], [y2])
                kb.ts("dve", y2[:], y2[:], 0.044715, 1.0, ALU.mult, ALU.add, [y2], [y2])
                kb.tt("pool", y2[:], y2[:], yv[:], ALU.mult, [y2, yv], [y2])
                kb.act(gl[:], y2[:], AF.Sigmoid, [y2], [gl], scale=1.5957691216057308)
                kb.tt("pool", gl[:], gl[:], yv[:], ALU.mult, [gl, yv], [gl])
                kb.tt("dve", yo[:], gl[:], hs[:], ALU.mult, [gl, hs], [yo])
                kb.dma("sp", self.y1s[i // 2][(i % 2) * 128:(i % 2 + 1) * 128, tg * 512:(tg + 1) * 512], yo[:], reads=[yo], writes=[self.y1s[i // 2]])
            if self.m1_stop in ("conv", "gate", "ab", "scan", "lru"):
                continue
            for i in range(2):
                pc = inproj(10 + i)
                ph = inproj(12 + i)
                xp = xp2[i]
                if tg > 0:
                    kb.cp("pool", xp[:, 0:2], xp[:, 512:514], [xp], [xp])
                kb.cp("act", cdv[:], pc[:, :], [pc], [cdv])
                kb.tt("dve", xp[:, 2:514], ph[:, :], cdv[:], ALU.mult, [ph, cdv], [xp])
                kb.ts("dve", cdv[:], xp[:, 0:512], scw[:, i, 0:1], None, ALU.mult, None, [xp, scw], [cdv])
                for k in range(1, 3):
                    kb.stt("dve", cdv[:], xp[:, k:k + 512], scw[:, i, k:k + 1], cdv[:], ALU.mult, ALU.add, [xp, scw, cdv], [cdv])
                pb_ = inproj(8 + i)
                kb.tt("dve", yo[:], pb_[:, :], cdv[:], ALU.mult, [pb_, cdv], [yo])
                kb.dma("sp", self.y1s[2][i * 128:(i + 1) * 128, tg * 512:(tg + 1) * 512], yo[:], reads=[yo], writes=[self.y1s[2]])
        kb.pop()

    def final_norm(self):
        kb, c = self.kb, self.c
        s = self.scr
        kb.push()
        fw = kb.sb("fwrow", [128, D], F32)
        kb.dma("sp", fw[:], bc(self.fnw[0:1, :], [128, D]), writes=[fw])
        ot = [kb.sb("ot%d" % i, [128, D], F32) for i in range(2)]
        for tt in range(16):
            xr = self.xres[tt]
            kb.act(s["junk"][:], xr[:], AF.Square, [xr], [s["junk"], s["ss"]], accum_out=s["ss"][:])
            kb.act(s["rs"][:], s["ss"][:], AF.Sqrt, [s["ss"], c["eps"]], [s["rs"]], scale=1.0 / D, bias=c["eps"][:, 0:1])
            kb.op("dve", lambda e: e.reciprocal(out=s["rs"][:], in_=s["rs"][:]), reads=[s["rs"]], writes=[s["rs"]])
            o = ot[tt % 2]
            kb.stt("dve", o[:], xr[:], s["rs"][:, 0:1], fw[:], ALU.mult, ALU.mult, [xr, s["rs"], fw], [o])
            kb.dma("sp", self.out[tt * 128:(tt + 1) * 128, :], o[:], reads=[o], writes=[self.out])
        kb.pop()

    def build(self):
        kb = self.kb
        self.declare()
        if self.debug and self.stop_after != "adaln":
            self.dbg_o = self.outp("dbg_o", [256, T])
            self.dbg_y0 = self.outp("dbg_y0", [512, T], BF16)
        self.consts()
        self.alloc_scr()
        if self.moeonly:
            kb.op("dve", lambda e: e.memset(self.mods[:], 1.0), writes=[self.mods])
            self.load_xres()
            self.moe()
            return self.dump_x()
        if self.m1only:
            kb.op("dve", lambda e: e.memset(self.mods[:], 1.0), writes=[self.mods])
            self.load_xres()
            self.mixer1()
            if self.m1_stop is None:
                self.gather(self.y1s, self.y1g)
                self.outproj(self.y1g, 12, self.wout1)
            return self.dump_x()
        if self.skip_ada:
            kb.op("dve", lambda e: e.memset(self.mods[:], 1.0), writes=[self.mods])
        else:
            self.adaln(0, 0)
        if self.stop_after == "adaln":
            self.dbg_mods = self.outp("dbg_mods", [128, 3 * D])
            kb.dma("sp", self.dbg_mods[:, :], self.mods[:].rearrange("p a b -> p (a b)"), reads=[self.mods], writes=[self.dbg_mods], grp="y")
            self.finish()
            return
        self.mixer0()
        kb.pop()
        if self.stop_after != "mixer0":
            self.gather(self.y0s, self.y0g)
            self.load_xres()
            self.outproj(self.y0g, 8, self.wout0)
            if self.stop_after == "op0":
                return self.dump_x()
            self.adaln(0, 1)
            self.ffn0()
            if self.stop_after == "ffn0":
                return self.dump_x()
            self.adaln(1, 0)
            self.mixer1()
            self.gather(self.y1s, self.y1g)
            self.outproj(self.y1g, 12, self.wout1)
            if self.stop_after == "premoe":
                return self.dump_x()
            self.adaln(1, 1)
            self.moe()
            self.final_norm()
            self.finish()
            return
        if self.stop_after == "mixer0":
            stg = kb.sb("stg", [128, 4, 512], BF16)
            for tg in range(self.ntg):
                for b in range(2):
                    kb.dma("sp", stg[:, 2 * b:2 * b + 2, :], self.y0s[b].h.ap()[:, tg * 512:(tg + 1) * 512].rearrange("(a p) t -> p a t", p=128),
                           reads=[self.y0s[b]], writes=[stg], grp="x")
                kb.dma("sp", self.dbg_y0.h.ap()[:, tg * 512:(tg + 1) * 512].rearrange("(a p) t -> p a t", p=128), stg[:],
                       reads=[stg], writes=[self.dbg_y0], grp="y")
            self.finish()
            return

    def dump_x(self):
        kb = self.kb
        for tt in range(16):
            kb.dma("sp", self.out[tt * 128:(tt + 1) * 128, :], self.xres[tt][:], reads=[self.xres[tt]], writes=[self.out])
        self.finish()

    def finish(self):
        kb = self.kb
        kb.barrier()


def bucket_tab():
    dist = np.arange(128)
    max_exact = 16
    d = np.maximum(dist, 0)
    large = max_exact + (np.log(np.maximum(d, 1) / max_exact) / np.log(128 / max_exact) * (32 - max_exact)).astype(np.int32)
    large = np.minimum(large, 31)
    return np.where(d < max_exact, d, large).astype(np.int32)


def host_inputs(inputs, c):
    b, half = c // 2, c % 2
    f32 = np.float32
    m = {}
    x = inputs["x"]
    m["xfull"] = np.ascontiguousarray(x[b])
    m["xown"] = np.ascontiguousarray(x[b, half * TH:(half + 1) * TH])
    fl = np.zeros((128, 2), f32)
    fl[:, 0] = 1.0 - half
    fl[:, 1] = half
    m["flag"] = fl
    m["cvec"] = np.ascontiguousarray(inputs["c"][b].reshape(8, 128).T)
    m["adaw"] = inputs["ada_w"]
    m["adab"] = inputs["ada_b"]
    m["nmw"] = inputs["norm_mix_w"]
    m["nfw"] = inputs["norm_ffn_w"]
    m["fnw"] = inputs["final_norm_w"].reshape(1, D)
    m["ident"] = np.eye(128, dtype=f32)
    r = np.arange(64)[:, None]
    cc = np.arange(64)[None, :]
    cm = np.zeros((128, 4, 64), f32)
    for q in range(2):
        cm[q * 64:(q + 1) * 64, 0] = np.where(cc >= r, 0.0, NEG)
        cm[q * 64:(q + 1) * 64, 1] = np.where(r > cc, 0.0, NEG)
        cm[q * 64:(q + 1) * 64, 2] = np.eye(64)
    m["cmask"] = cm
    sel = np.zeros((4, 2, 128), f32)
    for q in range(2):
        for j in range(2):
            sel[2 * q + j, j, q * 64:(q + 1) * 64] = 1.0
    m["sel"] = sel
    blk = np.zeros((128, 128), f32)
    blk[:64, :64] = 1.0
    blk[64:, 64:] = 1.0
    m["blk1"] = blk
    w = inputs["ab_w_in"][0]
    HA = lambda q, j: 4 * half + 2 * q + j
    cols = []
    for j in range(2):
        for q in range(2):
            cols += list(range(HA(q, j) * 64, HA(q, j) * 64 + 64))
    cols += list(range(512 + half * 64, 512 + half * 64 + 64)) * 2
    dncols = []
    for base in (768, 768 + 512, 768 + 1024, 2304):
        for j in range(2):
            for q in range(2):
                cols += list(range(base + HA(q, j) * 64, base + HA(q, j) * 64 + 64))
                if base < 2304:
                    dncols += list(range(base - 768 + HA(q, j) * 64, base - 768 + HA(q, j) * 64 + 64))
    cols += list(range(640 + half * 64, 640 + half * 64 + 64))
    cols += [2816 + 4 * half + hl for hl in range(4)]
    cols += [2824 + 4 * half + hl for hl in range(4)]
    assert len(cols) == NC0
    m["win0"] = np.ascontiguousarray(w[:, cols])
    cw = inputs["dn_conv_w"][0][:, dncols]
    m["cw0"] = np.ascontiguousarray(cw.reshape(4, 6, 128).transpose(2, 1, 0))
    hl = [4 * half + i for i in range(4)]
    m["dnsm"] = np.ascontiguousarray(np.stack([inputs["dn_a_log"][0][hl], inputs["dn_dt_bias"][0][hl]], axis=1))
    m["dnw"] = np.ascontiguousarray(np.tile(inputs["dn_norm_w"][0], 2).reshape(128, 1))
    sk = np.zeros((128, 2), f32)
    for q in range(2):
        for j in range(2):
            sk[q * 64:(q + 1) * 64, j] = inputs["attn_sinks"][0][HA(q, j)]
    m["sinkl"] = sk
    bt = bucket_tab()
    s_ = np.arange(128)[:, None]
    qi = np.arange(128)[None, :]
    bg = np.zeros((2, 128, 4, 128), f32)
    am = np.zeros((2, 128, 128), f32)
    for a in range(2):
        dist = qi + 128 - (s_ + 128 * a)
        valid = (dist >= 0) & (dist < 128)
        bk = bt[np.clip(dist, 0, 127)]
        am[a] = np.where(valid, 0.0, NEG)
        for q in range(2):
            for j in range(2):
                bg[a, :, q * 2 + j, :] = inputs["rel_bias"][bk, HA(q, j)]
    m["biasg"] = bg.reshape(2, 128, 512)
    m["amask"] = am
    rows = []
    for base in (0, 512):
        for r_ in range(2):
            for j in range(2):
                for q in range(2):
                    Hh = 4 * r_ + 2 * q + j
                    rows += list(range(base + Hh * 64, base + Hh * 64 + 64))
    m["wout0"] = np.ascontiguousarray(inputs["ab_w_out"][0][rows])
    m["wg0"] = inputs["ffn_w_gate"][0]
    m["wu0"] = inputs["ffn_w_up"][0]
    m["wd0"] = inputs["ffn_w_down"][0]
    w1 = inputs["cd_w_in"][0]
    cols = []
    for base in (0, 1024):
        for i in range(4):
            cols += list(range(base + (4 * half + i) * 128, base + (4 * half + i + 1) * 128))
    for base in (2048, 2560, 3072):
        for i in range(2):
            cols += list(range(base + (2 * half + i) * 128, base + (2 * half + i + 1) * 128))
    assert len(cols) == NC1
    m["win1"] = np.ascontiguousarray(w1[:, cols])
    ch = np.arange(512) + 512 * half
    sm = np.zeros((128, 4, 8), f32)
    sm[:, :, 0:4] = inputs["lru_conv_w"][0][:, ch].reshape(4, 4, 128).transpose(2, 1, 0)
    sm[:, :, 4] = inputs["lru_conv_b"][0][ch].reshape(4, 128).T
    sm[:, :, 5] = inputs["lru_gate_a_b"][0][ch].reshape(4, 128).T
    sm[:, :, 6] = inputs["lru_gate_x_b"][0][ch].reshape(4, 128).T
    sm[:, :, 7] = inputs["lru_lambda"][0][ch].reshape(4, 128).T
    m["l1sm"] = sm
    m["gaw"] = np.ascontiguousarray(inputs["lru_gate_a_w"][0][4 * half:4 * half + 4])
    m["gxw"] = np.ascontiguousarray(inputs["lru_gate_x_w"][0][4 * half:4 * half + 4])
    sc = inputs["sconv_w"][0][:, 256 * half:256 * half + 256]
    m["scw"] = np.ascontiguousarray(sc.reshape(3, 2, 128).transpose(2, 1, 0))
    rows = []
    for b_ in range(2):
        for r_ in range(2):
            for i in (2 * b_, 2 * b_ + 1):
                rows += list(range((4 * r_ + i) * 128, (4 * r_ + i + 1) * 128))
    for r_ in range(2):
        for i in range(2):
            rows += list(range(1024 + (2 * r_ + i) * 128, 1024 + (2 * r_ + i + 1) * 128))
    m["wout1"] = np.ascontiguousarray(inputs["cd_w_out"][0][rows])
    m["rw"] = np.ascontiguousarray(inputs["moe_router_w"][0].reshape(8, 128, 8).transpose(1, 0, 2))
    m["rb"] = inputs["moe_router_b"][0].reshape(1, 8)
    m["mwg"] = inputs["moe_w_gate"][0]
    m["mwu"] = inputs["moe_w_up"][0]
    m["mwd"] = inputs["moe_w_down"][0]
    return {k: np.ascontiguousarray(v, dtype=np.float32) for k, v in m.items()}


def run(inputs, stop_after=None, debug=False, **kw):
    pg = Prog(stop_after=stop_after, debug=debug, **kw)
    pg.build()
    in_maps = []
    for c in range(8):
        hm = host_inputs(inputs, c)
        in_maps.append({k: hm[k] for k in pg.inputs})
    res = run_bass_kernel_spmd(pg.kb.nc, in_maps, core_ids=list(range(8)))
    return res, pg


def kernel(**inputs):
    inputs = {k: np.asarray(v) for k, v in inputs.items()}
    res, pg = run(inputs)
    out = np.zeros((4, T, D), np.float32)
    for c in range(8):
        out[c // 2, (c % 2) * TH:(c % 2 + 1) * TH] = res.results[c]["out"]
    return out
```

```python
import numpy as np
import concourse.bass as bass
import concourse.mybir as mybir
from concourse.bass_utils import run_bass_kernel_spmd

F32 = mybir.dt.float32
BF16 = mybir.dt.bfloat16
AF = mybir.ActivationFunctionType
ALU = mybir.AluOpType
AX = mybir.AxisListType

T = 4096
TH = 2048
D = 1024
EPS = 1e-6
NEG = -30000.0
PAIRS = [[0, 1], [2, 3], [4, 5], [6, 7]]


class Tl:
    __slots__ = ("h", "name", "w", "r", "ds")

    def __init__(self, h, name):
        self.h = h
        self.name = name
        self.w = None
        self.r = {}
        self.ds = None

    def __getitem__(self, k):
        return self.h[k]


class Eng:
    def __init__(self, name, handle, sem):
        self.name = name
        self.h = handle
        self.sem = sem
        self.cnt = 0
        self.waited = {}

    def wait(self, ev):
        sem, val, _ = ev
        k = id(sem)
        if self.waited.get(k, 0) >= val:
            return
        self.waited[k] = val
        self.h.wait_ge(sem, val)


class KB:
    def __init__(self):
        self.nc = bass.Bass("TRN2", target_bir_lowering=False)
        nc = self.nc
        self.E = {}
        for n, h in (("pe", nc.tensor), ("act", nc.scalar), ("dve", nc.vector),
                     ("pool", nc.gpsimd), ("sp", nc.sync)):
            self.E[n] = Eng(n, h, nc.alloc_semaphore("sem_" + n))
        self.dall = []
        self.dfree = {}
        self.ninst = 0
        self.nps = 0
        self.pst = [Tl(nc.alloc_psum_tensor("ps%d" % i, [128, 512], F32), "ps%d" % i) for i in range(8)]
        self.nrot = 6

    def sb(self, name, shape, dt=F32):
        if not hasattr(self, "scopes"):
            self.scopes = [[]]
        cm = self.nc.sbuf_tensor(name, list(shape), dt)
        h = cm.__enter__()
        t = Tl(h, name)
        self.scopes[-1].append((cm, t))
        return t

    def push(self):
        if not hasattr(self, "scopes"):
            self.scopes = [[]]
        self.scopes.append([])

    def pop(self):
        self.barrier()
        for cm, t in reversed(self.scopes.pop()):
            if t.ds is not None:
                self.dfree.setdefault(t.ds[2], []).append(t.ds)
                t.ds = None
            cm.__exit__(None, None, None)

    def ps(self):
        t = self.pst[self.nps % self.nrot]
        self.nps += 1
        return t

    def dram(self, name, shape, dt, kind="Internal"):
        return Tl(self.nc.dram_tensor(name, list(shape), dt, kind=kind), name)

    def _deps(self, eng, reads, writes):
        E = self.E[eng]
        for t in reads:
            if t.w is not None:
                ev = t.w
                if ev[2] == eng and eng == "pe":
                    continue
                E.wait(ev)
        for t in writes:
            if t.w is not None and not (t.w[2] == eng and eng == "pe"):
                E.wait(t.w)
            for ev in t.r.values():
                if not (ev[2] == eng and eng == "pe"):
                    E.wait(ev)

    def _commit(self, ev, reads, writes):
        for t in reads:
            t.r[id(ev[0])] = ev
        for t in writes:
            t.w = ev
            t.r = {}

    def op(self, eng, fn, reads=(), writes=(), sig=True):
        E = self.E[eng]
        self._deps(eng, reads, writes)
        ins = fn(E.h)
        self.ninst += 1
        if sig:
            E.cnt += 1
            ins.then_inc(E.sem, 1)
            ev = (E.sem, E.cnt, eng)
        else:
            ev = (E.sem, E.cnt + 1, eng)
        self._commit(ev, reads, writes)
        return ins

    def _dsem(self, t, kind):
        if t.ds is None:
            fl = self.dfree.setdefault(kind, [])
            if fl:
                t.ds = fl.pop()
            else:
                t.ds = [self.nc.alloc_semaphore("dsem%d" % len(self.dall)), 0, kind]
                self.dall.append(t.ds)
        assert t.ds[2] == kind, (t.name, t.ds[2], kind)
        return t.ds

    def dma(self, q, out_ap, in_ap, reads=(), writes=(), grp="d"):
        E = self.E[q]
        self._deps(q, reads, writes)
        d = self._dsem(writes[0], "sw" if q == "pool" else "hw")
        d[1] += 16
        E.h.dma_start(out=out_ap, in_=in_ap).then_inc(d[0], 16)
        self.ninst += 1
        self._commit((d[0], d[1], "dma"), reads, writes)

    def collective(self, kind, groups, in_t, out_t, in_ap, out_ap, grp="cc"):
        E = self.E["pool"]
        self._deps("pool", [in_t], [out_t])
        d = self._dsem(out_t, "cc")
        d[1] += 1
        E.h.collective_compute(kind, ALU.bypass, replica_groups=groups,
                               ins=[in_ap], outs=[out_ap]).then_inc(d[0])
        self._commit((d[0], d[1], "dma"), [in_t], [out_t])

    def barrier(self):
        evs = []
        for n, E in self.E.items():
            if E.cnt > 0:
                evs.append((E.sem, E.cnt, n))
        for d in self.dall:
            if d[1] > 0:
                evs.append((d[0], d[1], "dma"))
        for n, E in self.E.items():
            for ev in evs:
                if ev[2] != n:
                    E.wait(ev)

    def mm(self, pst, out, lhsT, rhs, reads, start=True, stop=True, tp=None):
        kw = {}
        if tp is not None:
            kw["tile_position"] = tp
        return self.op("pe", lambda e: e.matmul(out, lhsT, rhs, start=start, stop=stop, **kw),
                       reads=reads, writes=[pst], sig=stop)

    def tr(self, pst, out, in_, ident, reads, tp=None):
        kw = {}
        if tp is not None:
            kw["tile_position"] = tp
        return self.op("pe", lambda e: e.transpose(out, in_, ident, **kw), reads=reads, writes=[pst])

    def act(self, out, in_, func, reads, writes, eng="act", **kw):
        return self.op(eng, lambda e: e.activation(out=out, in_=in_, func=func, **kw), reads=reads, writes=writes)

    def tt(self, eng, out, a, b, op, reads, writes):
        return self.op(eng, lambda e: e.tensor_tensor(out=out, in0=a, in1=b, op=op), reads=reads, writes=writes)

    def ts(self, eng, out, a, s1, s2, op0, op1, reads, writes):
        if op1 is None:
            return self.op(eng, lambda e: e.tensor_scalar(out=out, in0=a, scalar1=s1, scalar2=None, op0=op0),
                           reads=reads, writes=writes)
        return self.op(eng, lambda e: e.tensor_scalar(out=out, in0=a, scalar1=s1, scalar2=s2, op0=op0, op1=op1),
                       reads=reads, writes=writes)

    def stt(self, eng, out, a, s, b, op0, op1, reads, writes):
        return self.op(eng, lambda e: e.scalar_tensor_tensor(out=out, in0=a, scalar=s, in1=b, op0=op0, op1=op1),
                       reads=reads, writes=writes)

    def cp(self, eng, out, in_, reads, writes):
        if eng == "act":
            return self.op(eng, lambda e: e.copy(out=out, in_=in_), reads=reads, writes=writes)
        return self.op(eng, lambda e: e.tensor_copy(out=out, in_=in_), reads=reads, writes=writes)


def bc(ap, shape):
    return ap.broadcast_to(list(shape))


NC0 = 11 * 128 + 64 + 8
NC1 = 14 * 128


class Prog:
    def __init__(self, stop_after=None, debug=False, ntg=8, m0_stop=None, skip_ada=False, m1only=False, m1_stop=None):
        self.m1only = m1only
        self.m1_stop = m1_stop
        self.moeonly = False
        self.ntg = ntg
        self.nexp = 8
        self.groups = PAIRS
        self.m0_stop = m0_stop
        self.skip_ada = skip_ada
        self.kb = KB()
        self.stop_after = stop_after
        self.debug = debug
        self.inputs = {}
        self.outputs = {}

    def inp(self, name, shape, dt=F32):
        t = self.kb.dram(name, shape, dt, kind="ExternalInput")
        self.inputs[name] = t
        return t

    def outp(self, name, shape, dt=F32):
        t = self.kb.dram(name, shape, dt, kind="ExternalOutput")
        self.outputs[name] = t
        return t

    def declare(self):
        I = self.inp
        self.xfull = I("xfull", [T, D])
        self.xown = I("xown", [TH, D])
        self.flag = I("flag", [128, 2])
        self.cvec = I("cvec", [128, 8])
        self.adaw = I("adaw", [2, D, 6 * D])
        self.adab = I("adab", [2, 6 * D])
        self.nmw = I("nmw", [2, D])
        self.nfw = I("nfw", [2, D])
        self.fnw = I("fnw", [1, D])
        self.ident = I("ident", [128, 128])
        self.cmask = I("cmask", [128, 4, 64])
        self.sel = I("sel", [4, 2, 128])
        self.blk1 = I("blk1", [128, 128])
        self.win0 = I("win0", [D, NC0])
        self.cw0 = I("cw0", [128, 6, 4])
        self.dnsm = I("dnsm", [4, 2])
        self.dnw = I("dnw", [128, 1])
        self.sinkl = I("sinkl", [128, 2])
        self.biasg = I("biasg", [2, 128, 512])
        self.amask = I("amask", [2, 128, 128])
        if self.stop_after in ("adaln", "mixer0") and not self.m1only:
            self._internal()
            return
        if self.moeonly:
            self.rw = I("rw", [128, 8, 8])
            self.rb = I("rb", [1, 8])
            self.mwg = I("mwg", [self.nexp, D, 3584])
            self.mwu = I("mwu", [self.nexp, D, 3584])
            self.mwd = I("mwd", [self.nexp, 3584, D])
            self.out = self.outp("out", [TH, D])
            self.dbg_g = self.outp("dbg_g", [128, 128])
            self._internal()
            return
        if self.m1only:
            self.win1 = I("win1", [D, NC1])
            self.l1sm = I("l1sm", [128, 4, 8])
            self.gaw = I("gaw", [4, 128, 128])
            self.gxw = I("gxw", [4, 128, 128])
            self.scw = I("scw", [128, 2, 3])
            self.wout1 = I("wout1", [1536, D])
            self.out = self.outp("out", [TH, D])
            self._internal()
            return
        self.wout0 = I("wout0", [D, D])
        if self.stop_after == "op0":
            self.out = self.outp("out", [TH, D])
            self._internal()
            return
        self.wg0 = I("wg0", [D, 2816])
        self.wu0 = I("wu0", [D, 2816])
        self.wd0 = I("wd0", [2816, D])
        if self.stop_after == "ffn0":
            self.out = self.outp("out", [TH, D])
            self._internal()
            return
        self.win1 = I("win1", [D, NC1])
        self.l1sm = I("l1sm", [128, 4, 8])
        self.gaw = I("gaw", [4, 128, 128])
        self.gxw = I("gxw", [4, 128, 128])
        self.scw = I("scw", [128, 2, 3])
        self.wout1 = I("wout1", [1536, D])
        if self.stop_after != "premoe":
            self.rw = I("rw", [128, 8, 8])
            self.rb = I("rb", [1, 8])
            self.mwg = I("mwg", [8, D, 3584])
            self.mwu = I("mwu", [8, D, 3584])
            self.mwd = I("mwd", [8, 3584, D])
        self.out = self.outp("out", [TH, D])
        self._internal()

    def _internal(self):
        kb = self.kb
        self.y0s = [kb.dram("y0s%d" % i, [256, T], BF16) for i in range(2)]
        self.y0g = [kb.dram("y0g%d" % i, [512, T], BF16) for i in range(2)]
        self.h1s = [kb.dram("h1s%d" % i, [512, TH], BF16) for i in range(2)]
        self.h1g = [kb.dram("h1g%d" % i, [1024, TH], BF16) for i in range(2)]
        self.y1s = [kb.dram("y1s%d" % i, [256, T], BF16) for i in range(3)]
        self.y1g = [kb.dram("y1g%d" % i, [512, T], BF16) for i in range(3)]

    def consts(self):
        kb = self.kb
        c = {}
        self.c = c
        c["idf"] = kb.sb("idf", [128, 128], F32)
        c["idb"] = kb.sb("idb", [128, 128], BF16)
        c["cm"] = kb.sb("cm", [128, 4, 64], F32)
        c["i64b"] = kb.sb("i64b", [128, 64], BF16)
        c["sel"] = kb.sb("selc", [4, 2, 128], F32)
        c["blk1f"] = kb.sb("blk1f", [128, 128], F32)
        c["blk1"] = kb.sb("blk1b", [128, 128], BF16)
        c["ones"] = kb.sb("onesb", [128, 128], BF16)
        c["flag"] = kb.sb("flagc", [128, 2], F32)
        c["cv"] = kb.sb("cv", [128, 8], F32)
        c["cond"] = kb.sb("cond", [128, 8], F32)
        c["condB"] = kb.sb("condB", [128, 8, 128], BF16)
        c["eps"] = kb.sb("epsc", [128, 1], F32)
        c["one"] = kb.sb("onec", [128, 1], F32)
        q = "sp"
        kb.dma(q, c["idf"][:], self.ident[:, :], writes=[c["idf"]], grp="c")
        kb.dma(q, c["cm"][:], self.cmask[:, :, :], writes=[c["cm"]], grp="c")
        kb.dma(q, c["sel"][:], self.sel[:, :, :], writes=[c["sel"]], grp="c")
        kb.dma(q, c["blk1f"][:], self.blk1[:, :], writes=[c["blk1f"]], grp="c")
        kb.dma(q, c["flag"][:], self.flag[:, :], writes=[c["flag"]], grp="c")
        kb.dma(q, c["cv"][:], self.cvec[:, :], writes=[c["cv"]], grp="c")
        kb.cp("dve", c["idb"][:], c["idf"][:], [c["idf"]], [c["idb"]])
        kb.cp("dve", c["blk1"][:], c["blk1f"][:], [c["blk1f"]], [c["blk1"]])
        kb.cp("dve", c["i64b"][:], c["cm"][:, 2, :], [c["cm"]], [c["i64b"]])
        kb.op("dve", lambda e: e.memset(c["ones"][:], 1.0), writes=[c["ones"]])
        kb.op("dve", lambda e: e.memset(c["eps"][:], EPS), writes=[c["eps"]])
        kb.op("dve", lambda e: e.memset(c["one"][:], 1.0), writes=[c["one"]])
        kb.act(c["cond"][:], c["cv"][:], AF.Silu, [c["cv"]], [c["cond"]])
        kb.cp("dve", c["condB"][:], bc(c["cond"][:].unsqueeze(2), [128, 8, 128]), [c["cond"]], [c["condB"]])
        self.mods = kb.sb("mods", [128, 3, D], F32)

    def adaln(self, l, part):
        kb, c = self.kb, self.c
        kb.push()
        self.wbuf = [kb.sb("wbuf%d_%d_%d" % (i, l, part), [128, 8, 512], BF16) for i in range(6)]
        rowts = [kb.sb("rwt%d_%d_%d" % (i, l, part), [128, 512], F32) for i in range(6)]
        self.rowt2 = kb.sb("rowt2_%d_%d" % (l, part), [128, D], F32)
        for n in range(part * 6, part * 6 + 6):
            kb.dma("pool", self.wbuf[n % 6][:], self.adaw[l, :, n * 512:(n + 1) * 512].rearrange("(k p) n -> p k n", p=128),
                   writes=[self.wbuf[n % 6]], grp="w")
            kb.dma("sp", rowts[n % 6][:], bc(self.adab[l:l + 1, n * 512:(n + 1) * 512], [128, 512]),
                   writes=[rowts[n % 6]], grp="c")
        for n in range(part * 6, part * 6 + 6):
            wb = self.wbuf[n % 6]
            self.rowt = rowts[n % 6]
            p = kb.ps()
            for k in range(8):
                kb.mm(p, p[:, :], c["condB"][:, k, :], wb[:, k, :], [c["condB"], wb], start=(k == 0), stop=(k == 7))
            kb.tt("dve", self.mods[:, (n // 2) % 3, (n % 2) * 512:(n % 2) * 512 + 512], p[:, :], self.rowt[:], ALU.add,
                  [p, self.rowt], [self.mods])
        w = self.nmw if part == 0 else self.nfw
        kb.dma("sp", self.rowt2[:], bc(w[l:l + 1, :], [128, D]), writes=[self.rowt2], grp="c")
        kb.stt("dve", self.mods[:, 1, :], self.mods[:, 1, :], 1.0, self.rowt2[:], ALU.add, ALU.mult,
               [self.mods, self.rowt2], [self.mods])
        kb.pop()

    def norm_tile(self, xt_ap, xt_tl, ia, ib, hnT_ap, hnT_tl, eng2="dve"):
        kb, c = self.kb, self.c
        s = self.scr
        kb.act(s["junk"][:], xt_ap, AF.Square, [xt_tl], [s["junk"], s["ss"]], accum_out=s["ss"][:])
        kb.act(s["rs"][:], s["ss"][:], AF.Sqrt, [s["ss"], c["eps"]], [s["rs"]], scale=1.0 / D, bias=c["eps"][:, 0:1])
        kb.op("dve", lambda e: e.reciprocal(out=s["rs"][:], in_=s["rs"][:]), reads=[s["rs"]], writes=[s["rs"]])
        kb.stt("dve", s["t1"][:], xt_ap, s["rs"][:, 0:1], self.mods[:, ia, :], ALU.mult, ALU.mult,
               [xt_tl, s["rs"], self.mods], [s["t1"]])
        kb.tt(eng2, s["hn"][:], s["t1"][:], self.mods[:, ib, :], ALU.add, [s["t1"], self.mods], [s["hn"]])
        p = kb.ps()
        pb = p[:, :].bitcast(BF16)
        for k in range(8):
            kb.tr(p, pb[:, k * 128:(k + 1) * 128], s["hn"][:, k * 128:(k + 1) * 128], c["idb"][:], [s["hn"], c["idb"]])
        kb.cp("act", hnT_ap, pb[:, 0:1024].rearrange("p (k t) -> p k t", k=8), [p], [hnT_tl])

    def alloc_scr(self):
        kb = self.kb
        s = {}
        self.scr = s
        s["junk"] = kb.sb("junk", [128, D], BF16)
        s["ss"] = kb.sb("ss", [128, 1], F32)
        s["rs"] = kb.sb("rs", [128, 1], F32)
        s["t1"] = kb.sb("t1", [128, D], F32)
        s["hn"] = kb.sb("hn", [128, D], BF16)
        self.xin = [kb.sb("xin%d" % i, [128, D], F32) for i in range(2)]

    def mixer0(self):
        kb, c = self.kb, self.c
        sb = kb.sb
        kb.push()
        w0 = sb("w0", [128, 8, NC0], BF16)
        for i in range(0, NC0, 512):
            j = min(i + 512, NC0)
            kb.dma("pool", w0[:, :, i:j], self.win0[:, i:j].rearrange("(k p) n -> p k n", p=128), writes=[w0], grp="w")
        cw = sb("cw", [128, 6, 4], F32)
        kb.dma("sp", cw[:], self.cw0[:, :, :], writes=[cw], grp="c")
        dnsm = sb("dnsmc", [4, 2], F32)
        kb.dma("sp", dnsm[:], self.dnsm[:, :], writes=[dnsm], grp="c")
        nega = sb("nega", [4, 1], F32)
        kb.act(nega[:], dnsm[:, 0:1], AF.Exp, [dnsm], [nega])
        kb.ts("dve", nega[:], nega[:], -1.0, None, ALU.mult, None, [nega], [nega])
        dnw = sb("dnwc", [128, 1], F32)
        kb.dma("sp", dnw[:], self.dnw[:, :], writes=[dnw], grp="c")
        sinkE = sb("sinkE", [128, 2], F32)
        kb.dma("sp", sinkE[:], self.sinkl[:, :], writes=[sinkE], grp="c")
        kb.act(sinkE[:], sinkE[:], AF.Exp, [sinkE], [sinkE])
        biasm = sb("biasm", [128, 2, 512], F32)
        am = sb("am", [128, 2, 128], F32)
        kb.dma("sp", biasm[:], self.biasg.h.ap().rearrange("a p n -> p a n"), writes=[biasm], grp="c")
        kb.dma("sp", am[:], self.amask.h.ap().rearrange("a p n -> p a n"), writes=[am], grp="c")
        for a in range(2):
            kb.tt("dve", biasm[:, a, :].rearrange("p (s q) -> p s q", s=4),
                  biasm[:, a, :].rearrange("p (s q) -> p s q", s=4),
                  bc(am[:, a, :].unsqueeze(1), [128, 4, 128]), ALU.add, [biasm, am], [biasm])
        rmask = sb("rmask", [4, 8, 64], F32)
        kb.op("dve", lambda e: e.memset(rmask[:], 1.0), writes=[rmask])
        kb.op("dve", lambda e: e.memset(rmask[:, :, 0:1], 0.0), writes=[rmask])

        mU8 = sb("mU8", [128, 8, 64], F32)
        mL8 = sb("mL8", [128, 8, 64], F32)
        kb.cp("dve", mU8[:], bc(c["cm"][:, 0, :].unsqueeze(1), [128, 8, 64]), [c["cm"]], [mU8])
        kb.cp("dve", mL8[:], bc(c["cm"][:, 1, :].unsqueeze(1), [128, 8, 64]), [c["cm"]], [mL8])
        hnT = sb("hnT0", [128, 8, 512], BF16)
        qaT = sb("qaT", [128, 2, 512], BF16)
        kaT = sb("kaT", [128, 2, 128 + T], BF16)
        vat = sb("vat", [128, 33, 64], BF16)
        kb.op("pool", lambda e: e.memset(kaT[:], 0.0), writes=[kaT])
        kb.op("dve", lambda e: e.memset(vat[:, 0, :], 0.0), writes=[vat])
        xpre = [sb("xpre%d" % i, [128, 3 + 512], F32) for i in range(6)]
        for i in range(6):
            kb.op("pool", lambda e, i=i: e.memset(xpre[i][:, 0:3], 0.0), writes=[xpre[i]])
        gates = sb("gates", [128, 2, 512], F32)
        tl = sb("tl", [128, 512], F32)
        cacc = sb("cacc", [128, 512], F32)
        ysil = sb("ysil", [128, 512], F32)
        sqb = sb("sqb", [128, 512], BF16)
        rstd = sb("rstd", [128, 512], F32)
        qn = [sb("qn%d" % j, [128, 512], BF16) for j in range(2)]
        qnf = [sb("qnf%d" % j, [128, 512], F32) for j in range(2)]
        i8f = sb("i8f", [128, 8, 64], F32)
        kb.cp("dve", i8f[:], bc(c["cm"][:, 2, :].unsqueeze(1), [128, 8, 64]), [c["cm"]], [i8f])
        A32 = sb("A32", [128, 8, 64], F32)
        Xs = sb("Xs", [128, 4, 64], F32)
        kn = [sb("kn%d" % j, [128, 512], BF16) for j in range(2)]
        vT = [sb("vT%d" % j, [128, 512], BF16) for j in range(2)]
        bt = sb("bt", [4, 512], F32)
        gt = sb("gt", [4, 512], F32)
        Gs = sb("Gs", [4, 512], F32)
        Es = sb("Es", [4, 512], F32)
        BEs = sb("BEs", [4, 512], F32)
        DKs = sb("DKs", [4, 512], F32)
        nbt = gt
        tk4 = sb("tk4", [128, 5, 8, 4], F32)
        TK = sb("TK", [128, 5, 8, 2], F32)
        EGLc = sb("EGLc", [128, 2, 8], F32)
        t0 = sb("t0", [128, 8, 64], F32)
        tU = sb("tU", [128, 8, 64], F32)
        tL = sb("tL", [128, 8, 64], F32)
        Du = tU
        Dl = tL
        tmpf = t0
        NTp = [sb("NTp%d" % i, [128, 8, 64], BF16) for i in range(2)]
        Np = [sb("Np%d" % i, [128, 8, 64], BF16) for i in range(2)]
        Am = sb("Am", [128, 8, 64], BF16)
        KV = sb("KV", [128, 8, 128], BF16)
        KQ = sb("KQ", [128, 8, 128], BF16)
        Wu = sb("Wu", [128, 8, 128], BF16)
        qdec = sb("qdec", [128, 8, 64], F32)
        UT = [sb("UT%d" % j, [128, 8, 64], BF16) for j in range(2)]
        RT = [sb("RT%d" % j, [128, 8, 64], BF16) for j in range(2)]
        O0 = [sb("O0%d" % j, [128, 8, 64], F32) for j in range(2)]
        Qs = [sb("Qs%d" % j, [128, 8, 64], F32) for j in range(2)]
        S32 = [sb("S32_%d" % j, [128, 64], F32) for j in range(2)]
        Sbf = [sb("Sbf_%d" % j, [128, 64], BF16) for j in range(2)]
        pre = [sb("pre%d" % j, [128, 64], F32) for j in range(2)]
        cS = [sb("cS%d" % j, [128, 64], F32) for j in range(2)]
        oT = sb("oT", [128, 8, 64], F32)
        yo = sb("yo", [128, 512], BF16)
        for j in range(2):
            kb.op("dve", lambda e, j=j: e.memset(S32[j][:], 0.0), writes=[S32[j]])
            kb.op("dve", lambda e, j=j: e.memset(Sbf[j][:], 0.0), writes=[Sbf[j]])
        PT = [sb("PT%d" % i, [128, 4, 128], BF16) for i in range(2)]
        den = sb("den", [128, 2, 128], F32)
        ao = sb("ao", [128, 2, 128], BF16)
        H = (slice(0, 64), slice(64, 128))

        for tg in range(self.ntg):
            for tt in range(4):
                xt = self.xin[tt % 2]
                r0 = tg * 512 + tt * 128
                kb.dma("sp", xt[:], self.xfull[r0:r0 + 128, :], writes=[xt], grp="x")
                self.norm_tile(xt[:], xt, 1, 0, hnT[:, :, tt * 128:(tt + 1) * 128], hnT)
            for ci in range(11):
                p = kb.ps()
                for k in range(8):
                    kb.mm(p, p[:, :], w0[:, k, ci * 128:(ci + 1) * 128], hnT[:, k, :], [w0, hnT], start=(k == 0), stop=(k == 7))
                if ci < 2:
                    kb.act(qaT[:, ci, :], p[:, :], AF.Copy, [p], [qaT], scale=0.125)
                elif ci == 2:
                    for q in range(2):
                        kb.cp("act", kaT[H[q], q, 128 + tg * 512:128 + (tg + 1) * 512], p[H[q], :], [p], [kaT])
                elif ci < 9:
                    xp = xpre[ci - 3]
                    if tg > 0:
                        kb.cp("pool", xp[:, 0:3], xp[:, 512:515], [xp], [xp])
                    kb.cp("act", xp[:, 3:515], p[:, :], [p], [xp])
                else:
                    kb.act(gates[:, ci - 9, :], p[:, :], AF.Silu, [p], [gates])
            for tt in range(4):
                p = kb.ps()
                for k in range(8):
                    kb.mm(p, p[:, 0:64], hnT[:, k, tt * 128:(tt + 1) * 128], w0[:, k, 1408:1472], [w0, hnT], start=(k == 0), stop=(k == 7))
                kb.cp("act", vat[:, 1 + tg * 4 + tt, :], p[:, 0:64], [p], [vat])
            pb_ = kb.ps()
            pd_ = kb.ps()
            for k in range(8):
                kb.mm(pb_, pb_[0:4, :], w0[:, k, 1472:1476], hnT[:, k, :], [w0, hnT], start=(k == 0), stop=(k == 7))
            for k in range(8):
                kb.mm(pd_, pd_[0:4, :], w0[:, k, 1476:1480], hnT[:, k, :], [w0, hnT], start=(k == 0), stop=(k == 7))
            kb.act(bt[:], pb_[0:4, :], AF.Sigmoid, [pb_], [bt])
            kb.act(gt[:], pd_[0:4, :], AF.Exp, [pd_, dnsm], [gt], bias=dnsm[:, 1:2])
            kb.act(gt[:], gt[:], AF.Ln, [gt, c["one"]], [gt], bias=c["one"][0:4, 0:1])
            kb.ts("dve", gt[:], gt[:], nega[:, 0:1], None, ALU.mult, None, [gt, nega], [gt])
            kb.op("dve", lambda e: e.tensor_tensor_scan(out=Gs[:], data0=rmask[:].rearrange("p a b -> p (a b)"), data1=gt[:],
                                                        initial=0.0, op0=ALU.mult, op1=ALU.add), reads=[rmask, gt], writes=[Gs])
            kb.act(Es[:], Gs[:], AF.Exp, [Gs], [Es])
            kb.tt("dve", BEs[:], bt[:], Es[:], ALU.mult, [bt, Es], [BEs])
            G3 = Gs[:].rearrange("p (a b) -> p a b", a=8)
            kb.tt("dve", DKs[:].rearrange("p (a b) -> p a b", a=8), G3, bc(G3[:, :, 63:64], [4, 8, 64]), ALU.subtract, [Gs], [DKs])
            kb.act(DKs[:], DKs[:], AF.Exp, [DKs], [DKs], scale=-1.0)
            kb.ts("dve", nbt[:], bt[:], -1.0, None, ALU.mult, None, [bt], [nbt])
            p = kb.ps()
            for qi, X in enumerate((Gs, nbt, BEs, DKs, bt)):
                for n in range(8):
                    for q in range(2):
                        kb.mm(p, p[H[q], (qi * 8 + n) * 4:(qi * 8 + n) * 4 + 4], X[:, n * 64:(n + 1) * 64], c["idf"][0:4, 0:4],
                              [X, c["idf"]], tp=(0, 64 * q))
            kb.cp("dve", tk4[:].rearrange("p a n h -> p (a n h)"), p[:, 0:160], [p], [tk4])
            for q in range(2):
                kb.cp("dve", TK[H[q], :, :, :], tk4[H[q], :, :, 2 * q:2 * q + 2], [tk4], [TK])
            p = kb.ps()
            for j in range(2):
                kb.mm(p, p[:, j * 8:j * 8 + 8], c["sel"][:, j, :], Es[:].rearrange("p (a b) -> p a b", a=8)[:, :, 63], [c["sel"], Es])
            kb.cp("dve", EGLc[:].rearrange("p j n -> p (j n)"), p[:, 0:16], [p], [EGLc])

            def gen_attn():
                for qb in range(4):
                    n = tg * 4 + qb
                    kbs = [n - 1, n] if n > 0 else [n]
                    for ki, kbk in enumerate(kbs):
                        sel_ = 0 if kbk == n - 1 else 1
                        pl = kb.ps()
                        for q in range(2):
                            for j in range(2):
                                sl = q * 2 + j
                                kb.mm(pl, pl[:, sl * 128:(sl + 1) * 128], kaT[:, q, 128 + kbk * 128:128 + (kbk + 1) * 128],
                                      qaT[:, j, qb * 128:(qb + 1) * 128], [kaT, qaT])
                        kb.tt("dve", tl[:], pl[:, :], biasm[:, sel_, :], ALU.add, [pl, biasm], [tl])
                        kb.act(PT[ki][:].rearrange("p s q -> p (s q)"), tl[:], AF.Exp, [tl], [PT[ki]])
                        yield
                    po = kb.ps()
                    pdn = kb.ps()
                    for q in range(2):
                        for j in range(2):
                            sl = q * 2 + j
                            for ki, kbk in enumerate(kbs):
                                kb.mm(po, po[H[q], j * 128:(j + 1) * 128], vat[:, 1 + kbk, :], PT[ki][:, sl, :], [vat, PT[ki]],
                                      start=(ki == 0), stop=(ki == len(kbs) - 1), tp=(0, 64 * q))
                            for ki, kbk in enumerate(kbs):
                                kb.mm(pdn, pdn[H[q], j * 128:(j + 1) * 128], c["ones"][:, 0:64], PT[ki][:, sl, :], [c["ones"], PT[ki]],
                                      start=(ki == 0), stop=(ki == len(kbs) - 1), tp=(0, 64 * q))
                    kb.tt("dve", den[:], pdn[:, 0:256].rearrange("p (j t) -> p j t", j=2), bc(sinkE[:].unsqueeze(2), [128, 2, 128]),
                          ALU.add, [pdn, sinkE], [den])
                    kb.op("dve", lambda e: e.reciprocal(out=den[:], in_=den[:]), reads=[den], writes=[den])
                    kb.tt("dve", ao[:], po[:, 0:256].rearrange("p (j t) -> p j t", j=2), den[:], ALU.mult, [po, den], [ao])
                    for j in range(2):
                        kb.dma("sp", self.y0s[0][j * 128:(j + 1) * 128, n * 128:(n + 1) * 128], ao[:, j, :], reads=[ao], writes=[self.y0s[0]], grp="y")
                    yield

            def gen_dn():
                for ci in range(6):
                    xp = xpre[ci]
                    j = ci % 2
                    kind = ci // 2
                    kb.ts("dve", cacc[:], xp[:, 0:512], cw[:, ci, 0:1], None, ALU.mult, None, [xp, cw], [cacc])
                    for k in range(1, 4):
                        kb.stt("dve", cacc[:], xp[:, k:k + 512], cw[:, ci, k:k + 1], cacc[:], ALU.mult, ALU.add, [xp, cw, cacc], [cacc])
                    if kind == 2:
                        kb.act(vT[j][:], cacc[:], AF.Silu, [cacc], [vT[j]])
                        yield
                        continue
                    kb.act(ysil[:], cacc[:], AF.Silu, [cacc], [ysil])
                    kb.act(sqb[:], ysil[:], AF.Square, [ysil], [sqb])
                    p = kb.ps()
                    kb.mm(p, p[:, :], c["blk1"][:], sqb[:], [c["blk1"], sqb])
                    kb.act(rstd[:], p[:, :], AF.Sqrt, [p, c["eps"]], [rstd], bias=c["eps"][:, 0:1])
                    kb.op("dve", lambda e: e.reciprocal(out=rstd[:], in_=rstd[:]), reads=[rstd], writes=[rstd])
                    if kind == 0:
                        kb.stt("dve", qnf[j][:], ysil[:], 0.125, rstd[:], ALU.mult, ALU.mult, [ysil, rstd], [qnf[j]])
                        kb.cp("act", qn[j][:], qnf[j][:], [qnf[j]], [qn[j]])
                    else:
                        kb.tt("dve", kn[j][:], ysil[:], rstd[:], ALU.mult, [ysil, rstd], [kn[j]])
                    yield

                for j in range(2):
                    k3 = kn[j][:].rearrange("p (n c) -> p n c", n=8)
                    q3 = qn[j][:].rearrange("p (n c) -> p n c", n=8)
                    v3 = vT[j][:].rearrange("p (n c) -> p n c", n=8)
                    pk = kb.ps()
                    pv = kb.ps()
                    pkb = pk[:, :].rearrange("p (n x) -> p n x", n=8)
                    pvb = pv[:, :].rearrange("p (n x) -> p n x", n=8)
                    for n in range(8):
                        for q in range(2):
                            kb.mm(pk, pkb[H[q], n, :], k3[H[q], n, :], c["idb"][H[q], 64 * q:64 * q + 64], [kn[j], c["idb"]], tp=(64 * q, 64 * q))
                    for n in range(8):
                        for q in range(2):
                            kb.mm(pv, pvb[H[q], n, :], v3[H[q], n, :], c["idb"][H[q], 64 * q:64 * q + 64], [vT[j], c["idb"]], tp=(64 * q, 64 * q))
                    kb.tt("dve", KQ[:, :, 0:64], bc(TK[:, 3, :, j:j + 1], [128, 8, 64]), pkb, ALU.mult, [pk, TK], [KQ])
                    kb.tt("dve", KV[:, :, 0:64], bc(TK[:, 2, :, j:j + 1], [128, 8, 64]), pkb, ALU.mult, [pk, TK], [KV])
                    kb.tt("dve", KV[:, :, 64:128], bc(TK[:, 4, :, j:j + 1], [128, 8, 64]), pvb, ALU.mult, [pv, TK], [KV])
                    yield
                    pg = kb.ps()
                    pe_ = kb.ps()
                    kb.mm(pg, pg[:, :], c["sel"][:, j, :], Gs[:], [c["sel"], Gs])
                    kb.mm(pe_, pe_[:, :], c["sel"][:, j, :], Es[:], [c["sel"], Es])
                    pg3 = pg[:, :].rearrange("p (n c) -> p n c", n=8)
                    kb.tt("dve", t0[:], pg3, bc(TK[:, 0, :, j:j + 1], [128, 8, 64]), ALU.subtract, [pg, TK], [t0])
                    kb.tt("pool", tU[:], t0[:], mU8[:], ALU.add, [t0, mU8], [tU])
                    kb.stt("dve", tL[:], t0[:], -1.0, mL8[:], ALU.mult, ALU.add, [t0, mL8], [tL])
                    kb.act(Du[:], tU[:], AF.Exp, [tU], [Du])
                    kb.act(Dl[:], tL[:], AF.Exp, [tL], [Dl])
                    kb.cp("act", cacc[:], pe_[:, :], [pe_], [cacc])
                    kb.tt("dve", qdec[:].rearrange("p n c -> p (n c)"), cacc[:], qnf[j][:], ALU.mult, [qnf[j], cacc], [qdec])
                    yield
                    pkk = kb.ps()
                    pqk = kb.ps()
                    kk3 = pkk[:, :].rearrange("p (n c) -> p n c", n=8)
                    qk3 = pqk[:, :].rearrange("p (n c) -> p n c", n=8)
                    for n in range(8):
                        for q in range(2):
                            kb.mm(pkk, kk3[H[q], n, :], k3[H[q], n, :], k3[H[q], n, :], [kn[j]], tp=(64 * q, 64 * q))
                    for n in range(8):
                        for q in range(2):
                            kb.mm(pqk, qk3[H[q], n, :], k3[H[q], n, :], q3[H[q], n, :], [kn[j], qn[j]], tp=(64 * q, 64 * q))
                    kb.tt("dve", tmpf[:], bc(TK[:, 1, :, j:j + 1], [128, 8, 64]), Dl[:], ALU.mult, [TK, Dl], [tmpf])
                    kb.cp("act", tl[:], pkk[:, :], [pkk], [tl])
                    kb.tt("dve", NTp[0][:], tl[:].rearrange("p (n c) -> p n c", n=8), tmpf[:], ALU.mult, [tl, tmpf], [NTp[0]])
                    kb.cp("act", cacc[:], pqk[:, :], [pqk], [cacc])
                    kb.tt("dve", KQ[:, :, 64:128], cacc[:].rearrange("p (n c) -> p n c", n=8), Du[:], ALU.mult, [cacc, Du], [KQ])
                    yield
                    pn = kb.ps()
                    pnb = pn[:, :].rearrange("p (n c) -> p n c", n=8)
                    for n in range(8):
                        for q in range(2):
                            kb.mm(pn, pnb[H[q], n, :], NTp[0][H[q], n, :], c["idb"][H[q], 64 * q:64 * q + 64], [NTp[0], c["idb"]], tp=(64 * q, 64 * q))
                    kb.cp("act", Np[0][:], pnb, [pn], [Np[0]])
                    kb.cp("act", A32[:], pnb, [pn], [A32])
                    kb.tt("pool", A32[:], A32[:], i8f[:], ALU.add, [A32, i8f], [A32])
                    kb.cp("act", Am[:], A32[:], [A32], [Am])
                    cur = 0
                    for lev in range(5):
                        nxt = 1 - cur
                        if lev < 4:
                            p1 = kb.ps()
                            p13 = p1[:, :].rearrange("p (n c) -> p n c", n=8)
                            for n in range(8):
                                for q in range(2):
                                    kb.mm(p1, p13[H[q], n, :], NTp[cur][H[q], n, :], Np[cur][H[q], n, :], [NTp[cur], Np[cur]], tp=(64 * q, 64 * q))
                        p2 = kb.ps()
                        p23 = p2[:, :].rearrange("p (n c) -> p n c", n=8)
                        for n in range(8):
                            for q in range(2):
                                kb.mm(p2, p23[H[q], n, :], Np[cur][H[q], n, :], NTp[cur][H[q], n, :], [NTp[cur], Np[cur]], tp=(64 * q, 64 * q))
                        if lev < 4:
                            kb.cp("act", Np[nxt][:], p13, [p1], [Np[nxt]])
                        kb.cp("act", NTp[nxt][:], p23, [p2], [NTp[nxt]])
                        p3 = kb.ps()
                        p33 = p3[:, :].rearrange("p (n c) -> p n c", n=8)
                        for n in range(8):
                            for q in range(2):
                                kb.mm(p3, p33[H[q], n, :], NTp[nxt][H[q], n, :], Am[H[q], n, :], [NTp[nxt], Am], tp=(64 * q, 64 * q))
                        kb.cp("act", tl[:], p3[:, :], [p3], [tl])
                        kb.tt("dve", A32[:], tl[:].rearrange("p (n c) -> p n c", n=8), A32[:], ALU.add, [A32, tl], [A32])
                        kb.cp("act", Am[:], A32[:], [A32], [Am])
                        cur = nxt
                        yield
                    yield
                    for hf in range(2):
                        pw = kb.ps()
                        pw3 = pw[:, :].rearrange("p (n c) -> p n c", n=4)
                        for n in range(4):
                            for q in range(2):
                                kb.mm(pw, pw3[H[q], n, :], Am[H[q], hf * 4 + n, :], KV[H[q], hf * 4 + n, :], [Am, KV], tp=(64 * q, 64 * q))
                        kb.cp("act", Wu[:, hf * 4:hf * 4 + 4, :], pw3, [pw], [Wu])
                    yield
                    for hf in range(2):
                        pu = kb.ps()
                        pu3 = pu[:, :].rearrange("p (n c) -> p n c", n=4)
                        for n in range(4):
                            for q in range(2):
                                kb.mm(pu, pu3[H[q], n, :], Wu[H[q], hf * 4 + n, 0:64], KQ[H[q], hf * 4 + n, :], [Wu, KQ], tp=(64 * q, 64 * q))
                        kb.cp("act", UT[j][:, hf * 4:hf * 4 + 4, :], pu3[:, :, 0:64], [pu], [UT[j]])
                        kb.cp("act", Xs[:], pu3[:, :, 64:128], [pu], [Xs])
                        kb.tt("pool", RT[j][:, hf * 4:hf * 4 + 4, :], qdec[:, hf * 4:hf * 4 + 4, :], Xs[:], ALU.subtract, [qdec, Xs], [RT[j]])
                    po0 = kb.ps()
                    pq_ = kb.ps()
                    po03 = po0[:, :].rearrange("p (n c) -> p n c", n=8)
                    pq3 = pq_[:, :].rearrange("p (n c) -> p n c", n=8)
                    for n in range(8):
                        for q in range(2):
                            kb.mm(po0, po03[H[q], n, :], Wu[H[q], n, 64:128], KQ[H[q], n, 64:128], [Wu, KQ], tp=(64 * q, 64 * q))
                    for n in range(8):
                        for q in range(2):
                            kb.mm(pq_, pq3[H[q], n, :], KQ[H[q], n, 0:64], Wu[H[q], n, 64:128], [Wu, KQ], tp=(64 * q, 64 * q))
                    kb.cp("act", O0[j][:], po03, [po0], [O0[j]])
                    kb.cp("act", Qs[j][:], pq3, [pq_], [Qs[j]])
                    yield

            alive = [gen_attn(), gen_dn()]
            while alive:
                for g_ in list(alive):
                    try:
                        next(g_)
                    except StopIteration:
                        alive.remove(g_)

            pOs = [kb.pst[6], kb.pst[7]]
            pO3s = [pp[:, :].rearrange("p (n c) -> p n c", n=8) for pp in pOs]
            for n in range(8):
                for j in range(2):
                    for q in range(2):
                        kb.mm(pOs[j], pO3s[j][H[q], n, :], Sbf[j][H[q], :], RT[j][H[q], n, :], [Sbf[j], RT[j]], tp=(64 * q, 64 * q))
                    pS = kb.ps()
                    for q in range(2):
                        kb.mm(pS, pS[H[q], 0:64], UT[j][H[q], n, :], Sbf[j][H[q], :], [Sbf[j], UT[j]], tp=(64 * q, 64 * q))
                    kb.stt("dve", pre[j][:], S32[j][:], EGLc[:, j, n:n + 1], Qs[j][:, n, :], ALU.mult, ALU.add, [S32[j], EGLc, Qs[j]], [pre[j]])
                    kb.cp("act", cS[j][:], pS[:, 0:64], [pS], [cS[j]])
                    kb.tt("dve", S32[j][:], pre[j][:], cS[j][:], ALU.subtract, [pre[j], cS[j]], [S32[j]])
                    kb.cp("act", Sbf[j][:], S32[j][:], [S32[j]], [Sbf[j]])
            for j in range(2):
                pO = pOs[j]
                pO3 = pO3s[j]
                kb.cp("act", oT[:], pO3, [pO], [oT])
                kb.tt("dve", oT[:], oT[:], O0[j][:], ALU.add, [oT, O0[j]], [oT])
                o2 = oT[:].rearrange("p n c -> p (n c)")
                kb.act(sqb[:], o2, AF.Square, [oT], [sqb])
                p = kb.ps()
                kb.mm(p, p[:, :], c["blk1"][:], sqb[:], [c["blk1"], sqb])
                kb.act(rstd[:], p[:, :], AF.Sqrt, [p, c["eps"]], [rstd], scale=1.0 / 64, bias=c["eps"][:, 0:1])
                kb.op("dve", lambda e: e.reciprocal(out=rstd[:], in_=rstd[:]), reads=[rstd], writes=[rstd])
                kb.stt("dve", ysil[:], o2, dnw[:, 0:1], rstd[:], ALU.mult, ALU.mult, [oT, dnw, rstd], [ysil])
                kb.tt("dve", yo[:], ysil[:], gates[:, j, :], ALU.mult, [ysil, gates], [yo])
                kb.dma("sp", self.y0s[1][j * 128:(j + 1) * 128, tg * 512:(tg + 1) * 512], yo[:], reads=[yo], writes=[self.y0s[1]], grp="y")
                if self.debug:
                    kb.dma("sp", self.dbg_o[j * 128:(j + 1) * 128, tg * 512:(tg + 1) * 512], o2, reads=[oT], writes=[self.dbg_o], grp="y")

    def gather(self, snd, gth):
        for a, b in zip(snd, gth):
            self.kb.collective("AllGather", self.groups, a, b, a.h.ap()[:, :], b.h.ap()[:, :])

    def outproj(self, gth, nk, wout):
        kb, c = self.kb, self.c
        sb = kb.sb
        kb.push()
        wo = sb("wo_%d" % nk, [128, nk, D], BF16)
        stg = [sb("wostg%d_%d" % (i, nk), [128, D], F32) for i in range(2)]
        for k in range(nk):
            st = stg[k % 2]
            kb.dma("sp", st[:], wout[k * 128:(k + 1) * 128, :], writes=[st])
            kb.tt("dve", wo[:, k, :], st[:], self.mods[:, 2, :], ALU.mult, [st, self.mods], [wo])
        ya = [sb("ya%d_%d" % (i, nk), [128, nk, 128], BF16) for i in range(2)]
        yb_ = [sb("yb%d_%d" % (i, nk), [128, nk, 128], BF16) for i in range(2)]
        ytmp = sb("ytmp_%d" % nk, [128, nk, 128], F32)
        yown = [sb("yown%d_%d" % (i, nk), [128, nk, 128], BF16) for i in range(2)]
        nb = len(gth)
        kpb = nk // nb
        for tt in range(16):
            A, B, Y = ya[tt % 2], yb_[tt % 2], yown[tt % 2]
            for b in range(nb):
                g3 = gth[b].h.ap().rearrange("(k p) t -> p k t", p=128)
                kb.dma("sp", A[:, b * kpb:(b + 1) * kpb, :], g3[:, :, tt * 128:(tt + 1) * 128], reads=[gth[b]], writes=[A])
                kb.dma("sp", B[:, b * kpb:(b + 1) * kpb, :], g3[:, :, TH + tt * 128:TH + (tt + 1) * 128], reads=[gth[b]], writes=[B])
            kb.ts("dve", ytmp[:], A[:], c["flag"][:, 0:1], None, ALU.mult, None, [A, c["flag"]], [ytmp])
            kb.stt("dve", Y[:], B[:], c["flag"][:, 1:2], ytmp[:], ALU.mult, ALU.add, [B, c["flag"], ytmp], [Y])
            for hf in range(2):
                p = kb.ps()
                for k in range(nk):
                    kb.mm(p, p[:, :], Y[:, k, :], wo[:, k, hf * 512:(hf + 1) * 512], [Y, wo], start=(k == 0), stop=(k == nk - 1))
                xr = self.xres[tt]
                kb.tt("dve", xr[:, hf * 512:(hf + 1) * 512], p[:, :], xr[:, hf * 512:(hf + 1) * 512], ALU.add, [p, xr], [xr])
        kb.pop()

    def load_xres(self):
        kb = self.kb
        self.xres = [kb.sb("xres%d" % i, [128, D], F32) for i in range(16)]
        for tt in range(16):
            kb.dma("sp", self.xres[tt][:], self.xown[tt * 128:(tt + 1) * 128, :], writes=[self.xres[tt]])

    def norm_own(self, hnT):
        for tt in range(16):
            self.norm_tile(self.xres[tt][:], self.xres[tt], 1, 0, hnT[:, :, tt * 128:(tt + 1) * 128], hnT)

    def ffn_run(self, hnT, specs, gates=None):
        kb, c = self.kb, self.c
        B = self.fb
        groups = []
        for (wg, wu, wd, nff, e) in specs:
            nch = nff // 128
            for gi in range((nch + 1) // 2):
                groups.append((wg, wu, wd, gi * 256, min(2, nch - gi * 2), e))

        def load_gu(it):
            wg, wu, wd, f0, fc, e = groups[it]
            wgb, wub = B["wg"][it % 2], B["wu"][it % 2]
            kb.dma("pool", wgb[:, :, 0:fc * 128], wg[:, f0:f0 + fc * 128].rearrange("(k p) n -> p k n", p=128), writes=[wgb])
            kb.dma("pool", wub[:, :, 0:fc * 128], wu[:, f0:f0 + fc * 128].rearrange("(k p) n -> p k n", p=128), writes=[wub])

        def load_d(it):
            wg, wu, wd, f0, fc, e = groups[it]
            st, wdb = B["stg"][it % 2], B["wd"][it % 2]
            kb.dma("sp", st[:, 0:fc, :], wd[f0:f0 + fc * 128, :].rearrange("(cc p) n -> p cc n", p=128), writes=[st])
            for cc in range(fc):
                kb.tt("pool", wdb[:, cc, :], st[:, cc, :], self.mods[:, 2, :], ALU.mult, [st, self.mods], [wdb])

        def gen_up(it):
            wg, wu, wd, f0, fc, e = groups[it]
            wgb, wub, h1 = B["wg"][it % 2], B["wu"][it % 2], B["h1"][it % 2]
            for cc in range(fc):
                for tg in range(4):
                    pgt = kb.ps()
                    put = kb.ps()
                    for k in range(8):
                        kb.mm(pgt, pgt[:, :], wgb[:, k, cc * 128:(cc + 1) * 128], hnT[:, k, tg * 512:(tg + 1) * 512], [wgb, hnT],
                              start=(k == 0), stop=(k == 7))
                    for k in range(8):
                        kb.mm(put, put[:, :], wub[:, k, cc * 128:(cc + 1) * 128], hnT[:, k, tg * 512:(tg + 1) * 512], [wub, hnT],
                              start=(k == 0), stop=(k == 7))
                    sl = B["sil"][(cc * 4 + tg) % 2]
                    kb.act(sl[:], pgt[:, :], AF.Silu, [pgt], [sl])
                    kb.tt("dve", h1[:, cc, tg * 512:(tg + 1) * 512], put[:, :], sl[:], ALU.mult, [put, sl], [h1])
                    yield

        def gen_down(it):
            wg, wu, wd, f0, fc, e = groups[it]
            wdb, h1 = B["wd"][it % 2], B["h1"][it % 2]
            for tt in range(16):
                xr = self.xres[tt]
                for hf in range(2):
                    p = kb.ps()
                    for cc in range(fc):
                        kb.mm(p, p[:, :], h1[:, cc, tt * 128:(tt + 1) * 128], wdb[:, cc, hf * 512:(hf + 1) * 512], [h1, wdb],
                              start=(cc == 0), stop=(cc == fc - 1))
                    if gates is None:
                        kb.tt("dve", xr[:, hf * 512:(hf + 1) * 512], p[:, :], xr[:, hf * 512:(hf + 1) * 512], ALU.add, [p, xr], [xr])
                    else:
                        ev = B["ev"][(tt * 2 + hf) % 2]
                        kb.act(ev[:], p[:, :], AF.Copy, [p, gates], [ev], scale=gates[:, tt, e:e + 1])
                        kb.tt("dve", xr[:, hf * 512:(hf + 1) * 512], ev[:], xr[:, hf * 512:(hf + 1) * 512], ALU.add, [ev, xr], [xr])
                if tt % 2 == 1:
                    yield

        n = len(groups)
        for it in range(min(2, n)):
            load_gu(it)
            load_d(it)
        for _ in gen_up(0):
            pass
        for it in range(n):
            if it + 2 < n:
                load_gu(it + 2)
            alive = [gen_down(it)]
            if it + 1 < n:
                alive.append(gen_up(it + 1))
            while alive:
                for g_ in list(alive):
                    try:
                        next(g_)
                    except StopIteration:
                        alive.remove(g_)
            if it + 2 < n:
                load_d(it + 2)

    def alloc_ffn(self, tag):
        kb = self.kb
        B = {}
        B["wg"] = [kb.sb("fwg%d_%s" % (i, tag), [128, 8, 256], BF16) for i in range(2)]
        B["wu"] = [kb.sb("fwu%d_%s" % (i, tag), [128, 8, 256], BF16) for i in range(2)]
        B["stg"] = [kb.sb("fst%d_%s" % (i, tag), [128, 2, D], F32) for i in range(2)]
        B["wd"] = [kb.sb("fwd%d_%s" % (i, tag), [128, 2, D], BF16) for i in range(2)]
        B["h1"] = [kb.sb("fh1%d_%s" % (i, tag), [128, 2, TH], BF16) for i in range(2)]
        B["sil"] = [kb.sb("fsil%d_%s" % (i, tag), [128, 512], F32) for i in range(2)]
        B["ev"] = [kb.sb("fev%d_%s" % (i, tag), [128, 512], F32) for i in range(2)]
        self.fb = B
        self.fcnt = 0

    def ffn0(self):
        kb = self.kb
        kb.push()
        hnT = kb.sb("hnTf0", [128, 8, TH], BF16)
        self.norm_own(hnT)
        self.alloc_ffn("f0")
        self.ffn_run(hnT, [(self.wg0.h.ap(), self.wu0.h.ap(), self.wd0.h.ap(), 2816, 0)])
        kb.pop()

    def moe(self):
        kb, c = self.kb, self.c
        kb.push()
        hnT = kb.sb("hnTf1", [128, 8, TH], BF16)
        self.norm_own(hnT)
        rwf = kb.sb("rwf", [128, 8, 8], F32)
        rwb = kb.sb("rwb", [128, 8, 8], BF16)
        kb.dma("sp", rwf[:], self.rw[:, :, :], writes=[rwf])
        kb.cp("dve", rwb[:], rwf[:], [rwf], [rwb])
        rbb = kb.sb("rbb", [128, 8], F32)
        kb.dma("sp", rbb[:], bc(self.rb[0:1, :], [128, 8]), writes=[rbb])
        lg = kb.sb("lg", [128, 16, 8], F32)
        p = kb.ps()
        for tt in range(16):
            for k in range(8):
                kb.mm(p, p[:, tt * 8:(tt + 1) * 8], hnT[:, k, tt * 128:(tt + 1) * 128], rwb[:, k, :], [hnT, rwb], start=(k == 0), stop=(k == 7))
        kb.cp("act", lg[:].rearrange("p a b -> p (a b)"), p[:, 0:128], [p], [lg])
        kb.tt("dve", lg[:], lg[:], bc(rbb[:].unsqueeze(1), [128, 16, 8]), ALU.add, [lg, rbb], [lg])
        m1 = kb.sb("m1", [128, 16], F32)
        m2 = kb.sb("m2", [128, 16], F32)
        eq1 = kb.sb("eq1", [128, 16, 8], F32)
        eq2 = kb.sb("eq2", [128, 16, 8], F32)
        lg2 = kb.sb("lg2", [128, 16, 8], F32)
        w1 = kb.sb("w1", [128, 16], F32)
        w2 = kb.sb("w2", [128, 16], F32)
        gates = kb.sb("gates1", [128, 16, 8], F32)
        kb.op("dve", lambda e: e.reduce_max(out=m1[:], in_=lg[:], axis=AX.X), reads=[lg], writes=[m1])
        kb.tt("dve", eq1[:], lg[:], bc(m1[:].unsqueeze(2), [128, 16, 8]), ALU.is_equal, [lg, m1], [eq1])
        kb.stt("dve", lg2[:], eq1[:], NEG, lg[:], ALU.mult, ALU.add, [eq1, lg], [lg2])
        kb.op("dve", lambda e: e.reduce_max(out=m2[:], in_=lg2[:], axis=AX.X), reads=[lg2], writes=[m2])
        kb.tt("dve", eq2[:], lg2[:], bc(m2[:].unsqueeze(2), [128, 16, 8]), ALU.is_equal, [lg2, m2], [eq2])
        kb.tt("dve", w2[:], m2[:], m1[:], ALU.subtract, [m1, m2], [w2])
        kb.act(w1[:], w2[:], AF.Sigmoid, [w2], [w1], scale=-1.0)
        kb.act(w2[:], w2[:], AF.Sigmoid, [w2], [w2])
        kb.tt("dve", eq1[:], eq1[:], bc(w1[:].unsqueeze(2), [128, 16, 8]), ALU.mult, [eq1, w1], [eq1])
        kb.tt("dve", eq2[:], eq2[:], bc(w2[:].unsqueeze(2), [128, 16, 8]), ALU.mult, [eq2, w2], [eq2])
        kb.tt("dve", gates[:], eq1[:], eq2[:], ALU.add, [eq1, eq2], [gates])
        if self.moeonly:
            kb.dma("sp", self.dbg_g[:, :], gates[:].rearrange("p a b -> p (a b)"), reads=[gates], writes=[self.dbg_g])
        self.alloc_ffn("f1")
        self.ffn_run(hnT, [(self.mwg.h.ap()[e], self.mwu.h.ap()[e], self.mwd.h.ap()[e], 3584, e) for e in range(self.nexp)], gates=gates)
        kb.pop()

    def mixer1(self):
        kb, c = self.kb, self.c
        sb = kb.sb
        kb.push()
        hnT = sb("hnTm1", [128, 8, TH], BF16)
        self.norm_own(hnT)
        for b in range(2):
            kb.dma("sp", self.h1s[b].h.ap().rearrange("(k p) t -> p k t", p=128), hnT[:, 4 * b:4 * b + 4, :], reads=[hnT], writes=[self.h1s[b]])
        kb.pop()
        self.gather(self.h1s, self.h1g)
        kb.push()
        w1 = sb("w1m", [128, 8, NC1], BF16)
        for i in range(0, NC1, 512):
            j = min(i + 512, NC1)
            kb.dma("pool", w1[:, :, i:j], self.win1[:, i:j].rearrange("(k p) n -> p k n", p=128), writes=[w1])
        sm = sb("l1smc", [128, 4, 8], F32)
        kb.dma("sp", sm[:], self.l1sm[:, :, :], writes=[sm])
        scw = sb("scwc", [128, 2, 3], F32)
        kb.dma("sp", scw[:], self.scw[:, :, :], writes=[scw])
        gwf = sb("gwf", [128, 8, 128], F32)
        gwb = sb("gwb", [128, 8, 128], BF16)
        kb.dma("sp", gwf[:, 0:4, :], self.gaw.h.ap().rearrange("n i j -> i n j"), writes=[gwf])
        kb.dma("sp", gwf[:, 4:8, :], self.gxw.h.ap().rearrange("n i j -> i n j"), writes=[gwf])
        kb.cp("dve", gwb[:], gwf[:], [gwf], [gwb])
        cj = sb("cj", [128, 4], F32)
        kb.act(cj[:], sm[:, :, 7], AF.Exp, [sm], [cj], scale=-1.0)
        kb.act(cj[:], cj[:], AF.Ln, [cj, c["one"]], [cj], bias=c["one"][:, 0:1])
        kb.ts("dve", cj[:], cj[:], -8.0, None, ALU.mult, None, [cj], [cj])
        hin = sb("hin1", [128, 8, 512], BF16)
        xp1 = [sb("xp1_%d" % i, [128, 3 + 512], F32) for i in range(4)]
        xp2 = [sb("xp2_%d" % i, [128, 2 + 512], F32) for i in range(2)]
        for t_ in xp1 + xp2:
            kb.op("pool", lambda e, t_=t_: e.memset(t_[:, 0:3], 0.0), writes=[t_])
        hprev = [sb("hprev%d" % i, [128, 1], F32) for i in range(4)]
        for t_ in hprev:
            kb.op("pool", lambda e, t_=t_: e.memset(t_[:], 0.0), writes=[t_])
        TS = []
        for u in range(2):
            t = {}
            for nm in ("xcv", "rg", "ig", "av", "bv", "hs", "yv", "y2", "gl", "cdv"):
                t[nm] = sb("%s_%d" % (nm, u), [128, 512], F32)
            t["xcb"] = sb("xcb_%d" % u, [128, 512], BF16)
            t["yo"] = sb("yo1_%d" % u, [128, 512], BF16)
            TS.append(t)
        g3 = [self.h1g[b].h.ap().rearrange("(r k p) t -> r p k t", r=2, p=128) for b in range(2)]

        def inproj(ci):
            p = kb.ps()
            for k in range(8):
                kb.mm(p, p[:, :], w1[:, k, ci * 128:(ci + 1) * 128], hin[:, k, :], [w1, hin], start=(k == 0), stop=(k == 7))
            return p

        def lru_gen(tg, i, t):
            xcv, rg, ig, av, bv, hs, yv, y2, gl, xcb, yo = (t[k] for k in ("xcv", "rg", "ig", "av", "bv", "hs", "yv", "y2", "gl", "xcb", "yo"))
            p = inproj(i)
            xp = xp1[i]
            if tg > 0:
                kb.cp("pool", xp[:, 0:3], xp[:, 512:515], [xp], [xp])
            kb.cp("act", xp[:, 3:515], p[:, :], [p], [xp])
            yield
            kb.ts("dve", xcv[:], xp[:, 0:512], sm[:, i, 0:1], sm[:, i, 4:5], ALU.mult, ALU.add, [xp, sm], [xcv])
            for k in range(1, 4):
                kb.stt("dve", xcv[:], xp[:, k:k + 512], sm[:, i, k:k + 1], xcv[:], ALU.mult, ALU.add, [xp, sm, xcv], [xcv])
            kb.cp("act", xcb[:], xcv[:], [xcv], [xcb])
            yield
            pr = kb.ps()
            pi = kb.ps()
            kb.mm(pr, pr[:, :], gwb[:, i, :], xcb[:], [gwb, xcb])
            kb.mm(pi, pi[:, :], gwb[:, 4 + i, :], xcb[:], [gwb, xcb])
            kb.act(rg[:], pr[:, :], AF.Sigmoid, [pr, sm], [rg], bias=sm[:, i, 5:6])
            kb.act(ig[:], pi[:, :], AF.Sigmoid, [pi, sm], [ig], bias=sm[:, i, 6:7])
            yield
            kb.act(av[:], rg[:], AF.Exp, [rg, cj], [av], scale=cj[:, i:i + 1])
            kb.tt("pool", bv[:], av[:], av[:], ALU.mult, [av], [bv])
            kb.ts("dve", bv[:], bv[:], -1.0, 1.0, ALU.mult, ALU.add, [bv], [bv])
            kb.act(bv[:], bv[:], AF.Sqrt, [bv], [bv])
            yield
            kb.tt("pool", bv[:], bv[:], ig[:], ALU.mult, [bv, ig], [bv])
            kb.tt("dve", bv[:], bv[:], xcv[:], ALU.mult, [bv, xcv], [bv])
            kb.op("dve", lambda e: e.tensor_tensor_scan(out=hs[:], data0=av[:], data1=bv[:], initial=hprev[i][:, 0:1],
                                                        op0=ALU.mult, op1=ALU.add), reads=[av, bv, hprev[i]], writes=[hs])
            kb.cp("dve", hprev[i][:, 0:1], hs[:, 511:512], [hs], [hprev[i]])
            yield
            p = inproj(4 + i)
            kb.cp("act", yv[:], p[:, :], [p], [yv])
            yield
            kb.tt("pool", y2[:], yv[:], yv[:], ALU.mult, [yv], [y2])
            kb.ts("dve", y2[:], y2[:], 0.044715, 1.0, ALU.mult, ALU.add, [y2], [y2])
            kb.tt("pool", y2[:], y2[:], yv[:], ALU.mult, [y2, yv], [y2])
            kb.act(gl[:], y2[:], AF.Sigmoid, [y2], [gl], scale=1.5957691216057308)
            yield
            kb.tt("pool", gl[:], gl[:], yv[:], ALU.mult, [gl, yv], [gl])
            kb.tt("dve", yo[:], gl[:], hs[:], ALU.mult, [gl, hs], [yo])
            kb.dma("sp", self.y1s[i // 2][(i % 2) * 128:(i % 2 + 1) * 128, tg * 512:(tg + 1) * 512], yo[:], reads=[yo], writes=[self.y1s[i // 2]])

        def sc_gen(tg, i, t):
            cdv, yo = t["cdv"], t["yo"]
            pc = inproj(10 + i)
            ph = inproj(12 + i)
            xp = xp2[i]
            if tg > 0:
                kb.cp("pool", xp[:, 0:2], xp[:, 512:514], [xp], [xp])
            kb.cp("act", cdv[:], pc[:, :], [pc], [cdv])
            kb.tt("dve", xp[:, 2:514], ph[:, :], cdv[:], ALU.mult, [ph, cdv], [xp])
            yield
            kb.ts("dve", cdv[:], xp[:, 0:512], scw[:, i, 0:1], None, ALU.mult, None, [xp, scw], [cdv])
            for k in range(1, 3):
                kb.stt("dve", cdv[:], xp[:, k:k + 512], scw[:, i, k:k + 1], cdv[:], ALU.mult, ALU.add, [xp, scw, cdv], [cdv])
            yield
            pb_ = inproj(8 + i)
            kb.tt("dve", yo[:], pb_[:, :], cdv[:], ALU.mult, [pb_, cdv], [yo])
            kb.dma("sp", self.y1s[2][i * 128:(i + 1) * 128, tg * 512:(tg + 1) * 512], yo[:], reads=[yo], writes=[self.y1s[2]])

        def rr(gens):
            alive = list(gens)
            while alive:
                for g_ in list(alive):
                    try:
                        next(g_)
                    except StopIteration:
                        alive.remove(g_)

        for tg in range(self.ntg):
            for b in range(2):
                kb.dma("sp", hin[:, 4 * b:4 * b + 4, :], g3[b][tg // 4, :, :, (tg % 4) * 512:(tg % 4 + 1) * 512], reads=[self.h1g[b]], writes=[hin])
            rr([lru_gen(tg, 0, TS[0]), lru_gen(tg, 1, TS[1])])
            rr([lru_gen(tg, 2, TS[0]), lru_gen(tg, 3, TS[1])])
            rr([sc_gen(tg, 0, TS[0]), sc_gen(tg, 1, TS[1])])
        kb.pop()

    def final_norm(self):
        kb, c = self.kb, self.c
        s = self.scr
        kb.push()
        fw = kb.sb("fwrow", [128, D], F32)
        kb.dma("sp", fw[:], bc(self.fnw[0:1, :], [128, D]), writes=[fw])
        ot = [kb.sb("ot%d" % i, [128, D], F32) for i in range(2)]
        for tt in range(16):
            xr = self.xres[tt]
            kb.act(s["junk"][:], xr[:], AF.Square, [xr], [s["junk"], s["ss"]], accum_out=s["ss"][:])
            kb.act(s["rs"][:], s["ss"][:], AF.Sqrt, [s["ss"], c["eps"]], [s["rs"]], scale=1.0 / D, bias=c["eps"][:, 0:1])
            kb.op("dve", lambda e: e.reciprocal(out=s["rs"][:], in_=s["rs"][:]), reads=[s["rs"]], writes=[s["rs"]])
            o = ot[tt % 2]
            kb.stt("dve", o[:], xr[:], s["rs"][:, 0:1], fw[:], ALU.mult, ALU.mult, [xr, s["rs"], fw], [o])
            kb.dma("sp", self.out[tt * 128:(tt + 1) * 128, :], o[:], reads=[o], writes=[self.out])
        kb.pop()

    def build(self):
        kb = self.kb
        self.declare()
        if self.debug and self.stop_after != "adaln":
            self.dbg_o = self.outp("dbg_o", [256, T])
            self.dbg_y0 = self.outp("dbg_y0", [512, T], BF16)
        self.consts()
        self.alloc_scr()
        if self.moeonly:
            kb.op("dve", lambda e: e.memset(self.mods[:], 1.0), writes=[self.mods])
            self.load_xres()
            self.moe()
            return self.dump_x()
        if self.m1only:
            kb.op("dve", lambda e: e.memset(self.mods[:], 1.0), writes=[self.mods])
            self.load_xres()
            self.mixer1()
            if self.m1_stop is None:
                self.gather(self.y1s, self.y1g)
                self.outproj(self.y1g, 12, self.wout1)
            return self.dump_x()
        if self.skip_ada:
            kb.op("dve", lambda e: e.memset(self.mods[:], 1.0), writes=[self.mods])
        else:
            self.adaln(0, 0)
        if self.stop_after == "adaln":
            self.dbg_mods = self.outp("dbg_mods", [128, 3 * D])
            kb.dma("sp", self.dbg_mods[:, :], self.mods[:].rearrange("p a b -> p (a b)"), reads=[self.mods], writes=[self.dbg_mods], grp="y")
            self.finish()
            return
        self.mixer0()
        kb.pop()
        if self.stop_after != "mixer0":
            self.gather(self.y0s, self.y0g)
            self.load_xres()
            self.outproj(self.y0g, 8, self.wout0)
            if self.stop_after == "op0":
                return self.dump_x()
            self.adaln(0, 1)
            self.ffn0()
            if self.stop_after == "ffn0":
                return self.dump_x()
            self.adaln(1, 0)
            self.mixer1()
            self.gather(self.y1s, self.y1g)
            self.outproj(self.y1g, 12, self.wout1)
            if self.stop_after == "premoe":
                return self.dump_x()
            self.adaln(1, 1)
            self.moe()
            self.final_norm()
            self.finish()
            return
        if self.stop_after == "mixer0":
            stg = kb.sb("stg", [128, 4, 512], BF16)
            for tg in range(self.ntg):
                for b in range(2):
                    kb.dma("sp", stg[:, 2 * b:2 * b + 2, :], self.y0s[b].h.ap()[:, tg * 512:(tg + 1) * 512].rearrange("(a p) t -> p a t", p=128),
                           reads=[self.y0s[b]], writes=[stg], grp="x")
                kb.dma("sp", self.dbg_y0.h.ap()[:, tg * 512:(tg + 1) * 512].rearrange("(a p) t -> p a t", p=128), stg[:],
                       reads=[stg], writes=[self.dbg_y0], grp="y")
            self.finish()
            return

    def dump_x(self):
        kb = self.kb
        for tt in range(16):
            kb.dma("sp", self.out[tt * 128:(tt + 1) * 128, :], self.xres[tt][:], reads=[self.xres[tt]], writes=[self.out])
        self.finish()

    def finish(self):
        kb = self.kb
        kb.barrier()


def bucket_tab():
    dist = np.arange(128)
    max_exact = 16
    d = np.maximum(dist, 0)
    large = max_exact + (np.log(np.maximum(d, 1) / max_exact) / np.log(128 / max_exact) * (32 - max_exact)).astype(np.int32)
    large = np.minimum(large, 31)
    return np.where(d < max_exact, d, large).astype(np.int32)


def host_inputs(inputs, c):
    b, half = c // 2, c % 2
    f32 = np.float32
    m = {}
    x = inputs["x"]
    m["xfull"] = np.ascontiguousarray(x[b])
    m["xown"] = np.ascontiguousarray(x[b, half * TH:(half + 1) * TH])
    fl = np.zeros((128, 2), f32)
    fl[:, 0] = 1.0 - half
    fl[:, 1] = half
    m["flag"] = fl
    m["cvec"] = np.ascontiguousarray(inputs["c"][b].reshape(8, 128).T)
    m["adaw"] = inputs["ada_w"]
    m["adab"] = inputs["ada_b"]
    m["nmw"] = inputs["norm_mix_w"]
    m["nfw"] = inputs["norm_ffn_w"]
    m["fnw"] = inputs["final_norm_w"].reshape(1, D)
    m["ident"] = np.eye(128, dtype=f32)
    r = np.arange(64)[:, None]
    cc = np.arange(64)[None, :]
    cm = np.zeros((128, 4, 64), f32)
    for q in range(2):
        cm[q * 64:(q + 1) * 64, 0] = np.where(cc >= r, 0.0, NEG)
        cm[q * 64:(q + 1) * 64, 1] = np.where(r > cc, 0.0, NEG)
        cm[q * 64:(q + 1) * 64, 2] = np.eye(64)
    m["cmask"] = cm
    sel = np.zeros((4, 2, 128), f32)
    for q in range(2):
        for j in range(2):
            sel[2 * q + j, j, q * 64:(q + 1) * 64] = 1.0
    m["sel"] = sel
    blk = np.zeros((128, 128), f32)
    blk[:64, :64] = 1.0
    blk[64:, 64:] = 1.0
    m["blk1"] = blk
    w = inputs["ab_w_in"][0]
    HA = lambda q, j: 4 * half + 2 * q + j
    cols = []
    for j in range(2):
        for q in range(2):
            cols += list(range(HA(q, j) * 64, HA(q, j) * 64 + 64))
    cols += list(range(512 + half * 64, 512 + half * 64 + 64)) * 2
    dncols = []
    for base in (768, 768 + 512, 768 + 1024, 2304):
        for j in range(2):
            for q in range(2):
                cols += list(range(base + HA(q, j) * 64, base + HA(q, j) * 64 + 64))
                if base < 2304:
                    dncols += list(range(base - 768 + HA(q, j) * 64, base - 768 + HA(q, j) * 64 + 64))
    cols += list(range(640 + half * 64, 640 + half * 64 + 64))
    cols += [2816 + 4 * half + hl for hl in range(4)]
    cols += [2824 + 4 * half + hl for hl in range(4)]
    assert len(cols) == NC0
    m["win0"] = np.ascontiguousarray(w[:, cols])
    cw = inputs["dn_conv_w"][0][:, dncols]
    m["cw0"] = np.ascontiguousarray(cw.reshape(4, 6, 128).transpose(2, 1, 0))
    hl = [4 * half + i for i in range(4)]
    m["dnsm"] = np.ascontiguousarray(np.stack([inputs["dn_a_log"][0][hl], inputs["dn_dt_bias"][0][hl]], axis=1))
    m["dnw"] = np.ascontiguousarray(np.tile(inputs["dn_norm_w"][0], 2).reshape(128, 1))
    sk = np.zeros((128, 2), f32)
    for q in range(2):
        for j in range(2):
            sk[q * 64:(q + 1) * 64, j] = inputs["attn_sinks"][0][HA(q, j)]
    m["sinkl"] = sk
    bt = bucket_tab()
    s_ = np.arange(128)[:, None]
    qi = np.arange(128)[None, :]
    bg = np.zeros((2, 128, 4, 128), f32)
    am = np.zeros((2, 128, 128), f32)
    for a in range(2):
        dist = qi + 128 - (s_ + 128 * a)
        valid = (dist >= 0) & (dist < 128)
        bk = bt[np.clip(dist, 0, 127)]
        am[a] = np.where(valid, 0.0, NEG)
        for q in range(2):
            for j in range(2):
                bg[a, :, q * 2 + j, :] = inputs["rel_bias"][bk, HA(q, j)]
    m["biasg"] = bg.reshape(2, 128, 512)
    m["amask"] = am
    rows = []
    for base in (0, 512):
        for r_ in range(2):
            for j in range(2):
                for q in range(2):
                    Hh = 4 * r_ + 2 * q + j
                    rows += list(range(base + Hh * 64, base + Hh * 64 + 64))
    m["wout0"] = np.ascontiguousarray(inputs["ab_w_out"][0][rows])
    m["wg0"] = inputs["ffn_w_gate"][0]
    m["wu0"] = inputs["ffn_w_up"][0]
    m["wd0"] = inputs["ffn_w_down"][0]
    w1 = inputs["cd_w_in"][0]
    cols = []
    for base in (0, 1024):
        for i in range(4):
            cols += list(range(base + (4 * half + i) * 128, base + (4 * half + i + 1) * 128))
    for base in (2048, 2560, 3072):
        for i in range(2):
            cols += list(range(base + (2 * half + i) * 128, base + (2 * half + i + 1) * 128))
    assert len(cols) == NC1
    m["win1"] = np.ascontiguousarray(w1[:, cols])
    ch = np.arange(512) + 512 * half
    sm = np.zeros((128, 4, 8), f32)
    sm[:, :, 0:4] = inputs["lru_conv_w"][0][:, ch].reshape(4, 4, 128).transpose(2, 1, 0)
    sm[:, :, 4] = inputs["lru_conv_b"][0][ch].reshape(4, 128).T
    sm[:, :, 5] = inputs["lru_gate_a_b"][0][ch].reshape(4, 128).T
    sm[:, :, 6] = inputs["lru_gate_x_b"][0][ch].reshape(4, 128).T
    sm[:, :, 7] = inputs["lru_lambda"][0][ch].reshape(4, 128).T
    m["l1sm"] = sm
    m["gaw"] = np.ascontiguousarray(inputs["lru_gate_a_w"][0][4 * half:4 * half + 4])
    m["gxw"] = np.ascontiguousarray(inputs["lru_gate_x_w"][0][4 * half:4 * half + 4])
    sc = inputs["sconv_w"][0][:, 256 * half:256 * half + 256]
    m["scw"] = np.ascontiguousarray(sc.reshape(3, 2, 128).transpose(2, 1, 0))
    rows = []
    for b_ in range(2):
        for r_ in range(2):
            for i in (2 * b_, 2 * b_ + 1):
                rows += list(range((4 * r_ + i) * 128, (4 * r_ + i + 1) * 128))
    for r_ in range(2):
        for i in range(2):
            rows += list(range(1024 + (2 * r_ + i) * 128, 1024 + (2 * r_ + i + 1) * 128))
    m["wout1"] = np.ascontiguousarray(inputs["cd_w_out"][0][rows])
    m["rw"] = np.ascontiguousarray(inputs["moe_router_w"][0].reshape(8, 128, 8).transpose(1, 0, 2))
    m["rb"] = inputs["moe_router_b"][0].reshape(1, 8)
    m["mwg"] = inputs["moe_w_gate"][0]
    m["mwu"] = inputs["moe_w_up"][0]
    m["mwd"] = inputs["moe_w_down"][0]
    return {k: np.ascontiguousarray(v, dtype=np.float32) for k, v in m.items()}


def run(inputs, stop_after=None, debug=False, **kw):
    pg = Prog(stop_after=stop_after, debug=debug, **kw)
    pg.build()
    in_maps = []
    for c in range(8):
        hm = host_inputs(inputs, c)
        in_maps.append({k: hm[k] for k in pg.inputs})
    res = run_bass_kernel_spmd(pg.kb.nc, in_maps, core_ids=list(range(8)))
    return res, pg


def kernel(**inputs):
    inputs = {k: np.asarray(v) for k, v in inputs.items()}
    res, pg = run(inputs)
    out = np.zeros((4, T, D), np.float32)
    for c in range(8):
        out[c // 2, (c % 2) * TH:(c % 2 + 1) * TH] = res.results[c]["out"]
    return out
```
